# Optimizing a Trainium2 kernel written in Bass

```python
import jax, jax.numpy as jnp
from jax import lax
import numpy as np

D_MODEL = 2048
BATCH = 4
SEQ = 4096
DEPTH = 2

CHUNK = 64
ROPE_THETA = 10000.0
LN_EPS = 1e-5
DEEPNORM_ALPHA = (2 * DEPTH) ** 0.25
DEEPNORM_BETA = (8 * DEPTH) ** -0.25

A_HEADS = 8
A_KV_HEADS = 2
A_HEAD_DIM = 128
A_WIDTH = A_HEADS * A_HEAD_DIM
IDX_HEADS = 4
IDX_DIM = 128
TOPK_MAX = 256
A_QBLK = 128
POOL_WINDOWS = (2, 4, 8, 16)
POOL_WIDTH = D_MODEL // 2
POOL_GROUP = POOL_WIDTH // len(POOL_WINDOWS)
IN0_SIZES = (A_WIDTH, A_KV_HEADS * A_HEAD_DIM, A_KV_HEADS * A_HEAD_DIM,
             IDX_HEADS * IDX_DIM, IDX_DIM, IDX_HEADS, POOL_WIDTH)
IN0 = sum(IN0_SIZES)
MIX0_WIDTH = A_WIDTH + POOL_WIDTH

C_HEADS = 8
C_V_DIM = D_MODEL // C_HEADS
C_QK_DIM = C_V_DIM // 2
C_QK_WIDTH = C_HEADS * C_QK_DIM
C_V_WIDTH = C_HEADS * C_V_DIM
CONV_K = 4
IN1_SIZES = (2 * C_QK_WIDTH, C_V_WIDTH, C_HEADS, C_HEADS, C_V_WIDTH)
IN1 = sum(IN1_SIZES)

N_EXPERTS = 32
TOP_E = 4
D_FF = D_MODEL
SWIGLU_LIMIT = 7.0
SWIGLU_ALPHA = 1.702
MOE_BLK = 256

N_EVEN = (DEPTH + 1) // 2
N_ODD = DEPTH // 2

kernel_name = 'hybrid_dsa_pool_mlstm_moe_deepnorm'


def _split(p, sizes):
    idx = np.cumsum(sizes)[:-1].tolist()
    return jnp.split(p, idx, axis=-1)


def layer_norm(x, g, b):
    xf = x.astype(jnp.float32)
    mu = jnp.mean(xf, -1, keepdims=True)
    var = jnp.mean(jnp.square(xf - mu), -1, keepdims=True)
    return ((xf - mu) * lax.rsqrt(var + LN_EPS) * g + b).astype(x.dtype)


def rope(x, pos):
    d = x.shape[-1]
    inv = ROPE_THETA ** (-jnp.arange(0, d, 2, dtype=jnp.float32) / d)
    ang = pos.astype(jnp.float32)[..., None] * inv
    cos = jnp.cos(ang)[:, :, None, :]
    sin = jnp.sin(ang)[:, :, None, :]
    xf = x.astype(jnp.float32)
    x1, x2 = xf[..., : d // 2], xf[..., d // 2:]
    return jnp.concatenate([x1 * cos - x2 * sin, x2 * cos + x1 * sin], -1).astype(x.dtype)


def dsa_attention(q, k, v, qi, ki, wi, pos):
    B, S = q.shape[:2]
    ksel = min(TOPK_MAX, S // 4)
    rep = A_HEADS // A_KV_HEADS
    nqb = S // A_QBLK
    q = rope(q, pos)
    k = rope(k, pos)
    qi = rope(qi, pos)
    ki = rope(ki[:, :, None, :], pos)[:, :, 0].astype(jnp.float32)
    wi = wi.astype(jnp.float32) * IDX_HEADS ** -0.5
    key_chunk = jnp.arange(S, dtype=jnp.int32) // CHUNK

    def blocks(t):
        return jnp.moveaxis(t.reshape((B, nqb, A_QBLK) + t.shape[2:]), 1, 0)

    def one_block(args):
        q_b, qi_b, w_b, t_b = args
        q_chunk = t_b // CHUNK
        s = jnp.einsum('bqhd,bsd->bqhs', qi_b.astype(jnp.float32), ki)
        score = jnp.einsum('bqhs,bqh->bqs', jax.nn.relu(s), w_b) * IDX_DIM ** -0.5
        allowed = key_chunk[None, :] <= q_chunk[:, None]
        score = jnp.where(allowed[None], score, -jnp.inf)
        _, sel = lax.top_k(score, ksel)
        valid = (sel // CHUNK) <= q_chunk[None, :, None]
        k_sel = jax.vmap(lambda kk, ii: kk[ii])(k, sel)
        v_sel = jax.vmap(lambda vv, ii: vv[ii])(v, sel)
        qg = q_b.reshape(B, A_QBLK, A_KV_HEADS, rep, A_HEAD_DIM).astype(jnp.float32)
        logits = jnp.einsum('bqgrd,bqkgd->bqgrk', qg, k_sel.astype(jnp.float32)) * A_HEAD_DIM ** -0.5
        logits = jnp.where(valid[:, :, None, None, :], logits, -jnp.inf)
        p = jax.nn.softmax(logits, axis=-1)
        o = jnp.einsum('bqgrk,bqkgd->bqgrd', p, v_sel.astype(jnp.float32))
        return o.reshape(B, A_QBLK, A_WIDTH).astype(q_b.dtype)

    t_blocks = jnp.arange(S, dtype=jnp.int32).reshape(nqb, A_QBLK)
    out = lax.map(one_block, (blocks(q), blocks(qi), blocks(wi), t_blocks))
    return jnp.moveaxis(out, 0, 1).reshape(B, S, A_WIDTH)


def multiscale_pool(u, w_pool, pool_scale):
    B, S, _ = u.shape
    uf = u.astype(jnp.float32)
    wmax = POOL_WINDOWS[-1]
    cs = jnp.pad(jnp.cumsum(uf, axis=1), ((0, 0), (wmax, 0), (0, 0)))
    t = jnp.arange(S, dtype=jnp.float32)
    outs = []
    for g, w in enumerate(POOL_WINDOWS):
        lo, hi = g * POOL_GROUP, (g + 1) * POOL_GROUP
        win_sum = cs[:, wmax:, lo:hi] - cs[:, wmax - w: wmax - w + S, lo:hi]
        cnt = jnp.minimum(t + 1.0, float(w))[None, :, None]
        outs.append(win_sum / cnt - uf[:, :, lo:hi])
    pooled = jnp.stack(outs, axis=2)
    mixed = jnp.einsum('bsgc,gcd->bsgd', pooled, w_pool.astype(jnp.float32)).reshape(B, S, POOL_WIDTH)
    return (mixed * pool_scale).astype(u.dtype)


def causal_conv(x, w, b):
    K = w.shape[0]
    S = x.shape[1]
    xp = jnp.pad(x, ((0, 0), (K - 1, 0), (0, 0)))
    y = xp[:, 0:S] * w[0]
    for i in range(1, K):
        y = y + xp[:, i:i + S] * w[i]
    return y + b


def mlstm_chunkwise(q, k, v, ig, lf):
    B, S, H, dk = q.shape
    dv = v.shape[-1]
    nc = S // CHUNK

    def chunks4(t):
        return t.reshape(B, nc, CHUNK, H, t.shape[-1]).transpose(1, 0, 3, 2, 4)

    def chunks3(t):
        return t.reshape(B, nc, CHUNK, H).transpose(1, 0, 3, 2)

    causal = jnp.tril(jnp.ones((CHUNK, CHUNK), dtype=bool))

    def step(carry, xs):
        C, n, m = carry
        qc, kc, vc, ic, fc = xs
        b = jnp.cumsum(fc, axis=-1)
        D = b[..., :, None] - b[..., None, :] + ic[..., None, :]
        D = jnp.where(causal, D, -jnp.inf)
        inter = b + m[..., None]
        m_t = jnp.maximum(inter, jnp.max(D, axis=-1))
        Sm = jnp.exp(D - m_t[..., None]) * jnp.einsum('bhld,bhsd->bhls', qc, kc)
        e_inter = jnp.exp(inter - m_t)
        num = e_inter[..., None] * jnp.einsum('bhld,bhvd->bhlv', qc, C) + jnp.einsum('bhls,bhsv->bhlv', Sm, vc)
        nq = e_inter * jnp.einsum('bhld,bhd->bhl', qc, n) + jnp.sum(Sm, axis=-1)
        h = num / jnp.maximum(jnp.abs(nq), jnp.exp(-m_t))[..., None]
        bL = b[..., -1]
        g = bL[..., None] - b + ic
        m_new = jnp.maximum(bL + m, jnp.max(g, axis=-1))
        decay = jnp.exp(bL + m - m_new)
        wgt = jnp.exp(g - m_new[..., None])
        C_new = decay[..., None, None] * C + jnp.einsum('bhl,bhlv,bhld->bhvd', wgt, vc, kc)
        n_new = decay[..., None] * n + jnp.einsum('bhl,bhld->bhd', wgt, kc)
        return (C_new, n_new, m_new), h

    init = (jnp.zeros((B, H, dv, dk), jnp.float32), jnp.zeros((B, H, dk), jnp.float32),
            jnp.zeros((B, H), jnp.float32))
    xs = (chunks4(q), chunks4(k), chunks4(v), chunks3(ig), chunks3(lf))
    _, h = lax.scan(step, init, xs)
    return h.transpose(1, 0, 3, 2, 4).reshape(B, S, H, dv)


def even_mixer(x, positions, w_in, w_pool, pool_scale, w_out):
    B, S, _ = x.shape
    p = x @ w_in
    q, k, v, qi, ki, wi, u = _split(p, IN0_SIZES)
    a = dsa_attention(q.reshape(B, S, A_HEADS, A_HEAD_DIM),
                      k.reshape(B, S, A_KV_HEADS, A_HEAD_DIM),
                      v.reshape(B, S, A_KV_HEADS, A_HEAD_DIM),
                      qi.reshape(B, S, IDX_HEADS, IDX_DIM), ki, wi, positions)
    bo = multiscale_pool(u, w_pool, pool_scale)
    return jnp.concatenate([a, bo], axis=-1) @ w_out


def odd_mixer(x, w_in, conv_w, conv_b, gate_i_b, gate_f_b, c_norm_g, w_out):
    B, S, _ = x.shape
    p = x @ w_in
    qk, v, ig, fg, og = _split(p, IN1_SIZES)
    qk = jax.nn.silu(causal_conv(qk, conv_w, conv_b))
    q, k = qk[..., :C_QK_WIDTH], qk[..., C_QK_WIDTH:]
    q = q.reshape(B, S, C_HEADS, C_QK_DIM).astype(jnp.float32) * C_QK_DIM ** -0.5
    k = k.reshape(B, S, C_HEADS, C_QK_DIM).astype(jnp.float32)
    v = v.reshape(B, S, C_HEADS, C_V_DIM).astype(jnp.float32)
    ig = (ig + gate_i_b).astype(jnp.float32)
    lf = jax.nn.log_sigmoid((fg + gate_f_b).astype(jnp.float32))
    h = mlstm_chunkwise(q, k, v, ig, lf)
    mu = jnp.mean(h, -1, keepdims=True)
    var = jnp.mean(jnp.square(h - mu), -1, keepdims=True)
    h = ((h - mu) * lax.rsqrt(var + LN_EPS)).reshape(B, S, C_V_WIDTH) * c_norm_g
    y = jax.nn.sigmoid(og.astype(jnp.float32)) * h
    return y.astype(x.dtype) @ w_out


def moe_ffn(x, router_w, router_b, w_gu, b_gu, w_dn, b_dn):
    B, S, D = x.shape
    T = B * S
    xt = x.reshape(T, D)
    logits = (xt @ router_w + router_b).astype(jnp.float32)
    top_val, top_idx = lax.top_k(logits, TOP_E)
    gates = jax.nn.softmax(top_val, axis=-1)
    n_assign = T * TOP_E
    flat_e = top_idx.reshape(-1)
    flat_tok = jnp.repeat(jnp.arange(T, dtype=jnp.int32), TOP_E)
    order = jnp.argsort(flat_e)
    e_sorted = flat_e[order]
    counts = jnp.bincount(flat_e, length=N_EXPERTS)
    starts = jnp.cumsum(counts) - counts
    padded = (counts + MOE_BLK - 1) // MOE_BLK * MOE_BLK
    pend = jnp.cumsum(padded)
    pstart = pend - padded
    dest = pstart[e_sorted] + jnp.arange(n_assign, dtype=jnp.int32) - starts[e_sorted]
    n_blocks = -(-n_assign // MOE_BLK) + N_EXPERTS
    rows = n_blocks * MOE_BLK
    buf_tok = jnp.zeros((rows,), jnp.int32).at[dest].set(flat_tok[order])
    buf_gate = jnp.zeros((rows,), jnp.float32).at[dest].set(gates.reshape(-1)[order])
    block_e = jnp.minimum(jnp.searchsorted(pend, jnp.arange(n_blocks, dtype=jnp.int32) * MOE_BLK, side='right'),
                          N_EXPERTS - 1)

    def expert_block(args):
        tok, gate_w, e = args
        h = xt[tok] @ w_gu[e] + b_gu[e]
        gt = jnp.minimum(h[:, :D_FF], SWIGLU_LIMIT)
        up = jnp.clip(h[:, D_FF:], -SWIGLU_LIMIT, SWIGLU_LIMIT)
        act = (up + 1.0) * gt * jax.nn.sigmoid(SWIGLU_ALPHA * gt)
        return (act @ w_dn[e] + b_dn[e]) * gate_w[:, None]

    y = lax.map(expert_block, (buf_tok.reshape(n_blocks, MOE_BLK), buf_gate.reshape(n_blocks, MOE_BLK), block_e))
    out = jax.ops.segment_sum(y.reshape(rows, D), buf_tok, num_segments=T)
    return out.reshape(B, S, D).astype(x.dtype)


def setup_inputs(seed: int = 0) -> dict:
    key = jax.random.key(seed)
    ks = jax.random.split(key, 24)
    f32 = jnp.float32

    def nrm(k, shape, scale):
        return jax.random.normal(k, shape, f32) * scale

    x = nrm(ks[0], (BATCH, SEQ, D_MODEL), 1.0)
    offs = jax.random.randint(ks[1], (BATCH, 1), 0, 1024, dtype=jnp.int32)
    positions = offs + jnp.arange(SEQ, dtype=jnp.int32)[None, :]
    w_in0 = nrm(ks[2], (N_EVEN, D_MODEL, IN0), D_MODEL ** -0.5)
    w_pool = nrm(ks[3], (N_EVEN, len(POOL_WINDOWS), POOL_GROUP, POOL_GROUP), POOL_GROUP ** -0.5)
    pool_scale = 1.0 + nrm(ks[4], (N_EVEN, POOL_WIDTH), 0.05)
    w_out0 = nrm(ks[5], (N_EVEN, MIX0_WIDTH, D_MODEL), MIX0_WIDTH ** -0.5 * DEEPNORM_BETA)
    w_in1 = nrm(ks[6], (N_ODD, D_MODEL, IN1), D_MODEL ** -0.5)
    conv_w = nrm(ks[7], (N_ODD, CONV_K, 2 * C_QK_WIDTH), CONV_K ** -0.5)
    conv_b = nrm(ks[8], (N_ODD, 2 * C_QK_WIDTH), 0.02)
    gate_i_b = nrm(ks[9], (N_ODD, C_HEADS), 0.1)
    gate_f_b = jnp.linspace(3.0, 6.0, C_HEADS, dtype=f32)[None, :] + nrm(ks[10], (N_ODD, C_HEADS), 0.1)
    c_norm_g = 1.0 + nrm(ks[11], (N_ODD, C_V_WIDTH), 0.02)
    w_out1 = nrm(ks[12], (N_ODD, C_V_WIDTH, D_MODEL), C_V_WIDTH ** -0.5 * DEEPNORM_BETA)
    ln_mix_g = 1.0 + nrm(ks[13], (DEPTH, D_MODEL), 0.02)
    ln_mix_b = nrm(ks[14], (DEPTH, D_MODEL), 0.02)
    router_w = nrm(ks[15], (DEPTH, D_MODEL, N_EXPERTS), D_MODEL ** -0.5)
    router_b = nrm(ks[16], (DEPTH, N_EXPERTS), 0.01)
    w_gu = nrm(ks[17], (DEPTH, N_EXPERTS, D_MODEL, 2 * D_FF), D_MODEL ** -0.5)
    b_gu = nrm(ks[18], (DEPTH, N_EXPERTS, 2 * D_FF), 0.02)
    w_dn = nrm(ks[19], (DEPTH, N_EXPERTS, D_FF, D_MODEL), D_FF ** -0.5 * DEEPNORM_BETA)
    b_dn = nrm(ks[20], (DEPTH, N_EXPERTS, D_MODEL), 0.02)
    ln_moe_g = 1.0 + nrm(ks[21], (DEPTH, D_MODEL), 0.02)
    ln_moe_b = nrm(ks[22], (DEPTH, D_MODEL), 0.02)
    return {'x': x, 'positions': positions, 'w_in0': w_in0, 'w_pool': w_pool, 'pool_scale': pool_scale,
            'w_out0': w_out0, 'w_in1': w_in1, 'conv_w': conv_w, 'conv_b': conv_b, 'gate_i_b': gate_i_b,
            'gate_f_b': gate_f_b, 'c_norm_g': c_norm_g, 'w_out1': w_out1, 'ln_mix_g': ln_mix_g,
            'ln_mix_b': ln_mix_b, 'router_w': router_w, 'router_b': router_b, 'w_gu': w_gu, 'b_gu': b_gu,
            'w_dn': w_dn, 'b_dn': b_dn, 'ln_moe_g': ln_moe_g, 'ln_moe_b': ln_moe_b}


def reference(x, positions, w_in0, w_pool, pool_scale, w_out0, w_in1, conv_w, conv_b, gate_i_b, gate_f_b,
              c_norm_g, w_out1, ln_mix_g, ln_mix_b, router_w, router_b, w_gu, b_gu, w_dn, b_dn,
              ln_moe_g, ln_moe_b):
    for l in range(DEPTH):
        j = l // 2
        if l % 2 == 0:
            mix = even_mixer(x, positions, w_in0[j], w_pool[j], pool_scale[j], w_out0[j])
        else:
            mix = odd_mixer(x, w_in1[j], conv_w[j], conv_b[j], gate_i_b[j], gate_f_b[j], c_norm_g[j], w_out1[j])
        x = layer_norm(DEEPNORM_ALPHA * x + mix, ln_mix_g[l], ln_mix_b[l])
        ffn = moe_ffn(x, router_w[l], router_b[l], w_gu[l], b_gu[l], w_dn[l], b_dn[l])
        x = layer_norm(DEEPNORM_ALPHA * x + ffn, ln_moe_g[l], ln_moe_b[l])
    return x
```

```python
import numpy as np
from contextlib import ExitStack
import concourse.bass as bass
import concourse.mybir as mybir
from concourse.bass_utils import run_bass_kernel_spmd

F32 = mybir.dt.float32
BF16 = mybir.dt.bfloat16
I32 = mybir.dt.int32
U32 = mybir.dt.uint32
AF = mybir.ActivationFunctionType
ALU = mybir.AluOpType
AX = mybir.AxisListType

D = 2048
NT = 2048
NTT = NT // 128
NE = 32
CAP = 384
NSB = CAP // 128
ALPHA = float(4 ** 0.25)
LN_EPS = 1e-5


class Buf:
    def __init__(self, name, t):
        self.name = name
        self.t = t
        self.dsem = None
        self.dcnt = 0

    def __getitem__(self, k):
        return self.t[k]


class P:
    def __init__(self, nc, es):
        self.nc = nc
        self.es = es
        self.eng = {"pe": nc.tensor, "dve": nc.vector, "act": nc.scalar, "pool": nc.gpsimd, "sp": nc.sync}
        self.sem = {k: es.enter_context(nc.semaphore("sem_" + k)) for k in self.eng}
        self.cnt = {k: 0 for k in self.eng}
        self.seen = {k: {} for k in self.eng}
        self.st = {}
        self.nbuf = 0
        self.dsems = []
        self.allbufs = []

    def sb(self, name, shape, dt, es=None):
        self.nbuf += 1
        t = (es or self.es).enter_context(self.nc.sbuf_tensor(f"{name}_{self.nbuf}", list(shape), dt))
        b = Buf(name, t)
        self.allbufs.append(b)
        return b

    def ps(self, name, shape, dt, es=None):
        self.nbuf += 1
        t = (es or self.es).enter_context(self.nc.psum_tensor(f"{name}_{self.nbuf}", list(shape), dt))
        return Buf(name, t)

    def _dsem(self, buf):
        if buf.dsem is None:
            buf.dsem = self.es.enter_context(self.nc.semaphore(f"ds_{buf.name}_{len(self.dsems)}"))
            self.dsems.append(buf)
        return buf.dsem

    def _state(self, k):
        s = self.st.get(k)
        if s is None:
            s = {"w": {}, "r": {}}
            self.st[k] = s
        return s

    def _wait_tok(self, e, src, val):
        if isinstance(src, Buf):
            val = 16 * src.dcnt
            sem = src.dsem
            key = ("d", id(src))
        else:
            if src == e and e == "pe":
                return
            sem = self.sem[src]
            key = src
        if val <= 0:
            return
        if self.seen[e].get(key, 0) >= val:
            return
        self.eng[e].wait_ge(sem, val)
        self.seen[e][key] = val

    def _deps(self, e, reads, writes):
        for k in reads:
            s = self._state(k)
            for src, val in list(s["w"].items()):
                self._wait_tok(e, src, val)
        for k in writes:
            s = self._state(k)
            for src, val in list(s["w"].items()):
                self._wait_tok(e, src, val)
            for src, val in list(s["r"].items()):
                self._wait_tok(e, src, val)

    def _record(self, src, val, reads, writes):
        for k in reads:
            s = self._state(k)
            s["r"][src] = val
        for k in writes:
            s = self._state(k)
            s["w"] = {src: val}
            s["r"] = {}

    def op(self, e, fn, reads=(), writes=()):
        self._deps(e, reads, writes)
        ins = fn()
        self.cnt[e] += 1
        ins.then_inc(self.sem[e], 1)
        self._record(e, self.cnt[e], reads, writes)
        return ins

    def dma(self, q, out, in_, sbuf, reads=(), writes=(), fn=None, **kw):
        self._deps(q, reads, writes)
        sem = self._dsem(sbuf)
        if fn is not None:
            ins = fn()
        else:
            ins = self.eng[q].dma_start(out=out, in_=in_, **kw)
        sbuf.dcnt += 1
        ins.then_inc(sem, 16)
        self._record(sbuf, 16 * sbuf.dcnt, reads, writes)
        return ins

    def barrier(self):
        for e in self.eng:
            for o in self.eng:
                if o != e:
                    self._wait_tok(e, o, self.cnt[o])
            for b in self.dsems:
                self._wait_tok(e, b, 0)
        self.st = {}

    def finish(self):
        for o in self.eng:
            if o != "sp":
                self._wait_tok("sp", o, self.cnt[o])
        for b in self.dsems:
            self._wait_tok("sp", b, 0)


def bcast_rows(ap_1d, nparts, n):
    return bass.AP(tensor=ap_1d.tensor, offset=ap_1d.offset, ap=[[0, nparts], [1, n]])


def emit_ln(p, z, out, gbc, bbc, stats, mv, rstd):
    nc = p.nc
    for c in range(4):
        p.op("dve", lambda c=c: nc.vector.bn_stats(out=stats[:, c * 6:(c + 1) * 6], in_=z[:, c * 512:(c + 1) * 512]),
             reads=[z], writes=[(stats, c)])
    p.op("dve", lambda: nc.vector.bn_aggr(out=mv[:, 0:2], in_=stats[:, 0:24]),
         reads=[(stats, c) for c in range(4)], writes=[mv])
    p.op("dve", lambda: nc.vector.tensor_scalar(out=rstd[:, 0:1], in0=mv[:, 1:2], scalar1=LN_EPS, scalar2=None, op0=ALU.add),
         reads=[mv], writes=[rstd])
    p.op("act", lambda: nc.scalar.activation(out=rstd[:, 0:1], in_=rstd[:, 0:1], func=AF.Sqrt), reads=[rstd], writes=[rstd])
    p.op("dve", lambda: nc.vector.reciprocal(out=rstd[:, 0:1], in_=rstd[:, 0:1]), reads=[rstd], writes=[rstd])
    p.op("dve", lambda: nc.vector.tensor_scalar(out=out[:, :], in0=z[:, :], scalar1=mv[:, 0:1], scalar2=rstd[:, 0:1],
                                                op0=ALU.subtract, op1=ALU.mult), reads=[z, mv, rstd], writes=[out])
    p.op("pool", lambda: nc.gpsimd.tensor_tensor(out=out[:, :], in0=out[:, :], in1=gbc[:, :], op=ALU.mult), reads=[out, gbc], writes=[out])
    p.op("dve", lambda: nc.vector.tensor_tensor(out=out[:, :], in0=out[:, :], in1=bbc[:, :], op=ALU.add), reads=[out, bbc], writes=[out])


NLOC = NE // 8
XCAP = 8 * CAP


def emit_dispatch(p, x_in, xeT_out, gs_out, idx_out, W, ident_f):
    nc = p.nc
    with ExitStack() as es:
        xbf = p.sb("xbf", [128, NTT, D], BF16, es)
        posm = p.sb("posm", [128, NTT, NE], F32, es)
        ghi = p.sb("ghi", [128, NTT, NE, 2], BF16, es)
        idx = p.sb("idx", [128, NTT, 4], I32, es)
        iotac = p.sb("iotac", [128, CAP], F32, es)
        ones_bf = p.sb("ones_bf", [128, 128], BF16, es)
        p.op("pool", lambda: nc.gpsimd.iota(iotac[:, :], pattern=[[1, CAP]], base=0, channel_multiplier=0,
                                            allow_small_or_imprecise_dtypes=True), writes=[iotac])
        p.op("dve", lambda: nc.vector.memset(ones_bf[:, :], 1.0), writes=[ones_bf])

        with ExitStack() as es2:
            wr = p.sb("wr", [128, 16, NE], F32, es2)
            rb = p.sb("rb", [128, NE], F32, es2)
            ustr = p.sb("ustr", [128, 128], BF16, es2)
            rowbase = p.sb("rowbase", [128, NE], F32, es2)
            cum = p.sb("cum", [128, NE], F32, es2)
            xa = [p.sb(f"xa{i}", [128, D], F32, es2) for i in range(2)]
            xT = [p.sb(f"xT{i}", [128, 16, 128], F32, es2) for i in range(2)]
            lg = p.sb("lg", [128, NE], F32, es2)
            m8 = p.sb("m8", [128, 8], F32, es2)
            nm = p.sb("nm", [128, 1], F32, es2)
            mask = p.sb("mask", [128, NE], F32, es2)
            maskb = p.sb("maskb", [128, NE], BF16, es2)
            ex = p.sb("ex", [128, NE], F32, es2)
            ssum = p.sb("ssum", [128, 1], F32, es2)
            gate = p.sb("gate", [128, NE], F32, es2)
            gres = p.sb("gres", [128, NE], F32, es2)
            pos = p.sb("pos", [128, NE], F32, es2)
            vv = p.sb("vv", [128, NE], F32, es2)
            ltc = p.sb("ltc", [128, NE], F32, es2)
            r8 = p.sb("r8", [128, 8], F32, es2)
            pt = [p.ps(f"pt{i}", [128, 512], F32, es2) for i in range(4)]
            plg = p.ps("plg", [128, NE], F32, es2)
            ppos = p.ps("ppos", [128, 2, NE], F32, es2)

            p.dma("sp", wr[:, :, :], W["router_w"], wr, writes=[wr])
            p.dma("sp", rb[:, :], bcast_rows(W["router_b"], 128, NE), rb, writes=[rb])
            p.op("dve", lambda: nc.vector.memset(ustr[:, :], 1.0), writes=[ustr])
            p.op("pool", lambda: nc.gpsimd.affine_select(out=ustr[:, :], in_=ustr[:, :], pattern=[[1, 128]],
                                                         compare_op=ALU.is_gt, fill=0.0, base=0, channel_multiplier=-1),
                 reads=[ustr], writes=[ustr])
            p.op("pool", lambda: nc.gpsimd.iota(rowbase[:, :], pattern=[[CAP, NE]], base=1, channel_multiplier=0,
                                                allow_small_or_imprecise_dtypes=True), writes=[rowbase])
            p.op("dve", lambda: nc.vector.memset(cum[:, :], 0.0), writes=[cum])

            for i in range(NTT):
                xai = xa[i % 2]
                xTi = xT[i % 2]
                p.dma("sp", xai[:, :], x_in[i * 128:(i + 1) * 128, :], xai, writes=[xai])
                p.op("act", lambda: nc.scalar.copy(out=xbf[:, i, :], in_=xai[:, :]), reads=[xai], writes=[(xbf, i)])
                for g in range(4):
                    ptg = pt[g]
                    for j in range(4):
                        c = g * 4 + j
                        p.op("pe", lambda: nc.tensor.transpose(out=ptg[:, j * 128:(j + 1) * 128],
                                                               in_=xai[:, c * 128:(c + 1) * 128], identity=ident_f[:, :]),
                             reads=[xai, ident_f], writes=[ptg])
                    if g % 2 == 0:
                        p.op("dve", lambda: nc.vector.tensor_copy(out=xTi[:, g * 4:(g + 1) * 4, :],
                                                                  in_=ptg[:, :].rearrange("p (a b) -> p a b", a=4)),
                             reads=[ptg], writes=[(xTi, g)])
                    else:
                        p.op("act", lambda: nc.scalar.copy(out=xTi[:, g * 4:(g + 1) * 4, :],
                                                           in_=ptg[:, :].rearrange("p (a b) -> p a b", a=4)),
                             reads=[ptg], writes=[(xTi, g)])
                for c in range(16):
                    p.op("pe", lambda: nc.tensor.matmul(plg[:, :], lhsT=xTi[:, c, :], rhs=wr[:, c, :], start=(c == 0), stop=(c == 15)),
                         reads=[(xTi, c // 4), wr], writes=[plg])
                p.op("dve", lambda: nc.vector.tensor_tensor(out=lg[:, :], in0=plg[:, :], in1=rb[:, :], op=ALU.add), reads=[plg, rb], writes=[lg])
                p.op("dve", lambda: nc.vector.max(out=m8[:, :], in_=lg[:, :]), reads=[lg], writes=[m8])
                p.op("dve", lambda: nc.vector.tensor_scalar(out=mask[:, :], in0=lg[:, :], scalar1=m8[:, 3:4], scalar2=None, op0=ALU.is_ge),
                     reads=[lg, m8], writes=[mask])
                p.op("pool", lambda: nc.gpsimd.tensor_copy(out=maskb[:, :], in_=mask[:, :]), reads=[mask], writes=[maskb])
                p.op("dve", lambda: nc.vector.tensor_scalar(out=nm[:, :], in0=m8[:, 0:1], scalar1=-1.0, scalar2=None, op0=ALU.mult),
                     reads=[m8], writes=[nm])
                p.op("act", lambda: nc.scalar.activation(out=ex[:, :], in_=lg[:, :], func=AF.Exp, bias=nm[:, 0:1], scale=1.0),
                     reads=[lg, nm], writes=[ex])
                p.op("dve", lambda: nc.vector.tensor_tensor(out=ex[:, :], in0=ex[:, :], in1=mask[:, :], op=ALU.mult), reads=[ex, mask], writes=[ex])
                p.op("dve", lambda: nc.vector.reduce_sum(out=ssum[:, :], in_=ex[:, :], axis=AX.X), reads=[ex], writes=[ssum])
                p.op("dve", lambda: nc.vector.reciprocal(out=ssum[:, :], in_=ssum[:, :]), reads=[ssum], writes=[ssum])
                p.op("dve", lambda: nc.vector.tensor_scalar(out=gate[:, :], in0=ex[:, :], scalar1=ssum[:, 0:1], scalar2=None, op0=ALU.mult),
                     reads=[ex, ssum], writes=[gate])
                p.op("dve", lambda: nc.vector.tensor_copy(out=ghi[:, i, :, 0], in_=gate[:, :]), reads=[gate], writes=[(ghi, i, 0)])
                p.op("dve", lambda: nc.vector.tensor_tensor(out=gres[:, :], in0=gate[:, :], in1=ghi[:, i, :, 0], op=ALU.subtract),
                     reads=[gate, (ghi, i, 0)], writes=[gres])
                p.op("dve", lambda: nc.vector.tensor_copy(out=ghi[:, i, :, 1], in_=gres[:, :]), reads=[gres], writes=[(ghi, i, 1)])
                p.op("pe", lambda: nc.tensor.matmul(ppos[:, 0, :], lhsT=ustr[:, :], rhs=maskb[:, :], start=True, stop=True),
                     reads=[ustr, maskb], writes=[(ppos, 0)])
                p.op("pe", lambda: nc.tensor.matmul(ppos[:, 1, :], lhsT=ones_bf[:, :], rhs=maskb[:, :], start=True, stop=True),
                     reads=[ones_bf, maskb], writes=[(ppos, 1)])
                p.op("dve", lambda: nc.vector.tensor_tensor(out=pos[:, :], in0=ppos[:, 0, :], in1=cum[:, :], op=ALU.add),
                     reads=[(ppos, 0), cum], writes=[pos])
                p.op("dve", lambda: nc.vector.tensor_tensor(out=cum[:, :], in0=ppos[:, 1, :], in1=cum[:, :], op=ALU.add),
                     reads=[(ppos, 1), cum], writes=[cum])
                p.op("dve", lambda: nc.vector.scalar_tensor_tensor(out=posm[:, i, :], in0=pos[:, :], scalar=1.0, in1=mask[:, :],
                                                                   op0=ALU.add, op1=ALU.mult), reads=[pos, mask], writes=[(posm, i)])
                p.op("dve", lambda: nc.vector.tensor_scalar(out=posm[:, i, :], in0=posm[:, i, :], scalar1=-1.0, scalar2=None, op0=ALU.add),
                     reads=[(posm, i)], writes=[(posm, i)])
                p.op("dve", lambda: nc.vector.tensor_scalar(out=ltc[:, :], in0=pos[:, :], scalar1=float(CAP), scalar2=None, op0=ALU.is_lt),
                     reads=[pos], writes=[ltc])
                p.op("dve", lambda: nc.vector.tensor_tensor(out=ltc[:, :], in0=ltc[:, :], in1=mask[:, :], op=ALU.mult), reads=[ltc, mask], writes=[ltc])
                p.op("dve", lambda: nc.vector.tensor_tensor(out=vv[:, :], in0=pos[:, :], in1=rowbase[:, :], op=ALU.add), reads=[pos, rowbase], writes=[vv])
                p.op("dve", lambda: nc.vector.tensor_tensor(out=vv[:, :], in0=vv[:, :], in1=ltc[:, :], op=ALU.mult), reads=[vv, ltc], writes=[vv])
                p.op("dve", lambda: nc.vector.max(out=r8[:, :], in_=vv[:, :]), reads=[vv], writes=[r8])
                p.op("dve", lambda: nc.vector.tensor_copy(out=idx[:, i, :], in_=r8[:, 0:4]), reads=[r8], writes=[(idx, i)])
            p.dma("sp", idx_out, idx[:, :, :].rearrange("p a b -> p (a b)"), idx, reads=[(idx, i) for i in range(NTT)], writes=["idx_out"])
            p.barrier()

        with ExitStack() as es3:
            O = [p.sb(f"O{i}", [128, NTT, CAP], BF16, es3) for i in range(2)]
            xeT = [p.sb(f"xeT{i}", [128, 16, CAP], BF16, es3) for i in range(2)]
            gsl = [p.sb(f"gsl{i}", [128, NSB], F32, es3) for i in range(2)]
            pd = [p.ps(f"pd{i}", [128, 512], F32, es3) for i in range(4)]
            pgs = [p.ps(f"pgs{i}", [128, NSB, 2], F32, es3) for i in range(2)]
            for e in range(NE):
                Oe = O[e % 2]
                xe = xeT[e % 2]
                gs = gsl[e % 2]
                pg_ = pgs[e % 2]
                for i in range(NTT):
                    eng = "dve" if i % 2 == 0 else "pool"
                    eo = nc.vector if eng == "dve" else nc.gpsimd
                    p.op(eng, lambda: eo.tensor_scalar(out=Oe[:, i, :], in0=iotac[:, :], scalar1=posm[:, i, e:e + 1],
                                                       scalar2=None, op0=ALU.is_equal),
                         reads=[iotac, (posm, i)], writes=[(Oe, i)])
                for sb_ in range(NSB):
                    for i in range(NTT):
                        p.op("pe", lambda: nc.tensor.matmul(pg_[:, sb_, :], lhsT=Oe[:, i, sb_ * 128:(sb_ + 1) * 128],
                                                            rhs=ghi[:, i, e, :], start=(i == 0), stop=(i == NTT - 1)),
                             reads=[(Oe, i), (ghi, i, 0), (ghi, i, 1)], writes=[(pg_, sb_)])
                p.op("dve", lambda: nc.vector.reduce_sum(out=gs[:, :], in_=pg_[:, :, :], axis=AX.X),
                     reads=[(pg_, s) for s in range(NSB)], writes=[gs])
                p.dma("sp", gs_out[e, :].rearrange("(b q) -> q b", q=128), gs[:, :], gs, reads=[gs], writes=[("gs_out", e)],
                      allow_slow_non_contiguous=True)
                for c in range(16):
                    pdc = pd[c % 4]
                    for i in range(NTT):
                        p.op("pe", lambda: nc.tensor.matmul(pdc[:, 0:CAP], lhsT=xbf[:, i, c * 128:(c + 1) * 128], rhs=Oe[:, i, :],
                                                            start=(i == 0), stop=(i == NTT - 1)),
                             reads=[(xbf, i), (Oe, i)], writes=[pdc])
                    if c % 2 == 0:
                        p.op("act", lambda: nc.scalar.copy(out=xe[:, c, :], in_=pdc[:, 0:CAP]), reads=[pdc], writes=[(xe, c)])
                    else:
                        p.op("dve", lambda: nc.vector.tensor_copy(out=xe[:, c, :], in_=pdc[:, 0:CAP]), reads=[pdc], writes=[(xe, c)])
                p.dma("sp", xeT_out[e, :, :].rearrange("(c q) s -> q c s", q=128), xe[:, :, :], xe,
                      reads=[(xe, c) for c in range(16)], writes=[("xeT_out", e)])
            p.barrier()


def emit_experts(p, xeT_in, gs_in, y_out, W):
    nc = p.nc
    HS = XCAP // 2
    NCB = HS // 512
    NSBH = HS // 128
    with ExitStack() as es:
        NWB = 4
        wbuf = [p.sb(f"wb{i}", [128, 16, 512], BF16, es) for i in range(NWB)]
        xeT = p.sb("xeT", [128, 16, HS], BF16, es)
        actT = p.sb("actT", [128, 16, HS], BF16, es)
        bgu = [p.sb(f"bgu{i}", [128, 32], F32, es) for i in range(2)]
        bdn = [p.sb(f"bdn{i}", [1, D], F32, es) for i in range(2)]
        gsl = [p.sb(f"gsl{i}", [128, XCAP // 128], F32, es) for i in range(2)]
        gt = [p.sb(f"gt{i}", [128, 512], F32, es) for i in range(2)]
        sg = [p.sb(f"sg{i}", [128, 512], F32, es) for i in range(2)]
        u2 = [p.sb(f"u2{i}", [128, 512], F32, es) for i in range(2)]
        ysb = [p.sb(f"ysb{i}", [128, 512], F32, es) for i in range(4)]
        ones_f = p.sb("ones_f", [1, 128], F32, es)
        pg = [p.ps(f"pg{i}", [128, 512], F32, es) for i in range(2)]
        pu = [p.ps(f"pu{i}", [128, 512], F32, es) for i in range(2)]
        pdn = [p.ps(f"pdn{i}", [128, 512], F32, es) for i in range(3)]
        p.op("dve", lambda: nc.vector.memset(ones_f[:, :], 1.0), writes=[ones_f])
        cnt = {"w": 0, "y": 0, "g": 0, "d": 0}

        def load_w(src_ap):
            wb = wbuf[cnt["w"] % NWB]
            cnt["w"] += 1
            p.dma("pool", wb[:, :, :], src_ap, wb, writes=[wb])
            return wb

        for e in range(NLOC):
            bg = bgu[e % 2]
            bd = bdn[e % 2]
            gs = gsl[e % 2]
            p.dma("sp", bg[:, :], W["b_gu"][e], bg, writes=[bg])
            p.dma("sp", bd[:, :], W["b_dn"][e:e + 1, :], bd, writes=[bd])
            p.dma("sp", gs[:, :], gs_in[e], gs, writes=[gs])
            for h in range(2):
                p.dma("sp", xeT[:, :, :], xeT_in[e, :, h * HS:(h + 1) * HS].rearrange("(c q) s -> q c s", q=128), xeT, writes=[xeT])
                for j in range(4):
                    wg = load_w(W["w_gu"][e, :, j * 512:(j + 1) * 512].rearrange("(c q) f -> q c f", q=128))
                    wu = load_w(W["w_gu"][e, :, D + j * 512:D + (j + 1) * 512].rearrange("(c q) f -> q c f", q=128))
                    for f in range(4):
                        fc = j * 4 + f
                        for cb in range(NCB):
                            k = cnt["g"] % 2
                            cnt["g"] += 1
                            sl = slice(cb * 512, (cb + 1) * 512)
                            for c in range(16):
                                p.op("pe", lambda: nc.tensor.matmul(pg[k][:, :], lhsT=wg[:, c, f * 128:(f + 1) * 128], rhs=xeT[:, c, sl],
                                                                    start=(c == 0), stop=(c == 15)), reads=[wg, xeT], writes=[pg[k]])
                            for c in range(16):
                                p.op("pe", lambda: nc.tensor.matmul(pu[k][:, :], lhsT=wu[:, c, f * 128:(f + 1) * 128], rhs=xeT[:, c, sl],
                                                                    start=(c == 0), stop=(c == 15)), reads=[wu, xeT], writes=[pu[k]])
                            p.op("dve", lambda: nc.vector.tensor_scalar(out=gt[k][:, :], in0=pg[k][:, :], scalar1=bg[:, fc:fc + 1], scalar2=7.0,
                                                                        op0=ALU.add, op1=ALU.min), reads=[pg[k], bg], writes=[gt[k]])
                            p.op("act", lambda: nc.scalar.activation(out=sg[k][:, :], in_=gt[k][:, :], func=AF.Sigmoid, scale=1.702),
                                 reads=[gt[k]], writes=[sg[k]])
                            p.op("dve", lambda: nc.vector.tensor_scalar(out=u2[k][:, :], in0=pu[k][:, :], scalar1=bg[:, 16 + fc:17 + fc], scalar2=7.0,
                                                                        op0=ALU.add, op1=ALU.min), reads=[pu[k], bg], writes=[u2[k]])
                            p.op("pool", lambda: nc.gpsimd.tensor_scalar(out=u2[k][:, :], in0=u2[k][:, :], scalar1=-7.0, scalar2=1.0,
                                                                         op0=ALU.max, op1=ALU.add), reads=[u2[k]], writes=[u2[k]])
                            p.op("pool", lambda: nc.gpsimd.tensor_tensor(out=u2[k][:, :], in0=u2[k][:, :], in1=gt[k][:, :], op=ALU.mult),
                                 reads=[u2[k], gt[k]], writes=[u2[k]])
                            p.op("dve", lambda: nc.vector.tensor_tensor(out=actT[:, fc, sl], in0=u2[k][:, :], in1=sg[k][:, :], op=ALU.mult),
                                 reads=[u2[k], sg[k]], writes=[(actT, fc)])
                for dc in range(4):
                    wd = load_w(W["w_dn"][e, :, dc * 512:(dc + 1) * 512].rearrange("(c q) f -> q c f", q=128))
                    for sb_ in range(NSBH):
                        pdk = pdn[cnt["d"] % 3]
                        cnt["d"] += 1
                        for fc in range(16):
                            p.op("pe", lambda: nc.tensor.matmul(pdk[:, :], lhsT=actT[:, fc, sb_ * 128:(sb_ + 1) * 128], rhs=wd[:, fc, :],
                                                                start=(fc == 0), stop=False), reads=[(actT, fc), wd], writes=[pdk])
                        p.op("pe", lambda: nc.tensor.matmul(pdk[:, :], lhsT=ones_f[0:1, :], rhs=bd[0:1, dc * 512:(dc + 1) * 512],
                                                            start=False, stop=True), reads=[ones_f, bd], writes=[pdk])
                        yb = ysb[cnt["y"] % 4]
                        cnt["y"] += 1
                        gcol = h * NSBH + sb_
                        if cnt["y"] % 2 == 0:
                            p.op("dve", lambda: nc.vector.tensor_scalar(out=yb[:, :], in0=pdk[:, :], scalar1=gs[:, gcol:gcol + 1], scalar2=None,
                                                                        op0=ALU.mult), reads=[pdk, gs], writes=[yb])
                        else:
                            p.op("act", lambda: nc.scalar.activation(out=yb[:, :], in_=pdk[:, :], func=AF.Copy, scale=gs[:, gcol:gcol + 1]),
                                 reads=[pdk, gs], writes=[yb])
                        r0 = h * HS + sb_ * 128
                        p.dma("sp", y_out[e, r0:r0 + 128, dc * 512:(dc + 1) * 512], yb[:, :], yb, reads=[yb], writes=[("y_out", e, r0, dc)])
        p.barrier()


def emit_combine(p, x_res, ybuf, idx_in, ln_g, ln_b, x_out):
    nc = p.nc
    with ExitStack() as es4:
        gbc = p.sb("gbc", [128, D], F32, es4)
        bbc = p.sb("bbc", [128, D], F32, es4)
        idx = p.sb("idxc", [128, NTT * 4], I32, es4)
        G = [[p.sb(f"G{k}_{i}", [128, D], F32, es4) for k in range(4)] for i in range(2)]
        xr = [p.sb(f"xr{i}", [128, D], F32, es4) for i in range(2)]
        zo = [p.sb(f"zo{i}", [128, D], F32, es4) for i in range(2)]
        stats = p.sb("stats", [128, 24], F32, es4)
        mv = p.sb("mv", [128, 2], F32, es4)
        rstd = p.sb("rstd", [128, 1], F32, es4)
        p.dma("sp", gbc[:, :], bcast_rows(ln_g, 128, D), gbc, writes=[gbc])
        p.dma("sp", bbc[:, :], bcast_rows(ln_b, 128, D), bbc, writes=[bbc])
        p.dma("sp", idx[:, :], idx_in, idx, writes=[idx])
        for i in range(NTT):
            Gi = G[i % 2]
            xri = xr[i % 2]
            zi = zo[i % 2]
            p.dma("sp", xri[:, :], x_res[i * 128:(i + 1) * 128, :], xri, writes=[xri])
            for k in range(4):
                p.dma("pool", None, None, Gi[k], reads=[idx], writes=[Gi[k]],
                      fn=lambda: nc.gpsimd.indirect_dma_start(out=Gi[k][:, :], out_offset=None, in_=ybuf[:, :],
                                                              in_offset=bass.IndirectOffsetOnAxis(ap=idx[:, i * 4 + k:i * 4 + k + 1], axis=0)))
            p.op("dve", lambda: nc.vector.tensor_tensor(out=Gi[0][:, :], in0=Gi[0][:, :], in1=Gi[1][:, :], op=ALU.add), reads=[Gi[0], Gi[1]], writes=[Gi[0]])
            p.op("pool", lambda: nc.gpsimd.tensor_tensor(out=Gi[2][:, :], in0=Gi[2][:, :], in1=Gi[3][:, :], op=ALU.add), reads=[Gi[2], Gi[3]], writes=[Gi[2]])
            p.op("dve", lambda: nc.vector.tensor_tensor(out=Gi[0][:, :], in0=Gi[0][:, :], in1=Gi[2][:, :], op=ALU.add), reads=[Gi[0], Gi[2]], writes=[Gi[0]])
            p.op("dve", lambda: nc.vector.scalar_tensor_tensor(out=xri[:, :], in0=xri[:, :], scalar=ALPHA, in1=Gi[0][:, :], op0=ALU.mult, op1=ALU.add),
                 reads=[xri, Gi[0]], writes=[xri])
            emit_ln(p, xri, zi, gbc, bbc, stats, mv, rstd)
            p.dma("sp", x_out[i * 128:(i + 1) * 128, :], zi[:, :], zi, reads=[zi], writes=[("xout", i)])
        p.barrier()


def make_ident(p, es):
    nc = p.nc
    ident = p.sb("ident", [128, 128], F32, es)
    p.op("dve", lambda: nc.vector.memset(ident[:, :], 1.0), writes=[ident])
    p.op("pool", lambda: nc.gpsimd.affine_select(out=ident[:, :], in_=ident[:, :], pattern=[[1, 128]], compare_op=ALU.is_equal,
                                                 fill=0.0, base=0, channel_multiplier=-1), reads=[ident], writes=[ident])
    return ident


def new_nc():
    return bass.Bass("TRN2", target_bir_lowering=False)


def din(nc, name, shape, dt=F32):
    return nc.dram_tensor(name, list(shape), dt, kind="ExternalInput").ap()


def dout(nc, name, shape, dt=F32):
    return nc.dram_tensor(name, list(shape), dt, kind="ExternalOutput").ap()


def build_dispatch_prog():
    nc = new_nc()
    x_in = din(nc, "x_in", [NT, D])
    W = {"router_w": din(nc, "router_w", [128, 16, NE]), "router_b": din(nc, "router_b", [NE])}
    xeT_out = dout(nc, "xeT_out", [NE, D, CAP], BF16)
    gs_out = dout(nc, "gs_out", [NE, CAP])
    idx_out = dout(nc, "idx_out", [128, NTT * 4], I32)
    with ExitStack() as es:
        p = P(nc, es)
        ident = make_ident(p, es)
        emit_dispatch(p, x_in, xeT_out, gs_out, idx_out, W, ident)
        p.finish()
    return nc


def build_experts_prog():
    nc = new_nc()
    xeT_in = din(nc, "xeT_in", [NLOC, D, XCAP], BF16)
    gs_in = din(nc, "gs_in", [NLOC, 128, XCAP // 128])
    W = {"w_gu": din(nc, "w_gu", [NLOC, D, 2 * D]), "b_gu": din(nc, "b_gu", [NLOC, 128, 32]),
         "w_dn": din(nc, "w_dn", [NLOC, D, D]), "b_dn": din(nc, "b_dn", [NLOC, D])}
    y_out = dout(nc, "y_out", [NLOC, XCAP, D])
    with ExitStack() as es:
        p = P(nc, es)
        emit_experts(p, xeT_in, gs_in, y_out, W)
        p.finish()
    return nc


def build_combine_prog():
    nc = new_nc()
    x_res = din(nc, "x_res", [NT, D])
    ybuf = din(nc, "ybuf", [1 + NE * CAP, D])
    idx_in = din(nc, "idx_in", [128, NTT * 4], I32)
    ln_g = din(nc, "ln_g", [D])
    ln_b = din(nc, "ln_b", [D])
    x_out = dout(nc, "x_out", [NT, D])
    with ExitStack() as es:
        p = P(nc, es)
        emit_combine(p, x_res, ybuf, idx_in, ln_g, ln_b, x_out)
        p.finish()
    return nc


def run_moe_layer(xa_cores, inputs, l):
    import ml_dtypes
    rw = np.ascontiguousarray(inputs["router_w"][l].reshape(16, 128, NE).transpose(1, 0, 2))
    rb = np.ascontiguousarray(inputs["router_b"][l])
    nc1 = build_dispatch_prog()
    r1 = run_bass_kernel_spmd(nc1, [{"x_in": xa_cores[c], "router_w": rw, "router_b": rb} for c in range(8)], core_ids=list(range(8))).results
    maps2 = []
    bgu = inputs["b_gu"][l].reshape(NE, 32, 128).transpose(0, 2, 1)
    for c in range(8):
        es_ = slice(c * NLOC, (c + 1) * NLOC)
        xe = np.concatenate([np.asarray(r1[s]["xeT_out"])[es_] for s in range(8)], axis=2)
        gs = np.concatenate([np.asarray(r1[s]["gs_out"])[es_] for s in range(8)], axis=1)
        gs = np.ascontiguousarray(gs.reshape(NLOC, XCAP // 128, 128).transpose(0, 2, 1))
        maps2.append({"xeT_in": np.ascontiguousarray(xe), "gs_in": gs,
                      "w_gu": np.ascontiguousarray(inputs["w_gu"][l][es_]), "b_gu": np.ascontiguousarray(bgu[es_]),
                      "w_dn": np.ascontiguousarray(inputs["w_dn"][l][es_]), "b_dn": np.ascontiguousarray(inputs["b_dn"][l][es_])})
    nc2 = build_experts_prog()
    r2 = run_bass_kernel_spmd(nc2, maps2, core_ids=list(range(8))).results
    yall = np.concatenate([np.asarray(r2[c]["y_out"]) for c in range(8)], axis=0)
    maps3 = []
    for c in range(8):
        yb = np.zeros((1 + NE * CAP, D), np.float32)
        yb[1:] = yall[:, c * CAP:(c + 1) * CAP, :].reshape(NE * CAP, D)
        maps3.append({"x_res": xa_cores[c], "ybuf": yb, "idx_in": np.asarray(r1[c]["idx_out"]),
                      "ln_g": np.ascontiguousarray(inputs["ln_moe_g"][l]), "ln_b": np.ascontiguousarray(inputs["ln_moe_b"][l])})
    nc3 = build_combine_prog()
    r3 = run_bass_kernel_spmd(nc3, maps3, core_ids=list(range(8))).results
    return [np.asarray(r3[c]["x_out"]) for c in range(8)]


S = 4096
BIG = 1.0e30
TWO_PI = 6.283185307179586
NQB = 16


def emit_rope_tabs(p, pos_ap, t0, n, tmp, consts):
    nc = p.nc
    posi, posf, tt, ki_, kf, C, Ss, m1 = tmp
    invf, sgn = consts
    p.dma("sp", posi[:, 0:n], bcast_rows(pos_ap[t0:t0 + n], 128, n), posi, writes=[posi])
    p.op("dve", lambda: nc.vector.tensor_copy(out=posf[:, 0:n], in_=posi[:, 0:n]), reads=[posi], writes=[posf])
    for which, off, dst in (("s", 0.0, Ss), ("c", 0.25, C)):
        p.op("dve", lambda: nc.vector.tensor_scalar(out=tt[:, 0:n], in0=posf[:, 0:n], scalar1=invf[:, 0:1], scalar2=off, op0=ALU.mult, op1=ALU.add),
             reads=[posf, invf], writes=[tt])
        p.op("dve", lambda: nc.vector.tensor_copy(out=ki_[:, 0:n], in_=tt[:, 0:n]), reads=[tt], writes=[ki_])
        p.op("dve", lambda: nc.vector.tensor_copy(out=kf[:, 0:n], in_=ki_[:, 0:n]), reads=[ki_], writes=[kf])
        p.op("dve", lambda: nc.vector.tensor_tensor(out=tt[:, 0:n], in0=tt[:, 0:n], in1=kf[:, 0:n], op=ALU.subtract), reads=[tt, kf], writes=[tt])
        p.op("dve", lambda: nc.vector.tensor_scalar(out=m1[:, 0:n], in0=tt[:, 0:n], scalar1=0.5, scalar2=None, op0=ALU.is_gt), reads=[tt], writes=[m1])
        p.op("dve", lambda: nc.vector.tensor_tensor(out=tt[:, 0:n], in0=tt[:, 0:n], in1=m1[:, 0:n], op=ALU.subtract), reads=[tt, m1], writes=[tt])
        p.op("dve", lambda: nc.vector.tensor_scalar(out=m1[:, 0:n], in0=tt[:, 0:n], scalar1=-0.5, scalar2=None, op0=ALU.is_lt), reads=[tt], writes=[m1])
        p.op("dve", lambda: nc.vector.tensor_tensor(out=tt[:, 0:n], in0=tt[:, 0:n], in1=m1[:, 0:n], op=ALU.add), reads=[tt, m1], writes=[tt])
        p.op("act", lambda: nc.scalar.activation(out=dst[:, 0:n], in_=tt[:, 0:n], func=AF.Sin, scale=TWO_PI), reads=[tt], writes=[dst])
    p.op("dve", lambda: nc.vector.tensor_scalar(out=Ss[:, 0:n], in0=Ss[:, 0:n], scalar1=sgn[:, 0:1], scalar2=None, op0=ALU.mult),
         reads=[Ss, sgn], writes=[Ss])
    return C, Ss


def emit_l0_attn(p, A):
    nc = p.nc
    SCALE = 128.0 ** -0.5
    with ExitStack() as es:
        kT = p.sb("kT", [128, 2, S], BF16, es)
        kiT = p.sb("kiT", [128, S], BF16, es)
        vtok = p.sb("vtok", [128, S // 128, 256], BF16, es)
        qT = p.sb("qT", [128, NQB, 8, 128], BF16, es)
        qiT = p.sb("qiT", [128, NQB, 4, 128], BF16, es)
        wi = p.sb("wi", [128, NQB, 4], F32, es)
        identb = p.sb("identb", [128, 128], BF16, es)
        ones_bf = p.sb("ones_bf", [128, 128], BF16, es)
        iota256 = p.sb("iota256", [128, 256], F32, es)
        qlim = p.sb("qlim", [128, NQB], F32, es)
        invf = p.sb("invf", [128, 1], F32, es)
        sgn = p.sb("sgn", [128, 1], F32, es)
        p.op("dve", lambda: nc.vector.memset(identb[:, :], 1.0), writes=[identb])
        p.op("pool", lambda: nc.gpsimd.affine_select(out=identb[:, :], in_=identb[:, :], pattern=[[1, 128]], compare_op=ALU.is_equal,
                                                     fill=0.0, base=0, channel_multiplier=-1), reads=[identb], writes=[identb])
        p.op("dve", lambda: nc.vector.memset(ones_bf[:, :], 1.0), writes=[ones_bf])
        p.op("pool", lambda: nc.gpsimd.iota(iota256[:, :], pattern=[[1, 256]], base=0, channel_multiplier=0,
                                            allow_small_or_imprecise_dtypes=True), writes=[iota256])
        p.dma("sp", qlim[:, :], A["qlimrel"], qlim, writes=[qlim])
        p.dma("sp", invf[:, :], A["invf"], invf, writes=[invf])
        p.dma("sp", sgn[:, :], A["sgn"], sgn, writes=[sgn])

        with ExitStack() as es2:
            wt = p.sb("wt", [128, 16, 512], BF16, es2)
            wtwi = p.sb("wtwi", [128, 16, 4], BF16, es2)
            xb = [p.sb(f"xb{i}", [128, 16, 512], BF16, es2) for i in range(2)]
            tmp = [p.sb("posi", [128, 512], I32, es2), p.sb("posf", [128, 512], F32, es2), p.sb("tt", [128, 512], F32, es2),
                   p.sb("ki_", [128, 512], I32, es2), p.sb("kf", [128, 512], F32, es2), p.sb("C", [128, 512], F32, es2),
                   p.sb("Ss", [128, 512], F32, es2), p.sb("m1", [128, 512], F32, es2)]
            t1 = [p.sb(f"t1_{i}", [128, 512], F32, es2) for i in range(2)]
            ysw = [p.sb(f"ysw{i}", [128, 512], F32, es2) for i in range(2)]
            py = [p.ps(f"py{i}", [128, 512], F32, es2) for i in range(2)]
            psw = [p.ps(f"psw{i}", [128, 512], F32, es2) for i in range(2)]
            pv = p.ps("pv", [128, 256], F32, es2)
            pw = p.ps("pw", [128, 4], F32, es2)
            cn = {"x": 0, "r": 0}

            def load_wt(w_ap):
                p.dma("pool", wt[:, :, :], w_ap.rearrange("(c q) f -> q c f", q=128), wt, writes=[wt])

            def load_xb(xT_ap, t0):
                b = xb[cn["x"] % 2]
                cn["x"] += 1
                p.dma("pool", b[:, :, :], xT_ap[:, t0:t0 + 512].rearrange("(c q) t -> q c t", q=128), b, writes=[b])
                return b

            def proj(ps_, oc, b):
                for c in range(16):
                    p.op("pe", lambda: nc.tensor.matmul(ps_[:, :], lhsT=wt[:, c, oc * 128:(oc + 1) * 128], rhs=b[:, c, :],
                                                        start=(c == 0), stop=(c == 15)), reads=[wt, b], writes=[ps_])

            def rope(oc_y, oc_sw, b, C, Ss, out_ap, out_key, in_view=None):
                k = cn["r"] % 2
                cn["r"] += 1
                proj(py[k], oc_y, b)
                proj(psw[k], oc_sw, b)
                p.op("dve", lambda: nc.vector.tensor_tensor(out=t1[k][:, :], in0=py[k][:, :], in1=C[:, :], op=ALU.mult), reads=[py[k], C], writes=[t1[k]])
                p.op("act", lambda: nc.scalar.copy(out=ysw[k][:, :], in_=psw[k][:, :]), reads=[psw[k]], writes=[ysw[k]])
                p.op("pool", lambda: nc.gpsimd.tensor_tensor(out=ysw[k][:, :], in0=ysw[k][:, :], in1=Ss[:, :], op=ALU.mult), reads=[ysw[k], Ss], writes=[ysw[k]])
                a0 = t1[k][:, :] if in_view is None else t1[k][:, :].rearrange(in_view, a=4)
                a1 = ysw[k][:, :] if in_view is None else ysw[k][:, :].rearrange(in_view, a=4)
                p.op("pool", lambda: nc.gpsimd.tensor_tensor(out=out_ap, in0=a0, in1=a1, op=ALU.add), reads=[t1[k], ysw[k]], writes=[out_key])

            load_wt(A["wK"])
            for blk in range(S // 512):
                b = load_xb(A["xT_all"], blk * 512)
                C, Ss = emit_rope_tabs(p, A["pos_all"], blk * 512, 512, tmp, (invf, sgn))
                for g in range(2):
                    rope(g, 2 + g, b, C, Ss, kT[:, g, blk * 512:(blk + 1) * 512], (kT, g, blk))
            load_wt(A["wKIV"])
            for blk in range(S // 512):
                b = load_xb(A["xT_all"], blk * 512)
                C, Ss = emit_rope_tabs(p, A["pos_all"], blk * 512, 512, tmp, (invf, sgn))
                rope(0, 1, b, C, Ss, kiT[:, blk * 512:(blk + 1) * 512], (kiT, blk))
                for s_ in range(4):
                    for c in range(16):
                        p.op("pe", lambda: nc.tensor.matmul(pv[:, :], lhsT=b[:, c, s_ * 128:(s_ + 1) * 128], rhs=wt[:, c, 256:512],
                                                            start=(c == 0), stop=(c == 15)), reads=[wt, b], writes=[pv])
                    p.op("act", lambda: nc.scalar.copy(out=vtok[:, blk * 4 + s_, :], in_=pv[:, :]), reads=[pv], writes=[(vtok, blk * 4 + s_)])
            for pp in range(4):
                load_wt(A["wQ"][pp])
                for blk in range(NT // 512):
                    b = load_xb(A["xT_own"], blk * 512)
                    C, Ss = emit_rope_tabs(p, A["pos_own"], blk * 512, 512, tmp, (invf, sgn))
                    for hh in range(2):
                        rope(hh, 2 + hh, b, C, Ss, qT[:, blk * 4:(blk + 1) * 4, 2 * pp + hh, :], (qT, blk, 2 * pp + hh), in_view="p (a b) -> p a b")
            p.dma("pool", wtwi[:, :, :], A["wWI"].rearrange("(c q) f -> q c f", q=128), wtwi, writes=[wtwi])
            for pp in range(2):
                load_wt(A["wQI"][pp])
                for blk in range(NT // 512):
                    b = load_xb(A["xT_own"], blk * 512)
                    C, Ss = emit_rope_tabs(p, A["pos_own"], blk * 512, 512, tmp, (invf, sgn))
                    for hh in range(2):
                        rope(hh, 2 + hh, b, C, Ss, qiT[:, blk * 4:(blk + 1) * 4, 2 * pp + hh, :], (qiT, blk, 2 * pp + hh), in_view="p (a b) -> p a b")
                    if pp == 1:
                        for s_ in range(4):
                            for c in range(16):
                                p.op("pe", lambda: nc.tensor.matmul(pw[:, :], lhsT=b[:, c, s_ * 128:(s_ + 1) * 128], rhs=wtwi[:, c, :],
                                                                    start=(c == 0), stop=(c == 15)), reads=[wtwi, b], writes=[pw])
                            p.op("act", lambda: nc.scalar.copy(out=wi[:, blk * 4 + s_, :], in_=pw[:, :]), reads=[pw], writes=[(wi, blk * 4 + s_)])
            p.barrier()

        with ExitStack() as es3:
            sc = p.sb("sc", [128, S], F32, es3)
            scw = p.sb("scw", [128, S], F32, es3)
            sel = p.sb("sel", [128, S], BF16, es3)
            selT = p.sb("selT", [128, S // 128, 128], BF16, es3)
            rl = [p.sb(f"rl{i}", [128, 512], F32, es3) for i in range(2)]
            m256 = p.sb("m256", [128, 256], F32, es3)
            pen = p.sb("pen", [128, 256], F32, es3)
            m8 = p.sb("m8", [128, 8], F32, es3)
            thr = p.sb("thr", [128, 1], F32, es3)
            PT = [p.sb(f"PT{i}", [128, 4, 128], BF16, es3) for i in range(3)]
            rden = p.sb("rden", [128, 512], F32, es3)
            pi_ = [p.ps(f"pi{i}", [128, 512], F32, es3) for i in range(2)]
            ptr = p.ps("ptr", [128, 512], BF16, es3)
            pss = [p.ps(f"pss{i}", [128, 512], F32, es3) for i in range(2)]
            pot = p.ps("pot", [128, 512], F32, es3)
            pden = p.ps("pden", [128, 512], F32, es3)
            cn2 = {"i": 0, "s": 0, "p": 0}
            for j in range(NQB):
                NK = (2 * j + 2) * 128
                NKB = NK // 128
                ncb = (NK + 511) // 512
                for cb in range(ncb):
                    w_ = min(512, NK - cb * 512)
                    cs = slice(cb * 512, cb * 512 + w_)
                    for ih in range(4):
                        k = cn2["i"] % 2
                        cn2["i"] += 1
                        p.op("pe", lambda: nc.tensor.matmul(pi_[k][:, 0:w_], lhsT=qiT[:, j, ih, :], rhs=kiT[:, cs], start=True, stop=True),
                             reads=[qiT, kiT], writes=[pi_[k]])
                        p.op("act", lambda: nc.scalar.activation(out=rl[k][:, 0:w_], in_=pi_[k][:, 0:w_], func=AF.Relu), reads=[pi_[k]], writes=[rl[k]])
                        if ih == 0:
                            p.op("dve", lambda: nc.vector.tensor_scalar(out=sc[:, cs], in0=rl[k][:, 0:w_], scalar1=wi[:, j, 0:1], scalar2=None, op0=ALU.mult),
                                 reads=[rl[k], wi], writes=[(sc, cb)])
                        else:
                            p.op("dve", lambda: nc.vector.scalar_tensor_tensor(out=sc[:, cs], in0=rl[k][:, 0:w_], scalar=wi[:, j, ih:ih + 1], in1=sc[:, cs],
                                                                               op0=ALU.mult, op1=ALU.add), reads=[rl[k], wi, (sc, cb)], writes=[(sc, cb)])
                sck = [(sc, cb) for cb in range(ncb)]
                ls = slice(NK - 256, NK)
                p.op("dve", lambda: nc.vector.tensor_scalar(out=m256[:, :], in0=iota256[:, :], scalar1=qlim[:, j:j + 1], scalar2=None, op0=ALU.is_lt),
                     reads=[iota256, qlim], writes=[m256])
                p.op("dve", lambda: nc.vector.tensor_scalar(out=pen[:, :], in0=m256[:, :], scalar1=BIG, scalar2=-BIG, op0=ALU.mult, op1=ALU.add),
                     reads=[m256], writes=[pen])
                p.op("dve", lambda: nc.vector.tensor_tensor(out=sc[:, ls], in0=sc[:, ls], in1=m256[:, :], op=ALU.mult), reads=sck + [m256], writes=sck)
                p.op("dve", lambda: nc.vector.tensor_tensor(out=sc[:, ls], in0=sc[:, ls], in1=pen[:, :], op=ALU.add), reads=sck + [pen], writes=sck)
                if NK > 256:
                    cur = sc
                    for r in range(32):
                        p.op("dve", lambda: nc.vector.max(out=m8[:, :], in_=cur[:, 0:NK]), reads=sck + [scw], writes=[m8])
                        if r < 31:
                            p.op("dve", lambda: nc.vector.match_replace(out=scw[:, 0:NK], in_to_replace=m8[:, :], in_values=cur[:, 0:NK], imm_value=-BIG),
                                 reads=sck + [m8, scw], writes=[scw])
                            cur = scw
                    p.op("dve", lambda: nc.vector.tensor_scalar(out=thr[:, :], in0=m8[:, 7:8], scalar1=-BIG / 2, scalar2=None, op0=ALU.max),
                         reads=[m8], writes=[thr])
                else:
                    p.op("dve", lambda: nc.vector.memset(thr[:, :], -BIG / 2), writes=[thr])
                p.op("dve", lambda: nc.vector.tensor_scalar(out=sel[:, 0:NK], in0=sc[:, 0:NK], scalar1=thr[:, 0:1], scalar2=None, op0=ALU.is_ge),
                     reads=sck + [thr], writes=[sel])
                for kb0 in range(0, NKB, 4):
                    nb = min(4, NKB - kb0)
                    for t_ in range(nb):
                        kb = kb0 + t_
                        p.op("pe", lambda: nc.tensor.transpose(out=ptr[:, t_ * 128:(t_ + 1) * 128], in_=sel[:, kb * 128:(kb + 1) * 128], identity=identb[:, :]),
                             reads=[sel, identb], writes=[ptr])
                    p.op("act", lambda: nc.scalar.copy(out=selT[:, kb0:kb0 + nb, :], in_=ptr[:, 0:nb * 128].rearrange("p (a b) -> p a b", b=128)),
                         reads=[ptr], writes=[(selT, kb0 // 4)])
                for g in range(2):
                    for kb in range(NKB):
                        ks_ = cn2["s"] % 2
                        cn2["s"] += 1
                        pk = cn2["p"] % 3
                        cn2["p"] += 1
                        p.op("pe", lambda: nc.tensor.matmul(pss[ks_][:, :], lhsT=kT[:, g, kb * 128:(kb + 1) * 128], rhs=qT[:, j, 4 * g:4 * g + 4, :],
                                                            start=True, stop=True), reads=[kT, (qT, j, g)], writes=[pss[ks_]])
                        p.op("act", lambda: nc.scalar.activation(out=PT[pk][:, :, :], in_=pss[ks_][:, :].rearrange("p (a b) -> p a b", a=4),
                                                                 func=AF.Exp, scale=SCALE), reads=[pss[ks_]], writes=[PT[pk]])
                        eng = "pool" if kb % 2 == 0 else "dve"
                        eo = nc.gpsimd if eng == "pool" else nc.vector
                        p.op(eng, lambda: eo.tensor_tensor(out=PT[pk][:, :, :], in0=PT[pk][:, :, :],
                                                           in1=selT[:, kb, :].unsqueeze(1).to_broadcast([128, 4, 128]), op=ALU.mult),
                             reads=[PT[pk], (selT, kb // 4)], writes=[PT[pk]])
                        p.op("pe", lambda: nc.tensor.matmul(pot[:, :], lhsT=vtok[:, kb, g * 128:(g + 1) * 128], rhs=PT[pk][:, :, :],
                                                            start=(kb == 0), stop=(kb == NKB - 1)), reads=[vtok, PT[pk]], writes=[pot])
                        p.op("pe", lambda: nc.tensor.matmul(pden[:, :], lhsT=ones_bf[:, :], rhs=PT[pk][:, :, :],
                                                            start=(kb == 0), stop=(kb == NKB - 1)), reads=[ones_bf, PT[pk]], writes=[pden])
                    p.op("dve", lambda: nc.vector.reciprocal(out=rden[:, :], in_=pden[:, :]), reads=[pden], writes=[rden])
                    p.op("dve", lambda: nc.vector.tensor_tensor(out=qT[:, j, 4 * g:4 * g + 4, :], in0=pot[:, :].rearrange("p (a b) -> p a b", a=4),
                                                                in1=rden[:, :].rearrange("p (a b) -> p a b", a=4), op=ALU.mult),
                         reads=[pot, rden], writes=[(qT, j, g)])
            p.dma("sp", A["aT_out"].rearrange("h d (j q) -> d j h q", q=128), qT[:, :, :, :], qT,
                  reads=[(qT, j, g) for j in range(NQB) for g in range(2)], writes=["aT_out"])
            p.barrier()


def build_l0_attn_prog():
    nc = new_nc()
    A = {"xT_all": din(nc, "xT_all", [D, S]), "xT_own": din(nc, "xT_own", [D, NT]),
         "pos_all": din(nc, "pos_all", [S], I32), "pos_own": din(nc, "pos_own", [NT], I32),
         "qlimrel": din(nc, "qlimrel", [128, NQB]), "invf": din(nc, "invf", [128, 1]), "sgn": din(nc, "sgn", [128, 1]),
         "wK": din(nc, "wK", [D, 512]), "wKIV": din(nc, "wKIV", [D, 512]),
         "wQ": din(nc, "wQ", [4, D, 512]), "wQI": din(nc, "wQI", [2, D, 512]), "wWI": din(nc, "wWI", [D, 4]),
         "aT_out": dout(nc, "aT_out", [8, 128, NT], BF16)}
    with ExitStack() as es:
        p = P(nc, es)
        emit_l0_attn(p, A)
        p.finish()
    return nc


def _sw(c0):
    return list(range(c0 + 64, c0 + 128)) + list(range(c0, c0 + 64))


def l0_attn_host(inputs):
    x = inputs["x"]
    pos = inputs["positions"]
    w = inputs["w_in0"][0]
    oq, ok, ov, oqi, oki, owi, ou = 0, 1024, 1280, 1536, 2048, 2176, 2180
    def cols(c0):
        return list(range(c0, c0 + 128))
    wK = w[:, cols(ok) + cols(ok + 128) + _sw(ok) + _sw(ok + 128)]
    wKIV = w[:, cols(oki) + _sw(oki) + list(range(ov, ov + 256))]
    wQ = np.stack([w[:, cols(oq + 256 * pp) + cols(oq + 256 * pp + 128) + _sw(oq + 256 * pp) + _sw(oq + 256 * pp + 128)] for pp in range(4)])
    wQI = np.stack([w[:, cols(oqi + 256 * pp) + cols(oqi + 256 * pp + 128) + _sw(oqi + 256 * pp) + _sw(oqi + 256 * pp + 128)] for pp in range(2)])
    wWI = w[:, owi:owi + 4]
    invf = (10000.0 ** (-(np.arange(128) % 64) * 2.0 / 128.0) / (2 * np.pi)).astype(np.float32).reshape(128, 1)
    sgn = np.where(np.arange(128) < 64, -1.0, 1.0).astype(np.float32).reshape(128, 1)
    maps = []
    for c in range(8):
        b, h = c // 2, c % 2
        own = own_tokens(h)
        tq = own.reshape(NQB, 128)
        qlim = ((tq // 64 + 1) * 64).T.astype(np.float32)
        qlimrel = qlim - (np.arange(NQB) * 256)[None, :]
        xT = np.ascontiguousarray(x[b].T)
        maps.append({"xT_all": xT, "xT_own": np.ascontiguousarray(xT[:, own]),
                     "pos_all": np.ascontiguousarray(pos[b]), "pos_own": np.ascontiguousarray(pos[b][own]),
                     "qlimrel": np.ascontiguousarray(qlimrel.astype(np.float32)), "invf": invf, "sgn": sgn,
                     "wK": np.ascontiguousarray(wK), "wKIV": np.ascontiguousarray(wKIV), "wQ": np.ascontiguousarray(wQ),
                     "wQI": np.ascontiguousarray(wQI), "wWI": np.ascontiguousarray(wWI)})
    return maps


def own_tokens(h):
    return np.concatenate([np.arange((2 * j + h) * 128, (2 * j + h + 1) * 128) for j in range(NQB)])


HALO = 16
SEG = 128 + HALO


def emit_l0_out(p, A):
    nc = p.nc
    NOH = NQB * SEG
    with ExitStack() as es:
        poolT = p.sb("poolT", [128, 8, NT], BF16, es)
        with ExitStack() as es2:
            wt = p.sb("wt", [128, 16, 512], BF16, es2)
            xb = [p.sb(f"xb{i}", [128, 16, 3 * SEG], BF16, es2) for i in range(2)]
            u = p.sb("u", [128, 4, NOH], F32, es2)
            ua = p.sb("ua", [128, NOH], F32, es2)
            ub = p.sb("ub", [128, NOH], F32, es2)
            icn = p.sb("icn", [128, 4, 128], F32, es2)
            pooled = p.sb("pooled", [128, 8, NT], BF16, es2)
            wp = p.sb("wp", [128, 4, 2, 256], BF16, es2)
            psc = p.sb("psc", [128, 8], F32, es2)
            pu_ = [p.ps(f"pu{i}", [128, 512], F32, es2) for i in range(2)]
            pm = [p.ps(f"pm{i}", [128, 512], F32, es2) for i in range(2)]
            p.dma("sp", icn[:, :, :], bass.AP(tensor=A["invcnt"].tensor, offset=A["invcnt"].offset, ap=[[0, 128], [NT, 4], [1, 128]]), icn, writes=[icn])
            p.dma("pool", wp[:, :, :, :], A["w_pool"].rearrange("g (c q) f -> q g c f", q=128), wp, writes=[wp])
            p.dma("sp", psc[:, :], A["pool_scale"], psc, writes=[psc])
            NXB = (NQB + 2) // 3
            cx = 0
            for pp in range(2):
                p.dma("pool", wt[:, :, :], A["wU"][pp].rearrange("(c q) f -> q c f", q=128), wt, writes=[wt])
                for blk in range(NXB):
                    s0 = blk * 3
                    ns = min(3, NQB - s0)
                    n = ns * SEG
                    b = xb[cx % 2]
                    cx += 1
                    p.dma("pool", b[:, :, 0:n], A["xT_oh"][:, s0 * SEG:s0 * SEG + n].rearrange("(c q) t -> q c t", q=128), b, writes=[b])
                    for oc in range(4):
                        ps_ = pu_[oc % 2]
                        for c in range(16):
                            p.op("pe", lambda: nc.tensor.matmul(ps_[:, 0:n], lhsT=wt[:, c, oc * 128:(oc + 1) * 128], rhs=b[:, c, 0:n],
                                                                start=(c == 0), stop=(c == 15)), reads=[wt, b], writes=[ps_])
                        if oc % 2 == 0:
                            p.op("act", lambda: nc.scalar.copy(out=u[:, oc, s0 * SEG:s0 * SEG + n], in_=ps_[:, 0:n]), reads=[ps_], writes=[(u, oc)])
                        else:
                            p.op("dve", lambda: nc.vector.tensor_copy(out=u[:, oc, s0 * SEG:s0 * SEG + n], in_=ps_[:, 0:n]), reads=[ps_], writes=[(u, oc)])
                for oc in range(4):
                    ch = pp * 4 + oc
                    g = ch // 2
                    cur = None
                    bufs = [ua, ub]
                    eng = "dve" if ch % 2 == 0 else "pool"
                    eo = nc.vector if eng == "dve" else nc.gpsimd
                    for st in range(g + 1):
                        sh = 1 << st
                        dst = bufs[st % 2]
                        s3 = (u[:, oc, :] if cur is None else cur[:, :]).rearrange("p (j s) -> p j s", s=SEG)
                        d3 = dst[:, :].rearrange("p (j s) -> p j s", s=SEG)
                        p.op(eng, lambda: eo.tensor_tensor(out=d3[:, :, sh:SEG], in0=s3[:, :, sh:SEG], in1=s3[:, :, 0:SEG - sh], op=ALU.add),
                             reads=[(u, oc) if cur is None else cur], writes=[dst])
                        cur = dst
                    c3 = cur[:, :].rearrange("p (j s) -> p j s", s=SEG)[:, :, HALO:SEG]
                    u3 = u[:, oc, :].rearrange("p (j s) -> p j s", s=SEG)[:, :, HALO:SEG]
                    o3 = pooled[:, ch, :].rearrange("p (j s) -> p j s", s=128)
                    p.op(eng, lambda: eo.tensor_tensor(out=c3[:, 0, :], in0=c3[:, 0, :], in1=icn[:, g, :], op=ALU.mult), reads=[cur, icn], writes=[cur])
                    p.op(eng, lambda: eo.tensor_scalar(out=c3[:, 1:NQB, :], in0=c3[:, 1:NQB, :], scalar1=1.0 / (2 << g), scalar2=None, op0=ALU.mult),
                         reads=[cur], writes=[cur])
                    p.op(eng, lambda: eo.tensor_tensor(out=o3, in0=c3, in1=u3, op=ALU.subtract), reads=[cur, (u, oc)], writes=[(pooled, ch)])
            km = 0
            for g in range(4):
                for do in range(2):
                    for tb in range(NT // 512):
                        ps_ = pm[km % 2]
                        km += 1
                        for cc in range(2):
                            p.op("pe", lambda: nc.tensor.matmul(ps_[:, :], lhsT=wp[:, g, cc, do * 128:(do + 1) * 128], rhs=pooled[:, 2 * g + cc, tb * 512:(tb + 1) * 512],
                                                                start=(cc == 0), stop=(cc == 1)), reads=[wp, (pooled, 2 * g + cc)], writes=[ps_])
                        ch = 2 * g + do
                        p.op("dve", lambda: nc.vector.tensor_scalar(out=poolT[:, ch, tb * 512:(tb + 1) * 512], in0=ps_[:, :], scalar1=psc[:, ch:ch + 1], scalar2=None,
                                                                    op0=ALU.mult), reads=[ps_, psc], writes=[(poolT, ch)])
            p.barrier()
        with ExitStack() as es3:
            wo = p.sb("wo", [128, 16, D], BF16, es3)
            aT = p.sb("aT", [128, 8, NT], BF16, es3)
            p.dma("sp", aT[:, :, :], A["aT"].rearrange("h d t -> d h t"), aT, writes=[aT])
            gbc = p.sb("gbc", [128, D], F32, es3)
            bbc = p.sb("bbc", [128, D], F32, es3)
            xr = [p.sb(f"xr{i}", [128, D], F32, es3) for i in range(2)]
            zo = [p.sb(f"zo{i}", [128, D], F32, es3) for i in range(2)]
            stats = p.sb("stats", [128, 24], F32, es3)
            mv = p.sb("mv", [128, 2], F32, es3)
            rstd = p.sb("rstd", [128, 1], F32, es3)
            po = [p.ps(f"po{i}", [128, 512], F32, es3) for i in range(4)]
            for c4 in range(4):
                p.dma("pool", wo[:, c4 * 4:(c4 + 1) * 4, :], A["w_out"][c4 * 512:(c4 + 1) * 512, :].rearrange("(c q) f -> q c f", q=128), wo, writes=[(wo, c4)])
            p.dma("sp", gbc[:, :], bcast_rows(A["ln_g"], 128, D), gbc, writes=[gbc])
            p.dma("sp", bbc[:, :], bcast_rows(A["ln_b"], 128, D), bbc, writes=[bbc])
            wok = [(wo, c4) for c4 in range(4)]
            for i in range(NTT):
                xri = xr[i % 2]
                zi = zo[i % 2]
                p.dma("sp", xri[:, :], A["x_own"][i * 128:(i + 1) * 128, :], xri, writes=[xri])
                for dc in range(4):
                    for fc in range(16):
                        src = aT if fc < 8 else poolT
                        p.op("pe", lambda: nc.tensor.matmul(po[dc][:, :], lhsT=src[:, fc % 8, i * 128:(i + 1) * 128], rhs=wo[:, fc, dc * 512:(dc + 1) * 512],
                                                            start=(fc == 0), stop=(fc == 15)), reads=wok + [aT] + [(poolT, ch) for ch in range(8)], writes=[po[dc]])
                    p.op("dve", lambda: nc.vector.scalar_tensor_tensor(out=xri[:, dc * 512:(dc + 1) * 512], in0=xri[:, dc * 512:(dc + 1) * 512], scalar=ALPHA,
                                                                       in1=po[dc][:, :], op0=ALU.mult, op1=ALU.add), reads=[xri, po[dc]], writes=[xri])
                emit_ln(p, xri, zi, gbc, bbc, stats, mv, rstd)
                p.dma("sp", A["xa_out"][i * 128:(i + 1) * 128, :], zi[:, :], zi, reads=[zi], writes=[("xa_out", i)])
            p.barrier()


def build_l0_out_prog():
    nc = new_nc()
    A = {"xT_oh": din(nc, "xT_oh", [D, NQB * SEG]), "wU": din(nc, "wU", [2, D, 512]), "invcnt": din(nc, "invcnt", [4, NT]),
         "w_pool": din(nc, "w_pool", [4, 256, 256]), "pool_scale": din(nc, "pool_scale", [128, 8]),
         "aT": din(nc, "aT", [8, 128, NT], BF16), "w_out": din(nc, "w_out", [D, D]), "x_own": din(nc, "x_own", [NT, D]),
         "ln_g": din(nc, "ln_g", [D]), "ln_b": din(nc, "ln_b", [D]), "xa_out": dout(nc, "xa_out", [NT, D])}
    with ExitStack() as es:
        p = P(nc, es)
        emit_l0_out(p, A)
        p.finish()
    return nc


def l0_out_host(inputs, aT_cores):
    x = inputs["x"]
    w = inputs["w_in0"][0]
    ou = 2180
    wU = np.ascontiguousarray(np.stack([w[:, ou + 512 * pp:ou + 512 * (pp + 1)] for pp in range(2)]))
    psc = np.ascontiguousarray(inputs["pool_scale"][0].reshape(8, 128).T)
    maps = []
    for c in range(8):
        b, h = c // 2, c % 2
        own = own_tokens(h)
        xoh = np.zeros((D, NQB * SEG), np.float32)
        for j in range(NQB):
            t0 = (2 * j + h) * 128
            lo = t0 - HALO
            if lo >= 0:
                xoh[:, j * SEG:(j + 1) * SEG] = x[b, lo:t0 + 128].T
            else:
                xoh[:, j * SEG + HALO:(j + 1) * SEG] = x[b, t0:t0 + 128].T
        invcnt = np.stack([1.0 / np.minimum(own + 1.0, float(wd)) for wd in (2, 4, 8, 16)]).astype(np.float32)
        maps.append({"xT_oh": xoh, "wU": wU, "invcnt": np.ascontiguousarray(invcnt), "w_pool": np.ascontiguousarray(inputs["w_pool"][0]),
                     "pool_scale": psc, "aT": aT_cores[c], "w_out": np.ascontiguousarray(inputs["w_out0"][0]),
                     "x_own": np.ascontiguousarray(x[b][own]), "ln_g": np.ascontiguousarray(inputs["ln_mix_g"][0]),
                     "ln_b": np.ascontiguousarray(inputs["ln_mix_b"][0])})
    return maps


HL = 4
NTILE = S // 128


def emit_l1_mlstm(p, A):
    nc = p.nc
    with ExitStack() as es:
        qT = p.sb("qT", [128, HL, S], BF16, es)
        kT = p.sb("kT", [128, HL, S], BF16, es)
        with ExitStack() as es2:
            wt = p.sb("wt", [128, 16, 512], BF16, es2)
            xb = [p.sb(f"xb{i}", [128, 16, 512], BF16, es2) for i in range(2)]
            raw = [p.sb(f"raw{i}", [128, 4, 3 + 512], F32, es2) for i in range(2)]
            yv = [p.sb(f"yv{i}", [128, 512], F32, es2) for i in range(2)]
            cw = p.sb("cw", [128, 8, 4], F32, es2)
            cb = p.sb("cb", [128, 8], F32, es2)
            pq = [p.ps(f"pq{i}", [128, 512], F32, es2) for i in range(2)]
            p.dma("sp", cw[:, :, :], A["conv_w"], cw, writes=[cw])
            p.dma("sp", cb[:, :], A["conv_b"], cb, writes=[cb])
            cx = 0
            for pp in range(2):
                p.dma("pool", wt[:, :, :], A["wQK"][pp].rearrange("(c q) f -> q c f", q=128), wt, writes=[wt])
                dstT = qT if pp == 0 else kT
                for blk in range(S // 512):
                    b = xb[cx % 2]
                    rw = raw[cx % 2]
                    rprev = raw[(cx + 1) % 2]
                    cx += 1
                    p.dma("pool", b[:, :, :], A["xT"][:, blk * 512:(blk + 1) * 512].rearrange("(c q) t -> q c t", q=128), b, writes=[b])
                    if blk == 0:
                        p.op("dve", lambda: nc.vector.memset(rw[:, :, 0:3], 0.0), writes=[(rw, "h")])
                    else:
                        p.op("dve", lambda: nc.vector.tensor_copy(out=rw[:, :, 0:3], in_=rprev[:, :, 512:515]),
                             reads=[(rprev, o) for o in range(4)], writes=[(rw, "h")])
                    for oc in range(4):
                        ps_ = pq[oc % 2]
                        for c in range(16):
                            p.op("pe", lambda: nc.tensor.matmul(ps_[:, :], lhsT=wt[:, c, oc * 128:(oc + 1) * 128], rhs=b[:, c, :],
                                                                start=(c == 0), stop=(c == 15)), reads=[wt, b], writes=[ps_])
                        p.op("act", lambda: nc.scalar.copy(out=rw[:, oc, 3:515], in_=ps_[:, :]), reads=[ps_], writes=[(rw, oc)])
                        ch = pp * 4 + oc
                        y = yv[oc % 2]
                        eng = "dve" if oc % 2 == 0 else "pool"
                        eo = nc.vector if eng == "dve" else nc.gpsimd
                        p.op("dve", lambda: nc.vector.tensor_scalar(out=y[:, :], in0=rw[:, oc, 3:515], scalar1=cw[:, ch, 3:4], scalar2=cb[:, ch:ch + 1],
                                                                    op0=ALU.mult, op1=ALU.add), reads=[(rw, oc), cw, cb], writes=[y])
                        for tap in range(3):
                            p.op("dve", lambda: nc.vector.scalar_tensor_tensor(out=y[:, :], in0=rw[:, oc, tap:tap + 512], scalar=cw[:, ch, tap:tap + 1], in1=y[:, :],
                                                                               op0=ALU.mult, op1=ALU.add), reads=[(rw, oc), (rw, "h"), cw, y], writes=[y])
                        if pp == 0:
                            p.op("act", lambda: nc.scalar.activation(out=y[:, :], in_=y[:, :], func=AF.Silu), reads=[y], writes=[y])
                            p.op("pool", lambda: nc.gpsimd.tensor_scalar(out=dstT[:, oc, blk * 512:(blk + 1) * 512], in0=y[:, :], scalar1=128.0 ** -0.5, scalar2=None,
                                                                         op0=ALU.mult), reads=[y], writes=[(dstT, oc, blk)])
                        else:
                            p.op("act", lambda: nc.scalar.activation(out=dstT[:, oc, blk * 512:(blk + 1) * 512], in_=y[:, :], func=AF.Silu), reads=[y], writes=[(dstT, oc, blk)])
            p.barrier()
        with ExitStack() as es3:
            wv = p.sb("wv", [128, 16, 2056], BF16, es3)
            xb = [p.sb(f"xt{i}", [128, 16, 128], BF16, es3) for i in range(2)]
            vext = [p.sb(f"vext{i}", [128, HL, 258], BF16, es3) for i in range(2)]
            ogs = p.sb("ogs", [128, 1024], F32, es3)
            gb = p.sb("gb", [128, 8], F32, es3)
            cng = p.sb("cng", [128, 1024], F32, es3)
            g8 = p.sb("g8", [128, 8], F32, es3)
            e4 = p.sb("e4", [128, 4], F32, es3)
            lf4 = p.sb("lf4", [128, 4], F32, es3)
            a4 = p.sb("a4", [128, 4], F32, es3)
            ei4 = p.sb("ei4", [128, 4], F32, es3)
            tri2 = p.sb("tri2", [128, 128], F32, es3)
            mask2 = p.sb("mask2", [128, 128], F32, es3)
            ind2 = p.sb("ind2", [128, 2], F32, es3)
            ones_f = p.sb("ones_f", [128, 128], F32, es3)
            identb = p.sb("identb", [128, 128], BF16, es3)
            lfb = [p.sb(f"lfb{i}", [128, 128], F32, es3) for i in range(2)]
            E = [p.sb(f"E{i}", [128, 128], F32, es3) for i in range(2)]
            SmT = [p.sb(f"SmT{i}", [128, 128], BF16, es3) for i in range(2)]
            dec2 = [p.sb(f"dec2{i}", [128, 2], F32, es3) for i in range(2)]
            bLo = [p.sb(f"bLo{i}", [128, 1], F32, es3) for i in range(2)]
            wgt = [p.sb(f"wgt{i}", [128, 1], F32, es3) for i in range(2)]
            kw = [p.sb(f"kw{i}", [128, 128], BF16, es3) for i in range(2)]
            intra = [p.sb(f"intra{i}", [128, 257], F32, es3) for i in range(2)]
            num = [p.sb(f"num{i}", [128, 257], F32, es3) for i in range(2)]
            den = [p.sb(f"den{i}", [128, 1], F32, es3) for i in range(2)]
            hh = [p.sb(f"hh{i}", [128, 256], F32, es3) for i in range(2)]
            st6 = [p.sb(f"st6{i}", [128, 6], F32, es3) for i in range(2)]
            mv = [p.sb(f"mv{i}", [128, 2], F32, es3) for i in range(2)]
            rs = [p.sb(f"rs{i}", [128, 1], F32, es3) for i in range(2)]
            CT = [p.sb(f"CT{h}", [128, 257], F32, es3) for h in range(HL)]
            CTb = [p.sb(f"CTb{h}", [128, 257], BF16, es3) for h in range(HL)]
            y = [p.sb(f"y{i}", [128, 1024], F32, es3) for i in range(2)]
            ybf = [p.sb(f"ybf{i}", [128, 1024], BF16, es3) for i in range(2)]
            yT = [p.sb(f"yT{i}", [128, 8, 128], BF16, es3) for i in range(2)]
            P1 = p.ps("P1", [128, 512], F32, es3)
            P3 = p.ps("P3", [128, 64], F32, es3)
            P4 = p.ps("P4", [128, 4, 128], F32, es3)
            P5 = p.ps("P5", [128, 4, 128], F32, es3)
            P6 = p.ps("P6", [128, 8, 128], BF16, es3)
            Pin = p.ps("Pin", [128, 257], F32, es3)
            Pit = p.ps("Pit", [128, 257], F32, es3)
            Pup = p.ps("Pup", [128, 257], F32, es3)

            for c4 in range(4):
                p.dma("pool", wv[:, c4 * 4:(c4 + 1) * 4, :], A["wVOG"][c4 * 512:(c4 + 1) * 512, :].rearrange("(c q) f -> q c f", q=128), wv, writes=[(wv, c4)])
            wvk = [(wv, c4) for c4 in range(4)]
            p.dma("sp", gb[:, :], bcast_rows(A["gate_b"], 128, 8), gb, writes=[gb])
            p.dma("sp", cng[:, :], bcast_rows(A["c_norm_g"], 128, 1024), cng, writes=[cng])
            p.op("dve", lambda: nc.vector.memset(ones_f[:, :], 1.0), writes=[ones_f])
            p.op("dve", lambda: nc.vector.memset(mask2[:, :], 1.0), writes=[mask2])
            p.op("pool", lambda: nc.gpsimd.affine_select(out=mask2[:, :], in_=mask2[:, :], pattern=[[1, 128]], compare_op=ALU.is_ge,
                                                         fill=0.0, base=0, channel_multiplier=-1), reads=[mask2], writes=[mask2])
            p.op("dve", lambda: nc.vector.memset(mask2[0:64, 64:128], 0.0), reads=[mask2], writes=[mask2])
            p.op("dve", lambda: nc.vector.tensor_copy(out=tri2[:, :], in_=mask2[:, :]), reads=[mask2], writes=[tri2])
            p.op("dve", lambda: nc.vector.memset(ind2[:, :], 0.0), writes=[ind2])
            p.op("dve", lambda: nc.vector.memset(ind2[0:64, 0:1], 1.0), reads=[ind2], writes=[ind2])
            p.op("dve", lambda: nc.vector.memset(ind2[64:128, 1:2], 1.0), reads=[ind2], writes=[ind2])
            p.op("dve", lambda: nc.vector.memset(identb[:, :], 1.0), writes=[identb])
            p.op("pool", lambda: nc.gpsimd.affine_select(out=identb[:, :], in_=identb[:, :], pattern=[[1, 128]], compare_op=ALU.is_equal,
                                                         fill=0.0, base=0, channel_multiplier=-1), reads=[identb], writes=[identb])
            for h in range(HL):
                p.op("dve", lambda: nc.vector.memset(CT[h][:, :], 0.0), writes=[CT[h]])
                p.op("dve", lambda: nc.vector.memset(CTb[h][:, :], 0.0), writes=[CTb[h]])
            for i2 in range(2):
                p.op("dve", lambda: nc.vector.memset(vext[i2][:, :, 256:258], 1.0), writes=[(vext[i2], "one")])

            for i in range(NTILE):
                xt = xb[i % 2]
                ve = vext[i % 2]
                yi = y[i % 2]
                ts_ = slice(i * 128, (i + 1) * 128)
                p.dma("pool", xt[:, :, :], A["xT"][:, ts_].rearrange("(c q) t -> q c t", q=128), xt, writes=[xt])
                for vb in range(2):
                    for c in range(16):
                        p.op("pe", lambda: nc.tensor.matmul(P1[:, :], lhsT=xt[:, c, :], rhs=wv[:, c, vb * 512:(vb + 1) * 512], start=(c == 0), stop=(c == 15)),
                             reads=[xt] + wvk, writes=[P1])
                    p.op("act", lambda: nc.scalar.copy(out=ve[:, 2 * vb:2 * vb + 2, 0:256], in_=P1[:, :].rearrange("p (h d) -> p h d", h=2)),
                         reads=[P1], writes=[(ve, vb)])
                for ob in range(2):
                    for c in range(16):
                        p.op("pe", lambda: nc.tensor.matmul(P1[:, :], lhsT=xt[:, c, :], rhs=wv[:, c, 1024 + ob * 512:1024 + (ob + 1) * 512], start=(c == 0), stop=(c == 15)),
                             reads=[xt] + wvk, writes=[P1])
                    p.op("act", lambda: nc.scalar.activation(out=ogs[:, ob * 512:(ob + 1) * 512], in_=P1[:, :], func=AF.Sigmoid), reads=[P1], writes=[(ogs, ob)])
                for c in range(16):
                    p.op("pe", lambda: nc.tensor.matmul(P3[:, 0:8], lhsT=xt[:, c, :], rhs=wv[:, c, 2048:2056], start=(c == 0), stop=(c == 15)),
                         reads=[xt] + wvk, writes=[(P3, "g")])
                p.op("dve", lambda: nc.vector.tensor_tensor(out=g8[:, :], in0=P3[:, 0:8], in1=gb[:, :], op=ALU.add), reads=[(P3, "g"), gb], writes=[g8])
                p.op("act", lambda: nc.scalar.activation(out=e4[:, :], in_=g8[:, 4:8], func=AF.Exp, scale=-1.0), reads=[g8], writes=[e4])
                p.op("act", lambda: nc.scalar.activation(out=e4[:, :], in_=e4[:, :], func=AF.Ln, bias=1.0), reads=[e4], writes=[e4])
                p.op("dve", lambda: nc.vector.tensor_scalar(out=lf4[:, :], in0=e4[:, :], scalar1=-1.0, scalar2=None, op0=ALU.mult), reads=[e4], writes=[lf4])
                p.op("pe", lambda: nc.tensor.matmul(P3[:, 8:12], lhsT=tri2[:, :], rhs=lf4[:, :], start=True, stop=True), reads=[tri2, lf4], writes=[(P3, "b")])
                p.op("dve", lambda: nc.vector.tensor_tensor(out=a4[:, :], in0=g8[:, 0:4], in1=P3[:, 8:12], op=ALU.subtract), reads=[g8, (P3, "b")], writes=[a4])
                p.op("act", lambda: nc.scalar.activation(out=ei4[:, :], in_=P3[:, 8:12], func=AF.Exp), reads=[(P3, "b")], writes=[ei4])
                for h in range(HL):
                    k2 = h % 2
                    p.op("pool", lambda: nc.gpsimd.tensor_scalar(out=lfb[k2][:, :], in0=ones_f[:, :], scalar1=lf4[:, h:h + 1], scalar2=None, op0=ALU.mult),
                         reads=[ones_f, lf4], writes=[lfb[k2]])
                    p.op("pe", lambda: nc.tensor.matmul(P4[:, h, :], lhsT=lfb[k2][:, :], rhs=tri2[:, :], start=True, stop=True), reads=[lfb[k2], tri2], writes=[(P4, h)])
                    p.op("pe", lambda: nc.tensor.matmul(P3[:, 16 + 2 * h:18 + 2 * h], lhsT=lfb[k2][:, :], rhs=ind2[:, :], start=True, stop=True),
                         reads=[lfb[k2], ind2], writes=[(P3, "L", h)])
                    p.op("act", lambda: nc.scalar.activation(out=E[k2][:, :], in_=P4[:, h, :], func=AF.Exp, bias=a4[:, h:h + 1]), reads=[(P4, h), a4], writes=[E[k2]])
                    p.op("pool", lambda: nc.gpsimd.tensor_tensor(out=E[k2][:, :], in0=E[k2][:, :], in1=mask2[:, :], op=ALU.mult), reads=[E[k2], mask2], writes=[E[k2]])
                    p.op("pe", lambda: nc.tensor.matmul(P5[:, h, :], lhsT=kT[:, h, ts_], rhs=qT[:, h, ts_], start=True, stop=True), reads=[kT, qT], writes=[(P5, h)])
                    p.op("dve", lambda: nc.vector.tensor_tensor(out=SmT[k2][:, :], in0=P5[:, h, :], in1=E[k2][:, :], op=ALU.mult), reads=[(P5, h), E[k2]], writes=[SmT[k2]])
                    p.op("act", lambda: nc.scalar.activation(out=dec2[k2][:, :], in_=P3[:, 16 + 2 * h:18 + 2 * h], func=AF.Exp), reads=[(P3, "L", h)], writes=[dec2[k2]])
                    p.op("dve", lambda: nc.vector.tensor_copy(out=bLo[k2][0:64, :], in_=P3[0:64, 16 + 2 * h:17 + 2 * h]), reads=[(P3, "L", h)], writes=[(bLo[k2], 0)])
                    p.op("dve", lambda: nc.vector.tensor_copy(out=bLo[k2][64:128, :], in_=P3[64:128, 17 + 2 * h:18 + 2 * h]), reads=[(P3, "L", h)], writes=[(bLo[k2], 1)])
                    p.op("act", lambda: nc.scalar.activation(out=wgt[k2][:, :], in_=bLo[k2][:, :], func=AF.Exp, bias=a4[:, h:h + 1]),
                         reads=[(bLo[k2], 0), (bLo[k2], 1), a4], writes=[wgt[k2]])
                    p.op("pe", lambda: nc.tensor.transpose(out=P6[:, h, :], in_=kT[:, h, ts_], identity=identb[:, :]), reads=[kT, identb], writes=[(P6, h)])
                    p.op("dve", lambda: nc.vector.tensor_scalar(out=kw[k2][:, :], in0=P6[:, h, :], scalar1=wgt[k2][:, 0:1], scalar2=None, op0=ALU.mult),
                         reads=[(P6, h), wgt[k2]], writes=[kw[k2]])
                    p.op("pe", lambda: nc.tensor.matmul(Pit[:, :], lhsT=SmT[k2][:, :], rhs=ve[:, h, 0:257], start=True, stop=True),
                         reads=[SmT[k2], (ve, h // 2), (ve, "one")], writes=[Pit])
                    p.op("act", lambda: nc.scalar.copy(out=intra[k2][:, :], in_=Pit[:, :]), reads=[Pit], writes=[intra[k2]])
                    for ck in range(2):
                        ps_l = slice(ck * 64, (ck + 1) * 64)
                        p.op("pe", lambda: nc.tensor.matmul(Pin[:, :], lhsT=qT[:, h, ts_], rhs=CTb[h][:, :], start=True, stop=True), reads=[qT, CTb[h]], writes=[Pin])
                        p.op("dve", lambda: nc.vector.scalar_tensor_tensor(out=num[k2][ps_l, :], in0=Pin[ps_l, :], scalar=ei4[ps_l, h:h + 1], in1=intra[k2][ps_l, :],
                                                                           op0=ALU.mult, op1=ALU.add), reads=[Pin, ei4, intra[k2]], writes=[(num[k2], ck)])
                        p.op("pe", lambda: nc.tensor.matmul(Pup[:, :], lhsT=kw[k2][ps_l, :], rhs=ve[ps_l, h, 0:257], start=True, stop=True),
                             reads=[kw[k2], (ve, h // 2), (ve, "one")], writes=[Pup])
                        p.op("dve", lambda: nc.vector.scalar_tensor_tensor(out=CT[h][:, :], in0=CT[h][:, :], scalar=dec2[k2][:, ck:ck + 1], in1=Pup[:, :],
                                                                           op0=ALU.mult, op1=ALU.add), reads=[CT[h], dec2[k2], Pup], writes=[CT[h]])
                        p.op("act", lambda: nc.scalar.copy(out=CTb[h][:, :], in_=CT[h][:, :]), reads=[CT[h]], writes=[CTb[h]])
                    numk = [(num[k2], 0), (num[k2], 1)]
                    p.op("act", lambda: nc.scalar.activation(out=den[k2][:, :], in_=num[k2][:, 256:257], func=AF.Abs), reads=numk, writes=[den[k2]])
                    p.op("dve", lambda: nc.vector.tensor_scalar(out=den[k2][:, :], in0=den[k2][:, :], scalar1=1.0, scalar2=None, op0=ALU.max),
                         reads=[den[k2]], writes=[den[k2]])
                    p.op("dve", lambda: nc.vector.reciprocal(out=den[k2][:, :], in_=den[k2][:, :]), reads=[den[k2]], writes=[den[k2]])
                    p.op("dve", lambda: nc.vector.tensor_scalar(out=hh[k2][:, :], in0=num[k2][:, 0:256], scalar1=den[k2][:, 0:1], scalar2=None, op0=ALU.mult),
                         reads=numk + [den[k2]], writes=[hh[k2]])
                    p.op("dve", lambda: nc.vector.bn_stats(out=st6[k2][:, :], in_=hh[k2][:, :]), reads=[hh[k2]], writes=[st6[k2]])
                    p.op("dve", lambda: nc.vector.bn_aggr(out=mv[k2][:, :], in_=st6[k2][:, :]), reads=[st6[k2]], writes=[mv[k2]])
                    p.op("dve", lambda: nc.vector.tensor_scalar(out=rs[k2][:, :], in0=mv[k2][:, 1:2], scalar1=LN_EPS, scalar2=None, op0=ALU.add), reads=[mv[k2]], writes=[rs[k2]])
                    p.op("act", lambda: nc.scalar.activation(out=rs[k2][:, :], in_=rs[k2][:, :], func=AF.Sqrt), reads=[rs[k2]], writes=[rs[k2]])
                    p.op("dve", lambda: nc.vector.reciprocal(out=rs[k2][:, :], in_=rs[k2][:, :]), reads=[rs[k2]], writes=[rs[k2]])
                    p.op("dve", lambda: nc.vector.tensor_scalar(out=yi[:, h * 256:(h + 1) * 256], in0=hh[k2][:, :], scalar1=mv[k2][:, 0:1], scalar2=rs[k2][:, 0:1],
                                                                op0=ALU.subtract, op1=ALU.mult), reads=[hh[k2], mv[k2], rs[k2]], writes=[(yi, h)])
                yk = [(yi, h) for h in range(HL)]
                p.op("pool", lambda: nc.gpsimd.tensor_tensor(out=yi[:, :], in0=yi[:, :], in1=cng[:, :], op=ALU.mult), reads=yk + [cng], writes=yk)
                p.op("dve", lambda: nc.vector.tensor_tensor(out=ybf[i % 2][:, :], in0=yi[:, :], in1=ogs[:, :], op=ALU.mult), reads=yk + [(ogs, 0), (ogs, 1)], writes=[ybf[i % 2]])
                for fcn in range(8):
                    p.op("pe", lambda: nc.tensor.transpose(out=P6[:, fcn, :], in_=ybf[i % 2][:, fcn * 128:(fcn + 1) * 128], identity=identb[:, :]),
                         reads=[ybf[i % 2], identb], writes=[(P6, fcn % 4)] if fcn < 4 else [(P6, "hi", fcn)])
                p.op("act", lambda: nc.scalar.copy(out=yT[i % 2][:, :, :], in_=P6[:, :, :]),
                     reads=[(P6, f_) for f_ in range(4)] + [(P6, "hi", f_) for f_ in range(4, 8)], writes=[yT[i % 2]])
                p.dma("sp", A["hgT_out"][:, ts_].rearrange("(c q) t -> q c t", q=128), yT[i % 2][:, :, :], yT[i % 2], reads=[yT[i % 2]], writes=[("hg", i)])
            p.barrier()


def build_l1_mlstm_prog():
    nc = new_nc()
    A = {"xT": din(nc, "xT", [D, S]), "wQK": din(nc, "wQK", [2, D, 512]), "wVOG": din(nc, "wVOG", [D, 2056]),
         "conv_w": din(nc, "conv_w", [128, 8, 4]), "conv_b": din(nc, "conv_b", [128, 8]), "gate_b": din(nc, "gate_b", [8]),
         "c_norm_g": din(nc, "c_norm_g", [1024]), "hgT_out": dout(nc, "hgT_out", [1024, S], BF16)}
    with ExitStack() as es:
        p = P(nc, es)
        emit_l1_mlstm(p, A)
        p.finish()
    return nc


def l1_mlstm_host(inputs, x1_full):
    w = inputs["w_in1"][0]
    cwf = inputs["conv_w"][0]
    cbf = inputs["conv_b"][0]
    maps = []
    for c in range(8):
        b, hh = c // 2, c % 2
        qc = np.arange(hh * 512, (hh + 1) * 512)
        kc = 1024 + qc
        vc = 2048 + np.arange(hh * 1024, (hh + 1) * 1024)
        igc = 4096 + np.arange(hh * 4, (hh + 1) * 4)
        fgc = 4104 + np.arange(hh * 4, (hh + 1) * 4)
        ogc = 4112 + np.arange(hh * 1024, (hh + 1) * 1024)
        wQK = np.stack([w[:, qc], w[:, kc]])
        wVOG = w[:, np.concatenate([vc, ogc, igc, fgc])]
        chans = np.concatenate([qc, kc])
        cw = cwf[:, chans].reshape(4, 8, 128).transpose(2, 1, 0)
        cb = cbf[chans].reshape(8, 128).T
        gate_b = np.concatenate([inputs["gate_i_b"][0][hh * 4:(hh + 1) * 4], inputs["gate_f_b"][0][hh * 4:(hh + 1) * 4]])
        maps.append({"xT": np.ascontiguousarray(x1_full[b].T), "wQK": np.ascontiguousarray(wQK), "wVOG": np.ascontiguousarray(wVOG),
                     "conv_w": np.ascontiguousarray(cw), "conv_b": np.ascontiguousarray(cb), "gate_b": np.ascontiguousarray(gate_b),
                     "c_norm_g": np.ascontiguousarray(inputs["c_norm_g"][0][hh * 1024:(hh + 1) * 1024])})
    return maps


def emit_l1_out(p, A):
    nc = p.nc
    with ExitStack() as es3:
        wo = p.sb("wo", [128, 16, D], BF16, es3)
        hg = p.sb("hg", [128, 16, NT], BF16, es3)
        gbc = p.sb("gbc", [128, D], F32, es3)
        bbc = p.sb("bbc", [128, D], F32, es3)
        xr = [p.sb(f"xr{i}", [128, D], F32, es3) for i in range(2)]
        zo = [p.sb(f"zo{i}", [128, D], F32, es3) for i in range(2)]
        stats = p.sb("stats", [128, 24], F32, es3)
        mv = p.sb("mv", [128, 2], F32, es3)
        rstd = p.sb("rstd", [128, 1], F32, es3)
        po = [p.ps(f"po{i}", [128, 512], F32, es3) for i in range(4)]
        p.dma("sp", hg[:, :, :], A["hgT"].rearrange("(c q) t -> q c t", q=128), hg, writes=[hg])
        for c4 in range(4):
            p.dma("pool", wo[:, c4 * 4:(c4 + 1) * 4, :], A["w_out"][c4 * 512:(c4 + 1) * 512, :].rearrange("(c q) f -> q c f", q=128), wo, writes=[(wo, c4)])
        p.dma("sp", gbc[:, :], bcast_rows(A["ln_g"], 128, D), gbc, writes=[gbc])
        p.dma("sp", bbc[:, :], bcast_rows(A["ln_b"], 128, D), bbc, writes=[bbc])
        wok = [(wo, c4) for c4 in range(4)]
        for i in range(NTT):
            xri = xr[i % 2]
            zi = zo[i % 2]
            p.dma("sp", xri[:, :], A["x_own"][i * 128:(i + 1) * 128, :], xri, writes=[xri])
            for dc in range(4):
                for fc in range(16):
                    p.op("pe", lambda: nc.tensor.matmul(po[dc][:, :], lhsT=hg[:, fc, i * 128:(i + 1) * 128], rhs=wo[:, fc, dc * 512:(dc + 1) * 512],
                                                        start=(fc == 0), stop=(fc == 15)), reads=wok + [hg], writes=[po[dc]])
                p.op("dve", lambda: nc.vector.scalar_tensor_tensor(out=xri[:, dc * 512:(dc + 1) * 512], in0=xri[:, dc * 512:(dc + 1) * 512], scalar=ALPHA,
                                                                   in1=po[dc][:, :], op0=ALU.mult, op1=ALU.add), reads=[xri, po[dc]], writes=[xri])
            emit_ln(p, xri, zi, gbc, bbc, stats, mv, rstd)
            p.dma("sp", A["xb_out"][i * 128:(i + 1) * 128, :], zi[:, :], zi, reads=[zi], writes=[("xb_out", i)])
        p.barrier()


def build_l1_out_prog():
    nc = new_nc()
    A = {"hgT": din(nc, "hgT", [D, NT], BF16), "w_out": din(nc, "w_out", [D, D]), "x_own": din(nc, "x_own", [NT, D]),
         "ln_g": din(nc, "ln_g", [D]), "ln_b": din(nc, "ln_b", [D]), "xb_out": dout(nc, "xb_out", [NT, D])}
    with ExitStack() as es:
        p = P(nc, es)
        emit_l1_out(p, A)
        p.finish()
    return nc


ALL8 = list(range(8))


def kernel(**inputs):
    inputs = {k: np.asarray(v) for k, v in inputs.items()}
    r = run_bass_kernel_spmd(build_l0_attn_prog(), l0_attn_host(inputs), core_ids=ALL8).results
    aT = [np.asarray(r[c]["aT_out"]) for c in range(8)]
    r = run_bass_kernel_spmd(build_l0_out_prog(), l0_out_host(inputs, aT), core_ids=ALL8).results
    xa = [np.asarray(r[c]["xa_out"]) for c in range(8)]
    x1 = run_moe_layer(xa, inputs, 0)
    x1_full = np.empty((4, S, D), np.float32)
    for c in range(8):
        x1_full[c // 2][own_tokens(c % 2)] = x1[c]
    r = run_bass_kernel_spmd(build_l1_mlstm_prog(), l1_mlstm_host(inputs, x1_full), core_ids=ALL8).results
    hg = [np.asarray(r[c]["hgT_out"]) for c in range(8)]
    maps = []
    for c in range(8):
        b, h = c // 2, c % 2
        hgT = np.concatenate([hg[2 * b], hg[2 * b + 1]], axis=0)[:, own_tokens(h)]
        maps.append({"hgT": np.ascontiguousarray(hgT), "w_out": np.ascontiguousarray(inputs["w_out1"][0]), "x_own": x1[c],
                     "ln_g": np.ascontiguousarray(inputs["ln_mix_g"][1]), "ln_b": np.ascontiguousarray(inputs["ln_mix_b"][1])})
    r = run_bass_kernel_spmd(build_l1_out_prog(), maps, core_ids=ALL8).results
    xb = [np.asarray(r[c]["xb_out"]) for c in range(8)]
    x2 = run_moe_layer(xb, inputs, 1)
    out = np.empty((4, S, D), np.float32)
    for c in range(8):
        out[c // 2][own_tokens(c % 2)] = x2[c]
    return out
```

```python
import numpy as np
from contextlib import ExitStack
import concourse.bass as bass
import concourse.mybir as mybir
from concourse.bass_utils import run_bass_kernel_spmd

F32 = mybir.dt.float32
BF16 = mybir.dt.bfloat16
I32 = mybir.dt.int32
U32 = mybir.dt.uint32
AF = mybir.ActivationFunctionType
ALU = mybir.AluOpType
AX = mybir.AxisListType

D = 2048
NT = 2048
NTT = NT // 128
NE = 32
CAP = 384
NSB = CAP // 128
ALPHA = float(4 ** 0.25)
LN_EPS = 1e-5


class Buf:
    def __init__(self, name, t):
        self.name = name
        self.t = t
        self.slot = None

    def __getitem__(self, k):
        return self.t[k]


NSLOT = 64


class P:
    def __init__(self, nc, es):
        self.nc = nc
        self.es = es
        self.eng = {"pe": nc.tensor, "dve": nc.vector, "act": nc.scalar, "pool": nc.gpsimd, "sp": nc.sync}
        self.sem = {k: es.enter_context(nc.semaphore("sem_" + k)) for k in self.eng}
        self.cnt = {k: 0 for k in self.eng}
        self.seen = {k: {} for k in self.eng}
        self.st = {}
        self.nbuf = 0
        self.slots = []
        self.nassign = 0
        self.cc_slot = None

    def sb(self, name, shape, dt, es=None):
        self.nbuf += 1
        t = (es or self.es).enter_context(self.nc.sbuf_tensor(f"{name}_{self.nbuf}", list(shape), dt))
        return Buf(name, t)

    def ps(self, name, shape, dt, es=None):
        self.nbuf += 1
        t = (es or self.es).enter_context(self.nc.psum_tensor(f"{name}_{self.nbuf}", list(shape), dt))
        return Buf(name, t)

    def _new_slot(self, unit):
        sem = self.es.enter_context(self.nc.semaphore(f"ds_{len(self.slots)}"))
        self.slots.append({"sem": sem, "cnt": 0, "unit": unit})
        return len(self.slots) - 1

    def _slot(self, buf):
        if buf.slot is None:
            if len(self.slots) < NSLOT:
                buf.slot = self._new_slot(16)
            else:
                cands = [i for i, s_ in enumerate(self.slots) if s_["unit"] == 16]
                buf.slot = cands[self.nassign % len(cands)]
            self.nassign += 1
        return buf.slot

    def _state(self, k):
        s = self.st.get(k)
        if s is None:
            s = {"w": {}, "r": {}}
            self.st[k] = s
        return s

    def _wait_tok(self, e, src, val):
        if isinstance(src, tuple):
            sl = self.slots[src[1]]
            val = sl["unit"] * sl["cnt"]
            sem = sl["sem"]
            key = src
        else:
            if src == e and e == "pe":
                return
            sem = self.sem[src]
            key = src
        if val <= 0:
            return
        if self.seen[e].get(key, 0) >= val:
            return
        self.eng[e].wait_ge(sem, val)
        self.seen[e][key] = val

    def _deps(self, e, reads, writes):
        for k in reads:
            s = self._state(k)
            for src, val in list(s["w"].items()):
                self._wait_tok(e, src, val)
        for k in writes:
            s = self._state(k)
            for src, val in list(s["w"].items()):
                self._wait_tok(e, src, val)
            for src, val in list(s["r"].items()):
                self._wait_tok(e, src, val)

    def _record(self, src, val, reads, writes):
        for k in reads:
            s = self._state(k)
            s["r"][src] = val
        for k in writes:
            s = self._state(k)
            s["w"] = {src: val}
            s["r"] = {}

    def op(self, e, fn, reads=(), writes=()):
        self._deps(e, reads, writes)
        ins = fn()
        self.cnt[e] += 1
        ins.then_inc(self.sem[e], 1)
        self._record(e, self.cnt[e], reads, writes)
        return ins

    def dma(self, q, out, in_, sbuf, reads=(), writes=(), fn=None, **kw):
        self._deps(q, reads, writes)
        si = self._slot(sbuf)
        sl = self.slots[si]
        if fn is not None:
            ins = fn()
        else:
            ins = self.eng[q].dma_start(out=out, in_=in_, **kw)
        sl["cnt"] += 1
        ins.then_inc(sl["sem"], 16)
        self._record(("slot", si), 16 * sl["cnt"], reads, writes)
        return ins

    def collective(self, kind, replica_groups, in_ap, out_ap, reads=(), writes=()):
        self._deps("pool", reads, writes)
        if self.cc_slot is None:
            self.cc_slot = self._new_slot(1)
        sl = self.slots[self.cc_slot]
        ins = self.nc.gpsimd.collective_compute(kind, ALU.bypass, replica_groups=replica_groups, ins=[in_ap], outs=[out_ap])
        sl["cnt"] += 1
        ins.then_inc(sl["sem"], 1)
        self._record(("slot", self.cc_slot), sl["cnt"], reads, writes)
        return ins

    def barrier(self):
        for e in self.eng:
            for o in self.eng:
                if o != e:
                    self._wait_tok(e, o, self.cnt[o])
            for i in range(len(self.slots)):
                self._wait_tok(e, ("slot", i), 0)
        self.st = {}

    def finish(self):
        for o in self.eng:
            if o != "sp":
                self._wait_tok("sp", o, self.cnt[o])
        for i in range(len(self.slots)):
            self._wait_tok("sp", ("slot", i), 0)


def bcast_rows(ap_1d, nparts, n):
    return bass.AP(tensor=ap_1d.tensor, offset=ap_1d.offset, ap=[[0, nparts], [1, n]])


def emit_ln(p, z, out, gbc, bbc, stats, mv, rstd):
    nc = p.nc
    for c in range(4):
        p.op("dve", lambda c=c: nc.vector.bn_stats(out=stats[:, c * 6:(c + 1) * 6], in_=z[:, c * 512:(c + 1) * 512]),
             reads=[z], writes=[(stats, c)])
    p.op("dve", lambda: nc.vector.bn_aggr(out=mv[:, 0:2], in_=stats[:, 0:24]),
         reads=[(stats, c) for c in range(4)], writes=[mv])
    p.op("dve", lambda: nc.vector.tensor_scalar(out=rstd[:, 0:1], in0=mv[:, 1:2], scalar1=LN_EPS, scalar2=None, op0=ALU.add),
         reads=[mv], writes=[rstd])
    p.op("act", lambda: nc.scalar.activation(out=rstd[:, 0:1], in_=rstd[:, 0:1], func=AF.Sqrt), reads=[rstd], writes=[rstd])
    p.op("dve", lambda: nc.vector.reciprocal(out=rstd[:, 0:1], in_=rstd[:, 0:1]), reads=[rstd], writes=[rstd])
    p.op("dve", lambda: nc.vector.tensor_scalar(out=out[:, :], in0=z[:, :], scalar1=mv[:, 0:1], scalar2=rstd[:, 0:1],
                                                op0=ALU.subtract, op1=ALU.mult), reads=[z, mv, rstd], writes=[out])
    p.op("pool", lambda: nc.gpsimd.tensor_tensor(out=out[:, :], in0=out[:, :], in1=gbc[:, :], op=ALU.mult), reads=[out, gbc], writes=[out])
    p.op("dve", lambda: nc.vector.tensor_tensor(out=out[:, :], in0=out[:, :], in1=bbc[:, :], op=ALU.add), reads=[out, bbc], writes=[out])


NLOC = NE // 8
XCAP = 8 * CAP


def emit_dispatch(p, x_in, xeT_out, gs_out, idx_out, W, ident_f):
    nc = p.nc
    with ExitStack() as es:
        xbf = p.sb("xbf", [128, NTT, D], BF16, es)
        posm = p.sb("posm", [128, NTT, NE], F32, es)
        ghi = p.sb("ghi", [128, NTT, NE, 2], BF16, es)
        idx = p.sb("idx", [128, NTT, 4], I32, es)
        iotac = p.sb("iotac", [128, CAP], F32, es)
        ones_bf = p.sb("ones_bf", [128, 128], BF16, es)
        p.op("pool", lambda: nc.gpsimd.iota(iotac[:, :], pattern=[[1, CAP]], base=0, channel_multiplier=0,
                                            allow_small_or_imprecise_dtypes=True), writes=[iotac])
        p.op("dve", lambda: nc.vector.memset(ones_bf[:, :], 1.0), writes=[ones_bf])

        with ExitStack() as es2:
            wr = p.sb("wr", [128, 16, NE], F32, es2)
            rb = p.sb("rb", [128, NE], F32, es2)
            ustr = p.sb("ustr", [128, 128], BF16, es2)
            rowbase = p.sb("rowbase", [128, NE], F32, es2)
            cum = p.sb("cum", [128, NE], F32, es2)
            xa = [p.sb(f"xa{i}", [128, D], F32, es2) for i in range(2)]
            xT = [p.sb(f"xT{i}", [128, 16, 128], F32, es2) for i in range(2)]
            lg = p.sb("lg", [128, NE], F32, es2)
            m8 = p.sb("m8", [128, 8], F32, es2)
            nm = p.sb("nm", [128, 1], F32, es2)
            mask = p.sb("mask", [128, NE], F32, es2)
            maskb = p.sb("maskb", [128, NE], BF16, es2)
            ex = p.sb("ex", [128, NE], F32, es2)
            ssum = p.sb("ssum", [128, 1], F32, es2)
            gate = p.sb("gate", [128, NE], F32, es2)
            gres = p.sb("gres", [128, NE], F32, es2)
            pos = p.sb("pos", [128, NE], F32, es2)
            vv = p.sb("vv", [128, NE], F32, es2)
            ltc = p.sb("ltc", [128, NE], F32, es2)
            r8 = p.sb("r8", [128, 8], F32, es2)
            pt = [p.ps(f"pt{i}", [128, 512], F32, es2) for i in range(4)]
            plg = p.ps("plg", [128, NE], F32, es2)
            ppos = p.ps("ppos", [128, 2, NE], F32, es2)

            p.dma("sp", wr[:, :, :], W["router_w"], wr, writes=[wr])
            p.dma("sp", rb[:, :], bcast_rows(W["router_b"], 128, NE), rb, writes=[rb])
            p.op("dve", lambda: nc.vector.memset(ustr[:, :], 1.0), writes=[ustr])
            p.op("pool", lambda: nc.gpsimd.affine_select(out=ustr[:, :], in_=ustr[:, :], pattern=[[1, 128]],
                                                         compare_op=ALU.is_gt, fill=0.0, base=0, channel_multiplier=-1),
                 reads=[ustr], writes=[ustr])
            p.op("pool", lambda: nc.gpsimd.iota(rowbase[:, :], pattern=[[CAP, NE]], base=1, channel_multiplier=0,
                                                allow_small_or_imprecise_dtypes=True), writes=[rowbase])
            p.op("dve", lambda: nc.vector.memset(cum[:, :], 0.0), writes=[cum])

            for i in range(NTT):
                xai = xa[i % 2]
                xTi = xT[i % 2]
                p.dma("sp", xai[:, :], x_in[i * 128:(i + 1) * 128, :], xai, writes=[xai])
                p.op("act", lambda: nc.scalar.copy(out=xbf[:, i, :], in_=xai[:, :]), reads=[xai], writes=[(xbf, i)])
                for g in range(4):
                    ptg = pt[g]
                    for j in range(4):
                        c = g * 4 + j
                        p.op("pe", lambda: nc.tensor.transpose(out=ptg[:, j * 128:(j + 1) * 128],
                                                               in_=xai[:, c * 128:(c + 1) * 128], identity=ident_f[:, :]),
                             reads=[xai, ident_f], writes=[ptg])
                    if g % 2 == 0:
                        p.op("dve", lambda: nc.vector.tensor_copy(out=xTi[:, g * 4:(g + 1) * 4, :],
                                                                  in_=ptg[:, :].rearrange("p (a b) -> p a b", a=4)),
                             reads=[ptg], writes=[(xTi, g)])
                    else:
                        p.op("act", lambda: nc.scalar.copy(out=xTi[:, g * 4:(g + 1) * 4, :],
                                                           in_=ptg[:, :].rearrange("p (a b) -> p a b", a=4)),
                             reads=[ptg], writes=[(xTi, g)])
                for c in range(16):
                    p.op("pe", lambda: nc.tensor.matmul(plg[:, :], lhsT=xTi[:, c, :], rhs=wr[:, c, :], start=(c == 0), stop=(c == 15)),
                         reads=[(xTi, c // 4), wr], writes=[plg])
                p.op("dve", lambda: nc.vector.tensor_tensor(out=lg[:, :], in0=plg[:, :], in1=rb[:, :], op=ALU.add), reads=[plg, rb], writes=[lg])
                p.op("dve", lambda: nc.vector.max(out=m8[:, :], in_=lg[:, :]), reads=[lg], writes=[m8])
                p.op("dve", lambda: nc.vector.tensor_scalar(out=mask[:, :], in0=lg[:, :], scalar1=m8[:, 3:4], scalar2=None, op0=ALU.is_ge),
                     reads=[lg, m8], writes=[mask])
                p.op("pool", lambda: nc.gpsimd.tensor_copy(out=maskb[:, :], in_=mask[:, :]), reads=[mask], writes=[maskb])
                p.op("dve", lambda: nc.vector.tensor_scalar(out=nm[:, :], in0=m8[:, 0:1], scalar1=-1.0, scalar2=None, op0=ALU.mult),
                     reads=[m8], writes=[nm])
                p.op("act", lambda: nc.scalar.activation(out=ex[:, :], in_=lg[:, :], func=AF.Exp, bias=nm[:, 0:1], scale=1.0),
                     reads=[lg, nm], writes=[ex])
                p.op("dve", lambda: nc.vector.tensor_tensor(out=ex[:, :], in0=ex[:, :], in1=mask[:, :], op=ALU.mult), reads=[ex, mask], writes=[ex])
                p.op("dve", lambda: nc.vector.reduce_sum(out=ssum[:, :], in_=ex[:, :], axis=AX.X), reads=[ex], writes=[ssum])
                p.op("dve", lambda: nc.vector.reciprocal(out=ssum[:, :], in_=ssum[:, :]), reads=[ssum], writes=[ssum])
                p.op("dve", lambda: nc.vector.tensor_scalar(out=gate[:, :], in0=ex[:, :], scalar1=ssum[:, 0:1], scalar2=None, op0=ALU.mult),
                     reads=[ex, ssum], writes=[gate])
                p.op("dve", lambda: nc.vector.tensor_copy(out=ghi[:, i, :, 0], in_=gate[:, :]), reads=[gate], writes=[(ghi, i, 0)])
                p.op("dve", lambda: nc.vector.tensor_tensor(out=gres[:, :], in0=gate[:, :], in1=ghi[:, i, :, 0], op=ALU.subtract),
                     reads=[gate, (ghi, i, 0)], writes=[gres])
                p.op("dve", lambda: nc.vector.tensor_copy(out=ghi[:, i, :, 1], in_=gres[:, :]), reads=[gres], writes=[(ghi, i, 1)])
                p.op("pe", lambda: nc.tensor.matmul(ppos[:, 0, :], lhsT=ustr[:, :], rhs=maskb[:, :], start=True, stop=True),
                     reads=[ustr, maskb], writes=[(ppos, 0)])
                p.op("pe", lambda: nc.tensor.matmul(ppos[:, 1, :], lhsT=ones_bf[:, :], rhs=maskb[:, :], start=True, stop=True),
                     reads=[ones_bf, maskb], writes=[(ppos, 1)])
                p.op("dve", lambda: nc.vector.tensor_tensor(out=pos[:, :], in0=ppos[:, 0, :], in1=cum[:, :], op=ALU.add),
                     reads=[(ppos, 0), cum], writes=[pos])
                p.op("dve", lambda: nc.vector.tensor_tensor(out=cum[:, :], in0=ppos[:, 1, :], in1=cum[:, :], op=ALU.add),
                     reads=[(ppos, 1), cum], writes=[cum])
                p.op("dve", lambda: nc.vector.scalar_tensor_tensor(out=posm[:, i, :], in0=pos[:, :], scalar=1.0, in1=mask[:, :],
                                                                   op0=ALU.add, op1=ALU.mult), reads=[pos, mask], writes=[(posm, i)])
                p.op("dve", lambda: nc.vector.tensor_scalar(out=posm[:, i, :], in0=posm[:, i, :], scalar1=-1.0, scalar2=None, op0=ALU.add),
                     reads=[(posm, i)], writes=[(posm, i)])
                p.op("dve", lambda: nc.vector.tensor_scalar(out=ltc[:, :], in0=pos[:, :], scalar1=float(CAP), scalar2=None, op0=ALU.is_lt),
                     reads=[pos], writes=[ltc])
                p.op("dve", lambda: nc.vector.tensor_tensor(out=ltc[:, :], in0=ltc[:, :], in1=mask[:, :], op=ALU.mult), reads=[ltc, mask], writes=[ltc])
                p.op("dve", lambda: nc.vector.tensor_tensor(out=vv[:, :], in0=pos[:, :], in1=rowbase[:, :], op=ALU.add), reads=[pos, rowbase], writes=[vv])
                p.op("dve", lambda: nc.vector.tensor_tensor(out=vv[:, :], in0=vv[:, :], in1=ltc[:, :], op=ALU.mult), reads=[vv, ltc], writes=[vv])
                p.op("dve", lambda: nc.vector.max(out=r8[:, :], in_=vv[:, :]), reads=[vv], writes=[r8])
                p.op("dve", lambda: nc.vector.tensor_copy(out=idx[:, i, :], in_=r8[:, 0:4]), reads=[r8], writes=[(idx, i)])
            p.dma("sp", idx_out, idx[:, :, :].rearrange("p a b -> p (a b)"), idx, reads=[(idx, i) for i in range(NTT)], writes=["idx_out"])
            p.barrier()

        with ExitStack() as es3:
            O = [p.sb(f"O{i}", [128, NTT, CAP], BF16, es3) for i in range(2)]
            xeT = [p.sb(f"xeT{i}", [128, 16, CAP], BF16, es3) for i in range(2)]
            gsl = [p.sb(f"gsl{i}", [128, NSB], F32, es3) for i in range(2)]
            pd = [p.ps(f"pd{i}", [128, 512], F32, es3) for i in range(4)]
            pgs = [p.ps(f"pgs{i}", [128, NSB, 2], F32, es3) for i in range(2)]
            for e in range(NE):
                Oe = O[e % 2]
                xe = xeT[e % 2]
                gs = gsl[e % 2]
                pg_ = pgs[e % 2]
                for i in range(NTT):
                    eng = "dve"
                    eo = nc.vector
                    p.op(eng, lambda: eo.tensor_scalar(out=Oe[:, i, :], in0=iotac[:, :], scalar1=posm[:, i, e:e + 1],
                                                       scalar2=None, op0=ALU.is_equal),
                         reads=[iotac, (posm, i)], writes=[(Oe, i)])
                for sb_ in range(NSB):
                    for i in range(NTT):
                        p.op("pe", lambda: nc.tensor.matmul(pg_[:, sb_, :], lhsT=Oe[:, i, sb_ * 128:(sb_ + 1) * 128],
                                                            rhs=ghi[:, i, e, :], start=(i == 0), stop=(i == NTT - 1)),
                             reads=[(Oe, i), (ghi, i, 0), (ghi, i, 1)], writes=[(pg_, sb_)])
                p.op("dve", lambda: nc.vector.reduce_sum(out=gs[:, :], in_=pg_[:, :, :], axis=AX.X),
                     reads=[(pg_, s) for s in range(NSB)], writes=[gs])
                p.dma("sp", gs_out[e, :].rearrange("(b q) -> q b", q=128), gs[:, :], gs, reads=[gs], writes=[("gs_out", e)],
                      allow_slow_non_contiguous=True)
                for c in range(16):
                    pdc = pd[c % 4]
                    for i in range(NTT):
                        p.op("pe", lambda: nc.tensor.matmul(pdc[:, 0:CAP], lhsT=xbf[:, i, c * 128:(c + 1) * 128], rhs=Oe[:, i, :],
                                                            start=(i == 0), stop=(i == NTT - 1)),
                             reads=[(xbf, i), (Oe, i)], writes=[pdc])
                    if c % 2 == 0:
                        p.op("act", lambda: nc.scalar.copy(out=xe[:, c, :], in_=pdc[:, 0:CAP]), reads=[pdc], writes=[(xe, c)])
                    else:
                        p.op("dve", lambda: nc.vector.tensor_copy(out=xe[:, c, :], in_=pdc[:, 0:CAP]), reads=[pdc], writes=[(xe, c)])
                p.dma("sp", xeT_out[e, :, :].rearrange("(c q) s -> q c s", q=128), xe[:, :, :], xe,
                      reads=[(xe, c) for c in range(16)], writes=[("xeT_out", e)])
            p.barrier()


def emit_experts(p, xeT_in, gs_in, y_out, W):
    nc = p.nc
    HS = XCAP // 2
    NCB = HS // 512
    NSBH = HS // 128
    with ExitStack() as es:
        NWB = 4
        wbuf = [p.sb(f"wb{i}", [128, 16, 512], BF16, es) for i in range(NWB)]
        xeT = p.sb("xeT", [128, 16, HS], BF16, es)
        actT = p.sb("actT", [128, 16, HS], BF16, es)
        bgu = [p.sb(f"bgu{i}", [128, 32], F32, es) for i in range(2)]
        bdn = [p.sb(f"bdn{i}", [1, D], F32, es) for i in range(2)]
        gsl = [p.sb(f"gsl{i}", [128, XCAP // 128], F32, es) for i in range(2)]
        gt = [p.sb(f"gt{i}", [128, 512], F32, es) for i in range(2)]
        sg = [p.sb(f"sg{i}", [128, 512], F32, es) for i in range(2)]
        u2 = [p.sb(f"u2{i}", [128, 512], F32, es) for i in range(2)]
        ysb = [p.sb(f"ysb{i}", [128, 512], F32, es) for i in range(4)]
        ones_f = p.sb("ones_f", [1, 128], F32, es)
        pg = [p.ps(f"pg{i}", [128, 512], F32, es) for i in range(2)]
        pu = [p.ps(f"pu{i}", [128, 512], F32, es) for i in range(2)]
        pdn = [p.ps(f"pdn{i}", [128, 512], F32, es) for i in range(3)]
        p.op("dve", lambda: nc.vector.memset(ones_f[:, :], 1.0), writes=[ones_f])
        b7 = p.sb("b7", [128, 1], F32, es)
        p.op("dve", lambda: nc.vector.memset(b7[:, :], 7.0), writes=[b7])
        cnt = {"w": 0, "y": 0, "g": 0, "d": 0}

        def load_w(src_ap):
            wb = wbuf[cnt["w"] % NWB]
            cnt["w"] += 1
            p.dma("pool", wb[:, :, :], src_ap, wb, writes=[wb])
            return wb

        for e in range(NLOC):
            bg = bgu[e % 2]
            bd = bdn[e % 2]
            gs = gsl[e % 2]
            p.dma("sp", bg[:, :], W["b_gu"][e], bg, writes=[bg])
            p.dma("sp", bd[:, :], W["b_dn"][e:e + 1, :], bd, writes=[bd])
            p.dma("sp", gs[:, :], gs_in[e], gs, writes=[gs])
            for h in range(2):
                p.dma("sp", xeT[:, :, :], xeT_in[e, :, h * HS:(h + 1) * HS].rearrange("(c q) s -> q c s", q=128), xeT, writes=[xeT])
                for j in range(4):
                    wg = load_w(W["w_gu"][e, :, j * 512:(j + 1) * 512].rearrange("(c q) f -> q c f", q=128))
                    wu = load_w(W["w_gu"][e, :, D + j * 512:D + (j + 1) * 512].rearrange("(c q) f -> q c f", q=128))
                    for f in range(4):
                        fc = j * 4 + f
                        for cb in range(NCB):
                            k = cnt["g"] % 2
                            cnt["g"] += 1
                            sl = slice(cb * 512, (cb + 1) * 512)
                            for c in range(16):
                                p.op("pe", lambda: nc.tensor.matmul(pg[k][:, :], lhsT=wg[:, c, f * 128:(f + 1) * 128], rhs=xeT[:, c, sl],
                                                                    start=(c == 0), stop=(c == 15)), reads=[wg, xeT], writes=[pg[k]])
                            for c in range(16):
                                p.op("pe", lambda: nc.tensor.matmul(pu[k][:, :], lhsT=wu[:, c, f * 128:(f + 1) * 128], rhs=xeT[:, c, sl],
                                                                    start=(c == 0), stop=(c == 15)), reads=[wu, xeT], writes=[pu[k]])
                            p.op("dve", lambda: nc.vector.tensor_scalar(out=gt[k][:, :], in0=pg[k][:, :], scalar1=bg[:, fc:fc + 1], scalar2=7.0,
                                                                        op0=ALU.add, op1=ALU.min), reads=[pg[k], bg], writes=[gt[k]])
                            p.op("act", lambda: nc.scalar.activation(out=sg[k][:, :], in_=gt[k][:, :], func=AF.Sigmoid, scale=1.702),
                                 reads=[gt[k]], writes=[sg[k]])
                            p.op("dve", lambda: nc.vector.tensor_scalar(out=u2[k][:, :], in0=pu[k][:, :], scalar1=bg[:, 16 + fc:17 + fc], scalar2=7.0,
                                                                        op0=ALU.add, op1=ALU.min), reads=[pu[k], bg], writes=[u2[k]])
                            p.op("act", lambda: nc.scalar.activation(out=u2[k][:, :], in_=u2[k][:, :], func=AF.Relu, bias=b7[:, 0:1]),
                                 reads=[u2[k], b7], writes=[u2[k]])
                            p.op("dve", lambda: nc.vector.scalar_tensor_tensor(out=u2[k][:, :], in0=u2[k][:, :], scalar=-6.0, in1=gt[k][:, :],
                                                                               op0=ALU.add, op1=ALU.mult), reads=[u2[k], gt[k]], writes=[u2[k]])
                            p.op("dve", lambda: nc.vector.tensor_tensor(out=actT[:, fc, sl], in0=u2[k][:, :], in1=sg[k][:, :], op=ALU.mult),
                                 reads=[u2[k], sg[k]], writes=[(actT, fc)])
                for dc in range(4):
                    wd = load_w(W["w_dn"][e, :, dc * 512:(dc + 1) * 512].rearrange("(c q) f -> q c f", q=128))
                    for sb_ in range(NSBH):
                        pdk = pdn[cnt["d"] % 3]
                        cnt["d"] += 1
                        for fc in range(16):
                            p.op("pe", lambda: nc.tensor.matmul(pdk[:, :], lhsT=actT[:, fc, sb_ * 128:(sb_ + 1) * 128], rhs=wd[:, fc, :],
                                                                start=(fc == 0), stop=False), reads=[(actT, fc), wd], writes=[pdk])
                        p.op("pe", lambda: nc.tensor.matmul(pdk[:, :], lhsT=ones_f[0:1, :], rhs=bd[0:1, dc * 512:(dc + 1) * 512],
                                                            start=False, stop=True), reads=[ones_f, bd], writes=[pdk])
                        yb = ysb[cnt["y"] % 4]
                        cnt["y"] += 1
                        gcol = h * NSBH + sb_
                        if cnt["y"] % 2 == 0:
                            p.op("dve", lambda: nc.vector.tensor_scalar(out=yb[:, :], in0=pdk[:, :], scalar1=gs[:, gcol:gcol + 1], scalar2=None,
                                                                        op0=ALU.mult), reads=[pdk, gs], writes=[yb])
                        else:
                            p.op("act", lambda: nc.scalar.activation(out=yb[:, :], in_=pdk[:, :], func=AF.Copy, scale=gs[:, gcol:gcol + 1]),
                                 reads=[pdk, gs], writes=[yb])
                        r0 = h * HS + sb_ * 128
                        p.dma("sp", y_out[e, r0:r0 + 128, dc * 512:(dc + 1) * 512], yb[:, :], yb, reads=[yb], writes=[("y_out", e, r0, dc)])
        p.barrier()


def emit_combine(p, x_res, ybuf, idx_in, ln_g, ln_b, x_out):
    nc = p.nc
    with ExitStack() as es4:
        gbc = p.sb("gbc", [128, D], F32, es4)
        bbc = p.sb("bbc", [128, D], F32, es4)
        idx = p.sb("idxc", [128, NTT * 4], I32, es4)
        G = [[p.sb(f"G{k}_{i}", [128, D], F32, es4) for k in range(4)] for i in range(2)]
        xr = [p.sb(f"xr{i}", [128, D], F32, es4) for i in range(2)]
        zo = [p.sb(f"zo{i}", [128, D], F32, es4) for i in range(2)]
        stats = p.sb("stats", [128, 24], F32, es4)
        mv = p.sb("mv", [128, 2], F32, es4)
        rstd = p.sb("rstd", [128, 1], F32, es4)
        p.dma("sp", gbc[:, :], bcast_rows(ln_g, 128, D), gbc, writes=[gbc])
        p.dma("sp", bbc[:, :], bcast_rows(ln_b, 128, D), bbc, writes=[bbc])
        p.dma("sp", idx[:, :], idx_in, idx, writes=[idx])
        for i in range(NTT):
            Gi = G[i % 2]
            xri = xr[i % 2]
            zi = zo[i % 2]
            p.dma("sp", xri[:, :], x_res[i * 128:(i + 1) * 128, :], xri, writes=[xri])
            for k in range(4):
                p.dma("pool", None, None, Gi[k], reads=[idx], writes=[Gi[k]],
                      fn=lambda: nc.gpsimd.indirect_dma_start(out=Gi[k][:, :], out_offset=None, in_=ybuf[:, :],
                                                              in_offset=bass.IndirectOffsetOnAxis(ap=idx[:, i * 4 + k:i * 4 + k + 1], axis=0)))
            p.op("dve", lambda: nc.vector.tensor_tensor(out=Gi[0][:, :], in0=Gi[0][:, :], in1=Gi[1][:, :], op=ALU.add), reads=[Gi[0], Gi[1]], writes=[Gi[0]])
            p.op("pool", lambda: nc.gpsimd.tensor_tensor(out=Gi[2][:, :], in0=Gi[2][:, :], in1=Gi[3][:, :], op=ALU.add), reads=[Gi[2], Gi[3]], writes=[Gi[2]])
            p.op("dve", lambda: nc.vector.tensor_tensor(out=Gi[0][:, :], in0=Gi[0][:, :], in1=Gi[2][:, :], op=ALU.add), reads=[Gi[0], Gi[2]], writes=[Gi[0]])
            p.op("dve", lambda: nc.vector.scalar_tensor_tensor(out=xri[:, :], in0=xri[:, :], scalar=ALPHA, in1=Gi[0][:, :], op0=ALU.mult, op1=ALU.add),
                 reads=[xri, Gi[0]], writes=[xri])
            emit_ln(p, xri, zi, gbc, bbc, stats, mv, rstd)
            p.dma("sp", x_out[i * 128:(i + 1) * 128, :], zi[:, :], zi, reads=[zi], writes=[("xout", i)])
        p.barrier()


def make_ident(p, es):
    nc = p.nc
    ident = p.sb("ident", [128, 128], F32, es)
    p.op("dve", lambda: nc.vector.memset(ident[:, :], 1.0), writes=[ident])
    p.op("pool", lambda: nc.gpsimd.affine_select(out=ident[:, :], in_=ident[:, :], pattern=[[1, 128]], compare_op=ALU.is_equal,
                                                 fill=0.0, base=0, channel_multiplier=-1), reads=[ident], writes=[ident])
    return ident


def new_nc():
    return bass.Bass("TRN2", target_bir_lowering=False)


def din(nc, name, shape, dt=F32):
    return nc.dram_tensor(name, list(shape), dt, kind="ExternalInput").ap()


def dout(nc, name, shape, dt=F32):
    return nc.dram_tensor(name, list(shape), dt, kind="ExternalOutput").ap()


def build_dispatch_prog():
    nc = new_nc()
    x_in = din(nc, "x_in", [NT, D])
    W = {"router_w": din(nc, "router_w", [128, 16, NE]), "router_b": din(nc, "router_b", [NE])}
    xeT_out = dout(nc, "xeT_out", [NE, D, CAP], BF16)
    gs_out = dout(nc, "gs_out", [NE, CAP])
    idx_out = dout(nc, "idx_out", [128, NTT * 4], I32)
    with ExitStack() as es:
        p = P(nc, es)
        ident = make_ident(p, es)
        emit_dispatch(p, x_in, xeT_out, gs_out, idx_out, W, ident)
        p.finish()
    return nc


def build_experts_prog():
    nc = new_nc()
    xeT_in = din(nc, "xeT_in", [NLOC, D, XCAP], BF16)
    gs_in = din(nc, "gs_in", [NLOC, 128, XCAP // 128])
    W = {"w_gu": din(nc, "w_gu", [NLOC, D, 2 * D]), "b_gu": din(nc, "b_gu", [NLOC, 128, 32]),
         "w_dn": din(nc, "w_dn", [NLOC, D, D]), "b_dn": din(nc, "b_dn", [NLOC, D])}
    y_out = dout(nc, "y_out", [NLOC, XCAP, D])
    with ExitStack() as es:
        p = P(nc, es)
        emit_experts(p, xeT_in, gs_in, y_out, W)
        p.finish()
    return nc


def build_combine_prog():
    nc = new_nc()
    x_res = din(nc, "x_res", [NT, D])
    ybuf = din(nc, "ybuf", [1 + NE * CAP, D])
    idx_in = din(nc, "idx_in", [128, NTT * 4], I32)
    ln_g = din(nc, "ln_g", [D])
    ln_b = din(nc, "ln_b", [D])
    x_out = dout(nc, "x_out", [NT, D])
    with ExitStack() as es:
        p = P(nc, es)
        emit_combine(p, x_res, ybuf, idx_in, ln_g, ln_b, x_out)
        p.finish()
    return nc


def run_moe_layer(xa_cores, inputs, l):
    import ml_dtypes
    rw = np.ascontiguousarray(inputs["router_w"][l].reshape(16, 128, NE).transpose(1, 0, 2))
    rb = np.ascontiguousarray(inputs["router_b"][l])
    nc1 = build_dispatch_prog()
    r1 = run_bass_kernel_spmd(nc1, [{"x_in": xa_cores[c], "router_w": rw, "router_b": rb} for c in range(8)], core_ids=list(range(8))).results
    maps2 = []
    bgu = inputs["b_gu"][l].reshape(NE, 32, 128).transpose(0, 2, 1)
    for c in range(8):
        es_ = slice(c * NLOC, (c + 1) * NLOC)
        xe = np.concatenate([np.asarray(r1[s]["xeT_out"])[es_] for s in range(8)], axis=2)
        gs = np.concatenate([np.asarray(r1[s]["gs_out"])[es_] for s in range(8)], axis=1)
        gs = np.ascontiguousarray(gs.reshape(NLOC, XCAP // 128, 128).transpose(0, 2, 1))
        maps2.append({"xeT_in": np.ascontiguousarray(xe), "gs_in": gs,
                      "w_gu": np.ascontiguousarray(inputs["w_gu"][l][es_]), "b_gu": np.ascontiguousarray(bgu[es_]),
                      "w_dn": np.ascontiguousarray(inputs["w_dn"][l][es_]), "b_dn": np.ascontiguousarray(inputs["b_dn"][l][es_])})
    nc2 = build_experts_prog()
    r2 = run_bass_kernel_spmd(nc2, maps2, core_ids=list(range(8))).results
    yall = np.concatenate([np.asarray(r2[c]["y_out"]) for c in range(8)], axis=0)
    maps3 = []
    for c in range(8):
        yb = np.zeros((1 + NE * CAP, D), np.float32)
        yb[1:] = yall[:, c * CAP:(c + 1) * CAP, :].reshape(NE * CAP, D)
        maps3.append({"x_res": xa_cores[c], "ybuf": yb, "idx_in": np.asarray(r1[c]["idx_out"]),
                      "ln_g": np.ascontiguousarray(inputs["ln_moe_g"][l]), "ln_b": np.ascontiguousarray(inputs["ln_moe_b"][l])})
    nc3 = build_combine_prog()
    r3 = run_bass_kernel_spmd(nc3, maps3, core_ids=list(range(8))).results
    return [np.asarray(r3[c]["x_out"]) for c in range(8)]


S = 4096
BIG = 1.0e30
TWO_PI = 6.283185307179586
NQB = 16


def emit_rope_tabs(p, pos_ap, t0, n, tmp, consts):
    nc = p.nc
    posi, posf, tt, ki_, kf, C, Ss, m1 = tmp
    invf, sgn = consts
    p.dma("sp", posi[:, 0:n], bcast_rows(pos_ap[t0:t0 + n], 128, n), posi, writes=[posi])
    p.op("dve", lambda: nc.vector.tensor_copy(out=posf[:, 0:n], in_=posi[:, 0:n]), reads=[posi], writes=[posf])
    for which, off, dst in (("s", 0.0, Ss), ("c", 0.25, C)):
        p.op("dve", lambda: nc.vector.tensor_scalar(out=tt[:, 0:n], in0=posf[:, 0:n], scalar1=invf[:, 0:1], scalar2=off, op0=ALU.mult, op1=ALU.add),
             reads=[posf, invf], writes=[tt])
        p.op("dve", lambda: nc.vector.tensor_copy(out=ki_[:, 0:n], in_=tt[:, 0:n]), reads=[tt], writes=[ki_])
        p.op("dve", lambda: nc.vector.tensor_copy(out=kf[:, 0:n], in_=ki_[:, 0:n]), reads=[ki_], writes=[kf])
        p.op("dve", lambda: nc.vector.tensor_tensor(out=tt[:, 0:n], in0=tt[:, 0:n], in1=kf[:, 0:n], op=ALU.subtract), reads=[tt, kf], writes=[tt])
        p.op("dve", lambda: nc.vector.tensor_scalar(out=m1[:, 0:n], in0=tt[:, 0:n], scalar1=0.5, scalar2=None, op0=ALU.is_gt), reads=[tt], writes=[m1])
        p.op("dve", lambda: nc.vector.tensor_tensor(out=tt[:, 0:n], in0=tt[:, 0:n], in1=m1[:, 0:n], op=ALU.subtract), reads=[tt, m1], writes=[tt])
        p.op("dve", lambda: nc.vector.tensor_scalar(out=m1[:, 0:n], in0=tt[:, 0:n], scalar1=-0.5, scalar2=None, op0=ALU.is_lt), reads=[tt], writes=[m1])
        p.op("dve", lambda: nc.vector.tensor_tensor(out=tt[:, 0:n], in0=tt[:, 0:n], in1=m1[:, 0:n], op=ALU.add), reads=[tt, m1], writes=[tt])
        p.op("act", lambda: nc.scalar.activation(out=dst[:, 0:n], in_=tt[:, 0:n], func=AF.Sin, scale=TWO_PI), reads=[tt], writes=[dst])
    p.op("dve", lambda: nc.vector.tensor_scalar(out=Ss[:, 0:n], in0=Ss[:, 0:n], scalar1=sgn[:, 0:1], scalar2=None, op0=ALU.mult),
         reads=[Ss, sgn], writes=[Ss])
    return C, Ss


def emit_l0_attn(p, A):
    nc = p.nc
    SCALE = 128.0 ** -0.5
    with ExitStack() as es:
        kT = p.sb("kT", [128, 2, S], BF16, es)
        kiT = p.sb("kiT", [128, S], BF16, es)
        vtok = p.sb("vtok", [128, S // 128, 256], BF16, es)
        qT = p.sb("qT", [128, NQB, 8, 128], BF16, es)
        qiT = p.sb("qiT", [128, NQB, 4, 128], BF16, es)
        wi = p.sb("wi", [128, NQB, 4], F32, es)
        identb = p.sb("identb", [128, 128], BF16, es)
        ones_bf = p.sb("ones_bf", [128, 128], BF16, es)
        iota256 = p.sb("iota256", [128, 256], F32, es)
        qlim = p.sb("qlim", [128, NQB], F32, es)
        invf = p.sb("invf", [128, 1], F32, es)
        sgn = p.sb("sgn", [128, 1], F32, es)
        p.op("dve", lambda: nc.vector.memset(identb[:, :], 1.0), writes=[identb])
        p.op("pool", lambda: nc.gpsimd.affine_select(out=identb[:, :], in_=identb[:, :], pattern=[[1, 128]], compare_op=ALU.is_equal,
                                                     fill=0.0, base=0, channel_multiplier=-1), reads=[identb], writes=[identb])
        p.op("dve", lambda: nc.vector.memset(ones_bf[:, :], 1.0), writes=[ones_bf])
        p.op("pool", lambda: nc.gpsimd.iota(iota256[:, :], pattern=[[1, 256]], base=0, channel_multiplier=0,
                                            allow_small_or_imprecise_dtypes=True), writes=[iota256])
        p.dma("sp", qlim[:, :], A["qlimrel"], qlim, writes=[qlim])
        p.dma("sp", invf[:, :], A["invf"], invf, writes=[invf])
        p.dma("sp", sgn[:, :], A["sgn"], sgn, writes=[sgn])

        with ExitStack() as es2:
            wt = p.sb("wt", [128, 16, 512], BF16, es2)
            wtwi = p.sb("wtwi", [128, 16, 4], BF16, es2)
            xb = [p.sb(f"xb{i}", [128, 16, 512], BF16, es2) for i in range(2)]
            tmp = [p.sb("posi", [128, 512], I32, es2), p.sb("posf", [128, 512], F32, es2), p.sb("tt", [128, 512], F32, es2),
                   p.sb("ki_", [128, 512], I32, es2), p.sb("kf", [128, 512], F32, es2), p.sb("C", [128, 512], F32, es2),
                   p.sb("Ss", [128, 512], F32, es2), p.sb("m1", [128, 512], F32, es2)]
            t1 = [p.sb(f"t1_{i}", [128, 512], F32, es2) for i in range(2)]
            ysw = [p.sb(f"ysw{i}", [128, 512], F32, es2) for i in range(2)]
            py = [p.ps(f"py{i}", [128, 512], F32, es2) for i in range(2)]
            psw = [p.ps(f"psw{i}", [128, 512], F32, es2) for i in range(2)]
            pv = p.ps("pv", [128, 256], F32, es2)
            pw = p.ps("pw", [128, 4], F32, es2)
            cn = {"x": 0, "r": 0}

            def load_wt(w_ap):
                p.dma("pool", wt[:, :, :], w_ap.rearrange("(c q) f -> q c f", q=128), wt, writes=[wt])

            def load_xb(xT_ap, t0):
                b = xb[cn["x"] % 2]
                cn["x"] += 1
                p.dma("pool", b[:, :, :], xT_ap[:, t0:t0 + 512].rearrange("(c q) t -> q c t", q=128), b, writes=[b])
                return b

            def proj(ps_, oc, b):
                for c in range(16):
                    p.op("pe", lambda: nc.tensor.matmul(ps_[:, :], lhsT=wt[:, c, oc * 128:(oc + 1) * 128], rhs=b[:, c, :],
                                                        start=(c == 0), stop=(c == 15)), reads=[wt, b], writes=[ps_])

            def rope(oc_y, oc_sw, b, C, Ss, out_ap, out_key, in_view=None):
                k = cn["r"] % 2
                cn["r"] += 1
                proj(py[k], oc_y, b)
                proj(psw[k], oc_sw, b)
                p.op("dve", lambda: nc.vector.tensor_tensor(out=t1[k][:, :], in0=py[k][:, :], in1=C[:, :], op=ALU.mult), reads=[py[k], C], writes=[t1[k]])
                p.op("act", lambda: nc.scalar.copy(out=ysw[k][:, :], in_=psw[k][:, :]), reads=[psw[k]], writes=[ysw[k]])
                p.op("pool", lambda: nc.gpsimd.tensor_tensor(out=ysw[k][:, :], in0=ysw[k][:, :], in1=Ss[:, :], op=ALU.mult), reads=[ysw[k], Ss], writes=[ysw[k]])
                a0 = t1[k][:, :] if in_view is None else t1[k][:, :].rearrange(in_view, a=4)
                a1 = ysw[k][:, :] if in_view is None else ysw[k][:, :].rearrange(in_view, a=4)
                p.op("pool", lambda: nc.gpsimd.tensor_tensor(out=out_ap, in0=a0, in1=a1, op=ALU.add), reads=[t1[k], ysw[k]], writes=[out_key])

            load_wt(A["wK"])
            for blk in range(S // 512):
                b = load_xb(A["xT_all"], blk * 512)
                C, Ss = emit_rope_tabs(p, A["pos_all"], blk * 512, 512, tmp, (invf, sgn))
                for g in range(2):
                    rope(g, 2 + g, b, C, Ss, kT[:, g, blk * 512:(blk + 1) * 512], (kT, g, blk))
            load_wt(A["wKIV"])
            for blk in range(S // 512):
                b = load_xb(A["xT_all"], blk * 512)
                C, Ss = emit_rope_tabs(p, A["pos_all"], blk * 512, 512, tmp, (invf, sgn))
                rope(0, 1, b, C, Ss, kiT[:, blk * 512:(blk + 1) * 512], (kiT, blk))
                for s_ in range(4):
                    for c in range(16):
                        p.op("pe", lambda: nc.tensor.matmul(pv[:, :], lhsT=b[:, c, s_ * 128:(s_ + 1) * 128], rhs=wt[:, c, 256:512],
                                                            start=(c == 0), stop=(c == 15)), reads=[wt, b], writes=[pv])
                    p.op("act", lambda: nc.scalar.copy(out=vtok[:, blk * 4 + s_, :], in_=pv[:, :]), reads=[pv], writes=[(vtok, blk * 4 + s_)])
            for pp in range(4):
                load_wt(A["wQ"][pp])
                for blk in range(NT // 512):
                    b = load_xb(A["xT_own"], blk * 512)
                    C, Ss = emit_rope_tabs(p, A["pos_own"], blk * 512, 512, tmp, (invf, sgn))
                    for hh in range(2):
                        rope(hh, 2 + hh, b, C, Ss, qT[:, blk * 4:(blk + 1) * 4, 2 * pp + hh, :], (qT, blk, 2 * pp + hh), in_view="p (a b) -> p a b")
            p.dma("pool", wtwi[:, :, :], A["wWI"].rearrange("(c q) f -> q c f", q=128), wtwi, writes=[wtwi])
            for pp in range(2):
                load_wt(A["wQI"][pp])
                for blk in range(NT // 512):
                    b = load_xb(A["xT_own"], blk * 512)
                    C, Ss = emit_rope_tabs(p, A["pos_own"], blk * 512, 512, tmp, (invf, sgn))
                    for hh in range(2):
                        rope(hh, 2 + hh, b, C, Ss, qiT[:, blk * 4:(blk + 1) * 4, 2 * pp + hh, :], (qiT, blk, 2 * pp + hh), in_view="p (a b) -> p a b")
                    if pp == 1:
                        for s_ in range(4):
                            for c in range(16):
                                p.op("pe", lambda: nc.tensor.matmul(pw[:, :], lhsT=b[:, c, s_ * 128:(s_ + 1) * 128], rhs=wtwi[:, c, :],
                                                                    start=(c == 0), stop=(c == 15)), reads=[wtwi, b], writes=[pw])
                            p.op("act", lambda: nc.scalar.copy(out=wi[:, blk * 4 + s_, :], in_=pw[:, :]), reads=[pw], writes=[(wi, blk * 4 + s_)])
            p.barrier()

        with ExitStack() as es3:
            sc = p.sb("sc", [128, S], F32, es3)
            scw = p.sb("scw", [128, S], F32, es3)
            sel = p.sb("sel", [128, S], BF16, es3)
            selT = p.sb("selT", [128, S // 128, 128], BF16, es3)
            rl = [p.sb(f"rl{i}", [128, 512], F32, es3) for i in range(2)]
            m256 = p.sb("m256", [128, 256], F32, es3)
            pen = p.sb("pen", [128, 256], F32, es3)
            m8 = p.sb("m8", [128, 8], F32, es3)
            thr = p.sb("thr", [128, 1], F32, es3)
            PT = [p.sb(f"PT{i}", [128, 4, 128], BF16, es3) for i in range(3)]
            rden = p.sb("rden", [128, 512], F32, es3)
            pi_ = [p.ps(f"pi{i}", [128, 512], F32, es3) for i in range(2)]
            ptr = p.ps("ptr", [128, 512], BF16, es3)
            pss = [p.ps(f"pss{i}", [128, 512], F32, es3) for i in range(2)]
            pot = p.ps("pot", [128, 512], F32, es3)
            pden = p.ps("pden", [128, 512], F32, es3)
            cn2 = {"i": 0, "s": 0, "p": 0}
            for j in range(NQB):
                NK = (2 * j + 2) * 128
                NKB = NK // 128
                ncb = (NK + 511) // 512
                for cb in range(ncb):
                    w_ = min(512, NK - cb * 512)
                    cs = slice(cb * 512, cb * 512 + w_)
                    for ih in range(4):
                        k = cn2["i"] % 2
                        cn2["i"] += 1
                        p.op("pe", lambda: nc.tensor.matmul(pi_[k][:, 0:w_], lhsT=qiT[:, j, ih, :], rhs=kiT[:, cs], start=True, stop=True),
                             reads=[qiT, kiT], writes=[pi_[k]])
                        p.op("act", lambda: nc.scalar.activation(out=rl[k][:, 0:w_], in_=pi_[k][:, 0:w_], func=AF.Relu), reads=[pi_[k]], writes=[rl[k]])
                        if ih == 0:
                            p.op("dve", lambda: nc.vector.tensor_scalar(out=sc[:, cs], in0=rl[k][:, 0:w_], scalar1=wi[:, j, 0:1], scalar2=None, op0=ALU.mult),
                                 reads=[rl[k], wi], writes=[(sc, cb)])
                        else:
                            p.op("dve", lambda: nc.vector.scalar_tensor_tensor(out=sc[:, cs], in0=rl[k][:, 0:w_], scalar=wi[:, j, ih:ih + 1], in1=sc[:, cs],
                                                                               op0=ALU.mult, op1=ALU.add), reads=[rl[k], wi, (sc, cb)], writes=[(sc, cb)])
                sck = [(sc, cb) for cb in range(ncb)]
                ls = slice(NK - 256, NK)
                p.op("dve", lambda: nc.vector.tensor_scalar(out=m256[:, :], in0=iota256[:, :], scalar1=qlim[:, j:j + 1], scalar2=None, op0=ALU.is_lt),
                     reads=[iota256, qlim], writes=[m256])
                p.op("dve", lambda: nc.vector.tensor_scalar(out=pen[:, :], in0=m256[:, :], scalar1=BIG, scalar2=-BIG, op0=ALU.mult, op1=ALU.add),
                     reads=[m256], writes=[pen])
                p.op("dve", lambda: nc.vector.tensor_tensor(out=sc[:, ls], in0=sc[:, ls], in1=m256[:, :], op=ALU.mult), reads=sck + [m256], writes=sck)
                p.op("dve", lambda: nc.vector.tensor_tensor(out=sc[:, ls], in0=sc[:, ls], in1=pen[:, :], op=ALU.add), reads=sck + [pen], writes=sck)
                if NK > 256:
                    cur = sc
                    for r in range(32):
                        p.op("dve", lambda: nc.vector.max(out=m8[:, :], in_=cur[:, 0:NK]), reads=sck + [scw], writes=[m8])
                        if r < 31:
                            p.op("dve", lambda: nc.vector.match_replace(out=scw[:, 0:NK], in_to_replace=m8[:, :], in_values=cur[:, 0:NK], imm_value=-BIG),
                                 reads=sck + [m8, scw], writes=[scw])
                            cur = scw
                    p.op("dve", lambda: nc.vector.tensor_scalar(out=thr[:, :], in0=m8[:, 7:8], scalar1=-BIG / 2, scalar2=None, op0=ALU.max),
                         reads=[m8], writes=[thr])
                else:
                    p.op("dve", lambda: nc.vector.memset(thr[:, :], -BIG / 2), writes=[thr])
                p.op("dve", lambda: nc.vector.tensor_scalar(out=sel[:, 0:NK], in0=sc[:, 0:NK], scalar1=thr[:, 0:1], scalar2=None, op0=ALU.is_ge),
                     reads=sck + [thr], writes=[sel])
                for kb0 in range(0, NKB, 4):
                    nb = min(4, NKB - kb0)
                    for t_ in range(nb):
                        kb = kb0 + t_
                        p.op("pe", lambda: nc.tensor.transpose(out=ptr[:, t_ * 128:(t_ + 1) * 128], in_=sel[:, kb * 128:(kb + 1) * 128], identity=identb[:, :]),
                             reads=[sel, identb], writes=[ptr])
                    p.op("act", lambda: nc.scalar.copy(out=selT[:, kb0:kb0 + nb, :], in_=ptr[:, 0:nb * 128].rearrange("p (a b) -> p a b", b=128)),
                         reads=[ptr], writes=[(selT, kb0 // 4)])
                for g in range(2):
                    for kb in range(NKB):
                        ks_ = cn2["s"] % 2
                        cn2["s"] += 1
                        pk = cn2["p"] % 3
                        cn2["p"] += 1
                        p.op("pe", lambda: nc.tensor.matmul(pss[ks_][:, :], lhsT=kT[:, g, kb * 128:(kb + 1) * 128], rhs=qT[:, j, 4 * g:4 * g + 4, :],
                                                            start=True, stop=True), reads=[kT, (qT, j, g)], writes=[pss[ks_]])
                        p.op("act", lambda: nc.scalar.activation(out=PT[pk][:, :, :], in_=pss[ks_][:, :].rearrange("p (a b) -> p a b", a=4),
                                                                 func=AF.Exp, scale=SCALE), reads=[pss[ks_]], writes=[PT[pk]])
                        eng = "pool" if kb % 2 == 0 else "dve"
                        eo = nc.gpsimd if eng == "pool" else nc.vector
                        p.op(eng, lambda: eo.tensor_tensor(out=PT[pk][:, :, :], in0=PT[pk][:, :, :],
                                                           in1=selT[:, kb, :].unsqueeze(1).to_broadcast([128, 4, 128]), op=ALU.mult),
                             reads=[PT[pk], (selT, kb // 4)], writes=[PT[pk]])
                        p.op("pe", lambda: nc.tensor.matmul(pot[:, :], lhsT=vtok[:, kb, g * 128:(g + 1) * 128], rhs=PT[pk][:, :, :],
                                                            start=(kb == 0), stop=(kb == NKB - 1)), reads=[vtok, PT[pk]], writes=[pot])
                        p.op("pe", lambda: nc.tensor.matmul(pden[:, :], lhsT=ones_bf[:, :], rhs=PT[pk][:, :, :],
                                                            start=(kb == 0), stop=(kb == NKB - 1)), reads=[ones_bf, PT[pk]], writes=[pden])
                    p.op("dve", lambda: nc.vector.reciprocal(out=rden[:, :], in_=pden[:, :]), reads=[pden], writes=[rden])
                    p.op("dve", lambda: nc.vector.tensor_tensor(out=qT[:, j, 4 * g:4 * g + 4, :], in0=pot[:, :].rearrange("p (a b) -> p a b", a=4),
                                                                in1=rden[:, :].rearrange("p (a b) -> p a b", a=4), op=ALU.mult),
                         reads=[pot, rden], writes=[(qT, j, g)])
            p.dma("sp", A["aT_out"].rearrange("h d (j q) -> d j h q", q=128), qT[:, :, :, :], qT,
                  reads=[(qT, j, g) for j in range(NQB) for g in range(2)], writes=["aT_out"])
            p.barrier()


def build_l0_attn_prog():
    nc = new_nc()
    A = {"xT_all": din(nc, "xT_all", [D, S]), "xT_own": din(nc, "xT_own", [D, NT]),
         "pos_all": din(nc, "pos_all", [S], I32), "pos_own": din(nc, "pos_own", [NT], I32),
         "qlimrel": din(nc, "qlimrel", [128, NQB]), "invf": din(nc, "invf", [128, 1]), "sgn": din(nc, "sgn", [128, 1]),
         "wK": din(nc, "wK", [D, 512]), "wKIV": din(nc, "wKIV", [D, 512]),
         "wQ": din(nc, "wQ", [4, D, 512]), "wQI": din(nc, "wQI", [2, D, 512]), "wWI": din(nc, "wWI", [D, 4]),
         "aT_out": dout(nc, "aT_out", [8, 128, NT], BF16)}
    with ExitStack() as es:
        p = P(nc, es)
        emit_l0_attn(p, A)
        p.finish()
    return nc


def _sw(c0):
    return list(range(c0 + 64, c0 + 128)) + list(range(c0, c0 + 64))


def l0_attn_host(inputs):
    x = inputs["x"]
    pos = inputs["positions"]
    w = inputs["w_in0"][0]
    oq, ok, ov, oqi, oki, owi, ou = 0, 1024, 1280, 1536, 2048, 2176, 2180
    def cols(c0):
        return list(range(c0, c0 + 128))
    wK = w[:, cols(ok) + cols(ok + 128) + _sw(ok) + _sw(ok + 128)]
    wKIV = w[:, cols(oki) + _sw(oki) + list(range(ov, ov + 256))]
    wQ = np.stack([w[:, cols(oq + 256 * pp) + cols(oq + 256 * pp + 128) + _sw(oq + 256 * pp) + _sw(oq + 256 * pp + 128)] for pp in range(4)])
    wQI = np.stack([w[:, cols(oqi + 256 * pp) + cols(oqi + 256 * pp + 128) + _sw(oqi + 256 * pp) + _sw(oqi + 256 * pp + 128)] for pp in range(2)])
    wWI = w[:, owi:owi + 4]
    invf = (10000.0 ** (-(np.arange(128) % 64) * 2.0 / 128.0) / (2 * np.pi)).astype(np.float32).reshape(128, 1)
    sgn = np.where(np.arange(128) < 64, -1.0, 1.0).astype(np.float32).reshape(128, 1)
    maps = []
    for c in range(8):
        b, h = c // 2, c % 2
        own = own_tokens(h)
        tq = own.reshape(NQB, 128)
        qlim = ((tq // 64 + 1) * 64).T.astype(np.float32)
        qlimrel = qlim - (np.arange(NQB) * 256)[None, :]
        xT = np.ascontiguousarray(x[b].T)
        maps.append({"xT_all": xT, "xT_own": np.ascontiguousarray(xT[:, own]),
                     "pos_all": np.ascontiguousarray(pos[b]), "pos_own": np.ascontiguousarray(pos[b][own]),
                     "qlimrel": np.ascontiguousarray(qlimrel.astype(np.float32)), "invf": invf, "sgn": sgn,
                     "wK": np.ascontiguousarray(wK), "wKIV": np.ascontiguousarray(wKIV), "wQ": np.ascontiguousarray(wQ),
                     "wQI": np.ascontiguousarray(wQI), "wWI": np.ascontiguousarray(wWI)})
    return maps


def own_tokens(h):
    return np.concatenate([np.arange((2 * j + h) * 128, (2 * j + h + 1) * 128) for j in range(NQB)])


HALO = 16
SEG = 128 + HALO


def emit_l0_out(p, A):
    nc = p.nc
    NOH = NQB * SEG
    with ExitStack() as es:
        poolT = p.sb("poolT", [128, 8, NT], BF16, es)
        with ExitStack() as es2:
            wt = p.sb("wt", [128, 16, 512], BF16, es2)
            xb = [p.sb(f"xb{i}", [128, 16, 3 * SEG], BF16, es2) for i in range(2)]
            u = p.sb("u", [128, 4, NOH], F32, es2)
            ua = p.sb("ua", [128, NOH], F32, es2)
            ub = p.sb("ub", [128, NOH], F32, es2)
            icn = p.sb("icn", [128, 4, 128], F32, es2)
            pooled = p.sb("pooled", [128, 8, NT], BF16, es2)
            wp = p.sb("wp", [128, 4, 2, 256], BF16, es2)
            psc = p.sb("psc", [128, 8], F32, es2)
            pu_ = [p.ps(f"pu{i}", [128, 512], F32, es2) for i in range(2)]
            pm = [p.ps(f"pm{i}", [128, 512], F32, es2) for i in range(2)]
            p.dma("sp", icn[:, :, :], bass.AP(tensor=A["invcnt"].tensor, offset=A["invcnt"].offset, ap=[[0, 128], [NT, 4], [1, 128]]), icn, writes=[icn])
            p.dma("pool", wp[:, :, :, :], A["w_pool"].rearrange("g (c q) f -> q g c f", q=128), wp, writes=[wp])
            p.dma("sp", psc[:, :], A["pool_scale"], psc, writes=[psc])
            NXB = (NQB + 2) // 3
            cx = 0
            for pp in range(2):
                p.dma("pool", wt[:, :, :], A["wU"][pp].rearrange("(c q) f -> q c f", q=128), wt, writes=[wt])
                for blk in range(NXB):
                    s0 = blk * 3
                    ns = min(3, NQB - s0)
                    n = ns * SEG
                    b = xb[cx % 2]
                    cx += 1
                    p.dma("pool", b[:, :, 0:n], A["xT_oh"][:, s0 * SEG:s0 * SEG + n].rearrange("(c q) t -> q c t", q=128), b, writes=[b])
                    for oc in range(4):
                        ps_ = pu_[oc % 2]
                        for c in range(16):
                            p.op("pe", lambda: nc.tensor.matmul(ps_[:, 0:n], lhsT=wt[:, c, oc * 128:(oc + 1) * 128], rhs=b[:, c, 0:n],
                                                                start=(c == 0), stop=(c == 15)), reads=[wt, b], writes=[ps_])
                        if oc % 2 == 0:
                            p.op("act", lambda: nc.scalar.copy(out=u[:, oc, s0 * SEG:s0 * SEG + n], in_=ps_[:, 0:n]), reads=[ps_], writes=[(u, oc)])
                        else:
                            p.op("dve", lambda: nc.vector.tensor_copy(out=u[:, oc, s0 * SEG:s0 * SEG + n], in_=ps_[:, 0:n]), reads=[ps_], writes=[(u, oc)])
                for oc in range(4):
                    ch = pp * 4 + oc
                    g = ch // 2
                    cur = None
                    bufs = [ua, ub]
                    eng = "dve" if ch % 2 == 0 else "pool"
                    eo = nc.vector if eng == "dve" else nc.gpsimd
                    for st in range(g + 1):
                        sh = 1 << st
                        dst = bufs[st % 2]
                        s3 = (u[:, oc, :] if cur is None else cur[:, :]).rearrange("p (j s) -> p j s", s=SEG)
                        d3 = dst[:, :].rearrange("p (j s) -> p j s", s=SEG)
                        p.op(eng, lambda: eo.tensor_tensor(out=d3[:, :, sh:SEG], in0=s3[:, :, sh:SEG], in1=s3[:, :, 0:SEG - sh], op=ALU.add),
                             reads=[(u, oc) if cur is None else cur], writes=[dst])
                        cur = dst
                    c3 = cur[:, :].rearrange("p (j s) -> p j s", s=SEG)[:, :, HALO:SEG]
                    u3 = u[:, oc, :].rearrange("p (j s) -> p j s", s=SEG)[:, :, HALO:SEG]
                    o3 = pooled[:, ch, :].rearrange("p (j s) -> p j s", s=128)
                    p.op(eng, lambda: eo.tensor_tensor(out=c3[:, 0, :], in0=c3[:, 0, :], in1=icn[:, g, :], op=ALU.mult), reads=[cur, icn], writes=[cur])
                    p.op(eng, lambda: eo.tensor_scalar(out=c3[:, 1:NQB, :], in0=c3[:, 1:NQB, :], scalar1=1.0 / (2 << g), scalar2=None, op0=ALU.mult),
                         reads=[cur], writes=[cur])
                    p.op(eng, lambda: eo.tensor_tensor(out=o3, in0=c3, in1=u3, op=ALU.subtract), reads=[cur, (u, oc)], writes=[(pooled, ch)])
            km = 0
            for g in range(4):
                for do in range(2):
                    for tb in range(NT // 512):
                        ps_ = pm[km % 2]
                        km += 1
                        for cc in range(2):
                            p.op("pe", lambda: nc.tensor.matmul(ps_[:, :], lhsT=wp[:, g, cc, do * 128:(do + 1) * 128], rhs=pooled[:, 2 * g + cc, tb * 512:(tb + 1) * 512],
                                                                start=(cc == 0), stop=(cc == 1)), reads=[wp, (pooled, 2 * g + cc)], writes=[ps_])
                        ch = 2 * g + do
                        p.op("dve", lambda: nc.vector.tensor_scalar(out=poolT[:, ch, tb * 512:(tb + 1) * 512], in0=ps_[:, :], scalar1=psc[:, ch:ch + 1], scalar2=None,
                                                                    op0=ALU.mult), reads=[ps_, psc], writes=[(poolT, ch)])
            p.barrier()
        with ExitStack() as es3:
            wo = p.sb("wo", [128, 16, D], BF16, es3)
            aT = p.sb("aT", [128, 8, NT], BF16, es3)
            p.dma("sp", aT[:, :, :], A["aT"].rearrange("h d t -> d h t"), aT, writes=[aT])
            gbc = p.sb("gbc", [128, D], F32, es3)
            bbc = p.sb("bbc", [128, D], F32, es3)
            xr = [p.sb(f"xr{i}", [128, D], F32, es3) for i in range(2)]
            zo = [p.sb(f"zo{i}", [128, D], F32, es3) for i in range(2)]
            stats = p.sb("stats", [128, 24], F32, es3)
            mv = p.sb("mv", [128, 2], F32, es3)
            rstd = p.sb("rstd", [128, 1], F32, es3)
            po = [p.ps(f"po{i}", [128, 512], F32, es3) for i in range(4)]
            for c4 in range(4):
                p.dma("pool", wo[:, c4 * 4:(c4 + 1) * 4, :], A["w_out"][c4 * 512:(c4 + 1) * 512, :].rearrange("(c q) f -> q c f", q=128), wo, writes=[(wo, c4)])
            p.dma("sp", gbc[:, :], bcast_rows(A["ln_g"], 128, D), gbc, writes=[gbc])
            p.dma("sp", bbc[:, :], bcast_rows(A["ln_b"], 128, D), bbc, writes=[bbc])
            wok = [(wo, c4) for c4 in range(4)]
            for i in range(NTT):
                xri = xr[i % 2]
                zi = zo[i % 2]
                p.dma("sp", xri[:, :], A["x_own"][i * 128:(i + 1) * 128, :], xri, writes=[xri])
                for dc in range(4):
                    for fc in range(16):
                        src = aT if fc < 8 else poolT
                        p.op("pe", lambda: nc.tensor.matmul(po[dc][:, :], lhsT=src[:, fc % 8, i * 128:(i + 1) * 128], rhs=wo[:, fc, dc * 512:(dc + 1) * 512],
                                                            start=(fc == 0), stop=(fc == 15)), reads=wok + [aT] + [(poolT, ch) for ch in range(8)], writes=[po[dc]])
                    p.op("dve", lambda: nc.vector.scalar_tensor_tensor(out=xri[:, dc * 512:(dc + 1) * 512], in0=xri[:, dc * 512:(dc + 1) * 512], scalar=ALPHA,
                                                                       in1=po[dc][:, :], op0=ALU.mult, op1=ALU.add), reads=[xri, po[dc]], writes=[xri])
                emit_ln(p, xri, zi, gbc, bbc, stats, mv, rstd)
                p.dma("sp", A["xa_out"][i * 128:(i + 1) * 128, :], zi[:, :], zi, reads=[zi], writes=[("xa_out", i)])
                if "xa_out2" in A:
                    p.dma("sp", A["xa_out2"][i * 128:(i + 1) * 128, :], zi[:, :], zi, reads=[zi], writes=[("xa_out2", i)])
            p.barrier()


def build_l0_out_prog():
    nc = new_nc()
    A = {"xT_oh": din(nc, "xT_oh", [D, NQB * SEG]), "wU": din(nc, "wU", [2, D, 512]), "invcnt": din(nc, "invcnt", [4, NT]),
         "w_pool": din(nc, "w_pool", [4, 256, 256]), "pool_scale": din(nc, "pool_scale", [128, 8]),
         "aT": din(nc, "aT", [8, 128, NT], BF16), "w_out": din(nc, "w_out", [D, D]), "x_own": din(nc, "x_own", [NT, D]),
         "ln_g": din(nc, "ln_g", [D]), "ln_b": din(nc, "ln_b", [D]), "xa_out": dout(nc, "xa_out", [NT, D])}
    with ExitStack() as es:
        p = P(nc, es)
        emit_l0_out(p, A)
        p.finish()
    return nc


def l0_out_host(inputs, aT_cores):
    x = inputs["x"]
    w = inputs["w_in0"][0]
    ou = 2180
    wU = np.ascontiguousarray(np.stack([w[:, ou + 512 * pp:ou + 512 * (pp + 1)] for pp in range(2)]))
    psc = np.ascontiguousarray(inputs["pool_scale"][0].reshape(8, 128).T)
    maps = []
    for c in range(8):
        b, h = c // 2, c % 2
        own = own_tokens(h)
        xoh = np.zeros((D, NQB * SEG), np.float32)
        for j in range(NQB):
            t0 = (2 * j + h) * 128
            lo = t0 - HALO
            if lo >= 0:
                xoh[:, j * SEG:(j + 1) * SEG] = x[b, lo:t0 + 128].T
            else:
                xoh[:, j * SEG + HALO:(j + 1) * SEG] = x[b, t0:t0 + 128].T
        invcnt = np.stack([1.0 / np.minimum(own + 1.0, float(wd)) for wd in (2, 4, 8, 16)]).astype(np.float32)
        maps.append({"xT_oh": xoh, "wU": wU, "invcnt": np.ascontiguousarray(invcnt), "w_pool": np.ascontiguousarray(inputs["w_pool"][0]),
                     "pool_scale": psc, "aT": aT_cores[c], "w_out": np.ascontiguousarray(inputs["w_out0"][0]),
                     "x_own": np.ascontiguousarray(x[b][own]), "ln_g": np.ascontiguousarray(inputs["ln_mix_g"][0]),
                     "ln_b": np.ascontiguousarray(inputs["ln_mix_b"][0])})
    return maps


HL = 4
NTILE = S // 128


def emit_l1_mlstm(p, A):
    nc = p.nc
    with ExitStack() as es:
        qT = p.sb("qT", [128, HL, S], BF16, es)
        kT = p.sb("kT", [128, HL, S], BF16, es)
        with ExitStack() as es2:
            wt = p.sb("wt", [128, 16, 512], BF16, es2)
            xb = [p.sb(f"xb{i}", [128, 16, 512], BF16, es2) for i in range(2)]
            raw = [p.sb(f"raw{i}", [128, 4, 3 + 512], F32, es2) for i in range(2)]
            yv = [p.sb(f"yv{i}", [128, 512], F32, es2) for i in range(2)]
            cw = p.sb("cw", [128, 8, 4], F32, es2)
            cb = p.sb("cb", [128, 8], F32, es2)
            pq = [p.ps(f"pq{i}", [128, 512], F32, es2) for i in range(2)]
            p.dma("sp", cw[:, :, :], A["conv_w"], cw, writes=[cw])
            p.dma("sp", cb[:, :], A["conv_b"], cb, writes=[cb])
            cx = 0
            for pp in range(2):
                p.dma("pool", wt[:, :, :], A["wQK"][pp].rearrange("(c q) f -> q c f", q=128), wt, writes=[wt])
                dstT = qT if pp == 0 else kT
                for blk in range(S // 512):
                    b = xb[cx % 2]
                    rw = raw[cx % 2]
                    rprev = raw[(cx + 1) % 2]
                    cx += 1
                    if "x1R" in A:
                        for h_ in range(2):
                            for jj in range(2):
                                p.dma("sp", b[:, :, (jj * 2 + h_) * 128:(jj * 2 + h_ + 1) * 128],
                                      A["x1R"][h_, :, (2 * blk + jj) * 128:(2 * blk + jj + 1) * 128].rearrange("(c p) q -> p c q", p=128), b, writes=[b])
                    else:
                        p.dma("pool", b[:, :, :], A["xT"][:, blk * 512:(blk + 1) * 512].rearrange("(c q) t -> q c t", q=128), b, writes=[b])
                    if blk == 0:
                        p.op("dve", lambda: nc.vector.memset(rw[:, :, 0:3], 0.0), writes=[(rw, "h")])
                    else:
                        p.op("dve", lambda: nc.vector.tensor_copy(out=rw[:, :, 0:3], in_=rprev[:, :, 512:515]),
                             reads=[(rprev, o) for o in range(4)], writes=[(rw, "h")])
                    for oc in range(4):
                        ps_ = pq[oc % 2]
                        for c in range(16):
                            p.op("pe", lambda: nc.tensor.matmul(ps_[:, :], lhsT=wt[:, c, oc * 128:(oc + 1) * 128], rhs=b[:, c, :],
                                                                start=(c == 0), stop=(c == 15)), reads=[wt, b], writes=[ps_])
                        p.op("act", lambda: nc.scalar.copy(out=rw[:, oc, 3:515], in_=ps_[:, :]), reads=[ps_], writes=[(rw, oc)])
                        ch = pp * 4 + oc
                        y = yv[oc % 2]
                        eng = "dve" if oc % 2 == 0 else "pool"
                        eo = nc.vector if eng == "dve" else nc.gpsimd
                        p.op("dve", lambda: nc.vector.tensor_scalar(out=y[:, :], in0=rw[:, oc, 3:515], scalar1=cw[:, ch, 3:4], scalar2=cb[:, ch:ch + 1],
                                                                    op0=ALU.mult, op1=ALU.add), reads=[(rw, oc), cw, cb], writes=[y])
                        for tap in range(3):
                            p.op("dve", lambda: nc.vector.scalar_tensor_tensor(out=y[:, :], in0=rw[:, oc, tap:tap + 512], scalar=cw[:, ch, tap:tap + 1], in1=y[:, :],
                                                                               op0=ALU.mult, op1=ALU.add), reads=[(rw, oc), (rw, "h"), cw, y], writes=[y])
                        if pp == 0:
                            p.op("act", lambda: nc.scalar.activation(out=y[:, :], in_=y[:, :], func=AF.Silu), reads=[y], writes=[y])
                            p.op("pool", lambda: nc.gpsimd.tensor_scalar(out=dstT[:, oc, blk * 512:(blk + 1) * 512], in0=y[:, :], scalar1=128.0 ** -0.5, scalar2=None,
                                                                         op0=ALU.mult), reads=[y], writes=[(dstT, oc, blk)])
                        else:
                            p.op("act", lambda: nc.scalar.activation(out=dstT[:, oc, blk * 512:(blk + 1) * 512], in_=y[:, :], func=AF.Silu), reads=[y], writes=[(dstT, oc, blk)])
            p.barrier()
        with ExitStack() as es3:
            wv = p.sb("wv", [128, 16, 2056], BF16, es3)
            xb = [p.sb(f"xt{i}", [128, 16, 128], BF16, es3) for i in range(2)]
            vext = [p.sb(f"vext{i}", [128, HL, 258], BF16, es3) for i in range(2)]
            ogs = p.sb("ogs", [128, 1024], F32, es3)
            gb = p.sb("gb", [128, 8], F32, es3)
            cng = p.sb("cng", [128, 1024], F32, es3)
            g8 = p.sb("g8", [128, 8], F32, es3)
            e4 = p.sb("e4", [128, 4], F32, es3)
            lf4 = p.sb("lf4", [128, 4], F32, es3)
            a4 = p.sb("a4", [128, 4], F32, es3)
            ei4 = p.sb("ei4", [128, 4], F32, es3)
            tri2 = p.sb("tri2", [128, 128], F32, es3)
            mask2 = p.sb("mask2", [128, 128], F32, es3)
            ind2 = p.sb("ind2", [128, 2], F32, es3)
            ones_f = p.sb("ones_f", [128, 128], F32, es3)
            identb = p.sb("identb", [128, 128], BF16, es3)
            lfb = [p.sb(f"lfb{i}", [128, 128], F32, es3) for i in range(2)]
            E = [p.sb(f"E{i}", [128, 128], F32, es3) for i in range(2)]
            SmT = [p.sb(f"SmT{i}", [128, 128], BF16, es3) for i in range(2)]
            dec2 = [p.sb(f"dec2{i}", [128, 2], F32, es3) for i in range(2)]
            bLo = [p.sb(f"bLo{i}", [128, 1], F32, es3) for i in range(2)]
            wgt = [p.sb(f"wgt{i}", [128, 1], F32, es3) for i in range(2)]
            kw = [p.sb(f"kw{i}", [128, 128], BF16, es3) for i in range(2)]
            intra = [p.sb(f"intra{i}", [128, 257], F32, es3) for i in range(2)]
            num = [p.sb(f"num{i}", [128, 257], F32, es3) for i in range(2)]
            den = [p.sb(f"den{i}", [128, 1], F32, es3) for i in range(2)]
            hh = [p.sb(f"hh{i}", [128, 256], F32, es3) for i in range(2)]
            st6 = [p.sb(f"st6{i}", [128, 6], F32, es3) for i in range(2)]
            mv = [p.sb(f"mv{i}", [128, 2], F32, es3) for i in range(2)]
            rs = [p.sb(f"rs{i}", [128, 1], F32, es3) for i in range(2)]
            CT = [p.sb(f"CT{h}", [128, 257], F32, es3) for h in range(HL)]
            CTb = [p.sb(f"CTb{h}", [128, 257], BF16, es3) for h in range(HL)]
            y = [p.sb(f"y{i}", [128, 1024], F32, es3) for i in range(2)]
            ybf = [p.sb(f"ybf{i}", [128, 1024], BF16, es3) for i in range(2)]
            yT = [p.sb(f"yT{i}", [128, 8, 128], BF16, es3) for i in range(2)]
            P1 = p.ps("P1", [128, 512], F32, es3)
            P3 = p.ps("P3", [128, 64], F32, es3)
            P4 = p.ps("P4", [128, 4, 128], F32, es3)
            P5 = p.ps("P5", [128, 4, 128], F32, es3)
            P6 = p.ps("P6", [128, 8, 128], BF16, es3)
            Pin = p.ps("Pin", [128, 257], F32, es3)
            Pit = p.ps("Pit", [128, 257], F32, es3)
            Pup = p.ps("Pup", [128, 257], F32, es3)

            for c4 in range(4):
                p.dma("pool", wv[:, c4 * 4:(c4 + 1) * 4, :], A["wVOG"][c4 * 512:(c4 + 1) * 512, :].rearrange("(c q) f -> q c f", q=128), wv, writes=[(wv, c4)])
            wvk = [(wv, c4) for c4 in range(4)]
            p.dma("sp", gb[:, :], bcast_rows(A["gate_b"], 128, 8), gb, writes=[gb])
            p.dma("sp", cng[:, :], bcast_rows(A["c_norm_g"], 128, 1024), cng, writes=[cng])
            p.op("dve", lambda: nc.vector.memset(ones_f[:, :], 1.0), writes=[ones_f])
            p.op("dve", lambda: nc.vector.memset(mask2[:, :], 1.0), writes=[mask2])
            p.op("pool", lambda: nc.gpsimd.affine_select(out=mask2[:, :], in_=mask2[:, :], pattern=[[1, 128]], compare_op=ALU.is_ge,
                                                         fill=0.0, base=0, channel_multiplier=-1), reads=[mask2], writes=[mask2])
            p.op("dve", lambda: nc.vector.memset(mask2[0:64, 64:128], 0.0), reads=[mask2], writes=[mask2])
            p.op("dve", lambda: nc.vector.tensor_copy(out=tri2[:, :], in_=mask2[:, :]), reads=[mask2], writes=[tri2])
            p.op("dve", lambda: nc.vector.memset(ind2[:, :], 0.0), writes=[ind2])
            p.op("dve", lambda: nc.vector.memset(ind2[0:64, 0:1], 1.0), reads=[ind2], writes=[ind2])
            p.op("dve", lambda: nc.vector.memset(ind2[64:128, 1:2], 1.0), reads=[ind2], writes=[ind2])
            p.op("dve", lambda: nc.vector.memset(identb[:, :], 1.0), writes=[identb])
            p.op("pool", lambda: nc.gpsimd.affine_select(out=identb[:, :], in_=identb[:, :], pattern=[[1, 128]], compare_op=ALU.is_equal,
                                                         fill=0.0, base=0, channel_multiplier=-1), reads=[identb], writes=[identb])
            for h in range(HL):
                p.op("dve", lambda: nc.vector.memset(CT[h][:, :], 0.0), writes=[CT[h]])
                p.op("dve", lambda: nc.vector.memset(CTb[h][:, :], 0.0), writes=[CTb[h]])
            for i2 in range(2):
                p.op("dve", lambda: nc.vector.memset(vext[i2][:, :, 256:258], 1.0), writes=[(vext[i2], "one")])

            for i in range(NTILE):
                xt = xb[i % 2]
                ve = vext[i % 2]
                yi = y[i % 2]
                ts_ = slice(i * 128, (i + 1) * 128)
                if "x1R" in A:
                    p.dma("sp", xt[:, :, :], A["x1R"][i % 2, :, (i // 2) * 128:(i // 2 + 1) * 128].rearrange("(c q) t -> q c t", q=128), xt, writes=[xt])
                else:
                    p.dma("pool", xt[:, :, :], A["xT"][:, ts_].rearrange("(c q) t -> q c t", q=128), xt, writes=[xt])
                for vb in range(2):
                    for c in range(16):
                        p.op("pe", lambda: nc.tensor.matmul(P1[:, :], lhsT=xt[:, c, :], rhs=wv[:, c, vb * 512:(vb + 1) * 512], start=(c == 0), stop=(c == 15)),
                             reads=[xt] + wvk, writes=[P1])
                    p.op("act", lambda: nc.scalar.copy(out=ve[:, 2 * vb:2 * vb + 2, 0:256], in_=P1[:, :].rearrange("p (h d) -> p h d", h=2)),
                         reads=[P1], writes=[(ve, vb)])
                for ob in range(2):
                    for c in range(16):
                        p.op("pe", lambda: nc.tensor.matmul(P1[:, :], lhsT=xt[:, c, :], rhs=wv[:, c, 1024 + ob * 512:1024 + (ob + 1) * 512], start=(c == 0), stop=(c == 15)),
                             reads=[xt] + wvk, writes=[P1])
                    p.op("act", lambda: nc.scalar.activation(out=ogs[:, ob * 512:(ob + 1) * 512], in_=P1[:, :], func=AF.Sigmoid), reads=[P1], writes=[(ogs, ob)])
                for c in range(16):
                    p.op("pe", lambda: nc.tensor.matmul(P3[:, 0:8], lhsT=xt[:, c, :], rhs=wv[:, c, 2048:2056], start=(c == 0), stop=(c == 15)),
                         reads=[xt] + wvk, writes=[(P3, "g")])
                p.op("dve", lambda: nc.vector.tensor_tensor(out=g8[:, :], in0=P3[:, 0:8], in1=gb[:, :], op=ALU.add), reads=[(P3, "g"), gb], writes=[g8])
                p.op("act", lambda: nc.scalar.activation(out=e4[:, :], in_=g8[:, 4:8], func=AF.Exp, scale=-1.0), reads=[g8], writes=[e4])
                p.op("act", lambda: nc.scalar.activation(out=e4[:, :], in_=e4[:, :], func=AF.Ln, bias=1.0), reads=[e4], writes=[e4])
                p.op("dve", lambda: nc.vector.tensor_scalar(out=lf4[:, :], in0=e4[:, :], scalar1=-1.0, scalar2=None, op0=ALU.mult), reads=[e4], writes=[lf4])
                p.op("pe", lambda: nc.tensor.matmul(P3[:, 8:12], lhsT=tri2[:, :], rhs=lf4[:, :], start=True, stop=True), reads=[tri2, lf4], writes=[(P3, "b")])
                p.op("dve", lambda: nc.vector.tensor_tensor(out=a4[:, :], in0=g8[:, 0:4], in1=P3[:, 8:12], op=ALU.subtract), reads=[g8, (P3, "b")], writes=[a4])
                p.op("act", lambda: nc.scalar.activation(out=ei4[:, :], in_=P3[:, 8:12], func=AF.Exp), reads=[(P3, "b")], writes=[ei4])
                for h in range(HL):
                    k2 = h % 2
                    p.op("pool", lambda: nc.gpsimd.tensor_scalar(out=lfb[k2][:, :], in0=ones_f[:, :], scalar1=lf4[:, h:h + 1], scalar2=None, op0=ALU.mult),
                         reads=[ones_f, lf4], writes=[lfb[k2]])
                    p.op("pe", lambda: nc.tensor.matmul(P4[:, h, :], lhsT=lfb[k2][:, :], rhs=tri2[:, :], start=True, stop=True), reads=[lfb[k2], tri2], writes=[(P4, h)])
                    p.op("pe", lambda: nc.tensor.matmul(P3[:, 16 + 2 * h:18 + 2 * h], lhsT=lfb[k2][:, :], rhs=ind2[:, :], start=True, stop=True),
                         reads=[lfb[k2], ind2], writes=[(P3, "L", h)])
                    p.op("act", lambda: nc.scalar.activation(out=E[k2][:, :], in_=P4[:, h, :], func=AF.Exp, bias=a4[:, h:h + 1]), reads=[(P4, h), a4], writes=[E[k2]])
                    p.op("pool", lambda: nc.gpsimd.tensor_tensor(out=E[k2][:, :], in0=E[k2][:, :], in1=mask2[:, :], op=ALU.mult), reads=[E[k2], mask2], writes=[E[k2]])
                    p.op("pe", lambda: nc.tensor.matmul(P5[:, h, :], lhsT=kT[:, h, ts_], rhs=qT[:, h, ts_], start=True, stop=True), reads=[kT, qT], writes=[(P5, h)])
                    p.op("dve", lambda: nc.vector.tensor_tensor(out=SmT[k2][:, :], in0=P5[:, h, :], in1=E[k2][:, :], op=ALU.mult), reads=[(P5, h), E[k2]], writes=[SmT[k2]])
                    p.op("act", lambda: nc.scalar.activation(out=dec2[k2][:, :], in_=P3[:, 16 + 2 * h:18 + 2 * h], func=AF.Exp), reads=[(P3, "L", h)], writes=[dec2[k2]])
                    p.op("dve", lambda: nc.vector.tensor_copy(out=bLo[k2][0:64, :], in_=P3[0:64, 16 + 2 * h:17 + 2 * h]), reads=[(P3, "L", h)], writes=[(bLo[k2], 0)])
                    p.op("dve", lambda: nc.vector.tensor_copy(out=bLo[k2][64:128, :], in_=P3[64:128, 17 + 2 * h:18 + 2 * h]), reads=[(P3, "L", h)], writes=[(bLo[k2], 1)])
                    p.op("act", lambda: nc.scalar.activation(out=wgt[k2][:, :], in_=bLo[k2][:, :], func=AF.Exp, bias=a4[:, h:h + 1]),
                         reads=[(bLo[k2], 0), (bLo[k2], 1), a4], writes=[wgt[k2]])
                    p.op("pe", lambda: nc.tensor.transpose(out=P6[:, h, :], in_=kT[:, h, ts_], identity=identb[:, :]), reads=[kT, identb], writes=[(P6, h)])
                    p.op("dve", lambda: nc.vector.tensor_scalar(out=kw[k2][:, :], in0=P6[:, h, :], scalar1=wgt[k2][:, 0:1], scalar2=None, op0=ALU.mult),
                         reads=[(P6, h), wgt[k2]], writes=[kw[k2]])
                    p.op("pe", lambda: nc.tensor.matmul(Pit[:, :], lhsT=SmT[k2][:, :], rhs=ve[:, h, 0:257], start=True, stop=True),
                         reads=[SmT[k2], (ve, h // 2), (ve, "one")], writes=[Pit])
                    p.op("act", lambda: nc.scalar.copy(out=intra[k2][:, :], in_=Pit[:, :]), reads=[Pit], writes=[intra[k2]])
                    for ck in range(2):
                        ps_l = slice(ck * 64, (ck + 1) * 64)
                        p.op("pe", lambda: nc.tensor.matmul(Pin[:, :], lhsT=qT[:, h, ts_], rhs=CTb[h][:, :], start=True, stop=True), reads=[qT, CTb[h]], writes=[Pin])
                        p.op("dve", lambda: nc.vector.scalar_tensor_tensor(out=num[k2][ps_l, :], in0=Pin[ps_l, :], scalar=ei4[ps_l, h:h + 1], in1=intra[k2][ps_l, :],
                                                                           op0=ALU.mult, op1=ALU.add), reads=[Pin, ei4, intra[k2]], writes=[(num[k2], ck)])
                        p.op("pe", lambda: nc.tensor.matmul(Pup[:, :], lhsT=kw[k2][ps_l, :], rhs=ve[ps_l, h, 0:257], start=True, stop=True),
                             reads=[kw[k2], (ve, h // 2), (ve, "one")], writes=[Pup])
                        p.op("dve", lambda: nc.vector.scalar_tensor_tensor(out=CT[h][:, :], in0=CT[h][:, :], scalar=dec2[k2][:, ck:ck + 1], in1=Pup[:, :],
                                                                           op0=ALU.mult, op1=ALU.add), reads=[CT[h], dec2[k2], Pup], writes=[CT[h]])
                        p.op("act", lambda: nc.scalar.copy(out=CTb[h][:, :], in_=CT[h][:, :]), reads=[CT[h]], writes=[CTb[h]])
                    numk = [(num[k2], 0), (num[k2], 1)]
                    p.op("act", lambda: nc.scalar.activation(out=den[k2][:, :], in_=num[k2][:, 256:257], func=AF.Abs), reads=numk, writes=[den[k2]])
                    p.op("dve", lambda: nc.vector.tensor_scalar(out=den[k2][:, :], in0=den[k2][:, :], scalar1=1.0, scalar2=None, op0=ALU.max),
                         reads=[den[k2]], writes=[den[k2]])
                    p.op("dve", lambda: nc.vector.reciprocal(out=den[k2][:, :], in_=den[k2][:, :]), reads=[den[k2]], writes=[den[k2]])
                    p.op("dve", lambda: nc.vector.tensor_scalar(out=hh[k2][:, :], in0=num[k2][:, 0:256], scalar1=den[k2][:, 0:1], scalar2=None, op0=ALU.mult),
                         reads=numk + [den[k2]], writes=[hh[k2]])
                    p.op("dve", lambda: nc.vector.bn_stats(out=st6[k2][:, :], in_=hh[k2][:, :]), reads=[hh[k2]], writes=[st6[k2]])
                    p.op("dve", lambda: nc.vector.bn_aggr(out=mv[k2][:, :], in_=st6[k2][:, :]), reads=[st6[k2]], writes=[mv[k2]])
                    p.op("dve", lambda: nc.vector.tensor_scalar(out=rs[k2][:, :], in0=mv[k2][:, 1:2], scalar1=LN_EPS, scalar2=None, op0=ALU.add), reads=[mv[k2]], writes=[rs[k2]])
                    p.op("act", lambda: nc.scalar.activation(out=rs[k2][:, :], in_=rs[k2][:, :], func=AF.Sqrt), reads=[rs[k2]], writes=[rs[k2]])
                    p.op("dve", lambda: nc.vector.reciprocal(out=rs[k2][:, :], in_=rs[k2][:, :]), reads=[rs[k2]], writes=[rs[k2]])
                    p.op("dve", lambda: nc.vector.tensor_scalar(out=yi[:, h * 256:(h + 1) * 256], in0=hh[k2][:, :], scalar1=mv[k2][:, 0:1], scalar2=rs[k2][:, 0:1],
                                                                op0=ALU.subtract, op1=ALU.mult), reads=[hh[k2], mv[k2], rs[k2]], writes=[(yi, h)])
                yk = [(yi, h) for h in range(HL)]
                p.op("pool", lambda: nc.gpsimd.tensor_tensor(out=yi[:, :], in0=yi[:, :], in1=cng[:, :], op=ALU.mult), reads=yk + [cng], writes=yk)
                p.op("dve", lambda: nc.vector.tensor_tensor(out=ybf[i % 2][:, :], in0=yi[:, :], in1=ogs[:, :], op=ALU.mult), reads=yk + [(ogs, 0), (ogs, 1)], writes=[ybf[i % 2]])
                for fcn in range(8):
                    p.op("pe", lambda: nc.tensor.transpose(out=P6[:, fcn, :], in_=ybf[i % 2][:, fcn * 128:(fcn + 1) * 128], identity=identb[:, :]),
                         reads=[ybf[i % 2], identb], writes=[(P6, fcn % 4)] if fcn < 4 else [(P6, "hi", fcn)])
                p.op("act", lambda: nc.scalar.copy(out=yT[i % 2][:, :, :], in_=P6[:, :, :]),
                     reads=[(P6, f_) for f_ in range(4)] + [(P6, "hi", f_) for f_ in range(4, 8)], writes=[yT[i % 2]])
                hdst = A["HS"][i % 2, :, (i // 2) * 128:(i // 2 + 1) * 128] if "HS" in A else A["hgT_out"][:, ts_]
                p.dma("sp", hdst.rearrange("(c q) t -> q c t", q=128), yT[i % 2][:, :, :], yT[i % 2], reads=[yT[i % 2]], writes=[("hg", i)])
            p.barrier()


def build_l1_mlstm_prog():
    nc = new_nc()
    A = {"xT": din(nc, "xT", [D, S]), "wQK": din(nc, "wQK", [2, D, 512]), "wVOG": din(nc, "wVOG", [D, 2056]),
         "conv_w": din(nc, "conv_w", [128, 8, 4]), "conv_b": din(nc, "conv_b", [128, 8]), "gate_b": din(nc, "gate_b", [8]),
         "c_norm_g": din(nc, "c_norm_g", [1024]), "hgT_out": dout(nc, "hgT_out", [1024, S], BF16)}
    with ExitStack() as es:
        p = P(nc, es)
        emit_l1_mlstm(p, A)
        p.finish()
    return nc


def l1_mlstm_host(inputs, x1_full):
    w = inputs["w_in1"][0]
    cwf = inputs["conv_w"][0]
    cbf = inputs["conv_b"][0]
    maps = []
    for c in range(8):
        b, hh = c // 2, c % 2
        qc = np.arange(hh * 512, (hh + 1) * 512)
        kc = 1024 + qc
        vc = 2048 + np.arange(hh * 1024, (hh + 1) * 1024)
        igc = 4096 + np.arange(hh * 4, (hh + 1) * 4)
        fgc = 4104 + np.arange(hh * 4, (hh + 1) * 4)
        ogc = 4112 + np.arange(hh * 1024, (hh + 1) * 1024)
        wQK = np.stack([w[:, qc], w[:, kc]])
        wVOG = w[:, np.concatenate([vc, ogc, igc, fgc])]
        chans = np.concatenate([qc, kc])
        cw = cwf[:, chans].reshape(4, 8, 128).transpose(2, 1, 0)
        cb = cbf[chans].reshape(8, 128).T
        gate_b = np.concatenate([inputs["gate_i_b"][0][hh * 4:(hh + 1) * 4], inputs["gate_f_b"][0][hh * 4:(hh + 1) * 4]])
        maps.append({"xT": (np.ascontiguousarray(x1_full[b].T) if x1_full is not None else None), "wQK": np.ascontiguousarray(wQK), "wVOG": np.ascontiguousarray(wVOG),
                     "conv_w": np.ascontiguousarray(cw), "conv_b": np.ascontiguousarray(cb), "gate_b": np.ascontiguousarray(gate_b),
                     "c_norm_g": np.ascontiguousarray(inputs["c_norm_g"][0][hh * 1024:(hh + 1) * 1024])})
    return maps


def emit_l1_out(p, A):
    nc = p.nc
    with ExitStack() as es3:
        wo = p.sb("wo", [128, 16, D], BF16, es3)
        hg = p.sb("hg", [128, 16, NT], BF16, es3)
        gbc = p.sb("gbc", [128, D], F32, es3)
        bbc = p.sb("bbc", [128, D], F32, es3)
        xr = [p.sb(f"xr{i}", [128, D], F32, es3) for i in range(2)]
        zo = [p.sb(f"zo{i}", [128, D], F32, es3) for i in range(2)]
        stats = p.sb("stats", [128, 24], F32, es3)
        mv = p.sb("mv", [128, 2], F32, es3)
        rstd = p.sb("rstd", [128, 1], F32, es3)
        po = [p.ps(f"po{i}", [128, 512], F32, es3) for i in range(4)]
        if "HR" in A:
            tabh = p.sb("tabH", [128, 16], I32, es3)
            p.dma("sp", tabh[:, :], A["tabH"], tabh, writes=[tabh])
            HRrows = A["HR"].rearrange("r h f t -> (r h f) t")
            for fc in range(16):
                p.dma("pool", None, None, hg, reads=[tabh], writes=[hg],
                      fn=lambda: nc.gpsimd.indirect_dma_start(out=hg[:, fc, :], out_offset=None, in_=HRrows,
                                                              in_offset=bass.IndirectOffsetOnAxis(ap=tabh[:, fc:fc + 1], axis=0)))
        else:
            p.dma("sp", hg[:, :, :], A["hgT"].rearrange("(c q) t -> q c t", q=128), hg, writes=[hg])
        for c4 in range(4):
            p.dma("pool", wo[:, c4 * 4:(c4 + 1) * 4, :], A["w_out"][c4 * 512:(c4 + 1) * 512, :].rearrange("(c q) f -> q c f", q=128), wo, writes=[(wo, c4)])
        p.dma("sp", gbc[:, :], bcast_rows(A["ln_g"], 128, D), gbc, writes=[gbc])
        p.dma("sp", bbc[:, :], bcast_rows(A["ln_b"], 128, D), bbc, writes=[bbc])
        wok = [(wo, c4) for c4 in range(4)]
        for i in range(NTT):
            xri = xr[i % 2]
            zi = zo[i % 2]
            p.dma("sp", xri[:, :], A["x_own"][i * 128:(i + 1) * 128, :], xri, writes=[xri])
            for dc in range(4):
                for fc in range(16):
                    p.op("pe", lambda: nc.tensor.matmul(po[dc][:, :], lhsT=hg[:, fc, i * 128:(i + 1) * 128], rhs=wo[:, fc, dc * 512:(dc + 1) * 512],
                                                        start=(fc == 0), stop=(fc == 15)), reads=wok + [hg], writes=[po[dc]])
                p.op("dve", lambda: nc.vector.scalar_tensor_tensor(out=xri[:, dc * 512:(dc + 1) * 512], in0=xri[:, dc * 512:(dc + 1) * 512], scalar=ALPHA,
                                                                   in1=po[dc][:, :], op0=ALU.mult, op1=ALU.add), reads=[xri, po[dc]], writes=[xri])
            emit_ln(p, xri, zi, gbc, bbc, stats, mv, rstd)
            p.dma("sp", A["xb_out"][i * 128:(i + 1) * 128, :], zi[:, :], zi, reads=[zi], writes=[("xb_out", i)])
            if "xb_out2" in A:
                p.dma("sp", A["xb_out2"][i * 128:(i + 1) * 128, :], zi[:, :], zi, reads=[zi], writes=[("xb_out2", i)])
        p.barrier()


def build_l1_out_prog():
    nc = new_nc()
    A = {"hgT": din(nc, "hgT", [D, NT], BF16), "w_out": din(nc, "w_out", [D, D]), "x_own": din(nc, "x_own", [NT, D]),
         "ln_g": din(nc, "ln_g", [D]), "ln_b": din(nc, "ln_b", [D]), "xb_out": dout(nc, "xb_out", [NT, D])}
    with ExitStack() as es:
        p = P(nc, es)
        emit_l1_out(p, A)
        p.finish()
    return nc


ALL8 = list(range(8))


def kernel_unfused(**inputs):
    inputs = {k: np.asarray(v) for k, v in inputs.items()}
    r = run_bass_kernel_spmd(build_l0_attn_prog(), l0_attn_host(inputs), core_ids=ALL8).results
    aT = [np.asarray(r[c]["aT_out"]) for c in range(8)]
    r = run_bass_kernel_spmd(build_l0_out_prog(), l0_out_host(inputs, aT), core_ids=ALL8).results
    xa = [np.asarray(r[c]["xa_out"]) for c in range(8)]
    x1 = run_moe_layer(xa, inputs, 0)
    x1_full = np.empty((4, S, D), np.float32)
    for c in range(8):
        x1_full[c // 2][own_tokens(c % 2)] = x1[c]
    r = run_bass_kernel_spmd(build_l1_mlstm_prog(), l1_mlstm_host(inputs, x1_full), core_ids=ALL8).results
    hg = [np.asarray(r[c]["hgT_out"]) for c in range(8)]
    maps = []
    for c in range(8):
        b, h = c // 2, c % 2
        hgT = np.concatenate([hg[2 * b], hg[2 * b + 1]], axis=0)[:, own_tokens(h)]
        maps.append({"hgT": np.ascontiguousarray(hgT), "w_out": np.ascontiguousarray(inputs["w_out1"][0]), "x_own": x1[c],
                     "ln_g": np.ascontiguousarray(inputs["ln_mix_g"][1]), "ln_b": np.ascontiguousarray(inputs["ln_mix_b"][1])})
    r = run_bass_kernel_spmd(build_l1_out_prog(), maps, core_ids=ALL8).results
    xb = [np.asarray(r[c]["xb_out"]) for c in range(8)]
    x2 = run_moe_layer(xb, inputs, 1)
    out = np.empty((4, S, D), np.float32)
    for c in range(8):
        out[c // 2][own_tokens(c % 2)] = x2[c]
    return out


NXR = (8 * 8 * NLOC)
G8 = [list(range(8))]


def pair_groups(r):
    return [[c, c ^ r] for c in range(8) if c < (c ^ r)]


def emit_dispatch2(p, x_in, XS, XR, idx_out, gk_out, W, ident_f):
    nc = p.nc
    with ExitStack() as es:
        xbf = p.sb("xbf", [128, NTT, D], BF16, es)
        posm = p.sb("posm", [128, NTT, NE], F32, es)
        idx = p.sb("idx", [128, NTT, 4], I32, es)
        gk = p.sb("gk", [128, NTT, 4], F32, es)
        iotac = p.sb("iotac", [128, CAP], F32, es)
        ones_bf = p.sb("ones_bf", [128, 128], BF16, es)
        p.op("pool", lambda: nc.gpsimd.iota(iotac[:, :], pattern=[[1, CAP]], base=0, channel_multiplier=0,
                                            allow_small_or_imprecise_dtypes=True), writes=[iotac])
        p.op("dve", lambda: nc.vector.memset(ones_bf[:, :], 1.0), writes=[ones_bf])
        with ExitStack() as es2:
            wr = p.sb("wr", [128, 16, NE], F32, es2)
            rb = p.sb("rb", [128, NE], F32, es2)
            ustr = p.sb("ustr", [128, 128], BF16, es2)
            rowbase = p.sb("rowbase", [128, NE], F32, es2)
            cum = p.sb("cum", [128, NE], F32, es2)
            xa = [p.sb(f"xa{i}", [128, D], F32, es2) for i in range(2)]
            xT = [p.sb(f"xT{i}", [128, 16, 128], F32, es2) for i in range(2)]
            lg = p.sb("lg", [128, NE], F32, es2)
            m8 = p.sb("m8", [128, 8], F32, es2)
            nm = p.sb("nm", [128, 1], F32, es2)
            mask = p.sb("mask", [128, NE], F32, es2)
            maskb = p.sb("maskb", [128, NE], BF16, es2)
            ex = p.sb("ex", [128, NE], F32, es2)
            ssum = p.sb("ssum", [128, 1], F32, es2)
            gate = p.sb("gate", [128, NE], F32, es2)
            pos = p.sb("pos", [128, NE], F32, es2)
            vv = p.sb("vv", [128, NE], F32, es2)
            ltc = p.sb("ltc", [128, NE], F32, es2)
            eqk = p.sb("eqk", [128, NE], F32, es2)
            r8 = p.sb("r8", [128, 8], F32, es2)
            pt = [p.ps(f"pt{i}", [128, 512], F32, es2) for i in range(4)]
            plg = p.ps("plg", [128, NE], F32, es2)
            ppos = p.ps("ppos", [128, 2, NE], F32, es2)
            p.dma("sp", wr[:, :, :], W["router_w"], wr, writes=[wr])
            p.dma("sp", rb[:, :], bcast_rows(W["router_b"], 128, NE), rb, writes=[rb])
            p.dma("sp", rowbase[:, :], bcast_rows(W["rowbase"], 128, NE), rowbase, writes=[rowbase])
            p.op("dve", lambda: nc.vector.memset(ustr[:, :], 1.0), writes=[ustr])
            p.op("pool", lambda: nc.gpsimd.affine_select(out=ustr[:, :], in_=ustr[:, :], pattern=[[1, 128]],
                                                         compare_op=ALU.is_gt, fill=0.0, base=0, channel_multiplier=-1),
                 reads=[ustr], writes=[ustr])
            p.op("dve", lambda: nc.vector.memset(cum[:, :], 0.0), writes=[cum])
            for i in range(NTT):
                xai = xa[i % 2]
                xTi = xT[i % 2]
                p.dma("sp", xai[:, :], x_in[i * 128:(i + 1) * 128, :], xai, writes=[xai])
                p.op("act", lambda: nc.scalar.copy(out=xbf[:, i, :], in_=xai[:, :]), reads=[xai], writes=[(xbf, i)])
                for g in range(4):
                    ptg = pt[g]
                    for j in range(4):
                        c = g * 4 + j
                        p.op("pe", lambda: nc.tensor.transpose(out=ptg[:, j * 128:(j + 1) * 128],
                                                               in_=xai[:, c * 128:(c + 1) * 128], identity=ident_f[:, :]),
                             reads=[xai, ident_f], writes=[ptg])
                    if g % 2 == 0:
                        p.op("dve", lambda: nc.vector.tensor_copy(out=xTi[:, g * 4:(g + 1) * 4, :],
                                                                  in_=ptg[:, :].rearrange("p (a b) -> p a b", a=4)),
                             reads=[ptg], writes=[(xTi, g)])
                    else:
                        p.op("act", lambda: nc.scalar.copy(out=xTi[:, g * 4:(g + 1) * 4, :],
                                                           in_=ptg[:, :].rearrange("p (a b) -> p a b", a=4)),
                             reads=[ptg], writes=[(xTi, g)])
                for c in range(16):
                    p.op("pe", lambda: nc.tensor.matmul(plg[:, :], lhsT=xTi[:, c, :], rhs=wr[:, c, :], start=(c == 0), stop=(c == 15)),
                         reads=[(xTi, c // 4), wr], writes=[plg])
                p.op("dve", lambda: nc.vector.tensor_tensor(out=lg[:, :], in0=plg[:, :], in1=rb[:, :], op=ALU.add), reads=[plg, rb], writes=[lg])
                p.op("dve", lambda: nc.vector.max(out=m8[:, :], in_=lg[:, :]), reads=[lg], writes=[m8])
                p.op("dve", lambda: nc.vector.tensor_scalar(out=mask[:, :], in0=lg[:, :], scalar1=m8[:, 3:4], scalar2=None, op0=ALU.is_ge),
                     reads=[lg, m8], writes=[mask])
                p.op("pool", lambda: nc.gpsimd.tensor_copy(out=maskb[:, :], in_=mask[:, :]), reads=[mask], writes=[maskb])
                p.op("dve", lambda: nc.vector.tensor_scalar(out=nm[:, :], in0=m8[:, 0:1], scalar1=-1.0, scalar2=None, op0=ALU.mult),
                     reads=[m8], writes=[nm])
                p.op("act", lambda: nc.scalar.activation(out=ex[:, :], in_=lg[:, :], func=AF.Exp, bias=nm[:, 0:1], scale=1.0),
                     reads=[lg, nm], writes=[ex])
                p.op("dve", lambda: nc.vector.tensor_tensor(out=ex[:, :], in0=ex[:, :], in1=mask[:, :], op=ALU.mult), reads=[ex, mask], writes=[ex])
                p.op("dve", lambda: nc.vector.reduce_sum(out=ssum[:, :], in_=ex[:, :], axis=AX.X), reads=[ex], writes=[ssum])
                p.op("dve", lambda: nc.vector.reciprocal(out=ssum[:, :], in_=ssum[:, :]), reads=[ssum], writes=[ssum])
                p.op("dve", lambda: nc.vector.tensor_scalar(out=gate[:, :], in0=ex[:, :], scalar1=ssum[:, 0:1], scalar2=None, op0=ALU.mult),
                     reads=[ex, ssum], writes=[gate])
                p.op("pe", lambda: nc.tensor.matmul(ppos[:, 0, :], lhsT=ustr[:, :], rhs=maskb[:, :], start=True, stop=True),
                     reads=[ustr, maskb], writes=[(ppos, 0)])
                p.op("pe", lambda: nc.tensor.matmul(ppos[:, 1, :], lhsT=ones_bf[:, :], rhs=maskb[:, :], start=True, stop=True),
                     reads=[ones_bf, maskb], writes=[(ppos, 1)])
                p.op("dve", lambda: nc.vector.tensor_tensor(out=pos[:, :], in0=ppos[:, 0, :], in1=cum[:, :], op=ALU.add),
                     reads=[(ppos, 0), cum], writes=[pos])
                p.op("dve", lambda: nc.vector.tensor_tensor(out=cum[:, :], in0=ppos[:, 1, :], in1=cum[:, :], op=ALU.add),
                     reads=[(ppos, 1), cum], writes=[cum])
                p.op("dve", lambda: nc.vector.scalar_tensor_tensor(out=posm[:, i, :], in0=pos[:, :], scalar=1.0, in1=mask[:, :],
                                                                   op0=ALU.add, op1=ALU.mult), reads=[pos, mask], writes=[(posm, i)])
                p.op("dve", lambda: nc.vector.tensor_scalar(out=posm[:, i, :], in0=posm[:, i, :], scalar1=-1.0, scalar2=None, op0=ALU.add),
                     reads=[(posm, i)], writes=[(posm, i)])
                p.op("dve", lambda: nc.vector.tensor_scalar(out=ltc[:, :], in0=pos[:, :], scalar1=float(CAP), scalar2=None, op0=ALU.is_lt),
                     reads=[pos], writes=[ltc])
                p.op("dve", lambda: nc.vector.tensor_tensor(out=ltc[:, :], in0=ltc[:, :], in1=mask[:, :], op=ALU.mult), reads=[ltc, mask], writes=[ltc])
                p.op("dve", lambda: nc.vector.tensor_tensor(out=vv[:, :], in0=pos[:, :], in1=rowbase[:, :], op=ALU.add), reads=[pos, rowbase], writes=[vv])
                p.op("dve", lambda: nc.vector.tensor_tensor(out=vv[:, :], in0=vv[:, :], in1=ltc[:, :], op=ALU.mult), reads=[vv, ltc], writes=[vv])
                p.op("dve", lambda: nc.vector.max(out=r8[:, :], in_=vv[:, :]), reads=[vv], writes=[r8])
                p.op("dve", lambda: nc.vector.tensor_copy(out=idx[:, i, :], in_=r8[:, 0:4]), reads=[r8], writes=[(idx, i)])
                for k in range(4):
                    p.op("dve", lambda: nc.vector.tensor_scalar(out=eqk[:, :], in0=vv[:, :], scalar1=r8[:, k:k + 1], scalar2=None, op0=ALU.is_equal),
                         reads=[vv, r8], writes=[eqk])
                    p.op("dve", lambda: nc.vector.tensor_tensor(out=eqk[:, :], in0=eqk[:, :], in1=gate[:, :], op=ALU.mult), reads=[eqk, gate], writes=[eqk])
                    p.op("dve", lambda: nc.vector.reduce_sum(out=gk[:, i, k:k + 1], in_=eqk[:, :], axis=AX.X), reads=[eqk], writes=[(gk, i, k)])
            p.dma("sp", idx_out, idx[:, :, :].rearrange("p a b -> p (a b)"), idx, reads=[(idx, i) for i in range(NTT)], writes=["idx_out"])
            p.dma("sp", gk_out, gk[:, :, :].rearrange("p a b -> p (a b)"), gk, reads=[(gk, i, k) for i in range(NTT) for k in range(4)], writes=["gk_out"])
            p.barrier()
        with ExitStack() as es3:
            O = [p.sb(f"O{i}", [128, NTT, CAP], BF16, es3) for i in range(2)]
            xeT = [p.sb(f"xeT{i}", [128, 16, CAP], BF16, es3) for i in range(2)]
            pd = [p.ps(f"pd{i}", [128, 512], F32, es3) for i in range(4)]
            for e in range(NE):
                Oe = O[e % 2]
                xe = xeT[e % 2]
                r, el = e // NLOC, e % NLOC
                for i in range(NTT):
                    eng = "dve"
                    eo = nc.vector
                    p.op(eng, lambda: eo.tensor_scalar(out=Oe[:, i, :], in0=iotac[:, :], scalar1=posm[:, i, e:e + 1],
                                                       scalar2=None, op0=ALU.is_equal),
                         reads=[iotac, (posm, i)], writes=[(Oe, i)])
                for c in range(16):
                    pdc = pd[c % 4]
                    for i in range(NTT):
                        p.op("pe", lambda: nc.tensor.matmul(pdc[:, 0:CAP], lhsT=xbf[:, i, c * 128:(c + 1) * 128], rhs=Oe[:, i, :],
                                                            start=(i == 0), stop=(i == NTT - 1)),
                             reads=[(xbf, i), (Oe, i)], writes=[pdc])
                    if c % 2 == 0:
                        p.op("act", lambda: nc.scalar.copy(out=xe[:, c, :], in_=pdc[:, 0:CAP]), reads=[pdc], writes=[(xe, c)])
                    else:
                        p.op("dve", lambda: nc.vector.tensor_copy(out=xe[:, c, :], in_=pdc[:, 0:CAP]), reads=[pdc], writes=[(xe, c)])
                dst = XS[r, el]
                p.dma("sp", dst.rearrange("(c q) s -> q c s", q=128), xe[:, :, :], xe,
                      reads=[(xe, c) for c in range(16)], writes=[("XS", r, el)])
                if el == NLOC - 1:
                    p.collective("AllGather", G8, XS[r].rearrange("e d s -> (e d) s").opt(),
                                 XR[r].rearrange("h e d s -> (h e d) s").opt(), reads=[("XS", r, q_) for q_ in range(NLOC)], writes=[("XR", r)])
            p.barrier()


def emit_experts2(p, XR, tabX, YS, YR, W):
    nc = p.nc
    HS = XCAP // 2
    NCB = HS // 512
    NSBH = HS // 128
    XRrows = [XR[r].rearrange("h e d s -> (h e d) s") for r in range(8)]
    with ExitStack() as es:
        NWB = 4
        wbuf = [p.sb(f"wb{i}", [128, 16, 512], BF16, es) for i in range(NWB)]
        xeT = p.sb("xeT", [128, 16, HS], BF16, es)
        actT = p.sb("actT", [128, 16, HS], BF16, es)
        tab = p.sb("tabX", [128, 8 * NLOC * 16], I32, es)
        bgu = [p.sb(f"bgu{i}", [128, 32], F32, es) for i in range(2)]
        bdn = [p.sb(f"bdn{i}", [1, D], F32, es) for i in range(2)]
        gt = [p.sb(f"gt{i}", [128, 512], F32, es) for i in range(2)]
        sg = [p.sb(f"sg{i}", [128, 512], F32, es) for i in range(2)]
        u2 = [p.sb(f"u2{i}", [128, 512], F32, es) for i in range(2)]
        ysb = [p.sb(f"ysb{i}", [128, 512], BF16, es) for i in range(4)]
        ones_f = p.sb("ones_f", [1, 128], F32, es)
        pg = [p.ps(f"pg{i}", [128, 512], F32, es) for i in range(2)]
        pu = [p.ps(f"pu{i}", [128, 512], F32, es) for i in range(2)]
        pdn = [p.ps(f"pdn{i}", [128, 512], F32, es) for i in range(3)]
        p.op("dve", lambda: nc.vector.memset(ones_f[:, :], 1.0), writes=[ones_f])
        b7 = p.sb("b7", [128, 1], F32, es)
        p.op("dve", lambda: nc.vector.memset(b7[:, :], 7.0), writes=[b7])
        p.dma("sp", tab[:, :], tabX, tab, writes=[tab])
        cnt = {"w": 0, "y": 0, "g": 0, "d": 0}

        def load_w(src_ap):
            wb = wbuf[cnt["w"] % NWB]
            cnt["w"] += 1
            p.dma("pool", wb[:, :, :], src_ap, wb, writes=[wb])
            return wb

        for e in range(NLOC):
            bg = bgu[e % 2]
            bd = bdn[e % 2]
            p.dma("sp", bg[:, :], W["b_gu"][e], bg, writes=[bg])
            p.dma("sp", bd[:, :], W["b_dn"][e:e + 1, :], bd, writes=[bd])
            for h in range(2):
                for rr in range(4):
                    r = h * 4 + rr
                    for c in range(16):
                        col = (r * NLOC + e) * 16 + c
                        p.dma("pool", None, None, xeT, reads=[tab, ("XR", r)], writes=[xeT],
                              fn=lambda: nc.gpsimd.indirect_dma_start(out=xeT[:, c, rr * CAP:(rr + 1) * CAP], out_offset=None, in_=XRrows[r],
                                                                      in_offset=bass.IndirectOffsetOnAxis(ap=tab[:, col:col + 1], axis=0)))
                for j in range(4):
                    wg = load_w(W["w_gu"][e, :, j * 512:(j + 1) * 512].rearrange("(c q) f -> q c f", q=128))
                    wu = load_w(W["w_gu"][e, :, D + j * 512:D + (j + 1) * 512].rearrange("(c q) f -> q c f", q=128))
                    for f in range(4):
                        fc = j * 4 + f
                        for cb in range(NCB):
                            k = cnt["g"] % 2
                            cnt["g"] += 1
                            sl = slice(cb * 512, (cb + 1) * 512)
                            for c in range(16):
                                p.op("pe", lambda: nc.tensor.matmul(pg[k][:, :], lhsT=wg[:, c, f * 128:(f + 1) * 128], rhs=xeT[:, c, sl],
                                                                    start=(c == 0), stop=(c == 15)), reads=[wg, xeT], writes=[pg[k]])
                            for c in range(16):
                                p.op("pe", lambda: nc.tensor.matmul(pu[k][:, :], lhsT=wu[:, c, f * 128:(f + 1) * 128], rhs=xeT[:, c, sl],
                                                                    start=(c == 0), stop=(c == 15)), reads=[wu, xeT], writes=[pu[k]])
                            p.op("dve", lambda: nc.vector.tensor_scalar(out=gt[k][:, :], in0=pg[k][:, :], scalar1=bg[:, fc:fc + 1], scalar2=7.0,
                                                                        op0=ALU.add, op1=ALU.min), reads=[pg[k], bg], writes=[gt[k]])
                            p.op("act", lambda: nc.scalar.activation(out=sg[k][:, :], in_=gt[k][:, :], func=AF.Sigmoid, scale=1.702),
                                 reads=[gt[k]], writes=[sg[k]])
                            p.op("dve", lambda: nc.vector.tensor_scalar(out=u2[k][:, :], in0=pu[k][:, :], scalar1=bg[:, 16 + fc:17 + fc], scalar2=7.0,
                                                                        op0=ALU.add, op1=ALU.min), reads=[pu[k], bg], writes=[u2[k]])
                            p.op("act", lambda: nc.scalar.activation(out=u2[k][:, :], in_=u2[k][:, :], func=AF.Relu, bias=b7[:, 0:1]),
                                 reads=[u2[k], b7], writes=[u2[k]])
                            p.op("dve", lambda: nc.vector.scalar_tensor_tensor(out=u2[k][:, :], in0=u2[k][:, :], scalar=-6.0, in1=gt[k][:, :],
                                                                               op0=ALU.add, op1=ALU.mult), reads=[u2[k], gt[k]], writes=[u2[k]])
                            p.op("dve", lambda: nc.vector.tensor_tensor(out=actT[:, fc, sl], in0=u2[k][:, :], in1=sg[k][:, :], op=ALU.mult),
                                 reads=[u2[k], sg[k]], writes=[(actT, fc)])
                for dc in range(4):
                    wd = load_w(W["w_dn"][e, :, dc * 512:(dc + 1) * 512].rearrange("(c q) f -> q c f", q=128))
                    for sb_ in range(NSBH):
                        pdk = pdn[cnt["d"] % 3]
                        cnt["d"] += 1
                        for fc in range(16):
                            p.op("pe", lambda: nc.tensor.matmul(pdk[:, :], lhsT=actT[:, fc, sb_ * 128:(sb_ + 1) * 128], rhs=wd[:, fc, :],
                                                                start=(fc == 0), stop=False), reads=[(actT, fc), wd], writes=[pdk])
                        p.op("pe", lambda: nc.tensor.matmul(pdk[:, :], lhsT=ones_f[0:1, :], rhs=bd[0:1, dc * 512:(dc + 1) * 512],
                                                            start=False, stop=True), reads=[ones_f, bd], writes=[pdk])
                        yb = ysb[cnt["y"] % 4]
                        cnt["y"] += 1
                        if cnt["y"] % 2 == 0:
                            p.op("dve", lambda: nc.vector.tensor_copy(out=yb[:, :], in_=pdk[:, :]), reads=[pdk], writes=[yb])
                        else:
                            p.op("act", lambda: nc.scalar.copy(out=yb[:, :], in_=pdk[:, :]), reads=[pdk], writes=[yb])
                        r = h * 4 + sb_ // NSB
                        s0 = (sb_ % NSB) * 128
                        dst = YS[dc // 2][r, e, s0:s0 + 128, (dc % 2) * 512:(dc % 2 + 1) * 512]
                        p.dma("sp", dst, yb[:, :], yb, reads=[yb], writes=[("YS", r, e, s0, dc)])
        p.barrier()
        BLK = 8 * NLOC * CAP
        for r in range(8):
            for h2 in range(2):
                p.collective("AllGather", G8, YS[h2][r].rearrange("e s d -> (e s) d").opt(),
                             YR[h2][1 + r * BLK:1 + (r + 1) * BLK, :].opt(), writes=[("YR", r, h2)])
        p.barrier()


def emit_combine2(p, x_res, YR, idx_in, gk_in, ln_g, ln_b, x_out, x1T_out=None, ident_f=None):
    nc = p.nc
    with ExitStack() as es4:
        gbc = p.sb("gbc", [128, D], F32, es4)
        bbc = p.sb("bbc", [128, D], F32, es4)
        idx = p.sb("idxc", [128, NTT * 4], I32, es4)
        gk = p.sb("gkc", [128, NTT * 4], F32, es4)
        G = [[p.sb(f"G{k}_{i}", [128, D], BF16, es4) for k in range(4)] for i in range(2)]
        xr = [p.sb(f"xr{i}", [128, D], F32, es4) for i in range(2)]
        zo = [p.sb(f"zo{i}", [128, D], F32, es4) for i in range(2)]
        zT = [p.sb(f"zT{i}", [128, 16, 128], BF16, es4) for i in range(2)]
        zrow = p.sb("zrow", [1, D], BF16, es4)
        stats = p.sb("stats", [128, 24], F32, es4)
        mv = p.sb("mv", [128, 2], F32, es4)
        rstd = p.sb("rstd", [128, 1], F32, es4)
        ptz = [p.ps(f"ptz{i}", [128, 512], F32, es4) for i in range(2)]
        p.op("dve", lambda: nc.vector.memset(zrow[:, :], 0.0), writes=[zrow])
        for h2 in range(2):
            p.dma("sp", YR[h2][0:1, :], zrow[:, h2 * 1024:(h2 + 1) * 1024], zrow, reads=[zrow], writes=[("YR0", h2)])
        p.dma("sp", gbc[:, :], bcast_rows(ln_g, 128, D), gbc, writes=[gbc])
        p.dma("sp", bbc[:, :], bcast_rows(ln_b, 128, D), bbc, writes=[bbc])
        p.dma("sp", idx[:, :], idx_in, idx, writes=[idx])
        p.dma("sp", gk[:, :], gk_in, gk, writes=[gk])
        for i in range(NTT):
            Gi = G[i % 2]
            xri = xr[i % 2]
            zi = zo[i % 2]
            p.dma("sp", xri[:, :], x_res[i * 128:(i + 1) * 128, :], xri, writes=[xri])
            for k in range(4):
                for h2 in range(2):
                    p.dma("pool", None, None, Gi[k], reads=[idx, ("YR0", 0), ("YR0", 1)], writes=[(Gi[k], h2)],
                          fn=lambda: nc.gpsimd.indirect_dma_start(out=Gi[k][:, h2 * 1024:(h2 + 1) * 1024], out_offset=None, in_=YR[h2][:, :],
                                                                  in_offset=bass.IndirectOffsetOnAxis(ap=idx[:, i * 4 + k:i * 4 + k + 1], axis=0)))
            p.op("dve", lambda: nc.vector.tensor_scalar(out=xri[:, :], in0=xri[:, :], scalar1=ALPHA, scalar2=None, op0=ALU.mult), reads=[xri], writes=[xri])
            for k in range(4):
                p.op("dve", lambda: nc.vector.scalar_tensor_tensor(out=xri[:, :], in0=Gi[k][:, :], scalar=gk[:, i * 4 + k:i * 4 + k + 1], in1=xri[:, :],
                                                                   op0=ALU.mult, op1=ALU.add), reads=[(Gi[k], 0), (Gi[k], 1), gk, xri], writes=[xri])
            emit_ln(p, xri, zi, gbc, bbc, stats, mv, rstd)
            p.dma("sp", x_out[i * 128:(i + 1) * 128, :], zi[:, :], zi, reads=[zi], writes=[("xout", i)])
            if x1T_out is not None:
                zt = zT[i % 2]
                for g in range(4):
                    ptg = ptz[g % 2]
                    for j in range(4):
                        c = g * 4 + j
                        p.op("pe", lambda: nc.tensor.transpose(out=ptg[:, j * 128:(j + 1) * 128], in_=zi[:, c * 128:(c + 1) * 128], identity=ident_f[:, :]),
                             reads=[zi, ident_f], writes=[ptg])
                    p.op("act", lambda: nc.scalar.copy(out=zt[:, g * 4:(g + 1) * 4, :], in_=ptg[:, :].rearrange("p (a b) -> p a b", a=4)),
                         reads=[ptg], writes=[(zt, g)])
                p.dma("sp", x1T_out[:, i * 128:(i + 1) * 128].rearrange("(c q) t -> q c t", q=128), zt[:, :, :], zt,
                      reads=[(zt, g) for g in range(4)], writes=[("x1T", i)])
        p.barrier()


def dint(nc, name, shape, dt=F32):
    return nc.dram_tensor(name, list(shape), dt).ap()


def build_fused_prog():
    nc = new_nc()
    A0 = {"xT_all": din(nc, "xT_all", [D, S]), "xT_own": din(nc, "xT_own", [D, NT]),
          "pos_all": din(nc, "pos_all", [S], I32), "pos_own": din(nc, "pos_own", [NT], I32),
          "qlimrel": din(nc, "qlimrel", [128, NQB]), "invf": din(nc, "invf", [128, 1]), "sgn": din(nc, "sgn", [128, 1]),
          "wK": din(nc, "wK", [D, 512]), "wKIV": din(nc, "wKIV", [D, 512]),
          "wQ": din(nc, "wQ", [4, D, 512]), "wQI": din(nc, "wQI", [2, D, 512]), "wWI": din(nc, "wWI", [D, 4])}
    AT = dint(nc, "AT", [8, 128, NT], BF16)
    A0["aT_out"] = AT
    XA = dint(nc, "XA", [NT, D])
    A1 = {"xT_oh": din(nc, "xT_oh", [D, NQB * SEG]), "wU": din(nc, "wU", [2, D, 512]), "invcnt": din(nc, "invcnt", [4, NT]),
          "w_pool": din(nc, "w_pool", [4, 256, 256]), "pool_scale": din(nc, "pool_scale", [128, 8]),
          "aT": AT, "w_out": din(nc, "w_out0", [D, D]), "x_own": din(nc, "x_own", [NT, D]),
          "ln_g": din(nc, "ln_mix_g0", [D]), "ln_b": din(nc, "ln_mix_b0", [D]), "xa_out": XA}
    WM = []
    for l in range(2):
        WM.append({"router_w": din(nc, f"router_w{l}", [128, 16, NE]), "router_b": din(nc, f"router_b{l}", [NE]),
                   "rowbase": din(nc, f"rowbase{l}", [NE]),
                   "w_gu": din(nc, f"w_gu{l}", [NLOC, D, 2 * D]), "b_gu": din(nc, f"b_gu{l}", [NLOC, 128, 32]),
                   "w_dn": din(nc, f"w_dn{l}", [NLOC, D, D]), "b_dn": din(nc, f"b_dn{l}", [NLOC, D]),
                   "ln_g": din(nc, f"ln_moe_g{l}", [D]), "ln_b": din(nc, f"ln_moe_b{l}", [D])})
    tabX = din(nc, "tabX", [128, 8 * NLOC * 16], I32)
    tabH = din(nc, "tabH", [128, 16], I32)
    AL = {"wQK": din(nc, "wQK", [2, D, 512]), "wVOG": din(nc, "wVOG", [D, 2056]),
          "conv_w": din(nc, "conv_w", [128, 8, 4]), "conv_b": din(nc, "conv_b", [128, 8]), "gate_b": din(nc, "gate_b", [8]),
          "c_norm_g": din(nc, "c_norm_g", [1024])}
    w_out1 = din(nc, "w_out1", [D, D])
    ln_mix_g1 = din(nc, "ln_mix_g1", [D])
    ln_mix_b1 = din(nc, "ln_mix_b1", [D])
    OUT = dout(nc, "out", [NT, D])
    XS = dint(nc, "XS", [8, NLOC, D, CAP], BF16)
    XR = [dint(nc, f"XR{r}", [8, NLOC, D, CAP], BF16) for r in range(8)]
    YS = [dint(nc, f"YS{h2}", [8, NLOC, CAP, 1024], BF16) for h2 in range(2)]
    YR = [dint(nc, f"YR{h2}", [1 + NXR * CAP, 1024], BF16) for h2 in range(2)]
    IDX = dint(nc, "IDX", [128, NTT * 4], I32)
    GK = dint(nc, "GK", [128, NTT * 4])
    X1 = dint(nc, "X1", [NT, D])
    X1S = dint(nc, "X1S", [D, NT], BF16)
    X1R = dint(nc, "X1R", [2, D, NT], BF16)
    HS = dint(nc, "HS", [2, 1024, NT], BF16)
    HR = dint(nc, "HR", [2, 2, 1024, NT], BF16)
    XB = dint(nc, "XB", [NT, D])
    with ExitStack() as es:
        p = P(nc, es)
        ident = make_ident(p, es)
        emit_l0_attn(p, A0)
        emit_l0_out(p, A1)
        emit_dispatch2(p, XA, XS, XR, IDX, GK, WM[0], ident)
        emit_experts2(p, XR, tabX, YS, YR, WM[0])
        emit_combine2(p, XA, YR, IDX, GK, WM[0]["ln_g"], WM[0]["ln_b"], X1, x1T_out=X1S, ident_f=ident)
        p.collective("AllGather", pair_groups(1), X1S.opt(), X1R.rearrange("r d t -> (r d) t").opt())
        p.barrier()
        AL["x1R"] = X1R
        AL["HS"] = HS
        emit_l1_mlstm(p, AL)
        p.collective("AllGather", pair_groups(1), HS.rearrange("h f t -> (h f) t").opt(), HR.rearrange("r h f t -> (r h f) t").opt())
        p.barrier()
        emit_l1_out(p, {"HR": HR, "tabH": tabH, "w_out": w_out1, "x_own": X1, "ln_g": ln_mix_g1, "ln_b": ln_mix_b1, "xb_out": XB})
        emit_dispatch2(p, XB, XS, XR, IDX, GK, WM[1], ident)
        emit_experts2(p, XR, tabX, YS, YR, WM[1])
        emit_combine2(p, XB, YR, IDX, GK, WM[1]["ln_g"], WM[1]["ln_b"], OUT)
        p.finish()
    return nc


def fused_host(inputs):
    m0 = l0_attn_host(inputs)
    m1 = l0_out_host(inputs, [None] * 8)
    ml = l1_mlstm_host(inputs, None)
    maps = []
    for c in range(8):
        h = c % 2
        m = dict(m0[c])
        for k, v in m1[c].items():
            if k == "aT":
                continue
            m[{"w_out": "w_out0", "ln_g": "ln_mix_g0", "ln_b": "ln_mix_b0"}.get(k, k)] = v
        sel = [0] + [1 if (c ^ r) > c else 0 for r in range(1, 8)]
        perm = np.array([NLOC * (c ^ r) + e for r in range(8) for e in range(NLOC)])
        rowbase = np.array([1 + ((r * 8 + (c ^ r)) * NLOC + e) * CAP for r in range(8) for e in range(NLOC)], np.float32)
        es_ = slice(c * NLOC, (c + 1) * NLOC)
        for l in range(2):
            rw = inputs["router_w"][l][:, perm]
            m[f"router_w{l}"] = np.ascontiguousarray(rw.reshape(16, 128, NE).transpose(1, 0, 2))
            m[f"router_b{l}"] = np.ascontiguousarray(inputs["router_b"][l][perm])
            m[f"rowbase{l}"] = rowbase
            m[f"w_gu{l}"] = np.ascontiguousarray(inputs["w_gu"][l][es_])
            m[f"b_gu{l}"] = np.ascontiguousarray(inputs["b_gu"][l].reshape(NE, 32, 128).transpose(0, 2, 1)[es_])
            m[f"w_dn{l}"] = np.ascontiguousarray(inputs["w_dn"][l][es_])
            m[f"b_dn{l}"] = np.ascontiguousarray(inputs["b_dn"][l][es_])
            m[f"ln_moe_g{l}"] = np.ascontiguousarray(inputs["ln_moe_g"][l])
            m[f"ln_moe_b{l}"] = np.ascontiguousarray(inputs["ln_moe_b"][l])
        tabX = np.empty((128, 8 * NLOC * 16), np.int32)
        pidx = np.arange(128)
        for r in range(8):
            for e in range(NLOC):
                for cc in range(16):
                    tabX[:, (r * NLOC + e) * 16 + cc] = ((c ^ r) * NLOC + e) * D + cc * 128 + pidx
        m["tabX"] = tabX
        tabH = np.empty((128, 16), np.int32)
        for fc in range(16):
            tabH[:, fc] = ((fc // 8) * 2 + h) * 1024 + (fc % 8) * 128 + pidx
        m["tabH"] = tabH
        for k in ("wQK", "wVOG", "conv_w", "conv_b", "gate_b", "c_norm_g"):
            m[k] = ml[c][k]
        m["w_out1"] = np.ascontiguousarray(inputs["w_out1"][0])
        m["ln_mix_g1"] = np.ascontiguousarray(inputs["ln_mix_g"][1])
        m["ln_mix_b1"] = np.ascontiguousarray(inputs["ln_mix_b"][1])
        maps.append(m)
    return maps


def kernel_fused_unsupported(**inputs):
    inputs = {k: np.asarray(v) for k, v in inputs.items()}
    r = run_bass_kernel_spmd(build_fused_prog(), fused_host(inputs), core_ids=ALL8).results
    out = np.empty((4, S, D), np.float32)
    for c in range(8):
        out[c // 2][own_tokens(c % 2)] = np.asarray(r[c]["out"])
    return out


def build_moe2_test_prog(with_x1t=False):
    nc = new_nc()
    XA = din(nc, "x_in", [NT, D])
    l = 0
    WMl = {"router_w": din(nc, f"router_w{l}", [128, 16, NE]), "router_b": din(nc, f"router_b{l}", [NE]),
           "rowbase": din(nc, f"rowbase{l}", [NE]),
           "w_gu": din(nc, f"w_gu{l}", [NLOC, D, 2 * D]), "b_gu": din(nc, f"b_gu{l}", [NLOC, 128, 32]),
           "w_dn": din(nc, f"w_dn{l}", [NLOC, D, D]), "b_dn": din(nc, f"b_dn{l}", [NLOC, D]),
           "ln_g": din(nc, f"ln_moe_g{l}", [D]), "ln_b": din(nc, f"ln_moe_b{l}", [D])}
    tabX = din(nc, "tabX", [128, 8 * NLOC * 16], I32)
    OUT = dout(nc, "out", [NT, D])
    XS = dint(nc, "XS", [8, NLOC, D, CAP], BF16)
    XR = [dint(nc, f"XR{r}", [8, NLOC, D, CAP], BF16) for r in range(8)]
    YS = [dint(nc, f"YS{h2}", [8, NLOC, CAP, 1024], BF16) for h2 in range(2)]
    YR = [dint(nc, f"YR{h2}", [1 + NXR * CAP, 1024], BF16) for h2 in range(2)]
    IDX = dint(nc, "IDX", [128, NTT * 4], I32)
    GK = dint(nc, "GK", [128, NTT * 4])
    with ExitStack() as es:
        p = P(nc, es)
        ident = make_ident(p, es)
        emit_dispatch2(p, XA, XS, XR, IDX, GK, WMl, ident)
        emit_experts2(p, XR, tabX, YS, YR, WMl)
        emit_combine2(p, XA, YR, IDX, GK, WMl["ln_g"], WMl["ln_b"], OUT)
        p.finish()
    return nc


def build_seg1_prog():
    nc = new_nc()
    A0 = {"xT_all": din(nc, "xT_all", [D, S]), "xT_own": din(nc, "xT_own", [D, NT]),
          "pos_all": din(nc, "pos_all", [S], I32), "pos_own": din(nc, "pos_own", [NT], I32),
          "qlimrel": din(nc, "qlimrel", [128, NQB]), "invf": din(nc, "invf", [128, 1]), "sgn": din(nc, "sgn", [128, 1]),
          "wK": din(nc, "wK", [D, 512]), "wKIV": din(nc, "wKIV", [D, 512]),
          "wQ": din(nc, "wQ", [4, D, 512]), "wQI": din(nc, "wQI", [2, D, 512]), "wWI": din(nc, "wWI", [D, 4])}
    AT = dint(nc, "AT", [8, 128, NT], BF16)
    A0["aT_out"] = AT
    XA2 = dint(nc, "XA2", [NT, D])
    A1 = {"xT_oh": din(nc, "xT_oh", [D, NQB * SEG]), "wU": din(nc, "wU", [2, D, 512]), "invcnt": din(nc, "invcnt", [4, NT]),
          "w_pool": din(nc, "w_pool", [4, 256, 256]), "pool_scale": din(nc, "pool_scale", [128, 8]),
          "aT": AT, "w_out": din(nc, "w_out", [D, D]), "x_own": din(nc, "x_own", [NT, D]),
          "ln_g": din(nc, "ln_g", [D]), "ln_b": din(nc, "ln_b", [D]), "xa_out": dout(nc, "xa_out", [NT, D]), "xa_out2": XA2}
    W = {"router_w": din(nc, "router_w", [128, 16, NE]), "router_b": din(nc, "router_b", [NE])}
    xeT_out = dout(nc, "xeT_out", [NE, D, CAP], BF16)
    gs_out = dout(nc, "gs_out", [NE, CAP])
    idx_out = dout(nc, "idx_out", [128, NTT * 4], I32)
    with ExitStack() as es:
        p = P(nc, es)
        ident = make_ident(p, es)
        emit_l0_attn(p, A0)
        emit_l0_out(p, A1)
        emit_dispatch(p, XA2, xeT_out, gs_out, idx_out, W, ident)
        p.finish()
    return nc


def build_seg5_prog():
    nc = new_nc()
    XB2 = dint(nc, "XB2", [NT, D])
    A = {"hgT": din(nc, "hgT", [D, NT], BF16), "w_out": din(nc, "w_out", [D, D]), "x_own": din(nc, "x_own", [NT, D]),
         "ln_g": din(nc, "ln_g", [D]), "ln_b": din(nc, "ln_b", [D]), "xb_out": dout(nc, "xb_out", [NT, D]), "xb_out2": XB2}
    W = {"router_w": din(nc, "router_w", [128, 16, NE]), "router_b": din(nc, "router_b", [NE])}
    xeT_out = dout(nc, "xeT_out", [NE, D, CAP], BF16)
    gs_out = dout(nc, "gs_out", [NE, CAP])
    idx_out = dout(nc, "idx_out", [128, NTT * 4], I32)
    with ExitStack() as es:
        p = P(nc, es)
        ident = make_ident(p, es)
        emit_l1_out(p, A)
        emit_dispatch(p, XB2, xeT_out, gs_out, idx_out, W, ident)
        p.finish()
    return nc


def moe_tail(r1, x_res, inputs, l):
    maps2 = []
    bgu = inputs["b_gu"][l].reshape(NE, 32, 128).transpose(0, 2, 1)
    for c in range(8):
        es_ = slice(c * NLOC, (c + 1) * NLOC)
        xe = np.concatenate([np.asarray(r1[s]["xeT_out"])[es_] for s in range(8)], axis=2)
        gs = np.concatenate([np.asarray(r1[s]["gs_out"])[es_] for s in range(8)], axis=1)
        gs = np.ascontiguousarray(gs.reshape(NLOC, XCAP // 128, 128).transpose(0, 2, 1))
        maps2.append({"xeT_in": np.ascontiguousarray(xe), "gs_in": gs,
                      "w_gu": np.ascontiguousarray(inputs["w_gu"][l][es_]), "b_gu": np.ascontiguousarray(bgu[es_]),
                      "w_dn": np.ascontiguousarray(inputs["w_dn"][l][es_]), "b_dn": np.ascontiguousarray(inputs["b_dn"][l][es_])})
    r2 = run_bass_kernel_spmd(build_experts_prog(), maps2, core_ids=ALL8).results
    yall = np.concatenate([np.asarray(r2[c]["y_out"]) for c in range(8)], axis=0)
    maps3 = []
    for c in range(8):
        yb = np.zeros((1 + NE * CAP, D), np.float32)
        yb[1:] = yall[:, c * CAP:(c + 1) * CAP, :].reshape(NE * CAP, D)
        maps3.append({"x_res": x_res[c], "ybuf": yb, "idx_in": np.asarray(r1[c]["idx_out"]),
                      "ln_g": np.ascontiguousarray(inputs["ln_moe_g"][l]), "ln_b": np.ascontiguousarray(inputs["ln_moe_b"][l])})
    r3 = run_bass_kernel_spmd(build_combine_prog(), maps3, core_ids=ALL8).results
    return [np.asarray(r3[c]["x_out"]) for c in range(8)]


def kernel(**inputs):
    inputs = {k: np.asarray(v) for k, v in inputs.items()}
    m0 = l0_attn_host(inputs)
    m1 = l0_out_host(inputs, [None] * 8)
    maps = []
    for c in range(8):
        m = dict(m0[c])
        m.update({k: v for k, v in m1[c].items() if k != "aT"})
        m["router_w"] = np.ascontiguousarray(inputs["router_w"][0].reshape(16, 128, NE).transpose(1, 0, 2))
        m["router_b"] = np.ascontiguousarray(inputs["router_b"][0])
        maps.append(m)
    r1 = run_bass_kernel_spmd(build_seg1_prog(), maps, core_ids=ALL8).results
    xa = [np.asarray(r1[c]["xa_out"]) for c in range(8)]
    x1 = moe_tail(r1, xa, inputs, 0)
    x1_full = np.empty((4, S, D), np.float32)
    for c in range(8):
        x1_full[c // 2][own_tokens(c % 2)] = x1[c]
    r = run_bass_kernel_spmd(build_l1_mlstm_prog(), l1_mlstm_host(inputs, x1_full), core_ids=ALL8).results
    hg = [np.asarray(r[c]["hgT_out"]) for c in range(8)]
    maps = []
    for c in range(8):
        b, h = c // 2, c % 2
        hgT = np.concatenate([hg[2 * b], hg[2 * b + 1]], axis=0)[:, own_tokens(h)]
        maps.append({"hgT": np.ascontiguousarray(hgT), "w_out": np.ascontiguousarray(inputs["w_out1"][0]), "x_own": x1[c],
                     "ln_g": np.ascontiguousarray(inputs["ln_mix_g"][1]), "ln_b": np.ascontiguousarray(inputs["ln_mix_b"][1]),
                     "router_w": np.ascontiguousarray(inputs["router_w"][1].reshape(16, 128, NE).transpose(1, 0, 2)),
                     "router_b": np.ascontiguousarray(inputs["router_b"][1])})
    r5 = run_bass_kernel_spmd(build_seg5_prog(), maps, core_ids=ALL8).results
    xb = [np.asarray(r5[c]["xb_out"]) for c in range(8)]
    x2 = moe_tail(r5, xb, inputs, 1)
    out = np.empty((4, S, D), np.float32)
    for c in range(8):
        out[c // 2][own_tokens(c % 2)] = x2[c]
    return out
```

```python
import numpy as np
from contextlib import ExitStack
import concourse.bass as bass
import concourse.mybir as mybir
from concourse.bass_utils import run_bass_kernel_spmd

F32 = mybir.dt.float32
BF16 = mybir.dt.bfloat16
I32 = mybir.dt.int32
U32 = mybir.dt.uint32
AF = mybir.ActivationFunctionType
ALU = mybir.AluOpType
AX = mybir.AxisListType

D = 2048
NT = 2048
NTT = NT // 128
NE = 32
CAP = 384
NSB = CAP // 128
ALPHA = float(4 ** 0.25)
LN_EPS = 1e-5


class Buf:
    def __init__(self, name, t):
        self.name = name
        self.t = t
        self.slot = None

    def __getitem__(self, k):
        return self.t[k]


NSLOT = 64


class P:
    def __init__(self, nc, es):
        self.nc = nc
        self.es = es
        self.eng = {"pe": nc.tensor, "dve": nc.vector, "act": nc.scalar, "pool": nc.gpsimd, "sp": nc.sync}
        self.sem = {k: es.enter_context(nc.semaphore("sem_" + k)) for k in self.eng}
        self.cnt = {k: 0 for k in self.eng}
        self.seen = {k: {} for k in self.eng}
        self.st = {}
        self.nbuf = 0
        self.slots = []
        self.nassign = 0
        self.cc_slot = None

    def sb(self, name, shape, dt, es=None):
        self.nbuf += 1
        t = (es or self.es).enter_context(self.nc.sbuf_tensor(f"{name}_{self.nbuf}", list(shape), dt))
        return Buf(name, t)

    def ps(self, name, shape, dt, es=None):
        self.nbuf += 1
        t = (es or self.es).enter_context(self.nc.psum_tensor(f"{name}_{self.nbuf}", list(shape), dt))
        return Buf(name, t)

    def _new_slot(self, unit):
        sem = self.es.enter_context(self.nc.semaphore(f"ds_{len(self.slots)}"))
        self.slots.append({"sem": sem, "cnt": 0, "unit": unit})
        return len(self.slots) - 1

    def _slot(self, buf):
        if buf.slot is None:
            if len(self.slots) < NSLOT:
                buf.slot = self._new_slot(16)
            else:
                cands = [i for i, s_ in enumerate(self.slots) if s_["unit"] == 16]
                buf.slot = cands[self.nassign % len(cands)]
            self.nassign += 1
        return buf.slot

    def _state(self, k):
        s = self.st.get(k)
        if s is None:
            s = {"w": {}, "r": {}}
            self.st[k] = s
        return s

    def _wait_tok(self, e, src, val):
        if isinstance(src, tuple):
            sl = self.slots[src[1]]
            val = sl["unit"] * sl["cnt"]
            sem = sl["sem"]
            key = src
        else:
            if src == e and e == "pe":
                return
            sem = self.sem[src]
            key = src
        if val <= 0:
            return
        if self.seen[e].get(key, 0) >= val:
            return
        self.eng[e].wait_ge(sem, val)
        self.seen[e][key] = val

    def _deps(self, e, reads, writes):
        for k in reads:
            s = self._state(k)
            for src, val in list(s["w"].items()):
                self._wait_tok(e, src, val)
        for k in writes:
            s = self._state(k)
            for src, val in list(s["w"].items()):
                self._wait_tok(e, src, val)
            for src, val in list(s["r"].items()):
                self._wait_tok(e, src, val)

    def _record(self, src, val, reads, writes):
        for k in reads:
            s = self._state(k)
            s["r"][src] = val
        for k in writes:
            s = self._state(k)
            s["w"] = {src: val}
            s["r"] = {}

    def op(self, e, fn, reads=(), writes=()):
        self._deps(e, reads, writes)
        ins = fn()
        self.cnt[e] += 1
        ins.then_inc(self.sem[e], 1)
        self._record(e, self.cnt[e], reads, writes)
        return ins

    def dma(self, q, out, in_, sbuf, reads=(), writes=(), fn=None, **kw):
        self._deps(q, reads, writes)
        si = self._slot(sbuf)
        sl = self.slots[si]
        if fn is not None:
            ins = fn()
        else:
            ins = self.eng[q].dma_start(out=out, in_=in_, **kw)
        sl["cnt"] += 1
        ins.then_inc(sl["sem"], 16)
        self._record(("slot", si), 16 * sl["cnt"], reads, writes)
        return ins

    def collective(self, kind, replica_groups, in_ap, out_ap, reads=(), writes=()):
        self._deps("pool", reads, writes)
        if self.cc_slot is None:
            self.cc_slot = self._new_slot(1)
        sl = self.slots[self.cc_slot]
        ins = self.nc.gpsimd.collective_compute(kind, ALU.bypass, replica_groups=replica_groups, ins=[in_ap], outs=[out_ap])
        sl["cnt"] += 1
        ins.then_inc(sl["sem"], 1)
        self._record(("slot", self.cc_slot), sl["cnt"], reads, writes)
        return ins

    def barrier(self):
        for e in self.eng:
            for o in self.eng:
                if o != e:
                    self._wait_tok(e, o, self.cnt[o])
            for i in range(len(self.slots)):
                self._wait_tok(e, ("slot", i), 0)
        self.st = {}

    def finish(self):
        for o in self.eng:
            if o != "sp":
                self._wait_tok("sp", o, self.cnt[o])
        for i in range(len(self.slots)):
            self._wait_tok("sp", ("slot", i), 0)


def bcast_rows(ap_1d, nparts, n):
    return bass.AP(tensor=ap_1d.tensor, offset=ap_1d.offset, ap=[[0, nparts], [1, n]])


def emit_ln(p, z, out, gbc, bbc, stats, mv, rstd):
    nc = p.nc
    for c in range(4):
        p.op("dve", lambda c=c: nc.vector.bn_stats(out=stats[:, c * 6:(c + 1) * 6], in_=z[:, c * 512:(c + 1) * 512]),
             reads=[z], writes=[(stats, c)])
    p.op("dve", lambda: nc.vector.bn_aggr(out=mv[:, 0:2], in_=stats[:, 0:24]),
         reads=[(stats, c) for c in range(4)], writes=[mv])
    p.op("dve", lambda: nc.vector.tensor_scalar(out=rstd[:, 0:1], in0=mv[:, 1:2], scalar1=LN_EPS, scalar2=None, op0=ALU.add),
         reads=[mv], writes=[rstd])
    p.op("act", lambda: nc.scalar.activation(out=rstd[:, 0:1], in_=rstd[:, 0:1], func=AF.Sqrt), reads=[rstd], writes=[rstd])
    p.op("dve", lambda: nc.vector.reciprocal(out=rstd[:, 0:1], in_=rstd[:, 0:1]), reads=[rstd], writes=[rstd])
    p.op("dve", lambda: nc.vector.tensor_scalar(out=out[:, :], in0=z[:, :], scalar1=mv[:, 0:1], scalar2=rstd[:, 0:1],
                                                op0=ALU.subtract, op1=ALU.mult), reads=[z, mv, rstd], writes=[out])
    p.op("pool", lambda: nc.gpsimd.tensor_tensor(out=out[:, :], in0=out[:, :], in1=gbc[:, :], op=ALU.mult), reads=[out, gbc], writes=[out])
    p.op("dve", lambda: nc.vector.tensor_tensor(out=out[:, :], in0=out[:, :], in1=bbc[:, :], op=ALU.add), reads=[out, bbc], writes=[out])


NLOC = NE // 8
XCAP = 8 * CAP


def emit_dispatch(p, x_in, xeT_out, gs_out, idx_out, W, ident_f):
    nc = p.nc
    with ExitStack() as es:
        xbf = p.sb("xbf", [128, NTT, D], BF16, es)
        posm = p.sb("posm", [128, NTT, NE], F32, es)
        ghi = p.sb("ghi", [128, NTT, NE, 2], BF16, es)
        idx = p.sb("idx", [128, NTT, 4], I32, es)
        iotac = p.sb("iotac", [128, CAP], F32, es)
        ones_bf = p.sb("ones_bf", [128, 128], BF16, es)
        p.op("pool", lambda: nc.gpsimd.iota(iotac[:, :], pattern=[[1, CAP]], base=0, channel_multiplier=0,
                                            allow_small_or_imprecise_dtypes=True), writes=[iotac])
        p.op("dve", lambda: nc.vector.memset(ones_bf[:, :], 1.0), writes=[ones_bf])

        with ExitStack() as es2:
            wr = p.sb("wr", [128, 16, NE], F32, es2)
            rb = p.sb("rb", [128, NE], F32, es2)
            ustr = p.sb("ustr", [128, 128], BF16, es2)
            rowbase = p.sb("rowbase", [128, NE], F32, es2)
            cum = p.sb("cum", [128, NE], F32, es2)
            xa = [p.sb(f"xa{i}", [128, D], F32, es2) for i in range(2)]
            xT = [p.sb(f"xT{i}", [128, 16, 128], F32, es2) for i in range(2)]
            lg = p.sb("lg", [128, NE], F32, es2)
            m8 = p.sb("m8", [128, 8], F32, es2)
            nm = p.sb("nm", [128, 1], F32, es2)
            mask = p.sb("mask", [128, NE], F32, es2)
            maskb = p.sb("maskb", [128, NE], BF16, es2)
            ex = p.sb("ex", [128, NE], F32, es2)
            ssum = p.sb("ssum", [128, 1], F32, es2)
            gate = p.sb("gate", [128, NE], F32, es2)
            gres = p.sb("gres", [128, NE], F32, es2)
            pos = p.sb("pos", [128, NE], F32, es2)
            vv = p.sb("vv", [128, NE], F32, es2)
            ltc = p.sb("ltc", [128, NE], F32, es2)
            r8 = p.sb("r8", [128, 8], F32, es2)
            pt = [p.ps(f"pt{i}", [128, 512], F32, es2) for i in range(4)]
            plg = p.ps("plg", [128, NE], F32, es2)
            ppos = p.ps("ppos", [128, 2, NE], F32, es2)

            p.dma("sp", wr[:, :, :], W["router_w"], wr, writes=[wr])
            p.dma("sp", rb[:, :], bcast_rows(W["router_b"], 128, NE), rb, writes=[rb])
            p.op("dve", lambda: nc.vector.memset(ustr[:, :], 1.0), writes=[ustr])
            p.op("pool", lambda: nc.gpsimd.affine_select(out=ustr[:, :], in_=ustr[:, :], pattern=[[1, 128]],
                                                         compare_op=ALU.is_gt, fill=0.0, base=0, channel_multiplier=-1),
                 reads=[ustr], writes=[ustr])
            p.op("pool", lambda: nc.gpsimd.iota(rowbase[:, :], pattern=[[CAP, NE]], base=1, channel_multiplier=0,
                                                allow_small_or_imprecise_dtypes=True), writes=[rowbase])
            p.op("dve", lambda: nc.vector.memset(cum[:, :], 0.0), writes=[cum])

            for i in range(NTT):
                xai = xa[i % 2]
                xTi = xT[i % 2]
                p.dma("sp", xai[:, :], x_in[i * 128:(i + 1) * 128, :], xai, writes=[xai])
                p.op("act", lambda: nc.scalar.copy(out=xbf[:, i, :], in_=xai[:, :]), reads=[xai], writes=[(xbf, i)])
                for g in range(4):
                    ptg = pt[g]
                    for j in range(4):
                        c = g * 4 + j
                        p.op("pe", lambda: nc.tensor.transpose(out=ptg[:, j * 128:(j + 1) * 128],
                                                               in_=xai[:, c * 128:(c + 1) * 128], identity=ident_f[:, :]),
                             reads=[xai, ident_f], writes=[ptg])
                    if g % 2 == 0:
                        p.op("dve", lambda: nc.vector.tensor_copy(out=xTi[:, g * 4:(g + 1) * 4, :],
                                                                  in_=ptg[:, :].rearrange("p (a b) -> p a b", a=4)),
                             reads=[ptg], writes=[(xTi, g)])
                    else:
                        p.op("act", lambda: nc.scalar.copy(out=xTi[:, g * 4:(g + 1) * 4, :],
                                                           in_=ptg[:, :].rearrange("p (a b) -> p a b", a=4)),
                             reads=[ptg], writes=[(xTi, g)])
                for c in range(16):
                    p.op("pe", lambda: nc.tensor.matmul(plg[:, :], lhsT=xTi[:, c, :], rhs=wr[:, c, :], start=(c == 0), stop=(c == 15)),
                         reads=[(xTi, c // 4), wr], writes=[plg])
                p.op("dve", lambda: nc.vector.tensor_tensor(out=lg[:, :], in0=plg[:, :], in1=rb[:, :], op=ALU.add), reads=[plg, rb], writes=[lg])
                p.op("dve", lambda: nc.vector.max(out=m8[:, :], in_=lg[:, :]), reads=[lg], writes=[m8])
                p.op("dve", lambda: nc.vector.tensor_scalar(out=mask[:, :], in0=lg[:, :], scalar1=m8[:, 3:4], scalar2=None, op0=ALU.is_ge),
                     reads=[lg, m8], writes=[mask])
                p.op("pool", lambda: nc.gpsimd.tensor_copy(out=maskb[:, :], in_=mask[:, :]), reads=[mask], writes=[maskb])
                p.op("dve", lambda: nc.vector.tensor_scalar(out=nm[:, :], in0=m8[:, 0:1], scalar1=-1.0, scalar2=None, op0=ALU.mult),
                     reads=[m8], writes=[nm])
                p.op("act", lambda: nc.scalar.activation(out=ex[:, :], in_=lg[:, :], func=AF.Exp, bias=nm[:, 0:1], scale=1.0),
                     reads=[lg, nm], writes=[ex])
                p.op("dve", lambda: nc.vector.tensor_tensor(out=ex[:, :], in0=ex[:, :], in1=mask[:, :], op=ALU.mult), reads=[ex, mask], writes=[ex])
                p.op("dve", lambda: nc.vector.reduce_sum(out=ssum[:, :], in_=ex[:, :], axis=AX.X), reads=[ex], writes=[ssum])
                p.op("dve", lambda: nc.vector.reciprocal(out=ssum[:, :], in_=ssum[:, :]), reads=[ssum], writes=[ssum])
                p.op("dve", lambda: nc.vector.tensor_scalar(out=gate[:, :], in0=ex[:, :], scalar1=ssum[:, 0:1], scalar2=None, op0=ALU.mult),
                     reads=[ex, ssum], writes=[gate])
                p.op("dve", lambda: nc.vector.tensor_copy(out=ghi[:, i, :, 0], in_=gate[:, :]), reads=[gate], writes=[(ghi, i, 0)])
                p.op("dve", lambda: nc.vector.tensor_tensor(out=gres[:, :], in0=gate[:, :], in1=ghi[:, i, :, 0], op=ALU.subtract),
                     reads=[gate, (ghi, i, 0)], writes=[gres])
                p.op("dve", lambda: nc.vector.tensor_copy(out=ghi[:, i, :, 1], in_=gres[:, :]), reads=[gres], writes=[(ghi, i, 1)])
                p.op("pe", lambda: nc.tensor.matmul(ppos[:, 0, :], lhsT=ustr[:, :], rhs=maskb[:, :], start=True, stop=True),
                     reads=[ustr, maskb], writes=[(ppos, 0)])
                p.op("pe", lambda: nc.tensor.matmul(ppos[:, 1, :], lhsT=ones_bf[:, :], rhs=maskb[:, :], start=True, stop=True),
                     reads=[ones_bf, maskb], writes=[(ppos, 1)])
                p.op("dve", lambda: nc.vector.tensor_tensor(out=pos[:, :], in0=ppos[:, 0, :], in1=cum[:, :], op=ALU.add),
                     reads=[(ppos, 0), cum], writes=[pos])
                p.op("dve", lambda: nc.vector.tensor_tensor(out=cum[:, :], in0=ppos[:, 1, :], in1=cum[:, :], op=ALU.add),
                     reads=[(ppos, 1), cum], writes=[cum])
                p.op("dve", lambda: nc.vector.scalar_tensor_tensor(out=posm[:, i, :], in0=pos[:, :], scalar=1.0, in1=mask[:, :],
                                                                   op0=ALU.add, op1=ALU.mult), reads=[pos, mask], writes=[(posm, i)])
                p.op("dve", lambda: nc.vector.tensor_scalar(out=posm[:, i, :], in0=posm[:, i, :], scalar1=-1.0, scalar2=None, op0=ALU.add),
                     reads=[(posm, i)], writes=[(posm, i)])
                p.op("dve", lambda: nc.vector.tensor_scalar(out=ltc[:, :], in0=pos[:, :], scalar1=float(CAP), scalar2=None, op0=ALU.is_lt),
                     reads=[pos], writes=[ltc])
                p.op("dve", lambda: nc.vector.tensor_tensor(out=ltc[:, :], in0=ltc[:, :], in1=mask[:, :], op=ALU.mult), reads=[ltc, mask], writes=[ltc])
                p.op("dve", lambda: nc.vector.tensor_tensor(out=vv[:, :], in0=pos[:, :], in1=rowbase[:, :], op=ALU.add), reads=[pos, rowbase], writes=[vv])
                p.op("dve", lambda: nc.vector.tensor_tensor(out=vv[:, :], in0=vv[:, :], in1=ltc[:, :], op=ALU.mult), reads=[vv, ltc], writes=[vv])
                p.op("dve", lambda: nc.vector.max(out=r8[:, :], in_=vv[:, :]), reads=[vv], writes=[r8])
                p.op("dve", lambda: nc.vector.tensor_copy(out=idx[:, i, :], in_=r8[:, 0:4]), reads=[r8], writes=[(idx, i)])
            p.dma("sp", idx_out, idx[:, :, :].rearrange("p a b -> p (a b)"), idx, reads=[(idx, i) for i in range(NTT)], writes=["idx_out"])
            p.barrier()

        with ExitStack() as es3:
            O = [p.sb(f"O{i}", [128, NTT, CAP], BF16, es3) for i in range(2)]
            xeT = [p.sb(f"xeT{i}", [128, 16, CAP], BF16, es3) for i in range(2)]
            gsl = [p.sb(f"gsl{i}", [128, NSB], F32, es3) for i in range(2)]
            pd = [p.ps(f"pd{i}", [128, 512], F32, es3) for i in range(4)]
            pgs = [p.ps(f"pgs{i}", [128, NSB, 2], F32, es3) for i in range(2)]
            for e in range(NE):
                Oe = O[e % 2]
                xe = xeT[e % 2]
                gs = gsl[e % 2]
                pg_ = pgs[e % 2]
                for i in range(NTT):
                    eng = "dve"
                    eo = nc.vector
                    p.op(eng, lambda: eo.tensor_scalar(out=Oe[:, i, :], in0=iotac[:, :], scalar1=posm[:, i, e:e + 1],
                                                       scalar2=None, op0=ALU.is_equal),
                         reads=[iotac, (posm, i)], writes=[(Oe, i)])
                for sb_ in range(NSB):
                    for i in range(NTT):
                        p.op("pe", lambda: nc.tensor.matmul(pg_[:, sb_, :], lhsT=Oe[:, i, sb_ * 128:(sb_ + 1) * 128],
                                                            rhs=ghi[:, i, e, :], start=(i == 0), stop=(i == NTT - 1)),
                             reads=[(Oe, i), (ghi, i, 0), (ghi, i, 1)], writes=[(pg_, sb_)])
                p.op("dve", lambda: nc.vector.reduce_sum(out=gs[:, :], in_=pg_[:, :, :], axis=AX.X),
                     reads=[(pg_, s) for s in range(NSB)], writes=[gs])
                p.dma("sp", gs_out[e, :].rearrange("(b q) -> q b", q=128), gs[:, :], gs, reads=[gs], writes=[("gs_out", e)],
                      allow_slow_non_contiguous=True)
                for c in range(16):
                    pdc = pd[c % 4]
                    for i in range(NTT):
                        p.op("pe", lambda: nc.tensor.matmul(pdc[:, 0:CAP], lhsT=xbf[:, i, c * 128:(c + 1) * 128], rhs=Oe[:, i, :],
                                                            start=(i == 0), stop=(i == NTT - 1)),
                             reads=[(xbf, i), (Oe, i)], writes=[pdc])
                    if c % 2 == 0:
                        p.op("act", lambda: nc.scalar.copy(out=xe[:, c, :], in_=pdc[:, 0:CAP]), reads=[pdc], writes=[(xe, c)])
                    else:
                        p.op("dve", lambda: nc.vector.tensor_copy(out=xe[:, c, :], in_=pdc[:, 0:CAP]), reads=[pdc], writes=[(xe, c)])
                p.dma("sp", xeT_out[e, :, :].rearrange("(c q) s -> q c s", q=128), xe[:, :, :], xe,
                      reads=[(xe, c) for c in range(16)], writes=[("xeT_out", e)])
            p.barrier()


def emit_experts(p, xeT_in, gs_in, y_out, W):
    nc = p.nc
    HS = XCAP // 2
    NCB = HS // 512
    NSBH = HS // 128
    with ExitStack() as es:
        NWB = 4
        wbuf = [p.sb(f"wb{i}", [128, 16, 512], BF16, es) for i in range(NWB)]
        xeT = p.sb("xeT", [128, 16, HS], BF16, es)
        actT = p.sb("actT", [128, 16, HS], BF16, es)
        bgu = [p.sb(f"bgu{i}", [128, 32], F32, es) for i in range(2)]
        bdn = [p.sb(f"bdn{i}", [1, D], F32, es) for i in range(2)]
        gsl = [p.sb(f"gsl{i}", [128, XCAP // 128], F32, es) for i in range(2)]
        gt = [p.sb(f"gt{i}", [128, 512], F32, es) for i in range(2)]
        sg = [p.sb(f"sg{i}", [128, 512], F32, es) for i in range(2)]
        u2 = [p.sb(f"u2{i}", [128, 512], F32, es) for i in range(2)]
        ysb = [p.sb(f"ysb{i}", [128, 512], F32, es) for i in range(4)]
        ones_f = p.sb("ones_f", [1, 128], F32, es)
        pg = [p.ps(f"pg{i}", [128, 512], F32, es) for i in range(2)]
        pu = [p.ps(f"pu{i}", [128, 512], F32, es) for i in range(2)]
        pdn = [p.ps(f"pdn{i}", [128, 512], F32, es) for i in range(3)]
        p.op("dve", lambda: nc.vector.memset(ones_f[:, :], 1.0), writes=[ones_f])
        b7 = p.sb("b7", [128, 1], F32, es)
        p.op("dve", lambda: nc.vector.memset(b7[:, :], 7.0), writes=[b7])
        cnt = {"w": 0, "y": 0, "g": 0, "d": 0}

        def load_w(src_ap):
            wb = wbuf[cnt["w"] % NWB]
            cnt["w"] += 1
            p.dma("pool", wb[:, :, :], src_ap, wb, writes=[wb])
            return wb

        for e in range(NLOC):
            bg = bgu[e % 2]
            bd = bdn[e % 2]
            gs = gsl[e % 2]
            p.dma("sp", bg[:, :], W["b_gu"][e], bg, writes=[bg])
            p.dma("sp", bd[:, :], W["b_dn"][e:e + 1, :], bd, writes=[bd])
            p.dma("sp", gs[:, :], gs_in[e], gs, writes=[gs])
            for h in range(2):
                p.dma("sp", xeT[:, :, :], xeT_in[e, :, h * HS:(h + 1) * HS].rearrange("(c q) s -> q c s", q=128), xeT, writes=[xeT])
                for j in range(4):
                    wg = load_w(W["w_gu"][e, :, j * 512:(j + 1) * 512].rearrange("(c q) f -> q c f", q=128))
                    wu = load_w(W["w_gu"][e, :, D + j * 512:D + (j + 1) * 512].rearrange("(c q) f -> q c f", q=128))
                    for f in range(4):
                        fc = j * 4 + f
                        for cb in range(NCB):
                            k = cnt["g"] % 2
                            cnt["g"] += 1
                            sl = slice(cb * 512, (cb + 1) * 512)
                            for c in range(16):
                                p.op("pe", lambda: nc.tensor.matmul(pg[k][:, :], lhsT=wg[:, c, f * 128:(f + 1) * 128], rhs=xeT[:, c, sl],
                                                                    start=(c == 0), stop=(c == 15)), reads=[wg, xeT], writes=[pg[k]])
                            for c in range(16):
                                p.op("pe", lambda: nc.tensor.matmul(pu[k][:, :], lhsT=wu[:, c, f * 128:(f + 1) * 128], rhs=xeT[:, c, sl],
                                                                    start=(c == 0), stop=(c == 15)), reads=[wu, xeT], writes=[pu[k]])
                            p.op("dve", lambda: nc.vector.tensor_scalar(out=gt[k][:, :], in0=pg[k][:, :], scalar1=bg[:, fc:fc + 1], scalar2=7.0,
                                                                        op0=ALU.add, op1=ALU.min), reads=[pg[k], bg], writes=[gt[k]])
                            p.op("act", lambda: nc.scalar.activation(out=sg[k][:, :], in_=gt[k][:, :], func=AF.Sigmoid, scale=1.702),
                                 reads=[gt[k]], writes=[sg[k]])
                            p.op("dve", lambda: nc.vector.tensor_scalar(out=u2[k][:, :], in0=pu[k][:, :], scalar1=bg[:, 16 + fc:17 + fc], scalar2=7.0,
                                                                        op0=ALU.add, op1=ALU.min), reads=[pu[k], bg], writes=[u2[k]])
                            p.op("act", lambda: nc.scalar.activation(out=u2[k][:, :], in_=u2[k][:, :], func=AF.Relu, bias=b7[:, 0:1]),
                                 reads=[u2[k], b7], writes=[u2[k]])
                            p.op("dve", lambda: nc.vector.scalar_tensor_tensor(out=u2[k][:, :], in0=u2[k][:, :], scalar=-6.0, in1=gt[k][:, :],
                                                                               op0=ALU.add, op1=ALU.mult), reads=[u2[k], gt[k]], writes=[u2[k]])
                            p.op("dve", lambda: nc.vector.tensor_tensor(out=actT[:, fc, sl], in0=u2[k][:, :], in1=sg[k][:, :], op=ALU.mult),
                                 reads=[u2[k], sg[k]], writes=[(actT, fc)])
                for dc in range(4):
                    wd = load_w(W["w_dn"][e, :, dc * 512:(dc + 1) * 512].rearrange("(c q) f -> q c f", q=128))
                    for sb_ in range(NSBH):
                        pdk = pdn[cnt["d"] % 3]
                        cnt["d"] += 1
                        for fc in range(16):
                            p.op("pe", lambda: nc.tensor.matmul(pdk[:, :], lhsT=actT[:, fc, sb_ * 128:(sb_ + 1) * 128], rhs=wd[:, fc, :],
                                                                start=(fc == 0), stop=False), reads=[(actT, fc), wd], writes=[pdk])
                        p.op("pe", lambda: nc.tensor.matmul(pdk[:, :], lhsT=ones_f[0:1, :], rhs=bd[0:1, dc * 512:(dc + 1) * 512],
                                                            start=False, stop=True), reads=[ones_f, bd], writes=[pdk])
                        yb = ysb[cnt["y"] % 4]
                        cnt["y"] += 1
                        gcol = h * NSBH + sb_
                        if cnt["y"] % 2 == 0:
                            p.op("dve", lambda: nc.vector.tensor_scalar(out=yb[:, :], in0=pdk[:, :], scalar1=gs[:, gcol:gcol + 1], scalar2=None,
                                                                        op0=ALU.mult), reads=[pdk, gs], writes=[yb])
                        else:
                            p.op("act", lambda: nc.scalar.activation(out=yb[:, :], in_=pdk[:, :], func=AF.Copy, scale=gs[:, gcol:gcol + 1]),
                                 reads=[pdk, gs], writes=[yb])
                        r0 = h * HS + sb_ * 128
                        p.dma("sp", y_out[e, r0:r0 + 128, dc * 512:(dc + 1) * 512], yb[:, :], yb, reads=[yb], writes=[("y_out", e, r0, dc)])
        p.barrier()


def emit_combine(p, x_res, ybuf, idx_in, ln_g, ln_b, x_out):
    nc = p.nc
    with ExitStack() as es4:
        gbc = p.sb("gbc", [128, D], F32, es4)
        bbc = p.sb("bbc", [128, D], F32, es4)
        idx = p.sb("idxc", [128, NTT * 4], I32, es4)
        G = [[p.sb(f"G{k}_{i}", [128, D], F32, es4) for k in range(4)] for i in range(2)]
        xr = [p.sb(f"xr{i}", [128, D], F32, es4) for i in range(2)]
        zo = [p.sb(f"zo{i}", [128, D], F32, es4) for i in range(2)]
        stats = p.sb("stats", [128, 24], F32, es4)
        mv = p.sb("mv", [128, 2], F32, es4)
        rstd = p.sb("rstd", [128, 1], F32, es4)
        p.dma("sp", gbc[:, :], bcast_rows(ln_g, 128, D), gbc, writes=[gbc])
        p.dma("sp", bbc[:, :], bcast_rows(ln_b, 128, D), bbc, writes=[bbc])
        p.dma("sp", idx[:, :], idx_in, idx, writes=[idx])
        for i in range(NTT):
            Gi = G[i % 2]
            xri = xr[i % 2]
            zi = zo[i % 2]
            p.dma("sp", xri[:, :], x_res[i * 128:(i + 1) * 128, :], xri, writes=[xri])
            for k in range(4):
                p.dma("pool", None, None, Gi[k], reads=[idx], writes=[Gi[k]],
                      fn=lambda: nc.gpsimd.indirect_dma_start(out=Gi[k][:, :], out_offset=None, in_=ybuf[:, :],
                                                              in_offset=bass.IndirectOffsetOnAxis(ap=idx[:, i * 4 + k:i * 4 + k + 1], axis=0)))
            p.op("dve", lambda: nc.vector.tensor_tensor(out=Gi[0][:, :], in0=Gi[0][:, :], in1=Gi[1][:, :], op=ALU.add), reads=[Gi[0], Gi[1]], writes=[Gi[0]])
            p.op("pool", lambda: nc.gpsimd.tensor_tensor(out=Gi[2][:, :], in0=Gi[2][:, :], in1=Gi[3][:, :], op=ALU.add), reads=[Gi[2], Gi[3]], writes=[Gi[2]])
            p.op("dve", lambda: nc.vector.tensor_tensor(out=Gi[0][:, :], in0=Gi[0][:, :], in1=Gi[2][:, :], op=ALU.add), reads=[Gi[0], Gi[2]], writes=[Gi[0]])
            p.op("dve", lambda: nc.vector.scalar_tensor_tensor(out=xri[:, :], in0=xri[:, :], scalar=ALPHA, in1=Gi[0][:, :], op0=ALU.mult, op1=ALU.add),
                 reads=[xri, Gi[0]], writes=[xri])
            emit_ln(p, xri, zi, gbc, bbc, stats, mv, rstd)
            p.dma("sp", x_out[i * 128:(i + 1) * 128, :], zi[:, :], zi, reads=[zi], writes=[("xout", i)])
        p.barrier()


def make_ident(p, es):
    nc = p.nc
    ident = p.sb("ident", [128, 128], F32, es)
    p.op("dve", lambda: nc.vector.memset(ident[:, :], 1.0), writes=[ident])
    p.op("pool", lambda: nc.gpsimd.affine_select(out=ident[:, :], in_=ident[:, :], pattern=[[1, 128]], compare_op=ALU.is_equal,
                                                 fill=0.0, base=0, channel_multiplier=-1), reads=[ident], writes=[ident])
    return ident


def new_nc():
    return bass.Bass("TRN2", target_bir_lowering=False)


def din(nc, name, shape, dt=F32):
    return nc.dram_tensor(name, list(shape), dt, kind="ExternalInput").ap()


def dout(nc, name, shape, dt=F32):
    return nc.dram_tensor(name, list(shape), dt, kind="ExternalOutput").ap()


def build_dispatch_prog():
    nc = new_nc()
    x_in = din(nc, "x_in", [NT, D])
    W = {"router_w": din(nc, "router_w", [128, 16, NE]), "router_b": din(nc, "router_b", [NE])}
    xeT_out = dout(nc, "xeT_out", [NE, D, CAP], BF16)
    gs_out = dout(nc, "gs_out", [NE, CAP])
    idx_out = dout(nc, "idx_out", [128, NTT * 4], I32)
    with ExitStack() as es:
        p = P(nc, es)
        ident = make_ident(p, es)
        emit_dispatch(p, x_in, xeT_out, gs_out, idx_out, W, ident)
        p.finish()
    return nc


def build_experts_prog():
    nc = new_nc()
    xeT_in = din(nc, "xeT_in", [NLOC, D, XCAP], BF16)
    gs_in = din(nc, "gs_in", [NLOC, 128, XCAP // 128])
    W = {"w_gu": din(nc, "w_gu", [NLOC, D, 2 * D]), "b_gu": din(nc, "b_gu", [NLOC, 128, 32]),
         "w_dn": din(nc, "w_dn", [NLOC, D, D]), "b_dn": din(nc, "b_dn", [NLOC, D])}
    y_out = dout(nc, "y_out", [NLOC, XCAP, D])
    with ExitStack() as es:
        p = P(nc, es)
        emit_experts(p, xeT_in, gs_in, y_out, W)
        p.finish()
    return nc


def build_combine_prog():
    nc = new_nc()
    x_res = din(nc, "x_res", [NT, D])
    ybuf = din(nc, "ybuf", [1 + NE * CAP, D])
    idx_in = din(nc, "idx_in", [128, NTT * 4], I32)
    ln_g = din(nc, "ln_g", [D])
    ln_b = din(nc, "ln_b", [D])
    x_out = dout(nc, "x_out", [NT, D])
    with ExitStack() as es:
        p = P(nc, es)
        emit_combine(p, x_res, ybuf, idx_in, ln_g, ln_b, x_out)
        p.finish()
    return nc


def run_moe_layer(xa_cores, inputs, l):
    import ml_dtypes
    rw = np.ascontiguousarray(inputs["router_w"][l].reshape(16, 128, NE).transpose(1, 0, 2))
    rb = np.ascontiguousarray(inputs["router_b"][l])
    nc1 = build_dispatch_prog()
    r1 = run_bass_kernel_spmd(nc1, [{"x_in": xa_cores[c], "router_w": rw, "router_b": rb} for c in range(8)], core_ids=list(range(8))).results
    maps2 = []
    bgu = inputs["b_gu"][l].reshape(NE, 32, 128).transpose(0, 2, 1)
    for c in range(8):
        es_ = slice(c * NLOC, (c + 1) * NLOC)
        xe = np.concatenate([np.asarray(r1[s]["xeT_out"])[es_] for s in range(8)], axis=2)
        gs = np.concatenate([np.asarray(r1[s]["gs_out"])[es_] for s in range(8)], axis=1)
        gs = np.ascontiguousarray(gs.reshape(NLOC, XCAP // 128, 128).transpose(0, 2, 1))
        maps2.append({"xeT_in": np.ascontiguousarray(xe), "gs_in": gs,
                      "w_gu": np.ascontiguousarray(inputs["w_gu"][l][es_]), "b_gu": np.ascontiguousarray(bgu[es_]),
                      "w_dn": np.ascontiguousarray(inputs["w_dn"][l][es_]), "b_dn": np.ascontiguousarray(inputs["b_dn"][l][es_])})
    nc2 = build_experts_prog()
    r2 = run_bass_kernel_spmd(nc2, maps2, core_ids=list(range(8))).results
    yall = np.concatenate([np.asarray(r2[c]["y_out"]) for c in range(8)], axis=0)
    maps3 = []
    for c in range(8):
        yb = np.zeros((1 + NE * CAP, D), np.float32)
        yb[1:] = yall[:, c * CAP:(c + 1) * CAP, :].reshape(NE * CAP, D)
        maps3.append({"x_res": xa_cores[c], "ybuf": yb, "idx_in": np.asarray(r1[c]["idx_out"]),
                      "ln_g": np.ascontiguousarray(inputs["ln_moe_g"][l]), "ln_b": np.ascontiguousarray(inputs["ln_moe_b"][l])})
    nc3 = build_combine_prog()
    r3 = run_bass_kernel_spmd(nc3, maps3, core_ids=list(range(8))).results
    return [np.asarray(r3[c]["x_out"]) for c in range(8)]


S = 4096
BIG = 1.0e30
TWO_PI = 6.283185307179586
NQB = 16


def emit_rope_tabs(p, pos_ap, t0, n, tmp, consts):
    nc = p.nc
    posi, posf, tt, ki_, kf, C, Ss, m1 = tmp
    invf, sgn = consts
    p.dma("sp", posi[:, 0:n], bcast_rows(pos_ap[t0:t0 + n], 128, n), posi, writes=[posi])
    p.op("dve", lambda: nc.vector.tensor_copy(out=posf[:, 0:n], in_=posi[:, 0:n]), reads=[posi], writes=[posf])
    for which, off, dst in (("s", 0.0, Ss), ("c", 0.25, C)):
        p.op("dve", lambda: nc.vector.tensor_scalar(out=tt[:, 0:n], in0=posf[:, 0:n], scalar1=invf[:, 0:1], scalar2=off, op0=ALU.mult, op1=ALU.add),
             reads=[posf, invf], writes=[tt])
        p.op("dve", lambda: nc.vector.tensor_copy(out=ki_[:, 0:n], in_=tt[:, 0:n]), reads=[tt], writes=[ki_])
        p.op("dve", lambda: nc.vector.tensor_copy(out=kf[:, 0:n], in_=ki_[:, 0:n]), reads=[ki_], writes=[kf])
        p.op("dve", lambda: nc.vector.tensor_tensor(out=tt[:, 0:n], in0=tt[:, 0:n], in1=kf[:, 0:n], op=ALU.subtract), reads=[tt, kf], writes=[tt])
        p.op("dve", lambda: nc.vector.tensor_scalar(out=m1[:, 0:n], in0=tt[:, 0:n], scalar1=0.5, scalar2=None, op0=ALU.is_gt), reads=[tt], writes=[m1])
        p.op("dve", lambda: nc.vector.tensor_tensor(out=tt[:, 0:n], in0=tt[:, 0:n], in1=m1[:, 0:n], op=ALU.subtract), reads=[tt, m1], writes=[tt])
        p.op("dve", lambda: nc.vector.tensor_scalar(out=m1[:, 0:n], in0=tt[:, 0:n], scalar1=-0.5, scalar2=None, op0=ALU.is_lt), reads=[tt], writes=[m1])
        p.op("dve", lambda: nc.vector.tensor_tensor(out=tt[:, 0:n], in0=tt[:, 0:n], in1=m1[:, 0:n], op=ALU.add), reads=[tt, m1], writes=[tt])
        p.op("act", lambda: nc.scalar.activation(out=dst[:, 0:n], in_=tt[:, 0:n], func=AF.Sin, scale=TWO_PI), reads=[tt], writes=[dst])
    p.op("dve", lambda: nc.vector.tensor_scalar(out=Ss[:, 0:n], in0=Ss[:, 0:n], scalar1=sgn[:, 0:1], scalar2=None, op0=ALU.mult),
         reads=[Ss, sgn], writes=[Ss])
    return C, Ss


def emit_l0_attn(p, A):
    nc = p.nc
    SCALE = 128.0 ** -0.5
    with ExitStack() as es:
        kT = p.sb("kT", [128, 2, S], BF16, es)
        kiT = p.sb("kiT", [128, S], BF16, es)
        vtok = p.sb("vtok", [128, S // 128, 256], BF16, es)
        qT = p.sb("qT", [128, NQB, 8, 128], BF16, es)
        qiT = p.sb("qiT", [128, NQB, 4, 128], BF16, es)
        wi = p.sb("wi", [128, NQB, 4], F32, es)
        identb = p.sb("identb", [128, 128], BF16, es)
        ones_bf = p.sb("ones_bf", [128, 128], BF16, es)
        iota256 = p.sb("iota256", [128, 256], F32, es)
        qlim = p.sb("qlim", [128, NQB], F32, es)
        invf = p.sb("invf", [128, 1], F32, es)
        sgn = p.sb("sgn", [128, 1], F32, es)
        p.op("dve", lambda: nc.vector.memset(identb[:, :], 1.0), writes=[identb])
        p.op("pool", lambda: nc.gpsimd.affine_select(out=identb[:, :], in_=identb[:, :], pattern=[[1, 128]], compare_op=ALU.is_equal,
                                                     fill=0.0, base=0, channel_multiplier=-1), reads=[identb], writes=[identb])
        p.op("dve", lambda: nc.vector.memset(ones_bf[:, :], 1.0), writes=[ones_bf])
        p.op("pool", lambda: nc.gpsimd.iota(iota256[:, :], pattern=[[1, 256]], base=0, channel_multiplier=0,
                                            allow_small_or_imprecise_dtypes=True), writes=[iota256])
        p.dma("sp", qlim[:, :], A["qlimrel"], qlim, writes=[qlim])
        p.dma("sp", invf[:, :], A["invf"], invf, writes=[invf])
        p.dma("sp", sgn[:, :], A["sgn"], sgn, writes=[sgn])

        with ExitStack() as es2:
            wt = p.sb("wt", [128, 16, 512], BF16, es2)
            wtwi = p.sb("wtwi", [128, 16, 4], BF16, es2)
            xb = [p.sb(f"xb{i}", [128, 16, 512], BF16, es2) for i in range(2)]
            tmp = [p.sb("posi", [128, 512], I32, es2), p.sb("posf", [128, 512], F32, es2), p.sb("tt", [128, 512], F32, es2),
                   p.sb("ki_", [128, 512], I32, es2), p.sb("kf", [128, 512], F32, es2), p.sb("C", [128, 512], F32, es2),
                   p.sb("Ss", [128, 512], F32, es2), p.sb("m1", [128, 512], F32, es2)]
            t1 = [p.sb(f"t1_{i}", [128, 512], F32, es2) for i in range(2)]
            ysw = [p.sb(f"ysw{i}", [128, 512], F32, es2) for i in range(2)]
            py = [p.ps(f"py{i}", [128, 512], F32, es2) for i in range(2)]
            psw = [p.ps(f"psw{i}", [128, 512], F32, es2) for i in range(2)]
            pv = p.ps("pv", [128, 256], F32, es2)
            pw = p.ps("pw", [128, 4], F32, es2)
            cn = {"x": 0, "r": 0}

            def load_wt(w_ap):
                p.dma("pool", wt[:, :, :], w_ap.rearrange("(c q) f -> q c f", q=128), wt, writes=[wt])

            def load_xb(xT_ap, t0):
                b = xb[cn["x"] % 2]
                cn["x"] += 1
                p.dma("pool", b[:, :, :], xT_ap[:, t0:t0 + 512].rearrange("(c q) t -> q c t", q=128), b, writes=[b])
                return b

            def proj(ps_, oc, b):
                for c in range(16):
                    p.op("pe", lambda: nc.tensor.matmul(ps_[:, :], lhsT=wt[:, c, oc * 128:(oc + 1) * 128], rhs=b[:, c, :],
                                                        start=(c == 0), stop=(c == 15)), reads=[wt, b], writes=[ps_])

            def rope(oc_y, oc_sw, b, C, Ss, out_ap, out_key, in_view=None):
                k = cn["r"] % 2
                cn["r"] += 1
                proj(py[k], oc_y, b)
                proj(psw[k], oc_sw, b)
                p.op("dve", lambda: nc.vector.tensor_tensor(out=t1[k][:, :], in0=py[k][:, :], in1=C[:, :], op=ALU.mult), reads=[py[k], C], writes=[t1[k]])
                p.op("act", lambda: nc.scalar.copy(out=ysw[k][:, :], in_=psw[k][:, :]), reads=[psw[k]], writes=[ysw[k]])
                p.op("pool", lambda: nc.gpsimd.tensor_tensor(out=ysw[k][:, :], in0=ysw[k][:, :], in1=Ss[:, :], op=ALU.mult), reads=[ysw[k], Ss], writes=[ysw[k]])
                a0 = t1[k][:, :] if in_view is None else t1[k][:, :].rearrange(in_view, a=4)
                a1 = ysw[k][:, :] if in_view is None else ysw[k][:, :].rearrange(in_view, a=4)
                p.op("pool", lambda: nc.gpsimd.tensor_tensor(out=out_ap, in0=a0, in1=a1, op=ALU.add), reads=[t1[k], ysw[k]], writes=[out_key])

            load_wt(A["wK"])
            for blk in range(S // 512):
                b = load_xb(A["xT_all"], blk * 512)
                C, Ss = emit_rope_tabs(p, A["pos_all"], blk * 512, 512, tmp, (invf, sgn))
                for g in range(2):
                    rope(g, 2 + g, b, C, Ss, kT[:, g, blk * 512:(blk + 1) * 512], (kT, g, blk))
            load_wt(A["wKIV"])
            for blk in range(S // 512):
                b = load_xb(A["xT_all"], blk * 512)
                C, Ss = emit_rope_tabs(p, A["pos_all"], blk * 512, 512, tmp, (invf, sgn))
                rope(0, 1, b, C, Ss, kiT[:, blk * 512:(blk + 1) * 512], (kiT, blk))
                for s_ in range(4):
                    for c in range(16):
                        p.op("pe", lambda: nc.tensor.matmul(pv[:, :], lhsT=b[:, c, s_ * 128:(s_ + 1) * 128], rhs=wt[:, c, 256:512],
                                                            start=(c == 0), stop=(c == 15)), reads=[wt, b], writes=[pv])
                    p.op("act", lambda: nc.scalar.copy(out=vtok[:, blk * 4 + s_, :], in_=pv[:, :]), reads=[pv], writes=[(vtok, blk * 4 + s_)])
            for pp in range(4):
                load_wt(A["wQ"][pp])
                for blk in range(NT // 512):
                    b = load_xb(A["xT_own"], blk * 512)
                    C, Ss = emit_rope_tabs(p, A["pos_own"], blk * 512, 512, tmp, (invf, sgn))
                    for hh in range(2):
                        rope(hh, 2 + hh, b, C, Ss, qT[:, blk * 4:(blk + 1) * 4, 2 * pp + hh, :], (qT, blk, 2 * pp + hh), in_view="p (a b) -> p a b")
            p.dma("pool", wtwi[:, :, :], A["wWI"].rearrange("(c q) f -> q c f", q=128), wtwi, writes=[wtwi])
            for pp in range(2):
                load_wt(A["wQI"][pp])
                for blk in range(NT // 512):
                    b = load_xb(A["xT_own"], blk * 512)
                    C, Ss = emit_rope_tabs(p, A["pos_own"], blk * 512, 512, tmp, (invf, sgn))
                    for hh in range(2):
                        rope(hh, 2 + hh, b, C, Ss, qiT[:, blk * 4:(blk + 1) * 4, 2 * pp + hh, :], (qiT, blk, 2 * pp + hh), in_view="p (a b) -> p a b")
                    if pp == 1:
                        for s_ in range(4):
                            for c in range(16):
                                p.op("pe", lambda: nc.tensor.matmul(pw[:, :], lhsT=b[:, c, s_ * 128:(s_ + 1) * 128], rhs=wtwi[:, c, :],
                                                                    start=(c == 0), stop=(c == 15)), reads=[wtwi, b], writes=[pw])
                            p.op("act", lambda: nc.scalar.copy(out=wi[:, blk * 4 + s_, :], in_=pw[:, :]), reads=[pw], writes=[(wi, blk * 4 + s_)])
            p.barrier()

        with ExitStack() as es3:
            sc = p.sb("sc", [128, S], F32, es3)
            scw = p.sb("scw", [128, S], F32, es3)
            sel = p.sb("sel", [128, S], BF16, es3)
            selT = p.sb("selT", [128, S // 128, 128], BF16, es3)
            rl = [p.sb(f"rl{i}", [128, 512], F32, es3) for i in range(2)]
            m256 = p.sb("m256", [128, 256], F32, es3)
            pen = p.sb("pen", [128, 256], F32, es3)
            m8 = p.sb("m8", [128, 8], F32, es3)
            thr = p.sb("thr", [128, 1], F32, es3)
            blo = p.sb("blo", [128, 1], F32, es3)
            bhalf = p.sb("bhalf", [128, 1], F32, es3)
            bmid = p.sb("bmid", [128, 1], F32, es3)
            bcnt = p.sb("bcnt", [128, 1], F32, es3)
            PT = [p.sb(f"PT{i}", [128, 4, 128], BF16, es3) for i in range(3)]
            rden = p.sb("rden", [128, 512], F32, es3)
            pi_ = [p.ps(f"pi{i}", [128, 512], F32, es3) for i in range(2)]
            ptr = p.ps("ptr", [128, 512], BF16, es3)
            pss = [p.ps(f"pss{i}", [128, 512], F32, es3) for i in range(2)]
            pot = p.ps("pot", [128, 512], F32, es3)
            pden = p.ps("pden", [128, 512], F32, es3)
            cn2 = {"i": 0, "s": 0, "p": 0}
            for j in range(NQB):
                NK = (2 * j + 2) * 128
                NKB = NK // 128
                ncb = (NK + 511) // 512
                for cb in range(ncb):
                    w_ = min(512, NK - cb * 512)
                    cs = slice(cb * 512, cb * 512 + w_)
                    for ih in range(4):
                        k = cn2["i"] % 2
                        cn2["i"] += 1
                        p.op("pe", lambda: nc.tensor.matmul(pi_[k][:, 0:w_], lhsT=qiT[:, j, ih, :], rhs=kiT[:, cs], start=True, stop=True),
                             reads=[qiT, kiT], writes=[pi_[k]])
                        p.op("act", lambda: nc.scalar.activation(out=rl[k][:, 0:w_], in_=pi_[k][:, 0:w_], func=AF.Relu), reads=[pi_[k]], writes=[rl[k]])
                        if ih == 0:
                            p.op("dve", lambda: nc.vector.tensor_scalar(out=sc[:, cs], in0=rl[k][:, 0:w_], scalar1=wi[:, j, 0:1], scalar2=None, op0=ALU.mult),
                                 reads=[rl[k], wi], writes=[(sc, cb)])
                        else:
                            p.op("dve", lambda: nc.vector.scalar_tensor_tensor(out=sc[:, cs], in0=rl[k][:, 0:w_], scalar=wi[:, j, ih:ih + 1], in1=sc[:, cs],
                                                                               op0=ALU.mult, op1=ALU.add), reads=[rl[k], wi, (sc, cb)], writes=[(sc, cb)])
                sck = [(sc, cb) for cb in range(ncb)]
                ls = slice(NK - 256, NK)
                p.op("dve", lambda: nc.vector.tensor_scalar(out=m256[:, :], in0=iota256[:, :], scalar1=qlim[:, j:j + 1], scalar2=None, op0=ALU.is_lt),
                     reads=[iota256, qlim], writes=[m256])
                p.op("dve", lambda: nc.vector.tensor_scalar(out=pen[:, :], in0=m256[:, :], scalar1=BIG, scalar2=-BIG, op0=ALU.mult, op1=ALU.add),
                     reads=[m256], writes=[pen])
                p.op("dve", lambda: nc.vector.tensor_tensor(out=sc[:, ls], in0=sc[:, ls], in1=m256[:, :], op=ALU.mult), reads=sck + [m256], writes=sck)
                p.op("dve", lambda: nc.vector.tensor_tensor(out=sc[:, ls], in0=sc[:, ls], in1=pen[:, :], op=ALU.add), reads=sck + [pen], writes=sck)
                if NK > 256:
                    p.op("dve", lambda: nc.vector.tensor_reduce(out=blo[:, :], in_=sc[:, 0:NK - 256], axis=AX.X, op=ALU.min), reads=sck, writes=[blo])
                    p.op("dve", lambda: nc.vector.reduce_max(out=bhalf[:, :], in_=sc[:, 0:NK], axis=AX.X), reads=sck, writes=[bhalf])
                    p.op("dve", lambda: nc.vector.tensor_tensor(out=bhalf[:, :], in0=bhalf[:, :], in1=blo[:, :], op=ALU.subtract), reads=[bhalf, blo], writes=[bhalf])
                    p.op("dve", lambda: nc.vector.tensor_scalar(out=bhalf[:, :], in0=bhalf[:, :], scalar1=0.5, scalar2=None, op0=ALU.mult), reads=[bhalf], writes=[bhalf])
                    for it in range(18):
                        p.op("dve", lambda: nc.vector.tensor_tensor(out=bmid[:, :], in0=blo[:, :], in1=bhalf[:, :], op=ALU.add), reads=[blo, bhalf], writes=[bmid])
                        p.op("dve", lambda: nc.vector.tensor_scalar(out=scw[:, 0:NK], in0=sc[:, 0:NK], scalar1=bmid[:, 0:1], scalar2=None,
                                                                    op0=ALU.is_ge, op1=ALU.add, accum_out=bcnt[:, 0:1]), reads=sck + [bmid], writes=[scw, bcnt])
                        p.op("dve", lambda: nc.vector.tensor_scalar(out=bcnt[:, :], in0=bcnt[:, :], scalar1=255.5, scalar2=bhalf[:, 0:1],
                                                                    op0=ALU.is_ge, op1=ALU.mult), reads=[bcnt, bhalf], writes=[bcnt])
                        p.op("dve", lambda: nc.vector.tensor_tensor(out=blo[:, :], in0=blo[:, :], in1=bcnt[:, :], op=ALU.add), reads=[blo, bcnt], writes=[blo])
                        p.op("dve", lambda: nc.vector.tensor_scalar(out=bhalf[:, :], in0=bhalf[:, :], scalar1=0.5, scalar2=None, op0=ALU.mult), reads=[bhalf], writes=[bhalf])
                    p.op("dve", lambda: nc.vector.tensor_scalar(out=thr[:, :], in0=blo[:, :], scalar1=-BIG / 2, scalar2=None, op0=ALU.max),
                         reads=[blo], writes=[thr])
                else:
                    p.op("dve", lambda: nc.vector.memset(thr[:, :], -BIG / 2), writes=[thr])
                p.op("dve", lambda: nc.vector.tensor_scalar(out=sel[:, 0:NK], in0=sc[:, 0:NK], scalar1=thr[:, 0:1], scalar2=None, op0=ALU.is_ge),
                     reads=sck + [thr], writes=[sel])
                for kb0 in range(0, NKB, 4):
                    nb = min(4, NKB - kb0)
                    for t_ in range(nb):
                        kb = kb0 + t_
                        p.op("pe", lambda: nc.tensor.transpose(out=ptr[:, t_ * 128:(t_ + 1) * 128], in_=sel[:, kb * 128:(kb + 1) * 128], identity=identb[:, :]),
                             reads=[sel, identb], writes=[ptr])
                    p.op("act", lambda: nc.scalar.copy(out=selT[:, kb0:kb0 + nb, :], in_=ptr[:, 0:nb * 128].rearrange("p (a b) -> p a b", b=128)),
                         reads=[ptr], writes=[(selT, kb0 // 4)])
                for g in range(2):
                    for kb in range(NKB):
                        ks_ = cn2["s"] % 2
                        cn2["s"] += 1
                        pk = cn2["p"] % 3
                        cn2["p"] += 1
                        p.op("pe", lambda: nc.tensor.matmul(pss[ks_][:, :], lhsT=kT[:, g, kb * 128:(kb + 1) * 128], rhs=qT[:, j, 4 * g:4 * g + 4, :],
                                                            start=True, stop=True), reads=[kT, (qT, j, g)], writes=[pss[ks_]])
                        p.op("act", lambda: nc.scalar.activation(out=PT[pk][:, :, :], in_=pss[ks_][:, :].rearrange("p (a b) -> p a b", a=4),
                                                                 func=AF.Exp, scale=SCALE), reads=[pss[ks_]], writes=[PT[pk]])
                        eng = "pool" if kb % 2 == 0 else "dve"
                        eo = nc.gpsimd if eng == "pool" else nc.vector
                        p.op(eng, lambda: eo.tensor_tensor(out=PT[pk][:, :, :], in0=PT[pk][:, :, :],
                                                           in1=selT[:, kb, :].unsqueeze(1).to_broadcast([128, 4, 128]), op=ALU.mult),
                             reads=[PT[pk], (selT, kb // 4)], writes=[PT[pk]])
                        p.op("pe", lambda: nc.tensor.matmul(pot[:, :], lhsT=vtok[:, kb, g * 128:(g + 1) * 128], rhs=PT[pk][:, :, :],
                                                            start=(kb == 0), stop=(kb == NKB - 1)), reads=[vtok, PT[pk]], writes=[pot])
                        p.op("pe", lambda: nc.tensor.matmul(pden[:, :], lhsT=ones_bf[:, :], rhs=PT[pk][:, :, :],
                                                            start=(kb == 0), stop=(kb == NKB - 1)), reads=[ones_bf, PT[pk]], writes=[pden])
                    p.op("dve", lambda: nc.vector.reciprocal(out=rden[:, :], in_=pden[:, :]), reads=[pden], writes=[rden])
                    p.op("dve", lambda: nc.vector.tensor_tensor(out=qT[:, j, 4 * g:4 * g + 4, :], in0=pot[:, :].rearrange("p (a b) -> p a b", a=4),
                                                                in1=rden[:, :].rearrange("p (a b) -> p a b", a=4), op=ALU.mult),
                         reads=[pot, rden], writes=[(qT, j, g)])
            p.dma("sp", A["aT_out"].rearrange("h d (j q) -> d j h q", q=128), qT[:, :, :, :], qT,
                  reads=[(qT, j, g) for j in range(NQB) for g in range(2)], writes=["aT_out"])
            p.barrier()


def build_l0_attn_prog():
    nc = new_nc()
    A = {"xT_all": din(nc, "xT_all", [D, S]), "xT_own": din(nc, "xT_own", [D, NT]),
         "pos_all": din(nc, "pos_all", [S], I32), "pos_own": din(nc, "pos_own", [NT], I32),
         "qlimrel": din(nc, "qlimrel", [128, NQB]), "invf": din(nc, "invf", [128, 1]), "sgn": din(nc, "sgn", [128, 1]),
         "wK": din(nc, "wK", [D, 512]), "wKIV": din(nc, "wKIV", [D, 512]),
         "wQ": din(nc, "wQ", [4, D, 512]), "wQI": din(nc, "wQI", [2, D, 512]), "wWI": din(nc, "wWI", [D, 4]),
         "aT_out": dout(nc, "aT_out", [8, 128, NT], BF16)}
    with ExitStack() as es:
        p = P(nc, es)
        emit_l0_attn(p, A)
        p.finish()
    return nc


def _sw(c0):
    return list(range(c0 + 64, c0 + 128)) + list(range(c0, c0 + 64))


def l0_attn_host(inputs):
    x = inputs["x"]
    pos = inputs["positions"]
    w = inputs["w_in0"][0]
    oq, ok, ov, oqi, oki, owi, ou = 0, 1024, 1280, 1536, 2048, 2176, 2180
    def cols(c0):
        return list(range(c0, c0 + 128))
    wK = w[:, cols(ok) + cols(ok + 128) + _sw(ok) + _sw(ok + 128)]
    wKIV = w[:, cols(oki) + _sw(oki) + list(range(ov, ov + 256))]
    wQ = np.stack([w[:, cols(oq + 256 * pp) + cols(oq + 256 * pp + 128) + _sw(oq + 256 * pp) + _sw(oq + 256 * pp + 128)] for pp in range(4)])
    wQI = np.stack([w[:, cols(oqi + 256 * pp) + cols(oqi + 256 * pp + 128) + _sw(oqi + 256 * pp) + _sw(oqi + 256 * pp + 128)] for pp in range(2)])
    wWI = w[:, owi:owi + 4]
    invf = (10000.0 ** (-(np.arange(128) % 64) * 2.0 / 128.0) / (2 * np.pi)).astype(np.float32).reshape(128, 1)
    sgn = np.where(np.arange(128) < 64, -1.0, 1.0).astype(np.float32).reshape(128, 1)
    maps = []
    for c in range(8):
        b, h = c // 2, c % 2
        own = own_tokens(h)
        tq = own.reshape(NQB, 128)
        qlim = ((tq // 64 + 1) * 64).T.astype(np.float32)
        qlimrel = qlim - (np.arange(NQB) * 256)[None, :]
        xT = np.ascontiguousarray(x[b].T)
        maps.append({"xT_all": xT, "xT_own": np.ascontiguousarray(xT[:, own]),
                     "pos_all": np.ascontiguousarray(pos[b]), "pos_own": np.ascontiguousarray(pos[b][own]),
                     "qlimrel": np.ascontiguousarray(qlimrel.astype(np.float32)), "invf": invf, "sgn": sgn,
                     "wK": np.ascontiguousarray(wK), "wKIV": np.ascontiguousarray(wKIV), "wQ": np.ascontiguousarray(wQ),
                     "wQI": np.ascontiguousarray(wQI), "wWI": np.ascontiguousarray(wWI)})
    return maps


def own_tokens(h):
    return np.concatenate([np.arange((2 * j + h) * 128, (2 * j + h + 1) * 128) for j in range(NQB)])


HALO = 16
SEG = 128 + HALO


def emit_l0_out(p, A):
    nc = p.nc
    NOH = NQB * SEG
    with ExitStack() as es:
        poolT = p.sb("poolT", [128, 8, NT], BF16, es)
        with ExitStack() as es2:
            wt = p.sb("wt", [128, 16, 512], BF16, es2)
            xb = [p.sb(f"xb{i}", [128, 16, 3 * SEG], BF16, es2) for i in range(2)]
            u = p.sb("u", [128, 4, NOH], F32, es2)
            ua = p.sb("ua", [128, NOH], F32, es2)
            ub = p.sb("ub", [128, NOH], F32, es2)
            icn = p.sb("icn", [128, 4, 128], F32, es2)
            pooled = p.sb("pooled", [128, 8, NT], BF16, es2)
            wp = p.sb("wp", [128, 4, 2, 256], BF16, es2)
            psc = p.sb("psc", [128, 8], F32, es2)
            pu_ = [p.ps(f"pu{i}", [128, 512], F32, es2) for i in range(2)]
            pm = [p.ps(f"pm{i}", [128, 512], F32, es2) for i in range(2)]
            p.dma("sp", icn[:, :, :], bass.AP(tensor=A["invcnt"].tensor, offset=A["invcnt"].offset, ap=[[0, 128], [NT, 4], [1, 128]]), icn, writes=[icn])
            p.dma("pool", wp[:, :, :, :], A["w_pool"].rearrange("g (c q) f -> q g c f", q=128), wp, writes=[wp])
            p.dma("sp", psc[:, :], A["pool_scale"], psc, writes=[psc])
            NXB = (NQB + 2) // 3
            cx = 0
            for pp in range(2):
                p.dma("pool", wt[:, :, :], A["wU"][pp].rearrange("(c q) f -> q c f", q=128), wt, writes=[wt])
                for blk in range(NXB):
                    s0 = blk * 3
                    ns = min(3, NQB - s0)
                    n = ns * SEG
                    b = xb[cx % 2]
                    cx += 1
                    p.dma("pool", b[:, :, 0:n], A["xT_oh"][:, s0 * SEG:s0 * SEG + n].rearrange("(c q) t -> q c t", q=128), b, writes=[b])
                    for oc in range(4):
                        ps_ = pu_[oc % 2]
                        for c in range(16):
                            p.op("pe", lambda: nc.tensor.matmul(ps_[:, 0:n], lhsT=wt[:, c, oc * 128:(oc + 1) * 128], rhs=b[:, c, 0:n],
                                                                start=(c == 0), stop=(c == 15)), reads=[wt, b], writes=[ps_])
                        if oc % 2 == 0:
                            p.op("act", lambda: nc.scalar.copy(out=u[:, oc, s0 * SEG:s0 * SEG + n], in_=ps_[:, 0:n]), reads=[ps_], writes=[(u, oc)])
                        else:
                            p.op("dve", lambda: nc.vector.tensor_copy(out=u[:, oc, s0 * SEG:s0 * SEG + n], in_=ps_[:, 0:n]), reads=[ps_], writes=[(u, oc)])
                for oc in range(4):
                    ch = pp * 4 + oc
                    g = ch // 2
                    cur = None
                    bufs = [ua, ub]
                    eng = "dve" if ch % 2 == 0 else "pool"
                    eo = nc.vector if eng == "dve" else nc.gpsimd
                    for st in range(g + 1):
                        sh = 1 << st
                        dst = bufs[st % 2]
                        s3 = (u[:, oc, :] if cur is None else cur[:, :]).rearrange("p (j s) -> p j s", s=SEG)
                        d3 = dst[:, :].rearrange("p (j s) -> p j s", s=SEG)
                        p.op(eng, lambda: eo.tensor_tensor(out=d3[:, :, sh:SEG], in0=s3[:, :, sh:SEG], in1=s3[:, :, 0:SEG - sh], op=ALU.add),
                             reads=[(u, oc) if cur is None else cur], writes=[dst])
                        cur = dst
                    c3 = cur[:, :].rearrange("p (j s) -> p j s", s=SEG)[:, :, HALO:SEG]
                    u3 = u[:, oc, :].rearrange("p (j s) -> p j s", s=SEG)[:, :, HALO:SEG]
                    o3 = pooled[:, ch, :].rearrange("p (j s) -> p j s", s=128)
                    p.op(eng, lambda: eo.tensor_tensor(out=c3[:, 0, :], in0=c3[:, 0, :], in1=icn[:, g, :], op=ALU.mult), reads=[cur, icn], writes=[cur])
                    p.op(eng, lambda: eo.tensor_scalar(out=c3[:, 1:NQB, :], in0=c3[:, 1:NQB, :], scalar1=1.0 / (2 << g), scalar2=None, op0=ALU.mult),
                         reads=[cur], writes=[cur])
                    p.op(eng, lambda: eo.tensor_tensor(out=o3, in0=c3, in1=u3, op=ALU.subtract), reads=[cur, (u, oc)], writes=[(pooled, ch)])
            km = 0
            for g in range(4):
                for do in range(2):
                    for tb in range(NT // 512):
                        ps_ = pm[km % 2]
                        km += 1
                        for cc in range(2):
                            p.op("pe", lambda: nc.tensor.matmul(ps_[:, :], lhsT=wp[:, g, cc, do * 128:(do + 1) * 128], rhs=pooled[:, 2 * g + cc, tb * 512:(tb + 1) * 512],
                                                                start=(cc == 0), stop=(cc == 1)), reads=[wp, (pooled, 2 * g + cc)], writes=[ps_])
                        ch = 2 * g + do
                        p.op("dve", lambda: nc.vector.tensor_scalar(out=poolT[:, ch, tb * 512:(tb + 1) * 512], in0=ps_[:, :], scalar1=psc[:, ch:ch + 1], scalar2=None,
                                                                    op0=ALU.mult), reads=[ps_, psc], writes=[(poolT, ch)])
            p.barrier()
        with ExitStack() as es3:
            wo = p.sb("wo", [128, 16, D], BF16, es3)
            aT = p.sb("aT", [128, 8, NT], BF16, es3)
            p.dma("sp", aT[:, :, :], A["aT"].rearrange("h d t -> d h t"), aT, writes=[aT])
            gbc = p.sb("gbc", [128, D], F32, es3)
            bbc = p.sb("bbc", [128, D], F32, es3)
            xr = [p.sb(f"xr{i}", [128, D], F32, es3) for i in range(2)]
            zo = [p.sb(f"zo{i}", [128, D], F32, es3) for i in range(2)]
            stats = p.sb("stats", [128, 24], F32, es3)
            mv = p.sb("mv", [128, 2], F32, es3)
            rstd = p.sb("rstd", [128, 1], F32, es3)
            po = [p.ps(f"po{i}", [128, 512], F32, es3) for i in range(4)]
            for c4 in range(4):
                p.dma("pool", wo[:, c4 * 4:(c4 + 1) * 4, :], A["w_out"][c4 * 512:(c4 + 1) * 512, :].rearrange("(c q) f -> q c f", q=128), wo, writes=[(wo, c4)])
            p.dma("sp", gbc[:, :], bcast_rows(A["ln_g"], 128, D), gbc, writes=[gbc])
            p.dma("sp", bbc[:, :], bcast_rows(A["ln_b"], 128, D), bbc, writes=[bbc])
            wok = [(wo, c4) for c4 in range(4)]
            for i in range(NTT):
                xri = xr[i % 2]
                zi = zo[i % 2]
                p.dma("sp", xri[:, :], A["x_own"][i * 128:(i + 1) * 128, :], xri, writes=[xri])
                for dc in range(4):
                    for fc in range(16):
                        src = aT if fc < 8 else poolT
                        p.op("pe", lambda: nc.tensor.matmul(po[dc][:, :], lhsT=src[:, fc % 8, i * 128:(i + 1) * 128], rhs=wo[:, fc, dc * 512:(dc + 1) * 512],
                                                            start=(fc == 0), stop=(fc == 15)), reads=wok + [aT] + [(poolT, ch) for ch in range(8)], writes=[po[dc]])
                    p.op("dve", lambda: nc.vector.scalar_tensor_tensor(out=xri[:, dc * 512:(dc + 1) * 512], in0=xri[:, dc * 512:(dc + 1) * 512], scalar=ALPHA,
                                                                       in1=po[dc][:, :], op0=ALU.mult, op1=ALU.add), reads=[xri, po[dc]], writes=[xri])
                emit_ln(p, xri, zi, gbc, bbc, stats, mv, rstd)
                p.dma("sp", A["xa_out"][i * 128:(i + 1) * 128, :], zi[:, :], zi, reads=[zi], writes=[("xa_out", i)])
                if "xa_out2" in A:
                    p.dma("sp", A["xa_out2"][i * 128:(i + 1) * 128, :], zi[:, :], zi, reads=[zi], writes=[("xa_out2", i)])
            p.barrier()


def build_l0_out_prog():
    nc = new_nc()
    A = {"xT_oh": din(nc, "xT_oh", [D, NQB * SEG]), "wU": din(nc, "wU", [2, D, 512]), "invcnt": din(nc, "invcnt", [4, NT]),
         "w_pool": din(nc, "w_pool", [4, 256, 256]), "pool_scale": din(nc, "pool_scale", [128, 8]),
         "aT": din(nc, "aT", [8, 128, NT], BF16), "w_out": din(nc, "w_out", [D, D]), "x_own": din(nc, "x_own", [NT, D]),
         "ln_g": din(nc, "ln_g", [D]), "ln_b": din(nc, "ln_b", [D]), "xa_out": dout(nc, "xa_out", [NT, D])}
    with ExitStack() as es:
        p = P(nc, es)
        emit_l0_out(p, A)
        p.finish()
    return nc


def l0_out_host(inputs, aT_cores):
    x = inputs["x"]
    w = inputs["w_in0"][0]
    ou = 2180
    wU = np.ascontiguousarray(np.stack([w[:, ou + 512 * pp:ou + 512 * (pp + 1)] for pp in range(2)]))
    psc = np.ascontiguousarray(inputs["pool_scale"][0].reshape(8, 128).T)
    maps = []
    for c in range(8):
        b, h = c // 2, c % 2
        own = own_tokens(h)
        xoh = np.zeros((D, NQB * SEG), np.float32)
        for j in range(NQB):
            t0 = (2 * j + h) * 128
            lo = t0 - HALO
            if lo >= 0:
                xoh[:, j * SEG:(j + 1) * SEG] = x[b, lo:t0 + 128].T
            else:
                xoh[:, j * SEG + HALO:(j + 1) * SEG] = x[b, t0:t0 + 128].T
        invcnt = np.stack([1.0 / np.minimum(own + 1.0, float(wd)) for wd in (2, 4, 8, 16)]).astype(np.float32)
        maps.append({"xT_oh": xoh, "wU": wU, "invcnt": np.ascontiguousarray(invcnt), "w_pool": np.ascontiguousarray(inputs["w_pool"][0]),
                     "pool_scale": psc, "aT": aT_cores[c], "w_out": np.ascontiguousarray(inputs["w_out0"][0]),
                     "x_own": np.ascontiguousarray(x[b][own]), "ln_g": np.ascontiguousarray(inputs["ln_mix_g"][0]),
                     "ln_b": np.ascontiguousarray(inputs["ln_mix_b"][0])})
    return maps


HL = 4
NTILE = S // 128


def emit_l1_mlstm(p, A):
    nc = p.nc
    with ExitStack() as es:
        qT = p.sb("qT", [128, HL, S], BF16, es)
        kT = p.sb("kT", [128, HL, S], BF16, es)
        with ExitStack() as es2:
            wt = p.sb("wt", [128, 16, 512], BF16, es2)
            xb = [p.sb(f"xb{i}", [128, 16, 512], BF16, es2) for i in range(2)]
            raw = [p.sb(f"raw{i}", [128, 4, 3 + 512], F32, es2) for i in range(2)]
            yv = [p.sb(f"yv{i}", [128, 512], F32, es2) for i in range(2)]
            cw = p.sb("cw", [128, 8, 4], F32, es2)
            cb = p.sb("cb", [128, 8], F32, es2)
            pq = [p.ps(f"pq{i}", [128, 512], F32, es2) for i in range(2)]
            p.dma("sp", cw[:, :, :], A["conv_w"], cw, writes=[cw])
            p.dma("sp", cb[:, :], A["conv_b"], cb, writes=[cb])
            cx = 0
            for pp in range(2):
                p.dma("pool", wt[:, :, :], A["wQK"][pp].rearrange("(c q) f -> q c f", q=128), wt, writes=[wt])
                dstT = qT if pp == 0 else kT
                for blk in range(S // 512):
                    b = xb[cx % 2]
                    rw = raw[cx % 2]
                    rprev = raw[(cx + 1) % 2]
                    cx += 1
                    if "x1R" in A:
                        for h_ in range(2):
                            for jj in range(2):
                                p.dma("sp", b[:, :, (jj * 2 + h_) * 128:(jj * 2 + h_ + 1) * 128],
                                      A["x1R"][h_, :, (2 * blk + jj) * 128:(2 * blk + jj + 1) * 128].rearrange("(c p) q -> p c q", p=128), b, writes=[b])
                    else:
                        p.dma("pool", b[:, :, :], A["xT"][:, blk * 512:(blk + 1) * 512].rearrange("(c q) t -> q c t", q=128), b, writes=[b])
                    if blk == 0:
                        p.op("dve", lambda: nc.vector.memset(rw[:, :, 0:3], 0.0), writes=[(rw, "h")])
                    else:
                        p.op("dve", lambda: nc.vector.tensor_copy(out=rw[:, :, 0:3], in_=rprev[:, :, 512:515]),
                             reads=[(rprev, o) for o in range(4)], writes=[(rw, "h")])
                    for oc in range(4):
                        ps_ = pq[oc % 2]
                        for c in range(16):
                            p.op("pe", lambda: nc.tensor.matmul(ps_[:, :], lhsT=wt[:, c, oc * 128:(oc + 1) * 128], rhs=b[:, c, :],
                                                                start=(c == 0), stop=(c == 15)), reads=[wt, b], writes=[ps_])
                        p.op("act", lambda: nc.scalar.copy(out=rw[:, oc, 3:515], in_=ps_[:, :]), reads=[ps_], writes=[(rw, oc)])
                        ch = pp * 4 + oc
                        y = yv[oc % 2]
                        eng = "dve" if oc % 2 == 0 else "pool"
                        eo = nc.vector if eng == "dve" else nc.gpsimd
                        p.op("dve", lambda: nc.vector.tensor_scalar(out=y[:, :], in0=rw[:, oc, 3:515], scalar1=cw[:, ch, 3:4], scalar2=cb[:, ch:ch + 1],
                                                                    op0=ALU.mult, op1=ALU.add), reads=[(rw, oc), cw, cb], writes=[y])
                        for tap in range(3):
                            p.op("dve", lambda: nc.vector.scalar_tensor_tensor(out=y[:, :], in0=rw[:, oc, tap:tap + 512], scalar=cw[:, ch, tap:tap + 1], in1=y[:, :],
                                                                               op0=ALU.mult, op1=ALU.add), reads=[(rw, oc), (rw, "h"), cw, y], writes=[y])
                        if pp == 0:
                            p.op("act", lambda: nc.scalar.activation(out=y[:, :], in_=y[:, :], func=AF.Silu), reads=[y], writes=[y])
                            p.op("pool", lambda: nc.gpsimd.tensor_scalar(out=dstT[:, oc, blk * 512:(blk + 1) * 512], in0=y[:, :], scalar1=128.0 ** -0.5, scalar2=None,
                                                                         op0=ALU.mult), reads=[y], writes=[(dstT, oc, blk)])
                        else:
                            p.op("act", lambda: nc.scalar.activation(out=dstT[:, oc, blk * 512:(blk + 1) * 512], in_=y[:, :], func=AF.Silu), reads=[y], writes=[(dstT, oc, blk)])
            p.barrier()
        with ExitStack() as es3:
            wv = p.sb("wv", [128, 16, 2056], BF16, es3)
            xb = [p.sb(f"xt{i}", [128, 16, 128], BF16, es3) for i in range(2)]
            vext = [p.sb(f"vext{i}", [128, HL, 258], BF16, es3) for i in range(2)]
            ogs = p.sb("ogs", [128, 1024], F32, es3)
            gb = p.sb("gb", [128, 8], F32, es3)
            cng = p.sb("cng", [128, 1024], F32, es3)
            g8 = p.sb("g8", [128, 8], F32, es3)
            e4 = p.sb("e4", [128, 4], F32, es3)
            lf4 = p.sb("lf4", [128, 4], F32, es3)
            a4 = p.sb("a4", [128, 4], F32, es3)
            ei4 = p.sb("ei4", [128, 4], F32, es3)
            tri2 = p.sb("tri2", [128, 128], F32, es3)
            mask2 = p.sb("mask2", [128, 128], F32, es3)
            ind2 = p.sb("ind2", [128, 2], F32, es3)
            ones_f = p.sb("ones_f", [128, 128], F32, es3)
            identb = p.sb("identb", [128, 128], BF16, es3)
            lfb = [p.sb(f"lfb{i}", [128, 128], F32, es3) for i in range(2)]
            E = [p.sb(f"E{i}", [128, 128], F32, es3) for i in range(2)]
            SmT = [p.sb(f"SmT{i}", [128, 128], BF16, es3) for i in range(2)]
            dec2 = [p.sb(f"dec2{i}", [128, 2], F32, es3) for i in range(2)]
            bLo = [p.sb(f"bLo{i}", [128, 1], F32, es3) for i in range(2)]
            wgt = [p.sb(f"wgt{i}", [128, 1], F32, es3) for i in range(2)]
            kw = [p.sb(f"kw{i}", [128, 128], BF16, es3) for i in range(2)]
            intra = [p.sb(f"intra{i}", [128, 257], F32, es3) for i in range(2)]
            num = [p.sb(f"num{i}", [128, 257], F32, es3) for i in range(2)]
            den = [p.sb(f"den{i}", [128, 1], F32, es3) for i in range(2)]
            hh = [p.sb(f"hh{i}", [128, 256], F32, es3) for i in range(2)]
            st6 = [p.sb(f"st6{i}", [128, 6], F32, es3) for i in range(2)]
            mv = [p.sb(f"mv{i}", [128, 2], F32, es3) for i in range(2)]
            rs = [p.sb(f"rs{i}", [128, 1], F32, es3) for i in range(2)]
            CT = [p.sb(f"CT{h}", [128, 257], F32, es3) for h in range(HL)]
            CTb = [p.sb(f"CTb{h}", [128, 257], BF16, es3) for h in range(HL)]
            y = [p.sb(f"y{i}", [128, 1024], F32, es3) for i in range(2)]
            ybf = [p.sb(f"ybf{i}", [128, 1024], BF16, es3) for i in range(2)]
            yT = [p.sb(f"yT{i}", [128, 8, 128], BF16, es3) for i in range(2)]
            P1 = p.ps("P1", [128, 512], F32, es3)
            P3 = p.ps("P3", [128, 64], F32, es3)
            P4 = p.ps("P4", [128, 4, 128], F32, es3)
            P5 = p.ps("P5", [128, 4, 128], F32, es3)
            P6 = p.ps("P6", [128, 8, 128], BF16, es3)
            Pin = p.ps("Pin", [128, 257], F32, es3)
            Pit = p.ps("Pit", [128, 257], F32, es3)
            Pup = p.ps("Pup", [128, 257], F32, es3)

            for c4 in range(4):
                p.dma("pool", wv[:, c4 * 4:(c4 + 1) * 4, :], A["wVOG"][c4 * 512:(c4 + 1) * 512, :].rearrange("(c q) f -> q c f", q=128), wv, writes=[(wv, c4)])
            wvk = [(wv, c4) for c4 in range(4)]
            p.dma("sp", gb[:, :], bcast_rows(A["gate_b"], 128, 8), gb, writes=[gb])
            p.dma("sp", cng[:, :], bcast_rows(A["c_norm_g"], 128, 1024), cng, writes=[cng])
            p.op("dve", lambda: nc.vector.memset(ones_f[:, :], 1.0), writes=[ones_f])
            p.op("dve", lambda: nc.vector.memset(mask2[:, :], 1.0), writes=[mask2])
            p.op("pool", lambda: nc.gpsimd.affine_select(out=mask2[:, :], in_=mask2[:, :], pattern=[[1, 128]], compare_op=ALU.is_ge,
                                                         fill=0.0, base=0, channel_multiplier=-1), reads=[mask2], writes=[mask2])
            p.op("dve", lambda: nc.vector.memset(mask2[0:64, 64:128], 0.0), reads=[mask2], writes=[mask2])
            p.op("dve", lambda: nc.vector.tensor_copy(out=tri2[:, :], in_=mask2[:, :]), reads=[mask2], writes=[tri2])
            p.op("dve", lambda: nc.vector.memset(ind2[:, :], 0.0), writes=[ind2])
            p.op("dve", lambda: nc.vector.memset(ind2[0:64, 0:1], 1.0), reads=[ind2], writes=[ind2])
            p.op("dve", lambda: nc.vector.memset(ind2[64:128, 1:2], 1.0), reads=[ind2], writes=[ind2])
            p.op("dve", lambda: nc.vector.memset(identb[:, :], 1.0), writes=[identb])
            p.op("pool", lambda: nc.gpsimd.affine_select(out=identb[:, :], in_=identb[:, :], pattern=[[1, 128]], compare_op=ALU.is_equal,
                                                         fill=0.0, base=0, channel_multiplier=-1), reads=[identb], writes=[identb])
            for h in range(HL):
                p.op("dve", lambda: nc.vector.memset(CT[h][:, :], 0.0), writes=[CT[h]])
                p.op("dve", lambda: nc.vector.memset(CTb[h][:, :], 0.0), writes=[CTb[h]])
            for i2 in range(2):
                p.op("dve", lambda: nc.vector.memset(vext[i2][:, :, 256:258], 1.0), writes=[(vext[i2], "one")])

            for i in range(NTILE):
                xt = xb[i % 2]
                ve = vext[i % 2]
                yi = y[i % 2]
                ts_ = slice(i * 128, (i + 1) * 128)
                if "x1R" in A:
                    p.dma("sp", xt[:, :, :], A["x1R"][i % 2, :, (i // 2) * 128:(i // 2 + 1) * 128].rearrange("(c q) t -> q c t", q=128), xt, writes=[xt])
                else:
                    p.dma("pool", xt[:, :, :], A["xT"][:, ts_].rearrange("(c q) t -> q c t", q=128), xt, writes=[xt])
                for vb in range(2):
                    for c in range(16):
                        p.op("pe", lambda: nc.tensor.matmul(P1[:, :], lhsT=xt[:, c, :], rhs=wv[:, c, vb * 512:(vb + 1) * 512], start=(c == 0), stop=(c == 15)),
                             reads=[xt] + wvk, writes=[P1])
                    p.op("act", lambda: nc.scalar.copy(out=ve[:, 2 * vb:2 * vb + 2, 0:256], in_=P1[:, :].rearrange("p (h d) -> p h d", h=2)),
                         reads=[P1], writes=[(ve, vb)])
                for ob in range(2):
                    for c in range(16):
                        p.op("pe", lambda: nc.tensor.matmul(P1[:, :], lhsT=xt[:, c, :], rhs=wv[:, c, 1024 + ob * 512:1024 + (ob + 1) * 512], start=(c == 0), stop=(c == 15)),
                             reads=[xt] + wvk, writes=[P1])
                    p.op("act", lambda: nc.scalar.activation(out=ogs[:, ob * 512:(ob + 1) * 512], in_=P1[:, :], func=AF.Sigmoid), reads=[P1], writes=[(ogs, ob)])
                for c in range(16):
                    p.op("pe", lambda: nc.tensor.matmul(P3[:, 0:8], lhsT=xt[:, c, :], rhs=wv[:, c, 2048:2056], start=(c == 0), stop=(c == 15)),
                         reads=[xt] + wvk, writes=[(P3, "g")])
                p.op("dve", lambda: nc.vector.tensor_tensor(out=g8[:, :], in0=P3[:, 0:8], in1=gb[:, :], op=ALU.add), reads=[(P3, "g"), gb], writes=[g8])
                p.op("act", lambda: nc.scalar.activation(out=e4[:, :], in_=g8[:, 4:8], func=AF.Exp, scale=-1.0), reads=[g8], writes=[e4])
                p.op("act", lambda: nc.scalar.activation(out=e4[:, :], in_=e4[:, :], func=AF.Ln, bias=1.0), reads=[e4], writes=[e4])
                p.op("dve", lambda: nc.vector.tensor_scalar(out=lf4[:, :], in0=e4[:, :], scalar1=-1.0, scalar2=None, op0=ALU.mult), reads=[e4], writes=[lf4])
                p.op("pe", lambda: nc.tensor.matmul(P3[:, 8:12], lhsT=tri2[:, :], rhs=lf4[:, :], start=True, stop=True), reads=[tri2, lf4], writes=[(P3, "b")])
                p.op("dve", lambda: nc.vector.tensor_tensor(out=a4[:, :], in0=g8[:, 0:4], in1=P3[:, 8:12], op=ALU.subtract), reads=[g8, (P3, "b")], writes=[a4])
                p.op("act", lambda: nc.scalar.activation(out=ei4[:, :], in_=P3[:, 8:12], func=AF.Exp), reads=[(P3, "b")], writes=[ei4])
                for h in range(HL):
                    k2 = h % 2
                    p.op("pool", lambda: nc.gpsimd.tensor_scalar(out=lfb[k2][:, :], in0=ones_f[:, :], scalar1=lf4[:, h:h + 1], scalar2=None, op0=ALU.mult),
                         reads=[ones_f, lf4], writes=[lfb[k2]])
                    p.op("pe", lambda: nc.tensor.matmul(P4[:, h, :], lhsT=lfb[k2][:, :], rhs=tri2[:, :], start=True, stop=True), reads=[lfb[k2], tri2], writes=[(P4, h)])
                    p.op("pe", lambda: nc.tensor.matmul(P3[:, 16 + 2 * h:18 + 2 * h], lhsT=lfb[k2][:, :], rhs=ind2[:, :], start=True, stop=True),
                         reads=[lfb[k2], ind2], writes=[(P3, "L", h)])
                    p.op("act", lambda: nc.scalar.activation(out=E[k2][:, :], in_=P4[:, h, :], func=AF.Exp, bias=a4[:, h:h + 1]), reads=[(P4, h), a4], writes=[E[k2]])
                    p.op("pool", lambda: nc.gpsimd.tensor_tensor(out=E[k2][:, :], in0=E[k2][:, :], in1=mask2[:, :], op=ALU.mult), reads=[E[k2], mask2], writes=[E[k2]])
                    p.op("pe", lambda: nc.tensor.matmul(P5[:, h, :], lhsT=kT[:, h, ts_], rhs=qT[:, h, ts_], start=True, stop=True), reads=[kT, qT], writes=[(P5, h)])
                    p.op("dve", lambda: nc.vector.tensor_tensor(out=SmT[k2][:, :], in0=P5[:, h, :], in1=E[k2][:, :], op=ALU.mult), reads=[(P5, h), E[k2]], writes=[SmT[k2]])
                    p.op("act", lambda: nc.scalar.activation(out=dec2[k2][:, :], in_=P3[:, 16 + 2 * h:18 + 2 * h], func=AF.Exp), reads=[(P3, "L", h)], writes=[dec2[k2]])
                    p.op("dve", lambda: nc.vector.tensor_copy(out=bLo[k2][0:64, :], in_=P3[0:64, 16 + 2 * h:17 + 2 * h]), reads=[(P3, "L", h)], writes=[(bLo[k2], 0)])
                    p.op("dve", lambda: nc.vector.tensor_copy(out=bLo[k2][64:128, :], in_=P3[64:128, 17 + 2 * h:18 + 2 * h]), reads=[(P3, "L", h)], writes=[(bLo[k2], 1)])
                    p.op("act", lambda: nc.scalar.activation(out=wgt[k2][:, :], in_=bLo[k2][:, :], func=AF.Exp, bias=a4[:, h:h + 1]),
                         reads=[(bLo[k2], 0), (bLo[k2], 1), a4], writes=[wgt[k2]])
                    p.op("pe", lambda: nc.tensor.transpose(out=P6[:, h, :], in_=kT[:, h, ts_], identity=identb[:, :]), reads=[kT, identb], writes=[(P6, h)])
                    p.op("dve", lambda: nc.vector.tensor_scalar(out=kw[k2][:, :], in0=P6[:, h, :], scalar1=wgt[k2][:, 0:1], scalar2=None, op0=ALU.mult),
                         reads=[(P6, h), wgt[k2]], writes=[kw[k2]])
                    p.op("pe", lambda: nc.tensor.matmul(Pit[:, :], lhsT=SmT[k2][:, :], rhs=ve[:, h, 0:257], start=True, stop=True),
                         reads=[SmT[k2], (ve, h // 2), (ve, "one")], writes=[Pit])
                    p.op("act", lambda: nc.scalar.copy(out=intra[k2][:, :], in_=Pit[:, :]), reads=[Pit], writes=[intra[k2]])
                    for ck in range(2):
                        ps_l = slice(ck * 64, (ck + 1) * 64)
                        p.op("pe", lambda: nc.tensor.matmul(Pin[:, :], lhsT=qT[:, h, ts_], rhs=CTb[h][:, :], start=True, stop=True), reads=[qT, CTb[h]], writes=[Pin])
                        p.op("dve", lambda: nc.vector.scalar_tensor_tensor(out=num[k2][ps_l, :], in0=Pin[ps_l, :], scalar=ei4[ps_l, h:h + 1], in1=intra[k2][ps_l, :],
                                                                           op0=ALU.mult, op1=ALU.add), reads=[Pin, ei4, intra[k2]], writes=[(num[k2], ck)])
                        p.op("pe", lambda: nc.tensor.matmul(Pup[:, :], lhsT=kw[k2][ps_l, :], rhs=ve[ps_l, h, 0:257], start=True, stop=True),
                             reads=[kw[k2], (ve, h // 2), (ve, "one")], writes=[Pup])
                        p.op("dve", lambda: nc.vector.scalar_tensor_tensor(out=CT[h][:, :], in0=CT[h][:, :], scalar=dec2[k2][:, ck:ck + 1], in1=Pup[:, :],
                                                                           op0=ALU.mult, op1=ALU.add), reads=[CT[h], dec2[k2], Pup], writes=[CT[h]])
                        p.op("act", lambda: nc.scalar.copy(out=CTb[h][:, :], in_=CT[h][:, :]), reads=[CT[h]], writes=[CTb[h]])
                    numk = [(num[k2], 0), (num[k2], 1)]
                    p.op("act", lambda: nc.scalar.activation(out=den[k2][:, :], in_=num[k2][:, 256:257], func=AF.Abs), reads=numk, writes=[den[k2]])
                    p.op("dve", lambda: nc.vector.tensor_scalar(out=den[k2][:, :], in0=den[k2][:, :], scalar1=1.0, scalar2=None, op0=ALU.max),
                         reads=[den[k2]], writes=[den[k2]])
                    p.op("dve", lambda: nc.vector.reciprocal(out=den[k2][:, :], in_=den[k2][:, :]), reads=[den[k2]], writes=[den[k2]])
                    p.op("dve", lambda: nc.vector.tensor_scalar(out=hh[k2][:, :], in0=num[k2][:, 0:256], scalar1=den[k2][:, 0:1], scalar2=None, op0=ALU.mult),
                         reads=numk + [den[k2]], writes=[hh[k2]])
                    p.op("dve", lambda: nc.vector.bn_stats(out=st6[k2][:, :], in_=hh[k2][:, :]), reads=[hh[k2]], writes=[st6[k2]])
                    p.op("dve", lambda: nc.vector.bn_aggr(out=mv[k2][:, :], in_=st6[k2][:, :]), reads=[st6[k2]], writes=[mv[k2]])
                    p.op("dve", lambda: nc.vector.tensor_scalar(out=rs[k2][:, :], in0=mv[k2][:, 1:2], scalar1=LN_EPS, scalar2=None, op0=ALU.add), reads=[mv[k2]], writes=[rs[k2]])
                    p.op("act", lambda: nc.scalar.activation(out=rs[k2][:, :], in_=rs[k2][:, :], func=AF.Sqrt), reads=[rs[k2]], writes=[rs[k2]])
                    p.op("dve", lambda: nc.vector.reciprocal(out=rs[k2][:, :], in_=rs[k2][:, :]), reads=[rs[k2]], writes=[rs[k2]])
                    p.op("dve", lambda: nc.vector.tensor_scalar(out=yi[:, h * 256:(h + 1) * 256], in0=hh[k2][:, :], scalar1=mv[k2][:, 0:1], scalar2=rs[k2][:, 0:1],
                                                                op0=ALU.subtract, op1=ALU.mult), reads=[hh[k2], mv[k2], rs[k2]], writes=[(yi, h)])
                yk = [(yi, h) for h in range(HL)]
                p.op("pool", lambda: nc.gpsimd.tensor_tensor(out=yi[:, :], in0=yi[:, :], in1=cng[:, :], op=ALU.mult), reads=yk + [cng], writes=yk)
                p.op("dve", lambda: nc.vector.tensor_tensor(out=ybf[i % 2][:, :], in0=yi[:, :], in1=ogs[:, :], op=ALU.mult), reads=yk + [(ogs, 0), (ogs, 1)], writes=[ybf[i % 2]])
                for fcn in range(8):
                    p.op("pe", lambda: nc.tensor.transpose(out=P6[:, fcn, :], in_=ybf[i % 2][:, fcn * 128:(fcn + 1) * 128], identity=identb[:, :]),
                         reads=[ybf[i % 2], identb], writes=[(P6, fcn % 4)] if fcn < 4 else [(P6, "hi", fcn)])
                p.op("act", lambda: nc.scalar.copy(out=yT[i % 2][:, :, :], in_=P6[:, :, :]),
                     reads=[(P6, f_) for f_ in range(4)] + [(P6, "hi", f_) for f_ in range(4, 8)], writes=[yT[i % 2]])
                hdst = A["HS"][i % 2, :, (i // 2) * 128:(i // 2 + 1) * 128] if "HS" in A else A["hgT_out"][:, ts_]
                p.dma("sp", hdst.rearrange("(c q) t -> q c t", q=128), yT[i % 2][:, :, :], yT[i % 2], reads=[yT[i % 2]], writes=[("hg", i)])
            p.barrier()


def build_l1_mlstm_prog():
    nc = new_nc()
    A = {"xT": din(nc, "xT", [D, S]), "wQK": din(nc, "wQK", [2, D, 512]), "wVOG": din(nc, "wVOG", [D, 2056]),
         "conv_w": din(nc, "conv_w", [128, 8, 4]), "conv_b": din(nc, "conv_b", [128, 8]), "gate_b": din(nc, "gate_b", [8]),
         "c_norm_g": din(nc, "c_norm_g", [1024]), "hgT_out": dout(nc, "hgT_out", [1024, S], BF16)}
    with ExitStack() as es:
        p = P(nc, es)
        emit_l1_mlstm(p, A)
        p.finish()
    return nc


def l1_mlstm_host(inputs, x1_full):
    w = inputs["w_in1"][0]
    cwf = inputs["conv_w"][0]
    cbf = inputs["conv_b"][0]
    maps = []
    for c in range(8):
        b, hh = c // 2, c % 2
        qc = np.arange(hh * 512, (hh + 1) * 512)
        kc = 1024 + qc
        vc = 2048 + np.arange(hh * 1024, (hh + 1) * 1024)
        igc = 4096 + np.arange(hh * 4, (hh + 1) * 4)
        fgc = 4104 + np.arange(hh * 4, (hh + 1) * 4)
        ogc = 4112 + np.arange(hh * 1024, (hh + 1) * 1024)
        wQK = np.stack([w[:, qc], w[:, kc]])
        wVOG = w[:, np.concatenate([vc, ogc, igc, fgc])]
        chans = np.concatenate([qc, kc])
        cw = cwf[:, chans].reshape(4, 8, 128).transpose(2, 1, 0)
        cb = cbf[chans].reshape(8, 128).T
        gate_b = np.concatenate([inputs["gate_i_b"][0][hh * 4:(hh + 1) * 4], inputs["gate_f_b"][0][hh * 4:(hh + 1) * 4]])
        maps.append({"xT": (np.ascontiguousarray(x1_full[b].T) if x1_full is not None else None), "wQK": np.ascontiguousarray(wQK), "wVOG": np.ascontiguousarray(wVOG),
                     "conv_w": np.ascontiguousarray(cw), "conv_b": np.ascontiguousarray(cb), "gate_b": np.ascontiguousarray(gate_b),
                     "c_norm_g": np.ascontiguousarray(inputs["c_norm_g"][0][hh * 1024:(hh + 1) * 1024])})
    return maps


def emit_l1_out(p, A):
    nc = p.nc
    with ExitStack() as es3:
        wo = p.sb("wo", [128, 16, D], BF16, es3)
        hg = p.sb("hg", [128, 16, NT], BF16, es3)
        gbc = p.sb("gbc", [128, D], F32, es3)
        bbc = p.sb("bbc", [128, D], F32, es3)
        xr = [p.sb(f"xr{i}", [128, D], F32, es3) for i in range(2)]
        zo = [p.sb(f"zo{i}", [128, D], F32, es3) for i in range(2)]
        stats = p.sb("stats", [128, 24], F32, es3)
        mv = p.sb("mv", [128, 2], F32, es3)
        rstd = p.sb("rstd", [128, 1], F32, es3)
        po = [p.ps(f"po{i}", [128, 512], F32, es3) for i in range(4)]
        if "HR" in A:
            tabh = p.sb("tabH", [128, 16], I32, es3)
            p.dma("sp", tabh[:, :], A["tabH"], tabh, writes=[tabh])
            HRrows = A["HR"].rearrange("r h f t -> (r h f) t")
            for fc in range(16):
                p.dma("pool", None, None, hg, reads=[tabh], writes=[hg],
                      fn=lambda: nc.gpsimd.indirect_dma_start(out=hg[:, fc, :], out_offset=None, in_=HRrows,
                                                              in_offset=bass.IndirectOffsetOnAxis(ap=tabh[:, fc:fc + 1], axis=0)))
        else:
            p.dma("sp", hg[:, :, :], A["hgT"].rearrange("(c q) t -> q c t", q=128), hg, writes=[hg])
        for c4 in range(4):
            p.dma("pool", wo[:, c4 * 4:(c4 + 1) * 4, :], A["w_out"][c4 * 512:(c4 + 1) * 512, :].rearrange("(c q) f -> q c f", q=128), wo, writes=[(wo, c4)])
        p.dma("sp", gbc[:, :], bcast_rows(A["ln_g"], 128, D), gbc, writes=[gbc])
        p.dma("sp", bbc[:, :], bcast_rows(A["ln_b"], 128, D), bbc, writes=[bbc])
        wok = [(wo, c4) for c4 in range(4)]
        for i in range(NTT):
            xri = xr[i % 2]
            zi = zo[i % 2]
            p.dma("sp", xri[:, :], A["x_own"][i * 128:(i + 1) * 128, :], xri, writes=[xri])
            for dc in range(4):
                for fc in range(16):
                    p.op("pe", lambda: nc.tensor.matmul(po[dc][:, :], lhsT=hg[:, fc, i * 128:(i + 1) * 128], rhs=wo[:, fc, dc * 512:(dc + 1) * 512],
                                                        start=(fc == 0), stop=(fc == 15)), reads=wok + [hg], writes=[po[dc]])
                p.op("dve", lambda: nc.vector.scalar_tensor_tensor(out=xri[:, dc * 512:(dc + 1) * 512], in0=xri[:, dc * 512:(dc + 1) * 512], scalar=ALPHA,
                                                                   in1=po[dc][:, :], op0=ALU.mult, op1=ALU.add), reads=[xri, po[dc]], writes=[xri])
            emit_ln(p, xri, zi, gbc, bbc, stats, mv, rstd)
            p.dma("sp", A["xb_out"][i * 128:(i + 1) * 128, :], zi[:, :], zi, reads=[zi], writes=[("xb_out", i)])
            if "xb_out2" in A:
                p.dma("sp", A["xb_out2"][i * 128:(i + 1) * 128, :], zi[:, :], zi, reads=[zi], writes=[("xb_out2", i)])
        p.barrier()


def build_l1_out_prog():
    nc = new_nc()
    A = {"hgT": din(nc, "hgT", [D, NT], BF16), "w_out": din(nc, "w_out", [D, D]), "x_own": din(nc, "x_own", [NT, D]),
         "ln_g": din(nc, "ln_g", [D]), "ln_b": din(nc, "ln_b", [D]), "xb_out": dout(nc, "xb_out", [NT, D])}
    with ExitStack() as es:
        p = P(nc, es)
        emit_l1_out(p, A)
        p.finish()
    return nc


ALL8 = list(range(8))


def kernel_unfused(**inputs):
    inputs = {k: np.asarray(v) for k, v in inputs.items()}
    r = run_bass_kernel_spmd(build_l0_attn_prog(), l0_attn_host(inputs), core_ids=ALL8).results
    aT = [np.asarray(r[c]["aT_out"]) for c in range(8)]
    r = run_bass_kernel_spmd(build_l0_out_prog(), l0_out_host(inputs, aT), core_ids=ALL8).results
    xa = [np.asarray(r[c]["xa_out"]) for c in range(8)]
    x1 = run_moe_layer(xa, inputs, 0)
    x1_full = np.empty((4, S, D), np.float32)
    for c in range(8):
        x1_full[c // 2][own_tokens(c % 2)] = x1[c]
    r = run_bass_kernel_spmd(build_l1_mlstm_prog(), l1_mlstm_host(inputs, x1_full), core_ids=ALL8).results
    hg = [np.asarray(r[c]["hgT_out"]) for c in range(8)]
    maps = []
    for c in range(8):
        b, h = c // 2, c % 2
        hgT = np.concatenate([hg[2 * b], hg[2 * b + 1]], axis=0)[:, own_tokens(h)]
        maps.append({"hgT": np.ascontiguousarray(hgT), "w_out": np.ascontiguousarray(inputs["w_out1"][0]), "x_own": x1[c],
                     "ln_g": np.ascontiguousarray(inputs["ln_mix_g"][1]), "ln_b": np.ascontiguousarray(inputs["ln_mix_b"][1])})
    r = run_bass_kernel_spmd(build_l1_out_prog(), maps, core_ids=ALL8).results
    xb = [np.asarray(r[c]["xb_out"]) for c in range(8)]
    x2 = run_moe_layer(xb, inputs, 1)
    out = np.empty((4, S, D), np.float32)
    for c in range(8):
        out[c // 2][own_tokens(c % 2)] = x2[c]
    return out


NXR = (8 * 8 * NLOC)
G8 = [list(range(8))]


def pair_groups(r):
    return [[c, c ^ r] for c in range(8) if c < (c ^ r)]


def emit_dispatch2(p, x_in, XS, XR, idx_out, gk_out, W, ident_f):
    nc = p.nc
    with ExitStack() as es:
        xbf = p.sb("xbf", [128, NTT, D], BF16, es)
        posm = p.sb("posm", [128, NTT, NE], F32, es)
        idx = p.sb("idx", [128, NTT, 4], I32, es)
        gk = p.sb("gk", [128, NTT, 4], F32, es)
        iotac = p.sb("iotac", [128, CAP], F32, es)
        ones_bf = p.sb("ones_bf", [128, 128], BF16, es)
        p.op("pool", lambda: nc.gpsimd.iota(iotac[:, :], pattern=[[1, CAP]], base=0, channel_multiplier=0,
                                            allow_small_or_imprecise_dtypes=True), writes=[iotac])
        p.op("dve", lambda: nc.vector.memset(ones_bf[:, :], 1.0), writes=[ones_bf])
        with ExitStack() as es2:
            wr = p.sb("wr", [128, 16, NE], F32, es2)
            rb = p.sb("rb", [128, NE], F32, es2)
            ustr = p.sb("ustr", [128, 128], BF16, es2)
            rowbase = p.sb("rowbase", [128, NE], F32, es2)
            cum = p.sb("cum", [128, NE], F32, es2)
            xa = [p.sb(f"xa{i}", [128, D], F32, es2) for i in range(2)]
            xT = [p.sb(f"xT{i}", [128, 16, 128], F32, es2) for i in range(2)]
            lg = p.sb("lg", [128, NE], F32, es2)
            m8 = p.sb("m8", [128, 8], F32, es2)
            nm = p.sb("nm", [128, 1], F32, es2)
            mask = p.sb("mask", [128, NE], F32, es2)
            maskb = p.sb("maskb", [128, NE], BF16, es2)
            ex = p.sb("ex", [128, NE], F32, es2)
            ssum = p.sb("ssum", [128, 1], F32, es2)
            gate = p.sb("gate", [128, NE], F32, es2)
            pos = p.sb("pos", [128, NE], F32, es2)
            vv = p.sb("vv", [128, NE], F32, es2)
            ltc = p.sb("ltc", [128, NE], F32, es2)
            eqk = p.sb("eqk", [128, NE], F32, es2)
            r8 = p.sb("r8", [128, 8], F32, es2)
            pt = [p.ps(f"pt{i}", [128, 512], F32, es2) for i in range(4)]
            plg = p.ps("plg", [128, NE], F32, es2)
            ppos = p.ps("ppos", [128, 2, NE], F32, es2)
            p.dma("sp", wr[:, :, :], W["router_w"], wr, writes=[wr])
            p.dma("sp", rb[:, :], bcast_rows(W["router_b"], 128, NE), rb, writes=[rb])
            p.dma("sp", rowbase[:, :], bcast_rows(W["rowbase"], 128, NE), rowbase, writes=[rowbase])
            p.op("dve", lambda: nc.vector.memset(ustr[:, :], 1.0), writes=[ustr])
            p.op("pool", lambda: nc.gpsimd.affine_select(out=ustr[:, :], in_=ustr[:, :], pattern=[[1, 128]],
                                                         compare_op=ALU.is_gt, fill=0.0, base=0, channel_multiplier=-1),
                 reads=[ustr], writes=[ustr])
            p.op("dve", lambda: nc.vector.memset(cum[:, :], 0.0), writes=[cum])
            for i in range(NTT):
                xai = xa[i % 2]
                xTi = xT[i % 2]
                p.dma("sp", xai[:, :], x_in[i * 128:(i + 1) * 128, :], xai, writes=[xai])
                p.op("act", lambda: nc.scalar.copy(out=xbf[:, i, :], in_=xai[:, :]), reads=[xai], writes=[(xbf, i)])
                for g in range(4):
                    ptg = pt[g]
                    for j in range(4):
                        c = g * 4 + j
                        p.op("pe", lambda: nc.tensor.transpose(out=ptg[:, j * 128:(j + 1) * 128],
                                                               in_=xai[:, c * 128:(c + 1) * 128], identity=ident_f[:, :]),
                             reads=[xai, ident_f], writes=[ptg])
                    if g % 2 == 0:
                        p.op("dve", lambda: nc.vector.tensor_copy(out=xTi[:, g * 4:(g + 1) * 4, :],
                                                                  in_=ptg[:, :].rearrange("p (a b) -> p a b", a=4)),
                             reads=[ptg], writes=[(xTi, g)])
                    else:
                        p.op("act", lambda: nc.scalar.copy(out=xTi[:, g * 4:(g + 1) * 4, :],
                                                           in_=ptg[:, :].rearrange("p (a b) -> p a b", a=4)),
                             reads=[ptg], writes=[(xTi, g)])
                for c in range(16):
                    p.op("pe", lambda: nc.tensor.matmul(plg[:, :], lhsT=xTi[:, c, :], rhs=wr[:, c, :], start=(c == 0), stop=(c == 15)),
                         reads=[(xTi, c // 4), wr], writes=[plg])
                p.op("dve", lambda: nc.vector.tensor_tensor(out=lg[:, :], in0=plg[:, :], in1=rb[:, :], op=ALU.add), reads=[plg, rb], writes=[lg])
                p.op("dve", lambda: nc.vector.max(out=m8[:, :], in_=lg[:, :]), reads=[lg], writes=[m8])
                p.op("dve", lambda: nc.vector.tensor_scalar(out=mask[:, :], in0=lg[:, :], scalar1=m8[:, 3:4], scalar2=None, op0=ALU.is_ge),
                     reads=[lg, m8], writes=[mask])
                p.op("pool", lambda: nc.gpsimd.tensor_copy(out=maskb[:, :], in_=mask[:, :]), reads=[mask], writes=[maskb])
                p.op("dve", lambda: nc.vector.tensor_scalar(out=nm[:, :], in0=m8[:, 0:1], scalar1=-1.0, scalar2=None, op0=ALU.mult),
                     reads=[m8], writes=[nm])
                p.op("act", lambda: nc.scalar.activation(out=ex[:, :], in_=lg[:, :], func=AF.Exp, bias=nm[:, 0:1], scale=1.0),
                     reads=[lg, nm], writes=[ex])
                p.op("dve", lambda: nc.vector.tensor_tensor(out=ex[:, :], in0=ex[:, :], in1=mask[:, :], op=ALU.mult), reads=[ex, mask], writes=[ex])
                p.op("dve", lambda: nc.vector.reduce_sum(out=ssum[:, :], in_=ex[:, :], axis=AX.X), reads=[ex], writes=[ssum])
                p.op("dve", lambda: nc.vector.reciprocal(out=ssum[:, :], in_=ssum[:, :]), reads=[ssum], writes=[ssum])
                p.op("dve", lambda: nc.vector.tensor_scalar(out=gate[:, :], in0=ex[:, :], scalar1=ssum[:, 0:1], scalar2=None, op0=ALU.mult),
                     reads=[ex, ssum], writes=[gate])
                p.op("pe", lambda: nc.tensor.matmul(ppos[:, 0, :], lhsT=ustr[:, :], rhs=maskb[:, :], start=True, stop=True),
                     reads=[ustr, maskb], writes=[(ppos, 0)])
                p.op("pe", lambda: nc.tensor.matmul(ppos[:, 1, :], lhsT=ones_bf[:, :], rhs=maskb[:, :], start=True, stop=True),
                     reads=[ones_bf, maskb], writes=[(ppos, 1)])
                p.op("dve", lambda: nc.vector.tensor_tensor(out=pos[:, :], in0=ppos[:, 0, :], in1=cum[:, :], op=ALU.add),
                     reads=[(ppos, 0), cum], writes=[pos])
                p.op("dve", lambda: nc.vector.tensor_tensor(out=cum[:, :], in0=ppos[:, 1, :], in1=cum[:, :], op=ALU.add),
                     reads=[(ppos, 1), cum], writes=[cum])
                p.op("dve", lambda: nc.vector.scalar_tensor_tensor(out=posm[:, i, :], in0=pos[:, :], scalar=1.0, in1=mask[:, :],
                                                                   op0=ALU.add, op1=ALU.mult), reads=[pos, mask], writes=[(posm, i)])
                p.op("dve", lambda: nc.vector.tensor_scalar(out=posm[:, i, :], in0=posm[:, i, :], scalar1=-1.0, scalar2=None, op0=ALU.add),
                     reads=[(posm, i)], writes=[(posm, i)])
                p.op("dve", lambda: nc.vector.tensor_scalar(out=ltc[:, :], in0=pos[:, :], scalar1=float(CAP), scalar2=None, op0=ALU.is_lt),
                     reads=[pos], writes=[ltc])
                p.op("dve", lambda: nc.vector.tensor_tensor(out=ltc[:, :], in0=ltc[:, :], in1=mask[:, :], op=ALU.mult), reads=[ltc, mask], writes=[ltc])
                p.op("dve", lambda: nc.vector.tensor_tensor(out=vv[:, :], in0=pos[:, :], in1=rowbase[:, :], op=ALU.add), reads=[pos, rowbase], writes=[vv])
                p.op("dve", lambda: nc.vector.tensor_tensor(out=vv[:, :], in0=vv[:, :], in1=ltc[:, :], op=ALU.mult), reads=[vv, ltc], writes=[vv])
                p.op("dve", lambda: nc.vector.max(out=r8[:, :], in_=vv[:, :]), reads=[vv], writes=[r8])
                p.op("dve", lambda: nc.vector.tensor_copy(out=idx[:, i, :], in_=r8[:, 0:4]), reads=[r8], writes=[(idx, i)])
                for k in range(4):
                    p.op("dve", lambda: nc.vector.tensor_scalar(out=eqk[:, :], in0=vv[:, :], scalar1=r8[:, k:k + 1], scalar2=None, op0=ALU.is_equal),
                         reads=[vv, r8], writes=[eqk])
                    p.op("dve", lambda: nc.vector.tensor_tensor(out=eqk[:, :], in0=eqk[:, :], in1=gate[:, :], op=ALU.mult), reads=[eqk, gate], writes=[eqk])
                    p.op("dve", lambda: nc.vector.reduce_sum(out=gk[:, i, k:k + 1], in_=eqk[:, :], axis=AX.X), reads=[eqk], writes=[(gk, i, k)])
            p.dma("sp", idx_out, idx[:, :, :].rearrange("p a b -> p (a b)"), idx, reads=[(idx, i) for i in range(NTT)], writes=["idx_out"])
            p.dma("sp", gk_out, gk[:, :, :].rearrange("p a b -> p (a b)"), gk, reads=[(gk, i, k) for i in range(NTT) for k in range(4)], writes=["gk_out"])
            p.barrier()
        with ExitStack() as es3:
            O = [p.sb(f"O{i}", [128, NTT, CAP], BF16, es3) for i in range(2)]
            xeT = [p.sb(f"xeT{i}", [128, 16, CAP], BF16, es3) for i in range(2)]
            pd = [p.ps(f"pd{i}", [128, 512], F32, es3) for i in range(4)]
            for e in range(NE):
                Oe = O[e % 2]
                xe = xeT[e % 2]
                r, el = e // NLOC, e % NLOC
                for i in range(NTT):
                    eng = "dve"
                    eo = nc.vector
                    p.op(eng, lambda: eo.tensor_scalar(out=Oe[:, i, :], in0=iotac[:, :], scalar1=posm[:, i, e:e + 1],
                                                       scalar2=None, op0=ALU.is_equal),
                         reads=[iotac, (posm, i)], writes=[(Oe, i)])
                for c in range(16):
                    pdc = pd[c % 4]
                    for i in range(NTT):
                        p.op("pe", lambda: nc.tensor.matmul(pdc[:, 0:CAP], lhsT=xbf[:, i, c * 128:(c + 1) * 128], rhs=Oe[:, i, :],
                                                            start=(i == 0), stop=(i == NTT - 1)),
                             reads=[(xbf, i), (Oe, i)], writes=[pdc])
                    if c % 2 == 0:
                        p.op("act", lambda: nc.scalar.copy(out=xe[:, c, :], in_=pdc[:, 0:CAP]), reads=[pdc], writes=[(xe, c)])
                    else:
                        p.op("dve", lambda: nc.vector.tensor_copy(out=xe[:, c, :], in_=pdc[:, 0:CAP]), reads=[pdc], writes=[(xe, c)])
                dst = XS[r, el]
                p.dma("sp", dst.rearrange("(c q) s -> q c s", q=128), xe[:, :, :], xe,
                      reads=[(xe, c) for c in range(16)], writes=[("XS", r, el)])
                if el == NLOC - 1:
                    p.collective("AllGather", G8, XS[r].rearrange("e d s -> (e d) s").opt(),
                                 XR[r].rearrange("h e d s -> (h e d) s").opt(), reads=[("XS", r, q_) for q_ in range(NLOC)], writes=[("XR", r)])
            p.barrier()


def emit_experts2(p, XR, tabX, YS, YR, W):
    nc = p.nc
    HS = XCAP // 2
    NCB = HS // 512
    NSBH = HS // 128
    XRrows = [XR[r].rearrange("h e d s -> (h e d) s") for r in range(8)]
    with ExitStack() as es:
        NWB = 4
        wbuf = [p.sb(f"wb{i}", [128, 16, 512], BF16, es) for i in range(NWB)]
        xeT = p.sb("xeT", [128, 16, HS], BF16, es)
        actT = p.sb("actT", [128, 16, HS], BF16, es)
        tab = p.sb("tabX", [128, 8 * NLOC * 16], I32, es)
        bgu = [p.sb(f"bgu{i}", [128, 32], F32, es) for i in range(2)]
        bdn = [p.sb(f"bdn{i}", [1, D], F32, es) for i in range(2)]
        gt = [p.sb(f"gt{i}", [128, 512], F32, es) for i in range(2)]
        sg = [p.sb(f"sg{i}", [128, 512], F32, es) for i in range(2)]
        u2 = [p.sb(f"u2{i}", [128, 512], F32, es) for i in range(2)]
        ysb = [p.sb(f"ysb{i}", [128, 512], BF16, es) for i in range(4)]
        ones_f = p.sb("ones_f", [1, 128], F32, es)
        pg = [p.ps(f"pg{i}", [128, 512], F32, es) for i in range(2)]
        pu = [p.ps(f"pu{i}", [128, 512], F32, es) for i in range(2)]
        pdn = [p.ps(f"pdn{i}", [128, 512], F32, es) for i in range(3)]
        p.op("dve", lambda: nc.vector.memset(ones_f[:, :], 1.0), writes=[ones_f])
        b7 = p.sb("b7", [128, 1], F32, es)
        p.op("dve", lambda: nc.vector.memset(b7[:, :], 7.0), writes=[b7])
        p.dma("sp", tab[:, :], tabX, tab, writes=[tab])
        cnt = {"w": 0, "y": 0, "g": 0, "d": 0}

        def load_w(src_ap):
            wb = wbuf[cnt["w"] % NWB]
            cnt["w"] += 1
            p.dma("pool", wb[:, :, :], src_ap, wb, writes=[wb])
            return wb

        for e in range(NLOC):
            bg = bgu[e % 2]
            bd = bdn[e % 2]
            p.dma("sp", bg[:, :], W["b_gu"][e], bg, writes=[bg])
            p.dma("sp", bd[:, :], W["b_dn"][e:e + 1, :], bd, writes=[bd])
            for h in range(2):
                for rr in range(4):
                    r = h * 4 + rr
                    for c in range(16):
                        col = (r * NLOC + e) * 16 + c
                        p.dma("pool", None, None, xeT, reads=[tab, ("XR", r)], writes=[xeT],
                              fn=lambda: nc.gpsimd.indirect_dma_start(out=xeT[:, c, rr * CAP:(rr + 1) * CAP], out_offset=None, in_=XRrows[r],
                                                                      in_offset=bass.IndirectOffsetOnAxis(ap=tab[:, col:col + 1], axis=0)))
                for j in range(4):
                    wg = load_w(W["w_gu"][e, :, j * 512:(j + 1) * 512].rearrange("(c q) f -> q c f", q=128))
                    wu = load_w(W["w_gu"][e, :, D + j * 512:D + (j + 1) * 512].rearrange("(c q) f -> q c f", q=128))
                    for f in range(4):
                        fc = j * 4 + f
                        for cb in range(NCB):
                            k = cnt["g"] % 2
                            cnt["g"] += 1
                            sl = slice(cb * 512, (cb + 1) * 512)
                            for c in range(16):
                                p.op("pe", lambda: nc.tensor.matmul(pg[k][:, :], lhsT=wg[:, c, f * 128:(f + 1) * 128], rhs=xeT[:, c, sl],
                                                                    start=(c == 0), stop=(c == 15)), reads=[wg, xeT], writes=[pg[k]])
                            for c in range(16):
                                p.op("pe", lambda: nc.tensor.matmul(pu[k][:, :], lhsT=wu[:, c, f * 128:(f + 1) * 128], rhs=xeT[:, c, sl],
                                                                    start=(c == 0), stop=(c == 15)), reads=[wu, xeT], writes=[pu[k]])
                            p.op("dve", lambda: nc.vector.tensor_scalar(out=gt[k][:, :], in0=pg[k][:, :], scalar1=bg[:, fc:fc + 1], scalar2=7.0,
                                                                        op0=ALU.add, op1=ALU.min), reads=[pg[k], bg], writes=[gt[k]])
                            p.op("act", lambda: nc.scalar.activation(out=sg[k][:, :], in_=gt[k][:, :], func=AF.Sigmoid, scale=1.702),
                                 reads=[gt[k]], writes=[sg[k]])
                            p.op("dve", lambda: nc.vector.tensor_scalar(out=u2[k][:, :], in0=pu[k][:, :], scalar1=bg[:, 16 + fc:17 + fc], scalar2=7.0,
                                                                        op0=ALU.add, op1=ALU.min), reads=[pu[k], bg], writes=[u2[k]])
                            p.op("act", lambda: nc.scalar.activation(out=u2[k][:, :], in_=u2[k][:, :], func=AF.Relu, bias=b7[:, 0:1]),
                                 reads=[u2[k], b7], writes=[u2[k]])
                            p.op("dve", lambda: nc.vector.scalar_tensor_tensor(out=u2[k][:, :], in0=u2[k][:, :], scalar=-6.0, in1=gt[k][:, :],
                                                                               op0=ALU.add, op1=ALU.mult), reads=[u2[k], gt[k]], writes=[u2[k]])
                            p.op("dve", lambda: nc.vector.tensor_tensor(out=actT[:, fc, sl], in0=u2[k][:, :], in1=sg[k][:, :], op=ALU.mult),
                                 reads=[u2[k], sg[k]], writes=[(actT, fc)])
                for dc in range(4):
                    wd = load_w(W["w_dn"][e, :, dc * 512:(dc + 1) * 512].rearrange("(c q) f -> q c f", q=128))
                    for sb_ in range(NSBH):
                        pdk = pdn[cnt["d"] % 3]
                        cnt["d"] += 1
                        for fc in range(16):
                            p.op("pe", lambda: nc.tensor.matmul(pdk[:, :], lhsT=actT[:, fc, sb_ * 128:(sb_ + 1) * 128], rhs=wd[:, fc, :],
                                                                start=(fc == 0), stop=False), reads=[(actT, fc), wd], writes=[pdk])
                        p.op("pe", lambda: nc.tensor.matmul(pdk[:, :], lhsT=ones_f[0:1, :], rhs=bd[0:1, dc * 512:(dc + 1) * 512],
                                                            start=False, stop=True), reads=[ones_f, bd], writes=[pdk])
                        yb = ysb[cnt["y"] % 4]
                        cnt["y"] += 1
                        if cnt["y"] % 2 == 0:
                            p.op("dve", lambda: nc.vector.tensor_copy(out=yb[:, :], in_=pdk[:, :]), reads=[pdk], writes=[yb])
                        else:
                            p.op("act", lambda: nc.scalar.copy(out=yb[:, :], in_=pdk[:, :]), reads=[pdk], writes=[yb])
                        r = h * 4 + sb_ // NSB
                        s0 = (sb_ % NSB) * 128
                        dst = YS[dc // 2][r, e, s0:s0 + 128, (dc % 2) * 512:(dc % 2 + 1) * 512]
                        p.dma("sp", dst, yb[:, :], yb, reads=[yb], writes=[("YS", r, e, s0, dc)])
        p.barrier()
        BLK = 8 * NLOC * CAP
        for r in range(8):
            for h2 in range(2):
                p.collective("AllGather", G8, YS[h2][r].rearrange("e s d -> (e s) d").opt(),
                             YR[h2][1 + r * BLK:1 + (r + 1) * BLK, :].opt(), writes=[("YR", r, h2)])
        p.barrier()


def emit_combine2(p, x_res, YR, idx_in, gk_in, ln_g, ln_b, x_out, x1T_out=None, ident_f=None):
    nc = p.nc
    with ExitStack() as es4:
        gbc = p.sb("gbc", [128, D], F32, es4)
        bbc = p.sb("bbc", [128, D], F32, es4)
        idx = p.sb("idxc", [128, NTT * 4], I32, es4)
        gk = p.sb("gkc", [128, NTT * 4], F32, es4)
        G = [[p.sb(f"G{k}_{i}", [128, D], BF16, es4) for k in range(4)] for i in range(2)]
        xr = [p.sb(f"xr{i}", [128, D], F32, es4) for i in range(2)]
        zo = [p.sb(f"zo{i}", [128, D], F32, es4) for i in range(2)]
        zT = [p.sb(f"zT{i}", [128, 16, 128], BF16, es4) for i in range(2)]
        zrow = p.sb("zrow", [1, D], BF16, es4)
        stats = p.sb("stats", [128, 24], F32, es4)
        mv = p.sb("mv", [128, 2], F32, es4)
        rstd = p.sb("rstd", [128, 1], F32, es4)
        ptz = [p.ps(f"ptz{i}", [128, 512], F32, es4) for i in range(2)]
        p.op("dve", lambda: nc.vector.memset(zrow[:, :], 0.0), writes=[zrow])
        for h2 in range(2):
            p.dma("sp", YR[h2][0:1, :], zrow[:, h2 * 1024:(h2 + 1) * 1024], zrow, reads=[zrow], writes=[("YR0", h2)])
        p.dma("sp", gbc[:, :], bcast_rows(ln_g, 128, D), gbc, writes=[gbc])
        p.dma("sp", bbc[:, :], bcast_rows(ln_b, 128, D), bbc, writes=[bbc])
        p.dma("sp", idx[:, :], idx_in, idx, writes=[idx])
        p.dma("sp", gk[:, :], gk_in, gk, writes=[gk])
        for i in range(NTT):
            Gi = G[i % 2]
            xri = xr[i % 2]
            zi = zo[i % 2]
            p.dma("sp", xri[:, :], x_res[i * 128:(i + 1) * 128, :], xri, writes=[xri])
            for k in range(4):
                for h2 in range(2):
                    p.dma("pool", None, None, Gi[k], reads=[idx, ("YR0", 0), ("YR0", 1)], writes=[(Gi[k], h2)],
                          fn=lambda: nc.gpsimd.indirect_dma_start(out=Gi[k][:, h2 * 1024:(h2 + 1) * 1024], out_offset=None, in_=YR[h2][:, :],
                                                                  in_offset=bass.IndirectOffsetOnAxis(ap=idx[:, i * 4 + k:i * 4 + k + 1], axis=0)))
            p.op("dve", lambda: nc.vector.tensor_scalar(out=xri[:, :], in0=xri[:, :], scalar1=ALPHA, scalar2=None, op0=ALU.mult), reads=[xri], writes=[xri])
            for k in range(4):
                p.op("dve", lambda: nc.vector.scalar_tensor_tensor(out=xri[:, :], in0=Gi[k][:, :], scalar=gk[:, i * 4 + k:i * 4 + k + 1], in1=xri[:, :],
                                                                   op0=ALU.mult, op1=ALU.add), reads=[(Gi[k], 0), (Gi[k], 1), gk, xri], writes=[xri])
            emit_ln(p, xri, zi, gbc, bbc, stats, mv, rstd)
            p.dma("sp", x_out[i * 128:(i + 1) * 128, :], zi[:, :], zi, reads=[zi], writes=[("xout", i)])
            if x1T_out is not None:
                zt = zT[i % 2]
                for g in range(4):
                    ptg = ptz[g % 2]
                    for j in range(4):
                        c = g * 4 + j
                        p.op("pe", lambda: nc.tensor.transpose(out=ptg[:, j * 128:(j + 1) * 128], in_=zi[:, c * 128:(c + 1) * 128], identity=ident_f[:, :]),
                             reads=[zi, ident_f], writes=[ptg])
                    p.op("act", lambda: nc.scalar.copy(out=zt[:, g * 4:(g + 1) * 4, :], in_=ptg[:, :].rearrange("p (a b) -> p a b", a=4)),
                         reads=[ptg], writes=[(zt, g)])
                p.dma("sp", x1T_out[:, i * 128:(i + 1) * 128].rearrange("(c q) t -> q c t", q=128), zt[:, :, :], zt,
                      reads=[(zt, g) for g in range(4)], writes=[("x1T", i)])
        p.barrier()


def dint(nc, name, shape, dt=F32):
    return nc.dram_tensor(name, list(shape), dt).ap()


def build_fused_prog():
    nc = new_nc()
    A0 = {"xT_all": din(nc, "xT_all", [D, S]), "xT_own": din(nc, "xT_own", [D, NT]),
          "pos_all": din(nc, "pos_all", [S], I32), "pos_own": din(nc, "pos_own", [NT], I32),
          "qlimrel": din(nc, "qlimrel", [128, NQB]), "invf": din(nc, "invf", [128, 1]), "sgn": din(nc, "sgn", [128, 1]),
          "wK": din(nc, "wK", [D, 512]), "wKIV": din(nc, "wKIV", [D, 512]),
          "wQ": din(nc, "wQ", [4, D, 512]), "wQI": din(nc, "wQI", [2, D, 512]), "wWI": din(nc, "wWI", [D, 4])}
    AT = dint(nc, "AT", [8, 128, NT], BF16)
    A0["aT_out"] = AT
    XA = dint(nc, "XA", [NT, D])
    A1 = {"xT_oh": din(nc, "xT_oh", [D, NQB * SEG]), "wU": din(nc, "wU", [2, D, 512]), "invcnt": din(nc, "invcnt", [4, NT]),
          "w_pool": din(nc, "w_pool", [4, 256, 256]), "pool_scale": din(nc, "pool_scale", [128, 8]),
          "aT": AT, "w_out": din(nc, "w_out0", [D, D]), "x_own": din(nc, "x_own", [NT, D]),
          "ln_g": din(nc, "ln_mix_g0", [D]), "ln_b": din(nc, "ln_mix_b0", [D]), "xa_out": XA}
    WM = []
    for l in range(2):
        WM.append({"router_w": din(nc, f"router_w{l}", [128, 16, NE]), "router_b": din(nc, f"router_b{l}", [NE]),
                   "rowbase": din(nc, f"rowbase{l}", [NE]),
                   "w_gu": din(nc, f"w_gu{l}", [NLOC, D, 2 * D]), "b_gu": din(nc, f"b_gu{l}", [NLOC, 128, 32]),
                   "w_dn": din(nc, f"w_dn{l}", [NLOC, D, D]), "b_dn": din(nc, f"b_dn{l}", [NLOC, D]),
                   "ln_g": din(nc, f"ln_moe_g{l}", [D]), "ln_b": din(nc, f"ln_moe_b{l}", [D])})
    tabX = din(nc, "tabX", [128, 8 * NLOC * 16], I32)
    tabH = din(nc, "tabH", [128, 16], I32)
    AL = {"wQK": din(nc, "wQK", [2, D, 512]), "wVOG": din(nc, "wVOG", [D, 2056]),
          "conv_w": din(nc, "conv_w", [128, 8, 4]), "conv_b": din(nc, "conv_b", [128, 8]), "gate_b": din(nc, "gate_b", [8]),
          "c_norm_g": din(nc, "c_norm_g", [1024])}
    w_out1 = din(nc, "w_out1", [D, D])
    ln_mix_g1 = din(nc, "ln_mix_g1", [D])
    ln_mix_b1 = din(nc, "ln_mix_b1", [D])
    OUT = dout(nc, "out", [NT, D])
    XS = dint(nc, "XS", [8, NLOC, D, CAP], BF16)
    XR = [dint(nc, f"XR{r}", [8, NLOC, D, CAP], BF16) for r in range(8)]
    YS = [dint(nc, f"YS{h2}", [8, NLOC, CAP, 1024], BF16) for h2 in range(2)]
    YR = [dint(nc, f"YR{h2}", [1 + NXR * CAP, 1024], BF16) for h2 in range(2)]
    IDX = dint(nc, "IDX", [128, NTT * 4], I32)
    GK = dint(nc, "GK", [128, NTT * 4])
    X1 = dint(nc, "X1", [NT, D])
    X1S = dint(nc, "X1S", [D, NT], BF16)
    X1R = dint(nc, "X1R", [2, D, NT], BF16)
    HS = dint(nc, "HS", [2, 1024, NT], BF16)
    HR = dint(nc, "HR", [2, 2, 1024, NT], BF16)
    XB = dint(nc, "XB", [NT, D])
    with ExitStack() as es:
        p = P(nc, es)
        ident = make_ident(p, es)
        emit_l0_attn(p, A0)
        emit_l0_out(p, A1)
        emit_dispatch2(p, XA, XS, XR, IDX, GK, WM[0], ident)
        emit_experts2(p, XR, tabX, YS, YR, WM[0])
        emit_combine2(p, XA, YR, IDX, GK, WM[0]["ln_g"], WM[0]["ln_b"], X1, x1T_out=X1S, ident_f=ident)
        p.collective("AllGather", pair_groups(1), X1S.opt(), X1R.rearrange("r d t -> (r d) t").opt())
        p.barrier()
        AL["x1R"] = X1R
        AL["HS"] = HS
        emit_l1_mlstm(p, AL)
        p.collective("AllGather", pair_groups(1), HS.rearrange("h f t -> (h f) t").opt(), HR.rearrange("r h f t -> (r h f) t").opt())
        p.barrier()
        emit_l1_out(p, {"HR": HR, "tabH": tabH, "w_out": w_out1, "x_own": X1, "ln_g": ln_mix_g1, "ln_b": ln_mix_b1, "xb_out": XB})
        emit_dispatch2(p, XB, XS, XR, IDX, GK, WM[1], ident)
        emit_experts2(p, XR, tabX, YS, YR, WM[1])
        emit_combine2(p, XB, YR, IDX, GK, WM[1]["ln_g"], WM[1]["ln_b"], OUT)
        p.finish()
    return nc


def fused_host(inputs):
    m0 = l0_attn_host(inputs)
    m1 = l0_out_host(inputs, [None] * 8)
    ml = l1_mlstm_host(inputs, None)
    maps = []
    for c in range(8):
        h = c % 2
        m = dict(m0[c])
        for k, v in m1[c].items():
            if k == "aT":
                continue
            m[{"w_out": "w_out0", "ln_g": "ln_mix_g0", "ln_b": "ln_mix_b0"}.get(k, k)] = v
        sel = [0] + [1 if (c ^ r) > c else 0 for r in range(1, 8)]
        perm = np.array([NLOC * (c ^ r) + e for r in range(8) for e in range(NLOC)])
        rowbase = np.array([1 + ((r * 8 + (c ^ r)) * NLOC + e) * CAP for r in range(8) for e in range(NLOC)], np.float32)
        es_ = slice(c * NLOC, (c + 1) * NLOC)
        for l in range(2):
            rw = inputs["router_w"][l][:, perm]
            m[f"router_w{l}"] = np.ascontiguousarray(rw.reshape(16, 128, NE).transpose(1, 0, 2))
            m[f"router_b{l}"] = np.ascontiguousarray(inputs["router_b"][l][perm])
            m[f"rowbase{l}"] = rowbase
            m[f"w_gu{l}"] = np.ascontiguousarray(inputs["w_gu"][l][es_])
            m[f"b_gu{l}"] = np.ascontiguousarray(inputs["b_gu"][l].reshape(NE, 32, 128).transpose(0, 2, 1)[es_])
            m[f"w_dn{l}"] = np.ascontiguousarray(inputs["w_dn"][l][es_])
            m[f"b_dn{l}"] = np.ascontiguousarray(inputs["b_dn"][l][es_])
            m[f"ln_moe_g{l}"] = np.ascontiguousarray(inputs["ln_moe_g"][l])
            m[f"ln_moe_b{l}"] = np.ascontiguousarray(inputs["ln_moe_b"][l])
        tabX = np.empty((128, 8 * NLOC * 16), np.int32)
        pidx = np.arange(128)
        for r in range(8):
            for e in range(NLOC):
                for cc in range(16):
                    tabX[:, (r * NLOC + e) * 16 + cc] = ((c ^ r) * NLOC + e) * D + cc * 128 + pidx
        m["tabX"] = tabX
        tabH = np.empty((128, 16), np.int32)
        for fc in range(16):
            tabH[:, fc] = ((fc // 8) * 2 + h) * 1024 + (fc % 8) * 128 + pidx
        m["tabH"] = tabH
        for k in ("wQK", "wVOG", "conv_w", "conv_b", "gate_b", "c_norm_g"):
            m[k] = ml[c][k]
        m["w_out1"] = np.ascontiguousarray(inputs["w_out1"][0])
        m["ln_mix_g1"] = np.ascontiguousarray(inputs["ln_mix_g"][1])
        m["ln_mix_b1"] = np.ascontiguousarray(inputs["ln_mix_b"][1])
        maps.append(m)
    return maps


def kernel_fused_unsupported(**inputs):
    inputs = {k: np.asarray(v) for k, v in inputs.items()}
    r = run_bass_kernel_spmd(build_fused_prog(), fused_host(inputs), core_ids=ALL8).results
    out = np.empty((4, S, D), np.float32)
    for c in range(8):
        out[c // 2][own_tokens(c % 2)] = np.asarray(r[c]["out"])
    return out


def build_moe2_test_prog(with_x1t=False):
    nc = new_nc()
    XA = din(nc, "x_in", [NT, D])
    l = 0
    WMl = {"router_w": din(nc, f"router_w{l}", [128, 16, NE]), "router_b": din(nc, f"router_b{l}", [NE]),
           "rowbase": din(nc, f"rowbase{l}", [NE]),
           "w_gu": din(nc, f"w_gu{l}", [NLOC, D, 2 * D]), "b_gu": din(nc, f"b_gu{l}", [NLOC, 128, 32]),
           "w_dn": din(nc, f"w_dn{l}", [NLOC, D, D]), "b_dn": din(nc, f"b_dn{l}", [NLOC, D]),
           "ln_g": din(nc, f"ln_moe_g{l}", [D]), "ln_b": din(nc, f"ln_moe_b{l}", [D])}
    tabX = din(nc, "tabX", [128, 8 * NLOC * 16], I32)
    OUT = dout(nc, "out", [NT, D])
    XS = dint(nc, "XS", [8, NLOC, D, CAP], BF16)
    XR = [dint(nc, f"XR{r}", [8, NLOC, D, CAP], BF16) for r in range(8)]
    YS = [dint(nc, f"YS{h2}", [8, NLOC, CAP, 1024], BF16) for h2 in range(2)]
    YR = [dint(nc, f"YR{h2}", [1 + NXR * CAP, 1024], BF16) for h2 in range(2)]
    IDX = dint(nc, "IDX", [128, NTT * 4], I32)
    GK = dint(nc, "GK", [128, NTT * 4])
    with ExitStack() as es:
        p = P(nc, es)
        ident = make_ident(p, es)
        emit_dispatch2(p, XA, XS, XR, IDX, GK, WMl, ident)
        emit_experts2(p, XR, tabX, YS, YR, WMl)
        emit_combine2(p, XA, YR, IDX, GK, WMl["ln_g"], WMl["ln_b"], OUT)
        p.finish()
    return nc


def build_seg1_prog():
    nc = new_nc()
    A0 = {"xT_all": din(nc, "xT_all", [D, S]), "xT_own": din(nc, "xT_own", [D, NT]),
          "pos_all": din(nc, "pos_all", [S], I32), "pos_own": din(nc, "pos_own", [NT], I32),
          "qlimrel": din(nc, "qlimrel", [128, NQB]), "invf": din(nc, "invf", [128, 1]), "sgn": din(nc, "sgn", [128, 1]),
          "wK": din(nc, "wK", [D, 512]), "wKIV": din(nc, "wKIV", [D, 512]),
          "wQ": din(nc, "wQ", [4, D, 512]), "wQI": din(nc, "wQI", [2, D, 512]), "wWI": din(nc, "wWI", [D, 4])}
    AT = dint(nc, "AT", [8, 128, NT], BF16)
    A0["aT_out"] = AT
    XA2 = dint(nc, "XA2", [NT, D])
    A1 = {"xT_oh": din(nc, "xT_oh", [D, NQB * SEG]), "wU": din(nc, "wU", [2, D, 512]), "invcnt": din(nc, "invcnt", [4, NT]),
          "w_pool": din(nc, "w_pool", [4, 256, 256]), "pool_scale": din(nc, "pool_scale", [128, 8]),
          "aT": AT, "w_out": din(nc, "w_out", [D, D]), "x_own": din(nc, "x_own", [NT, D]),
          "ln_g": din(nc, "ln_g", [D]), "ln_b": din(nc, "ln_b", [D]), "xa_out": dout(nc, "xa_out", [NT, D]), "xa_out2": XA2}
    W = {"router_w": din(nc, "router_w", [128, 16, NE]), "router_b": din(nc, "router_b", [NE])}
    xeT_out = dout(nc, "xeT_out", [NE, D, CAP], BF16)
    gs_out = dout(nc, "gs_out", [NE, CAP])
    idx_out = dout(nc, "idx_out", [128, NTT * 4], I32)
    with ExitStack() as es:
        p = P(nc, es)
        ident = make_ident(p, es)
        emit_l0_attn(p, A0)
        emit_l0_out(p, A1)
        emit_dispatch(p, XA2, xeT_out, gs_out, idx_out, W, ident)
        p.finish()
    return nc


def build_seg5_prog():
    nc = new_nc()
    XB2 = dint(nc, "XB2", [NT, D])
    A = {"hgT": din(nc, "hgT", [D, NT], BF16), "w_out": din(nc, "w_out", [D, D]), "x_own": din(nc, "x_own", [NT, D]),
         "ln_g": din(nc, "ln_g", [D]), "ln_b": din(nc, "ln_b", [D]), "xb_out": dout(nc, "xb_out", [NT, D]), "xb_out2": XB2}
    W = {"router_w": din(nc, "router_w", [128, 16, NE]), "router_b": din(nc, "router_b", [NE])}
    xeT_out = dout(nc, "xeT_out", [NE, D, CAP], BF16)
    gs_out = dout(nc, "gs_out", [NE, CAP])
    idx_out = dout(nc, "idx_out", [128, NTT * 4], I32)
    with ExitStack() as es:
        p = P(nc, es)
        ident = make_ident(p, es)
        emit_l1_out(p, A)
        emit_dispatch(p, XB2, xeT_out, gs_out, idx_out, W, ident)
        p.finish()
    return nc


def moe_tail(r1, x_res, inputs, l):
    maps2 = []
    bgu = inputs["b_gu"][l].reshape(NE, 32, 128).transpose(0, 2, 1)
    for c in range(8):
        es_ = slice(c * NLOC, (c + 1) * NLOC)
        xe = np.concatenate([np.asarray(r1[s]["xeT_out"])[es_] for s in range(8)], axis=2)
        gs = np.concatenate([np.asarray(r1[s]["gs_out"])[es_] for s in range(8)], axis=1)
        gs = np.ascontiguousarray(gs.reshape(NLOC, XCAP // 128, 128).transpose(0, 2, 1))
        maps2.append({"xeT_in": np.ascontiguousarray(xe), "gs_in": gs,
                      "w_gu": np.ascontiguousarray(inputs["w_gu"][l][es_]), "b_gu": np.ascontiguousarray(bgu[es_]),
                      "w_dn": np.ascontiguousarray(inputs["w_dn"][l][es_]), "b_dn": np.ascontiguousarray(inputs["b_dn"][l][es_])})
    r2 = run_bass_kernel_spmd(build_experts_prog(), maps2, core_ids=ALL8).results
    yall = np.concatenate([np.asarray(r2[c]["y_out"]) for c in range(8)], axis=0)
    maps3 = []
    for c in range(8):
        yb = np.zeros((1 + NE * CAP, D), np.float32)
        yb[1:] = yall[:, c * CAP:(c + 1) * CAP, :].reshape(NE * CAP, D)
        maps3.append({"x_res": x_res[c], "ybuf": yb, "idx_in": np.asarray(r1[c]["idx_out"]),
                      "ln_g": np.ascontiguousarray(inputs["ln_moe_g"][l]), "ln_b": np.ascontiguousarray(inputs["ln_moe_b"][l])})
    r3 = run_bass_kernel_spmd(build_combine_prog(), maps3, core_ids=ALL8).results
    return [np.asarray(r3[c]["x_out"]) for c in range(8)]


def kernel(**inputs):
    inputs = {k: np.asarray(v) for k, v in inputs.items()}
    m0 = l0_attn_host(inputs)
    m1 = l0_out_host(inputs, [None] * 8)
    maps = []
    for c in range(8):
        m = dict(m0[c])
        m.update({k: v for k, v in m1[c].items() if k != "aT"})
        m["router_w"] = np.ascontiguousarray(inputs["router_w"][0].reshape(16, 128, NE).transpose(1, 0, 2))
        m["router_b"] = np.ascontiguousarray(inputs["router_b"][0])
        maps.append(m)
    r1 = run_bass_kernel_spmd(build_seg1_prog(), maps, core_ids=ALL8).results
    xa = [np.asarray(r1[c]["xa_out"]) for c in range(8)]
    x1 = moe_tail(r1, xa, inputs, 0)
    x1_full = np.empty((4, S, D), np.float32)
    for c in range(8):
        x1_full[c // 2][own_tokens(c % 2)] = x1[c]
    r = run_bass_kernel_spmd(build_l1_mlstm_prog(), l1_mlstm_host(inputs, x1_full), core_ids=ALL8).results
    hg = [np.asarray(r[c]["hgT_out"]) for c in range(8)]
    maps = []
    for c in range(8):
        b, h = c // 2, c % 2
        hgT = np.concatenate([hg[2 * b], hg[2 * b + 1]], axis=0)[:, own_tokens(h)]
        maps.append({"hgT": np.ascontiguousarray(hgT), "w_out": np.ascontiguousarray(inputs["w_out1"][0]), "x_own": x1[c],
                     "ln_g": np.ascontiguousarray(inputs["ln_mix_g"][1]), "ln_b": np.ascontiguousarray(inputs["ln_mix_b"][1]),
                     "router_w": np.ascontiguousarray(inputs["router_w"][1].reshape(16, 128, NE).transpose(1, 0, 2)),
                     "router_b": np.ascontiguousarray(inputs["router_b"][1])})
    r5 = run_bass_kernel_spmd(build_seg5_prog(), maps, core_ids=ALL8).results
    xb = [np.asarray(r5[c]["xb_out"]) for c in range(8)]
    x2 = moe_tail(r5, xb, inputs, 1)
    out = np.empty((4, S, D), np.float32)
    for c in range(8):
        out[c // 2][own_tokens(c % 2)] = x2[c]
    return out
```
